# Optimizing a Trainium2 kernel written in Bass

```python
import jax, jax.numpy as jnp
from jax import lax
import numpy as np


D_MODEL = 1024
BATCH = 2
SEQ = 8192
DEPTH = 2

CHUNK = 64
HEAD_DIM = 64
H_SB = 8
H_CA = 8
W_SB = H_SB * HEAD_DIM
W_CA = H_CA * HEAD_DIM
N_PAST_CHUNKS = 8
BAND = (N_PAST_CHUNKS + 1) * CHUNK
REL_CLIP = 128
Q_BLOCK = 128
N_GROUPS = 4
EXPERTS_PER_GROUP = 8
TOP_K_IN_GROUP = 2
D_EXPERT = 256
ALPHA = (2.0 * DEPTH) ** 0.25
BETA = (8.0 * DEPTH) ** -0.25
LN_EPS = 1e-5
SPLIT_SIZES = (W_SB, W_SB, W_SB, W_CA, W_CA, W_CA, D_MODEL, D_MODEL)
D_IN = sum(SPLIT_SIZES)

kernel_name = 'hybrid_stickbreak_chunkrel_hmoe_deepnorm'


def _layer_norm(x, g, b):
    xf = x.astype(jnp.float32)
    mu = jnp.mean(xf, axis=-1, keepdims=True)
    var = jnp.mean(jnp.square(xf - mu), axis=-1, keepdims=True)
    y = (xf - mu) * lax.rsqrt(var + LN_EPS)
    return (y * g.astype(jnp.float32) + b.astype(jnp.float32)).astype(x.dtype)


def _stick_breaking_attention(q, k, v):
    b, s, h, d = q.shape
    nb = s // Q_BLOCK
    scale = d ** -0.5
    q_blocks = q.reshape(b, nb, Q_BLOCK, h, d).transpose(1, 0, 3, 2, 4)
    k_t = k.transpose(0, 2, 1, 3)
    v_t = v.transpose(0, 2, 1, 3)
    key_pos = jnp.arange(s, dtype=jnp.int32)

    def one_block(args):
        q_blk, blk = args
        q_pos = blk * Q_BLOCK + jnp.arange(Q_BLOCK, dtype=jnp.int32)
        before = key_pos[None, :] < q_pos[:, None]
        z = jnp.einsum('bhqd,bhkd->bhqk', q_blk, k_t).astype(jnp.float32) * scale
        log_beta = jax.nn.log_sigmoid(z)
        log_one_minus = jnp.where(before, log_beta - z, 0.0)
        log_remaining = lax.cumsum(log_one_minus, axis=3, reverse=True) - log_one_minus
        w = jnp.where(before, jnp.exp(log_beta + log_remaining), 0.0)
        return jnp.einsum('bhqk,bhkd->bhqd', w.astype(v.dtype), v_t)

    out = lax.map(one_block, (q_blocks, jnp.arange(nb, dtype=jnp.int32)))
    return out.transpose(1, 0, 3, 2, 4).reshape(b, s, h * d)


def _chunked_relpos_attention(q, k, v, rel_bias):
    b, s, h, d = q.shape
    nc = s // CHUNK
    q_c = q.reshape(b, nc, CHUNK, h, d)

    def band(x):
        xc = x.reshape(b, nc, CHUNK, h, d)
        xp = jnp.pad(xc, ((0, 0), (N_PAST_CHUNKS, 0), (0, 0), (0, 0), (0, 0)))
        return jnp.concatenate([xp[:, i:i + nc] for i in range(N_PAST_CHUNKS + 1)], axis=2)

    k_b = band(k)
    v_b = band(v)
    scores = jnp.einsum('bcqhd,bckhd->bhcqk', q_c, k_b).astype(jnp.float32) * (d ** -0.5)
    qi = jnp.arange(CHUNK, dtype=jnp.int32)
    kp = jnp.arange(BAND, dtype=jnp.int32)
    rel = jnp.clip(qi[:, None] + N_PAST_CHUNKS * CHUNK - kp[None, :], -REL_CLIP, REL_CLIP) + REL_CLIP
    bias = rel_bias.astype(jnp.float32)[:, rel]
    valid = (jnp.arange(nc, dtype=jnp.int32)[:, None] - N_PAST_CHUNKS + kp[None, :] // CHUNK) >= 0
    scores = scores + bias[None, :, None]
    scores = jnp.where(valid[None, None, :, None, :], scores, -1e30)
    probs = jax.nn.softmax(scores, axis=-1)
    out = jnp.einsum('bhcqk,bckhd->bcqhd', probs.astype(v.dtype), v_b)
    return out.reshape(b, s, h * d)


def _hierarchical_moe(x, w_group, b_group, w_erouter, b_erouter, w_gate, w_up, w_down):
    n = x.shape[0]
    g_logits = jnp.einsum('nd,dg->ng', x, w_group).astype(jnp.float32) + b_group.astype(jnp.float32)
    g_probs = jax.nn.softmax(g_logits, axis=-1)
    g_val, g_idx = lax.top_k(g_probs, 1)
    g_sel = g_idx[:, 0]
    e_logits_all = (jnp.einsum('nd,gde->nge', x, w_erouter).astype(jnp.float32)
                    + b_erouter.astype(jnp.float32)[None])
    e_logits = jnp.take_along_axis(e_logits_all, g_sel[:, None, None], axis=1)[:, 0]
    top_val, top_idx = lax.top_k(e_logits, TOP_K_IN_GROUP)
    top_w = jax.nn.softmax(top_val, axis=-1)
    expert_w = jnp.sum(jax.nn.one_hot(top_idx, EXPERTS_PER_GROUP, dtype=jnp.float32) * top_w[..., None], axis=1)
    combine = (jax.nn.one_hot(g_sel, N_GROUPS, dtype=jnp.float32) * g_val)[:, :, None] * expert_w[:, None, :]
    y = jnp.zeros((n, x.shape[1]), dtype=x.dtype)
    for g in range(N_GROUPS):
        hid = jax.nn.silu(jnp.einsum('nd,edf->nef', x, w_gate[g])) * jnp.einsum('nd,edf->nef', x, w_up[g])
        hid = hid * combine[:, g, :, None].astype(hid.dtype)
        y = y + jnp.einsum('nef,efd->nd', hid, w_down[g])
    return y


def _layer(x, w_in, b_gate, rel_bias, w_br_sb, w_br_ca, w_out, ln1_g, ln1_b,
           w_group, b_group, w_erouter, b_erouter, w_gate, w_up, w_down, ln2_g, ln2_b):
    b, s, d = x.shape
    proj = jnp.einsum('bsd,de->bse', x, w_in)
    idx = [int(i) for i in np.cumsum(SPLIT_SIZES)[:-1]]
    q_sb, k_sb, v_sb, q_ca, k_ca, v_ca, gl_sb, gl_ca = jnp.split(proj, idx, axis=-1)
    heads = lambda t, h: t.reshape(b, s, h, HEAD_DIM)
    y_sb = _stick_breaking_attention(heads(q_sb, H_SB), heads(k_sb, H_SB), heads(v_sb, H_SB))
    y_ca = _chunked_relpos_attention(heads(q_ca, H_CA), heads(k_ca, H_CA), heads(v_ca, H_CA), rel_bias)
    y_sb = jnp.einsum('bsw,wd->bsd', y_sb, w_br_sb)
    y_ca = jnp.einsum('bsw,wd->bsd', y_ca, w_br_ca)
    g_sb = jax.nn.sigmoid(gl_sb + b_gate[0])
    g_ca = jax.nn.sigmoid(gl_ca + b_gate[1])
    mixed = jnp.einsum('bsd,de->bse', g_sb * y_sb + g_ca * y_ca, w_out)
    x = _layer_norm(ALPHA * x + mixed, ln1_g, ln1_b)
    ffn = _hierarchical_moe(x.reshape(b * s, d), w_group, b_group, w_erouter, b_erouter,
                            w_gate, w_up, w_down).reshape(b, s, d)
    return _layer_norm(ALPHA * x + ffn, ln2_g, ln2_b)


def setup_inputs(seed: int = 0) -> dict:
    key = jax.random.key(seed)
    ks = jax.random.split(key, 20)
    f32 = jnp.float32
    d = D_MODEL
    nrm = lambda k, shape, sc: jax.random.normal(k, shape, dtype=f32) * sc
    col_scale = jnp.concatenate([
        jnp.ones((2 * W_SB,), f32), jnp.full((W_SB,), BETA, f32),
        jnp.ones((2 * W_CA,), f32), jnp.full((W_CA,), BETA, f32),
        jnp.ones((2 * d,), f32)])
    x = nrm(ks[0], (BATCH, SEQ, d), 1.0)
    w_in = nrm(ks[1], (DEPTH, d, D_IN), d ** -0.5) * col_scale
    b_gate = nrm(ks[2], (DEPTH, 2, d), 0.1)
    rel_bias = nrm(ks[3], (DEPTH, H_CA, 2 * REL_CLIP + 1), 0.2)
    w_br_sb = nrm(ks[4], (DEPTH, W_SB, d), BETA * W_SB ** -0.5)
    w_br_ca = nrm(ks[5], (DEPTH, W_CA, d), BETA * W_CA ** -0.5)
    w_out = nrm(ks[6], (DEPTH, d, d), BETA * d ** -0.5)
    ln1_g = 1.0 + nrm(ks[7], (DEPTH, d), 0.01)
    ln1_b = nrm(ks[8], (DEPTH, d), 0.01)
    w_group = nrm(ks[9], (DEPTH, d, N_GROUPS), d ** -0.5)
    b_group = nrm(ks[10], (DEPTH, N_GROUPS), 0.01)
    w_erouter = nrm(ks[11], (DEPTH, N_GROUPS, d, EXPERTS_PER_GROUP), d ** -0.5)
    b_erouter = nrm(ks[12], (DEPTH, N_GROUPS, EXPERTS_PER_GROUP), 0.01)
    w_gate = nrm(ks[13], (DEPTH, N_GROUPS, EXPERTS_PER_GROUP, d, D_EXPERT), d ** -0.5)
    w_up = nrm(ks[14], (DEPTH, N_GROUPS, EXPERTS_PER_GROUP, d, D_EXPERT), BETA * d ** -0.5)
    w_down = nrm(ks[15], (DEPTH, N_GROUPS, EXPERTS_PER_GROUP, D_EXPERT, d), BETA * D_EXPERT ** -0.5)
    ln2_g = 1.0 + nrm(ks[16], (DEPTH, d), 0.01)
    ln2_b = nrm(ks[17], (DEPTH, d), 0.01)
    return {'x': x, 'w_in': w_in, 'b_gate': b_gate, 'rel_bias': rel_bias,
            'w_br_sb': w_br_sb, 'w_br_ca': w_br_ca, 'w_out': w_out,
            'ln1_g': ln1_g, 'ln1_b': ln1_b, 'w_group': w_group, 'b_group': b_group,
            'w_erouter': w_erouter, 'b_erouter': b_erouter,
            'w_gate': w_gate, 'w_up': w_up, 'w_down': w_down,
            'ln2_g': ln2_g, 'ln2_b': ln2_b}


def reference(x, w_in, b_gate, rel_bias, w_br_sb, w_br_ca, w_out, ln1_g, ln1_b,
              w_group, b_group, w_erouter, b_erouter, w_gate, w_up, w_down, ln2_g, ln2_b):
    for l in range(DEPTH):
        x = _layer(x, w_in[l], b_gate[l], rel_bias[l], w_br_sb[l], w_br_ca[l], w_out[l],
                   ln1_g[l], ln1_b[l], w_group[l], b_group[l], w_erouter[l], b_erouter[l],
                   w_gate[l], w_up[l], w_down[l], ln2_g[l], ln2_b[l])
    return x
```

```python
import numpy as np
from contextlib import ExitStack
import concourse.bass as bass
import concourse.mybir as mybir
from concourse.bass_utils import run_bass_kernel_spmd

F32 = mybir.dt.float32
BF16 = mybir.dt.bfloat16
AF = mybir.ActivationFunctionType
ALU = mybir.AluOpType
AX = mybir.AxisListType

D_MODEL = 1024
BATCH = 2
SEQ = 8192
DEPTH = 2
HD = 64
ALPHA = (2.0 * DEPTH) ** 0.25
LN_EPS = 1e-5
NEG = -30000.0
HEAT = 0


class Sched:
    ENG = ("pe", "act", "dve", "pool", "sp")

    def __init__(self, nc):
        self.nc = nc
        self.ops = []
        self.last_w = {}
        self.readers = {}
        self.chan_last = {}
        self.chans = []
        self.eng_last = {}
        self.bar = set()

    def barrier(self):
        self.bar = set(self.eng_last.values()) | set(self.chan_last.values())

    def add(self, eng, fn, reads=(), writes=(), chan=None, inc=16):
        idx = len(self.ops)
        deps = set(self.bar)
        for r in reads:
            if r in self.last_w:
                deps.add(self.last_w[r])
        for w in writes:
            if w in self.last_w:
                deps.add(self.last_w[w])
            for rd in self.readers.get(w, ()):
                deps.add(rd)
        if chan is not None:
            if chan in self.chan_last:
                deps.add(self.chan_last[chan])
            else:
                self.chans.append(chan)
            self.chan_last[chan] = idx
        for r in reads:
            self.readers.setdefault(r, []).append(idx)
        for w in writes:
            self.last_w[w] = idx
            self.readers[w] = []
        self.ops.append(dict(eng=eng, fn=fn, deps=deps, chan=chan, inc=inc))
        if chan is None:
            self.eng_last[eng] = idx
        return idx

    def emit(self, block, sems, final_wait_eng="sp"):
        ops = self.ops
        n = len(ops)
        has_dep = [False] * n
        for o in ops:
            for d in o["deps"]:
                has_dep[d] = True
        eng_cnt = {e: 0 for e in self.ENG}
        chan_cnt = {}
        sig = [None] * n
        for i, o in enumerate(ops):
            if o["chan"] is not None:
                c = o["chan"]
                chan_cnt[c] = chan_cnt.get(c, 0) + o["inc"]
                sig[i] = ("ch:" + c, chan_cnt[c])
            elif has_dep[i]:
                e = o["eng"]
                eng_cnt[e] += 1
                sig[i] = (e, eng_cnt[e])
        per_eng = {e: [] for e in self.ENG}
        for i, o in enumerate(ops):
            per_eng[o["eng"]].append(i)
        self.eng_cnt = eng_cnt

        def run(ename, eng):
            seen = {}
            for i in per_eng[ename]:
                o = ops[i]
                need = {}
                for d in o["deps"]:
                    od = ops[d]
                    if od["chan"] is None and od["eng"] == "pe" and ename == "pe" and o["chan"] is None:
                        continue
                    s, v = sig[d]
                    if need.get(s, 0) < v:
                        need[s] = v
                for s, v in need.items():
                    if seen.get(s, 0) >= v:
                        continue
                    eng.wait_ge(sems[s], v)
                    seen[s] = v
                ins = o["fn"](eng)
                if sig[i] is not None:
                    s, v = sig[i]
                    ins.then_inc(sems[s], o["inc"] if o["chan"] is not None else 1)
            if ename == final_wait_eng:
                for c, v in chan_cnt.items():
                    if seen.get("ch:" + c, 0) < v:
                        eng.wait_ge(sems["ch:" + c], v)

        @block.tensor
        def _(e):
            run("pe", e)

        @block.scalar
        def _(e):
            run("act", e)

        @block.vector
        def _(e):
            run("dve", e)

        @block.gpsimd
        def _(e):
            run("pool", e)

        @block.sync
        def _(e):
            run("sp", e)


class Ctx:
    def __init__(self, nc, es):
        self.nc = nc
        self.es = es
        self.S = Sched(nc)
        self.sfx = ""

    def sb(self, name, shape, dt):
        return self.es.enter_context(self.nc.sbuf_tensor(name + self.sfx, shape, dt))

    def ps(self, name, shape, dt):
        return self.es.enter_context(self.nc.psum_tensor(name + self.sfx, shape, dt))

    def finish(self):
        nc, es, S = self.nc, self.es, self.S
        sems = {}
        for e in Sched.ENG:
            sems[e] = es.enter_context(nc.semaphore("sem_" + e))
        for c in S.chans:
            sems["ch:" + c] = es.enter_context(nc.semaphore("semch_" + c))
        block = es.enter_context(nc.Block())
        S.emit(block, sems)


def build_consts(cx):
    S = cx.S
    c = {}
    ident = cx.sb("ident", [128, 128], BF16)
    ident32 = cx.sb("ident32", [128, 128], F32)
    negtri = cx.sb("negtri", [128, 128], BF16)
    negones = cx.sb("negones", [128, 128], BF16)
    m01 = cx.sb("m01", [128, 896], BF16)
    mbias = cx.sb("mbias", [128, 896], BF16)
    S.add("pool", lambda e: e.memset(ident[:], 0.0), writes=["ident"])
    S.add("pool", lambda e: e.affine_select(out=ident[:], in_=ident[:], pattern=[[-1, 128]],
                                            compare_op=ALU.not_equal, fill=1.0, base=0,
                                            channel_multiplier=1), reads=["ident"], writes=["ident"])
    S.add("pool", lambda e: e.memset(ident32[:], 0.0), writes=["ident32"])
    S.add("pool", lambda e: e.affine_select(out=ident32[:], in_=ident32[:], pattern=[[-1, 128]],
                                            compare_op=ALU.not_equal, fill=1.0, base=0,
                                            channel_multiplier=1), reads=["ident32"], writes=["ident32"])
    S.add("pool", lambda e: e.memset(negtri[:], -1.0), writes=["negtri"])
    S.add("pool", lambda e: e.affine_select(out=negtri[:], in_=negtri[:], pattern=[[-1, 128]],
                                            compare_op=ALU.is_ge, fill=0.0, base=0,
                                            channel_multiplier=1), reads=["negtri"], writes=["negtri"])
    S.add("pool", lambda e: e.memset(negones[:], -1.0), writes=["negones"])
    S.add("pool", lambda e: e.memset(m01[:], 1.0), writes=["m01"])
    S.add("pool", lambda e: e.affine_select(out=m01[:], in_=m01[:], pattern=[[1, 896]],
                                            compare_op=ALU.is_gt, fill=0.0, base=-384,
                                            channel_multiplier=-1), reads=["m01"], writes=["m01"])
    S.add("pool", lambda e: e.memset(mbias[:], 0.0), writes=["mbias"])
    S.add("pool", lambda e: e.affine_select(out=mbias[:], in_=mbias[:], pattern=[[1, 896]],
                                            compare_op=ALU.is_gt, fill=NEG, base=-384,
                                            channel_multiplier=-1), reads=["mbias"], writes=["mbias"])
    ones32 = cx.sb("ones32", [1, 128], F32)
    S.add("pool", lambda e: e.memset(ones32[:], 1.0), writes=["ones32"])
    c["ones32"] = ones32
    c.update(ident=ident, ident32=ident32, negtri=negtri, negones=negones, m01=m01, mbias=mbias)
    return c


def MM(out, lhsT, rhs, start=True, stop=True, skip=False):
    if skip:
        return lambda e: e.matmul(out, lhsT=lhsT, rhs=rhs, start=start, stop=stop, skip_group_check=True)
    return lambda e: e.matmul(out, lhsT=lhsT, rhs=rhs, start=start, stop=stop)


def ACT(out, in_, func, **kw):
    return lambda e: e.activation(out=out, in_=in_, func=func, **kw)


def TT(out, in0, in1, op):
    return lambda e: e.tensor_tensor(out=out, in0=in0, in1=in1, op=op)


def TS(out, in0, s1, s2, op0, op1=None):
    if op1 is None:
        return lambda e: e.tensor_scalar(out=out, in0=in0, scalar1=s1, scalar2=s2, op0=op0)
    return lambda e: e.tensor_scalar(out=out, in0=in0, scalar1=s1, scalar2=s2, op0=op0, op1=op1)


def STT(out, in0, scalar, in1, op0, op1):
    return lambda e: e.scalar_tensor_tensor(out=out, in0=in0, scalar=scalar, in1=in1, op0=op0, op1=op1)


def CP(out, in_):
    return lambda e: e.tensor_copy(out=out, in_=in_)


def DMA(out, in_):
    return lambda e: e.dma_start(out=out, in_=in_)


def TR(out, in_, ident):
    return lambda e: e.transpose(out, in_, ident)


def RED(out, in_, op):
    return lambda e: e.tensor_reduce(out=out, in_=in_, axis=AX.X, op=op)


def MS(ap, val):
    return lambda e: e.memset(ap, val)


def RCP(out, in_):
    return lambda e: e.reciprocal(out=out, in_=in_)


def k1_body(cx, cst, S_len, x_bf16, xT, w1, cab, ot_sb, ot_ca, xt_tile=None, emit_out=None):
    nc, S = cx.nc, cx.S
    NT = S_len // 256
    NB = S_len // 128
    NQ = S_len // 512
    KC = D_MODEL // 128

    QT_sb = cx.sb("QT_sb", [128, S_len], BF16)
    KT_sb = cx.sb("KT_sb", [128, S_len], BF16)
    QT_ca = cx.sb("QT_ca", [128, S_len], BF16)
    KT_ca = cx.sb("KT_ca", [128, S_len], BF16)
    V_all = cx.sb("V_all", [128, NB, 256], BF16)
    x32 = [cx.sb(f"x32_{i}", [128, KC, 256], F32) for i in range(2)]
    xbf = [cx.sb(f"xbf_{i}", [128, KC, 256], BF16) for i in range(2)]
    wbf = cx.sb("wbf", [128, KC, 768], BF16)
    cabs = cx.sb("cabs", [128, 2, 640], F32)

    banks = [cx.ps(f"bank{i}", [128, 512], F32) for i in range(7)]
    bankT = cx.ps("bankT", [128, 512], F32)

    if xt_tile is None:
        xT_v = xT.rearrange("(kc p) s -> p kc s", p=128)
        xt_tile = lambda t: xT_v[:, :, t * 256:(t + 1) * 256]
    w1_v = w1.rearrange("(kc p) n -> p kc n", p=128)

    S.add("sp", DMA(cabs[:], cab.rearrange("h q k -> q h k")), writes=["cabs"], chan="cab")

    for pc in range(3):
        buf = x32[pc % 2]
        nm = f"x32_{pc % 2}"
        S.add("sp", DMA(buf[:], w1_v[:, :, pc * 256:(pc + 1) * 256]), writes=[nm], chan=f"x{pc % 2}")
        S.add("dve", CP(wbf[:, :, pc * 256:(pc + 1) * 256], buf[:]), reads=[nm], writes=[f"wbf{pc}"])
    wres = ["wbf0", "wbf1", "wbf2"]

    dsts = [QT_sb, KT_sb, QT_ca, KT_ca]
    dnames = ["QT_sb", "KT_sb", "QT_ca", "KT_ca"]
    for t in range(NT):
        i2 = t % 2
        xb = xbf[i2]
        xbn = f"xbf_{i2}"
        tsl = slice(t * 256, (t + 1) * 256)
        if x_bf16:
            S.add("sp", DMA(xb[:], xt_tile(t)), writes=[xbn], chan=f"x{i2}")
            xr = [xbn]
        else:
            xs = x32[i2]
            xsn = f"x32_{i2}"
            S.add("sp", DMA(xs[:], xt_tile(t)), writes=[xsn], chan=f"x{i2}")
            S.add("dve", CP(xb[:, 0:4, :], xs[:, 0:4, :]), reads=[xsn], writes=[xbn + "a"])
            S.add("pool", CP(xb[:, 4:8, :], xs[:, 4:8, :]), reads=[xsn], writes=[xbn + "b"])
            xr = [xbn + "a", xbn + "b"]
        for g in range(4):
            bk = banks[g]
            bkn = f"bank{g}"
            for kc in range(KC):
                S.add("pe", MM(bk[:, 0:256], wbf[:, kc, g * 128:(g + 1) * 128], xb[:, kc, :],
                               start=(kc == 0), stop=(kc == KC - 1)), reads=wres + xr, writes=[bkn])
            dst = dsts[g]
            if g in (0, 2):
                S.add("act", ACT(dst[:, tsl], bk[:, 0:256], AF.Identity, scale=0.125),
                      reads=[bkn], writes=[(dnames[g], t // 2)])
            else:
                S.add("dve", CP(dst[:, tsl], bk[:, 0:256]), reads=[bkn], writes=[(dnames[g], t // 2)])
        for sub in range(2):
            bk = banks[4 + sub]
            bkn = f"bank{4 + sub}"
            for kc in range(KC):
                S.add("pe", MM(bk[:, 0:256], xb[:, kc, sub * 128:(sub + 1) * 128], wbf[:, kc, 512:768],
                               start=(kc == 0), stop=(kc == KC - 1)), reads=wres + xr, writes=[bkn])
            blk = t * 2 + sub
            if sub == 0:
                S.add("dve", CP(V_all[:, blk, :], bk[:, 0:256]), reads=[bkn], writes=[("V", blk)])
            else:
                S.add("act", ACT(V_all[:, blk, :], bk[:, 0:256], AF.Identity), reads=[bkn], writes=[("V", blk)])

    negtri, negones, ident, m01, mbias = cst["negtri"], cst["negones"], cst["ident"], cst["m01"], cst["mbias"]
    E32 = [cx.sb(f"E32_{i}", [128, 512], F32) for i in range(2)]
    SPb = [cx.sb(f"SP_{i}", [128, 512], BF16) for i in range(3)]
    Wb = [cx.sb(f"W_{i}", [128, 512], BF16) for i in range(2)]
    C32 = cx.sb("C32", [128, 512], F32)
    Cbf = [cx.sb(f"Cbf_{i}", [128, 512], BF16) for i in range(4)]
    osb = [cx.sb(f"osb_{i}", [64, 512], BF16) for i in range(2)]
    blocks = []
    oi = 0
    for h in range(2):
        for qb in range(NQ):
            n = 4 * (qb + 1)
            for i in range(n):
                blocks.append(dict(h=h, qb=qb, i=i, n=n, kb=n - 1 - i, oi=oi))
            oi += 1
    NBK = len(blocks)

    def R(g):
        bl = blocks[g]
        h, qb, kb = bl["h"], bl["qb"], bl["kb"]
        d_ = dict(bl)
        d_.update(hp=slice(h * 64, (h + 1) * 64), qs=slice(qb * 512, (qb + 1) * 512),
                  pz=banks[g % 6], pzn=f"bank{g % 6}", E=E32[g % 2], En=f"E32_{g % 2}",
                  SP=SPb[g % 3], SPn=f"SP_{g % 3}", W=Wb[g % 2], Wn=f"W_{g % 2}",
                  C=Cbf[g % 4], Cn=f"Cbf_{g % 4}", Cp=Cbf[(g + 1) % 4], Cpn=f"Cbf_{(g + 1) % 4}",
                  po=banks[6] if bl["oi"] % 2 == 0 else bankT, pon="bank6" if bl["oi"] % 2 == 0 else "bankT",
                  diag=kb >= 4 * qb, off=(kb - 4 * qb) * 128)
        return d_

    for t in range(NBK + 5):
        g = t
        if g < NBK:
            r = R(g)
            S.add("pe", MM(r["pz"][:], KT_sb[r["hp"], r["kb"] * 128:(r["kb"] + 1) * 128], QT_sb[r["hp"], r["qs"]]),
                  reads=[("KT_sb", r["kb"] // 4), ("QT_sb", r["qb"])], writes=[r["pzn"]])
        g = t - 1
        if 0 <= g < NBK:
            r = R(g)
            S.add("act", ACT(r["E"][:], r["pz"][:], AF.Exp), reads=[r["pzn"]], writes=[r["En"]])
        g = t - 2
        if 0 <= g < NBK:
            r = R(g)
            off = r["off"]
            S.add("act", ACT(r["SP"][:], r["E"][:], AF.Ln, bias=1.0), reads=[r["En"]], writes=[r["SPn"]])
            if r["diag"]:
                S.add("dve", TT(r["SP"][:], r["SP"][:], m01[:, 384 - off:896 - off], ALU.mult),
                      reads=[r["SPn"], "m01"], writes=[r["SPn"]])
            if r["i"] < r["n"] - 1:
                if r["i"] == 0:
                    S.add("dve", CP(C32[:], r["SP"][:]), reads=[r["SPn"]], writes=["C32"])
                    S.add("dve", CP(r["Cp"][:], r["SP"][:]), reads=[r["SPn"]], writes=[r["Cpn"]])
                else:
                    S.add("dve", TT(C32[:], C32[:], r["SP"][:], ALU.add), reads=[r["SPn"], "C32"], writes=["C32"])
                    S.add("dve", CP(r["Cp"][:], C32[:]), reads=["C32"], writes=[r["Cpn"]])
        g = t - 3
        if 0 <= g < NBK:
            r = R(g)
            off = r["off"]
            S.add("pe", MM(r["pz"][:], negtri[:], r["SP"][:], start=False, stop=False, skip=True),
                  reads=[r["SPn"], "negtri"], writes=[r["pzn"]])
            if r["i"] > 0:
                S.add("pe", MM(r["pz"][:], negones[:], r["C"][:], start=False, stop=False, skip=True),
                      reads=[r["Cn"], "negones"], writes=[r["pzn"]])
            if r["diag"]:
                S.add("pe", MM(r["pz"][:], ident[:], mbias[:, 384 - off:896 - off],
                               start=False, stop=False, skip=True),
                      reads=["ident", "mbias"], writes=[r["pzn"]])
        g = t - 4
        if 0 <= g < NBK:
            r = R(g)
            S.add("act", ACT(r["W"][:], r["pz"][:], AF.Exp), reads=[r["pzn"]], writes=[r["Wn"]])
        g = t - 5
        if 0 <= g < NBK:
            r = R(g)
            h = r["h"]
            po, pon = r["po"], r["pon"]
            S.add("pe", MM(po[0:64, 0:512], V_all[:, r["kb"], h * 64:(h + 1) * 64], r["W"][:],
                           start=(r["i"] == 0), stop=(r["i"] == r["n"] - 1)),
                  reads=[r["Wn"], ("V", r["kb"])], writes=[pon])
            for _ in range(HEAT):
                S.add("pe", MM(po[64:128, 0:512], negones[:, 0:64], m01[:, 0:512]), reads=["negones", "m01"])
            if r["i"] == r["n"] - 1:
                ob = osb[r["oi"] % 2]
                obn = f"osb_{r['oi'] % 2}"
                S.add("dve", CP(ob[:], po[0:64, 0:512]), reads=[pon], writes=[obn])
                if emit_out is not None:
                    emit_out(0, h, r["qb"], ob, obn)
                else:
                    S.add("sp", DMA(ot_sb[h * 64:(h + 1) * 64, r["qs"]], ob[:]), reads=[obn], chan=f"o{r['oi'] % 2}")

    T32 = [cx.sb(f"T32_{i}", [128, 640], F32) for i in range(2)]
    Pb = [cx.sb(f"P_{i}", [128, 640], BF16) for i in range(2)]
    Pn = [cx.sb(f"Pn_{i}", [128, 640], BF16) for i in range(2)]
    WT = [cx.sb(f"WT_{i}", [128, 640], BF16) for i in range(2)]
    st = [cx.sb(f"st_{i}", [128, 4], F32) for i in range(2)]
    oca = [cx.sb(f"oca_{i}", [64, 512], BF16) for i in range(2)]
    ci = 0
    po = banks[6]
    bankTb = bankT[:].bitcast(BF16)
    for h in range(2):
        hp = slice(h * 64, (h + 1) * 64)
        for m in range(NB):
            nk = min(640, 128 * (m + 1))
            ks = 128 * (m + 1) - nk
            p0 = 640 - nk
            n1 = min(nk, 512)
            i2 = ci % 2
            pa = banks[i2 * 2]
            pan = f"bank{i2 * 2}"
            pb = banks[i2 * 2 + 1]
            pbn = f"bank{i2 * 2 + 1}"
            T, Tn = T32[i2], f"T32_{i2}"
            P, Pnm = Pb[i2], f"P_{i2}"
            PN, PNn = Pn[i2], f"Pn_{i2}"
            W, Wn = WT[i2], f"WT_{i2}"
            sx, sxn = st[i2], f"st_{i2}"
            qsl = slice(m * 128, (m + 1) * 128)
            krd = [("KT_ca", j) for j in range(ks // 512, (ks + nk - 1) // 512 + 1)]
            S.add("pe", MM(pa[:, 0:n1], QT_ca[hp, qsl], KT_ca[hp, ks:ks + n1]),
                  reads=[("QT_ca", m // 4)] + krd, writes=[pan])
            S.add("dve", TT(T[:, 0:n1], pa[:, 0:n1], cabs[:, h, p0:p0 + n1], ALU.add),
                  reads=[pan, "cabs"], writes=[Tn])
            if nk > 512:
                S.add("pe", MM(pb[:, 0:128], QT_ca[hp, qsl], KT_ca[hp, ks + 512:ks + 640]),
                      reads=[("QT_ca", m // 4)] + krd, writes=[pbn])
                S.add("dve", TT(T[:, 512:640], pb[:, 0:128], cabs[:, h, 512:640], ALU.add),
                      reads=[pbn, "cabs", Tn], writes=[Tn])
            S.add("dve", RED(sx[:, 0:1], T[:, 0:nk], ALU.max), reads=[Tn], writes=[sxn])
            S.add("dve", TS(sx[:, 1:2], sx[:, 0:1], -1.0, None, ALU.mult), reads=[sxn], writes=[sxn])
            S.add("pool", MS(sx[:, 2:3], 0.0), reads=[sxn], writes=[sxn])
            S.add("act", ACT(P[:, 0:nk], T[:, 0:nk], AF.Exp, bias=sx[:, 1:2], accum_out=sx[:, 2:3]),
                  reads=[Tn, sxn], writes=[Pnm, sxn])
            S.add("dve", RCP(sx[:, 3:4], sx[:, 2:3]), reads=[sxn], writes=[sxn])
            S.add("dve", TS(PN[:, 0:nk], P[:, 0:nk], sx[:, 3:4], None, ALU.mult),
                  reads=[Pnm, sxn], writes=[PNn])
            nj = nk // 128
            for j in range(nj):
                S.add("pe", TR(bankTb[:, j * 128:(j + 1) * 128], PN[:, j * 128:(j + 1) * 128], ident[:]),
                      reads=[PNn, "ident"], writes=["bankT"])
            S.add("act", ACT(W[:, 0:nk], bankTb[:, 0:nk], AF.Identity), reads=["bankT"], writes=[Wn])
            mcol = (m % 4) * 128
            for j in range(nj):
                kb = ks // 128 + j
                S.add("pe", MM(po[0:64, mcol:mcol + 128], V_all[:, kb, 128 + h * 64:128 + (h + 1) * 64],
                               W[:, j * 128:(j + 1) * 128], start=(j == 0), stop=(j == nj - 1)),
                      reads=[Wn, ("V", kb)], writes=["bank6"])
            if m % 4 == 3:
                oq = (h * NB + m) // 4
                ob, obn = oca[oq % 2], f"oca_{oq % 2}"
                S.add("act", ACT(ob[:], po[0:64, :], AF.Identity), reads=["bank6"], writes=[obn])
                if emit_out is not None:
                    emit_out(1, h, m // 4, ob, obn)
                else:
                    S.add("sp", DMA(ot_ca[h * 64:(h + 1) * 64, (m - 3) * 128:(m + 1) * 128], ob[:]),
                          reads=[obn], chan=f"oc{oq % 2}")
            ci += 1


def layer_norm_tile(cx, S, u, un, out, outn, gbc, bbc, st, stn, junk, junkn, gn, tmp=None, tmpn=None):
    D = D_MODEL
    S.add("pool", MS(st[:, 0:2], 0.0), reads=[stn], writes=[stn])
    S.add("act", ACT(junk[:], u, AF.Identity, accum_out=st[:, 0:1]), reads=[un, stn], writes=[junkn, stn])
    S.add("act", ACT(junk[:], u, AF.Square, accum_out=st[:, 1:2]), reads=[un, stn, junkn], writes=[junkn, stn])
    S.add("dve", TS(st[:, 2:4], st[:, 0:2], 1.0 / D, None, ALU.mult), reads=[stn], writes=[stn])
    S.add("dve", STT(st[:, 4:5], st[:, 2:3], st[:, 2:3], st[:, 3:4], ALU.mult, ALU.subtract),
          reads=[stn], writes=[stn])
    S.add("dve", TS(st[:, 5:6], st[:, 4:5], -1.0, LN_EPS, ALU.mult, ALU.add), reads=[stn], writes=[stn])
    S.add("act", ACT(st[:, 5:6], st[:, 5:6], AF.Ln), reads=[stn], writes=[stn])
    S.add("act", ACT(st[:, 6:7], st[:, 5:6], AF.Exp, scale=-0.5), reads=[stn], writes=[stn])
    S.add("dve", STT(st[:, 7:8], st[:, 2:3], -1.0, st[:, 6:7], ALU.mult, ALU.mult), reads=[stn], writes=[stn])
    S.add("act", ACT(out, u, AF.Identity, scale=st[:, 6:7], bias=st[:, 7:8]), reads=[un, stn], writes=[outn])
    S.add("dve", TT(out, out, gbc[:], ALU.mult), reads=[outn, gn], writes=[outn])
    S.add("pool", TT(out, out, bbc[:], ALU.add), reads=[outn, gn], writes=[outn])


def k2_body(cx, cst, NTOK, x_bf16, d):
    nc, S = cx.nc, cx.S
    KC = D_MODEL // 128
    TS_ = min(512, NTOK)
    NST = NTOK // TS_
    NSUB = TS_ // 128
    NS = NTOK // 128
    ident32 = cst["ident32"]
    banks = [cx.ps(f"kb{i}", [128, 512], F32) for i in range(8)]
    bn = [f"kb{i}" for i in range(8)]

    lnbc = {}
    for nm in ("ln1_g", "ln1_b", "ln2_g", "ln2_b"):
        t = cx.sb("bc_" + nm, [128, D_MODEL], F32)
        S.add("sp", DMA(t[:], d[nm].partition_broadcast(128)), writes=["lnbc"], chan="prm")
        lnbc[nm] = t
    bg = cx.sb("bg", [128, 16], F32)
    S.add("sp", lambda e, o_=bg[:], i_=d["b_gate"].rearrange("(j p) -> p j", p=128): e.dma_start(
        out=o_, in_=i_, allow_slow_non_contiguous=True), writes=["bg"], chan="prm")
    wr32 = cx.sb("wr32", [128, KC, 36], F32)
    S.add("sp", DMA(wr32[:], d["w_router"].rearrange("(kc p) n -> p kc n", p=128)), writes=["wr32"], chan="prm")
    brt = cx.sb("brt", [128, 36], F32)
    S.add("sp", DMA(brt[:], d["b_router"].partition_broadcast(128)), writes=["brt"], chan="prm")

    mT_d = d["mT_scratch"]
    xT_v = d["xT"].rearrange("(kc p) s -> p kc s", p=128)
    if d.get("y_rs") is None:
        ysb_v = d["yT_sb"].rearrange("(kc p) s -> p kc s", p=128)
        yca_v = d["yT_ca"].rearrange("(kc p) s -> p kc s", p=128)

    with ExitStack() as es:
        sb = lambda name, shape, dt: es.enter_context(nc.sbuf_tensor(name + cx.sfx, shape, dt))
        wg_bf = sb("wg_bf", [128, KC, 2048], BF16)
        wbs_bf = sb("wbs_bf", [128, 4, 1024], BF16)
        wbc_bf = sb("wbc_bf", [128, 4, 1024], BF16)
        stg = [sb(f"stgA{i}", [128, 2048], F32) for i in range(2)]
        xbf = [sb(f"xbfA{i}", [128, KC, TS_], BF16) for i in range(2)]
        ysb = [sb(f"ysb{i}", [128, 4, TS_], BF16) for i in range(2)]
        yca = [sb(f"yca{i}", [128, 4, TS_], BF16) for i in range(2)]
        gs = [sb(f"gs{i}", [128, TS_], F32) for i in range(2)]
        gc = [sb(f"gc{i}", [128, TS_], F32) for i in range(2)]
        t1 = sb("t1", [128, TS_], F32)
        t2 = sb("t2", [128, TS_], F32)
        mT = [sb(f"mT{i}", [128, KC, TS_], BF16) for i in range(2)]
        si = 0

        def stage_cast(dst, src_ap, ncols, rn, k=None):
            nonlocal si
            b = si % 2
            si += 1
            sv = stg[b][:, 0:ncols]
            if k is not None:
                sv = sv.rearrange("p (k t) -> p k t", k=k)
            S.add("sp", DMA(sv, src_ap), writes=[f"stgA{b}"], chan=f"stgA{b}")
            S.add("dve" if b == 0 else "pool", CP(dst, sv), reads=[f"stgA{b}"], writes=[rn])

        wg_v = d["wg"].rearrange("(kc p) n -> p kc n", p=128)
        for kc in range(KC):
            stage_cast(wg_bf[:, kc, :], wg_v[:, kc, :], 2048, "wg_bf")
        wbs_v = d["w_br_sb"].rearrange("(kc p) n -> p kc n", p=128)
        wbc_v = d["w_br_ca"].rearrange("(kc p) n -> p kc n", p=128)
        for kc in range(0, 4, 2):
            stage_cast(wbs_bf[:, kc:kc + 2, :], wbs_v[:, kc:kc + 2, :], 2048, "wbs_bf", k=2)
            stage_cast(wbc_bf[:, kc:kc + 2, :], wbc_v[:, kc:kc + 2, :], 2048, "wbc_bf", k=2)
        for T in range(NST):
            i2 = T % 2
            tsl = slice(T * TS_, (T + 1) * TS_)
            xb, xbn = xbf[i2], f"xbfA{i2}"
            if x_bf16:
                S.add("sp", DMA(xb[:], xT_v[:, :, tsl]), writes=[xbn], chan=f"xA{i2}")
            else:
                per = 2048 // TS_
                for k0 in range(0, KC, per):
                    stage_cast(xb[:, k0:k0 + per, :], xT_v[:, k0:k0 + per, tsl], per * TS_, xbn, k=per)
            if d.get("y_rs") is not None:
                yv = d["y_rs"][T].rearrange("(b kc p) s -> b p kc s", b=2, p=128)
                per = 2048 // TS_
                for k0 in range(0, 4, per):
                    stage_cast(ysb[i2][:, k0:k0 + per, :], yv[0][:, k0:k0 + per, :], per * TS_, f"ysb{i2}", k=per)
                    stage_cast(yca[i2][:, k0:k0 + per, :], yv[1][:, k0:k0 + per, :], per * TS_, f"yca{i2}", k=per)
            else:
                S.add("sp", DMA(ysb[i2][:], ysb_v[:, :, tsl]), writes=[f"ysb{i2}"], chan=f"yA{i2}")
                S.add("sp", DMA(yca[i2][:], yca_v[:, :, tsl]), writes=[f"yca{i2}"], chan=f"yB{i2}")
            mt, mtn = mT[i2], f"mT{i2}"
            for fo in range(KC):
                j2 = fo % 2
                fsl = slice(fo * 128, (fo + 1) * 128)
                fsl2 = slice(1024 + fo * 128, 1024 + (fo + 1) * 128)
                b0, b1, b2, b3 = j2 * 4, j2 * 4 + 1, j2 * 4 + 2, j2 * 4 + 3
                for kc in range(KC):
                    S.add("pe", MM(banks[b0][:, 0:TS_], wg_bf[:, kc, fsl], xb[:, kc, :],
                                   start=(kc == 0), stop=(kc == KC - 1)), reads=["wg_bf", xbn], writes=[bn[b0]])
                S.add("act", ACT(gs[j2][:], banks[b0][:, 0:TS_], AF.Sigmoid, bias=bg[:, fo:fo + 1]),
                      reads=[bn[b0], "bg"], writes=[f"gs{j2}"])
                for kc in range(KC):
                    S.add("pe", MM(banks[b1][:, 0:TS_], wg_bf[:, kc, fsl2], xb[:, kc, :],
                                   start=(kc == 0), stop=(kc == KC - 1)), reads=["wg_bf", xbn], writes=[bn[b1]])
                S.add("act", ACT(gc[j2][:], banks[b1][:, 0:TS_], AF.Sigmoid, bias=bg[:, 8 + fo:9 + fo]),
                      reads=[bn[b1], "bg"], writes=[f"gc{j2}"])
                for kc in range(4):
                    S.add("pe", MM(banks[b2][:, 0:TS_], wbs_bf[:, kc, fsl], ysb[i2][:, kc, :],
                                   start=(kc == 0), stop=(kc == 3)), reads=["wbs_bf", f"ysb{i2}"], writes=[bn[b2]])
                for kc in range(4):
                    S.add("pe", MM(banks[b3][:, 0:TS_], wbc_bf[:, kc, fsl], yca[i2][:, kc, :],
                                   start=(kc == 0), stop=(kc == 3)), reads=["wbc_bf", f"yca{i2}"], writes=[bn[b3]])
                S.add("dve", TT(t1[:], gs[j2][:], banks[b2][:, 0:TS_], ALU.mult),
                      reads=[f"gs{j2}", bn[b2]], writes=["t1"])
                S.add("dve", TT(t2[:], gc[j2][:], banks[b3][:, 0:TS_], ALU.mult),
                      reads=[f"gc{j2}", bn[b3]], writes=["t2"])
                S.add("pool", TT(mt[:, fo, :], t1[:], t2[:], ALU.add), reads=["t1", "t2"], writes=[mtn])
            S.add("sp", DMA(mT_d[:, :, tsl], mt[:]), reads=[mtn], writes=["mT_d"], chan=f"mTo{i2}")
    S.barrier()

    yacc = cx.sb("yacc", [128, NS, D_MODEL], F32)
    X1T = cx.sb("X1T", [128, KC, NTOK], BF16)
    combT = cx.sb("combT", [32, NTOK], F32)

    with ExitStack() as es:
        sb = lambda name, shape, dt: es.enter_context(nc.sbuf_tensor(name + cx.sfx, shape, dt))
        wo_bf = sb("wo_bf", [128, KC, 1024], BF16)
        stg = [sb(f"stgB{i}", [128, 2048], F32) for i in range(2)]
        wo_v = d["w_out"].rearrange("(kc p) n -> p kc n", p=128)
        for k0 in range(0, KC, 2):
            b = (k0 // 2) % 2
            sv = stg[b][:].rearrange("p (k t) -> p k t", k=2)
            S.add("sp", DMA(sv, wo_v[:, k0:k0 + 2, :]), writes=[f"stgB{b}"], chan=f"stgB{b}")
            S.add("dve" if b == 0 else "pool", CP(wo_bf[:, k0:k0 + 2, :], sv),
                  reads=[f"stgB{b}"], writes=["wo_bf"])
        mTt = [sb(f"mTt{i}", [128, KC, 128], BF16) for i in range(2)]
        xtok = [sb(f"xtok{i}", [128, D_MODEL], F32) for i in range(2)]
        u = [sb(f"u{i}", [128, D_MODEL], F32) for i in range(2)]
        x1 = [sb(f"x1_{i}", [128, D_MODEL], F32) for i in range(2)]
        junk = sb("junk", [128, D_MODEL], BF16)
        x1T32 = sb("x1T32", [128, KC, 128], F32)
        stt = [sb(f"lnst{i}", [128, 8], F32) for i in range(2)]
        rt = [sb(f"rt{i}", [128, 128], F32) for i in range(2)]
        comb = [sb(f"comb{i}", [128, 32], F32) for i in range(2)]
        for sI in range(NS):
            i2 = sI % 2
            tok = slice(sI * 128, (sI + 1) * 128)
            S.add("sp", DMA(mTt[i2][:], mT_d[:, :, tok]), reads=["mT_d"], writes=[f"mTt{i2}"], chan=f"mTi{i2}")
            S.add("sp", DMA(xtok[i2][:], d["x_tok"][tok, :]), writes=[f"xtok{i2}"], chan=f"xt{i2}")
            for half in range(2):
                bk = banks[half]
                for kc in range(KC):
                    S.add("pe", MM(bk[:], mTt[i2][:, kc, :], wo_bf[:, kc, half * 512:(half + 1) * 512],
                                   start=(kc == 0), stop=(kc == KC - 1)),
                          reads=[f"mTt{i2}", "wo_bf"], writes=[bn[half]])
                S.add("dve", STT(u[i2][:, half * 512:(half + 1) * 512], xtok[i2][:, half * 512:(half + 1) * 512],
                                 ALPHA, bk[:], ALU.mult, ALU.add),
                      reads=[f"xtok{i2}", bn[half]], writes=[f"u{i2}"])
            layer_norm_tile(cx, S, u[i2][:], f"u{i2}", x1[i2][:], f"x1_{i2}", lnbc["ln1_g"], lnbc["ln1_b"],
                            stt[i2], f"lnst{i2}", junk, "junk", "lnbc")
            S.add("pool", TS(yacc[:, sI, :], x1[i2][:], ALPHA, None, ALU.mult),
                  reads=[f"x1_{i2}"], writes=[("yacc", sI)])
            for kc in range(KC):
                bk = banks[2 + (kc // 4)]
                S.add("pe", TR(bk[:, (kc % 4) * 128:(kc % 4 + 1) * 128], x1[i2][:, kc * 128:(kc + 1) * 128],
                               ident32[:]), reads=[f"x1_{i2}", "ident32"], writes=[bn[2 + kc // 4]])
            for q in range(2):
                S.add("act", ACT(x1T32[:, q * 4:(q + 1) * 4, :],
                                 banks[2 + q][:].rearrange("p (k t) -> p k t", k=4), AF.Identity),
                      reads=[bn[2 + q]], writes=["x1T32"])
            S.add("pool", CP(X1T[:, :, tok], x1T32[:]), reads=["x1T32"], writes=[("X1T", sI)])
            lg = banks[4]
            for kc in range(KC):
                S.add("pe", MM(lg[:, 0:36], x1T32[:, kc, :], wr32[:, kc, :], start=(kc == 0), stop=(kc == KC - 1)),
                      reads=["x1T32", "wr32"], writes=[bn[4]])
            R_, rn = rt[i2], f"rt{i2}"
            L = R_[:, 0:36]

            def V(eng, fn):
                S.add(eng, fn, reads=[rn, "brt", bn[4]] if eng != "pool" else [rn, "brt"], writes=[rn])
            V("dve", TT(L, lg[:, 0:36], brt[:], ALU.add))
            gmax, ngmax, sg, gval = R_[:, 36:37], R_[:, 37:38], R_[:, 38:39], R_[:, 39:40]
            ohg, eg, esel = R_[:, 40:44], R_[:, 44:48], R_[:, 48:56]
            m1, oh1, e2, m2, oh2 = R_[:, 56:57], R_[:, 64:72], R_[:, 72:80], R_[:, 57:58], R_[:, 80:88]
            dd, ed, den, w1, w2 = R_[:, 58:59], R_[:, 59:60], R_[:, 60:61], R_[:, 61:62], R_[:, 62:63]
            ew, gw = R_[:, 88:96], R_[:, 96:100]
            V("dve", RED(gmax, L[:, 0:4], ALU.max))
            V("dve", TS(ohg, L[:, 0:4], gmax, None, ALU.is_equal))
            V("dve", TS(ngmax, gmax, -1.0, None, ALU.mult))
            V("pool", MS(sg, 0.0))
            V("act", ACT(eg, L[:, 0:4], AF.Exp, bias=ngmax, accum_out=sg))
            V("dve", RCP(gval, sg))
            V("dve", TS(esel, L[:, 4:12], ohg[:, 0:1], None, ALU.mult))
            for g in range(1, 4):
                V("dve", STT(esel, L[:, 4 + 8 * g:12 + 8 * g], ohg[:, g:g + 1], esel, ALU.mult, ALU.add))
            V("dve", RED(m1, esel, ALU.max))
            V("dve", TS(oh1, esel, m1, None, ALU.is_equal))
            V("dve", STT(e2, oh1, -1e30, esel, ALU.mult, ALU.add))
            V("dve", RED(m2, e2, ALU.max))
            V("dve", TS(oh2, e2, m2, None, ALU.is_equal))
            V("dve", TT(dd, m2, m1, ALU.subtract))
            V("act", ACT(ed, dd, AF.Exp))
            V("dve", TS(den, ed, 1.0, None, ALU.add))
            V("dve", RCP(w1, den))
            V("dve", TT(w2, ed, w1, ALU.mult))
            V("dve", TS(ew, oh1, w1, None, ALU.mult))
            V("dve", STT(ew, oh2, w2, ew, ALU.mult, ALU.add))
            V("dve", TS(gw, ohg, gval, None, ALU.mult))
            cb_, cbn = comb[i2], f"comb{i2}"
            for g in range(4):
                S.add("dve", TS(cb_[:, 8 * g:8 * g + 8], ew, gw[:, g:g + 1], None, ALU.mult),
                      reads=[rn], writes=[cbn])
            S.add("pe", TR(banks[5][0:32, 0:128], cb_[:], ident32[:]), reads=[cbn, "ident32"], writes=[bn[5]])
            S.add("act", ACT(combT[:, tok], banks[5][0:32, 0:128], AF.Identity), reads=[bn[5]],
                  writes=[("combT", sI)])
    S.barrier()

    NE = 32
    with ExitStack() as es:
        sb = lambda name, shape, dt: es.enter_context(nc.sbuf_tensor(name + cx.sfx, shape, dt))
        wgt = [sb(f"wgt{i}", [128, KC, 256], BF16) for i in range(2)]
        wup = [sb(f"wup{i}", [128, KC, 256], BF16) for i in range(2)]
        wdn = [sb(f"wdn{i}", [128, 2, 1024], BF16) for i in range(2)]
        stg = [sb(f"stgC{i}", [128, 2048], F32) for i in range(3)]
        sel = [sb(f"sel{i}", [32, 128], F32) for i in range(2)]
        sl = [sb(f"sl{i}", [128, TS_], F32) for i in range(2)]
        tl = [sb(f"tl{i}", [128, TS_], F32) for i in range(2)]
        cbs = [sb(f"cbs{i}", [128, TS_], F32) for i in range(2)]
        hid = [sb(f"hid{i}", [128, 2, TS_], BF16) for i in range(2)]
        wg_v = d["w_gate"].rearrange("e (kc p) f -> e p kc f", p=128)
        wu_v = d["w_up"].rearrange("e (kc p) f -> e p kc f", p=128)
        wd_v = d["w_down"].rearrange("e (fc p) n -> e p fc n", p=128)
        it = 0
        pyi = 0
        for e_ in range(NE):
            b = e_ % 2
            S.add("sp", DMA(stg[0][:].rearrange("p (k f) -> p k f", k=KC), wg_v[e_]), writes=["stgC0"], chan="stgC0")
            S.add("act", ACT(wgt[b][:], stg[0][:].rearrange("p (k f) -> p k f", k=KC), AF.Identity),
                  reads=["stgC0"], writes=[f"wgt{b}"])
            S.add("sp", DMA(stg[1][:].rearrange("p (k f) -> p k f", k=KC), wu_v[e_]), writes=["stgC1"], chan="stgC1")
            S.add("act", ACT(wup[b][:], stg[1][:].rearrange("p (k f) -> p k f", k=KC), AF.Identity),
                  reads=["stgC1"], writes=[f"wup{b}"])
            S.add("sp", DMA(stg[2][:].rearrange("p (k f) -> p k f", k=2), wd_v[e_]), writes=["stgC2"], chan="stgC2")
            S.add("pool", CP(wdn[b][:], stg[2][:].rearrange("p (k f) -> p k f", k=2)),
                  reads=["stgC2"], writes=[f"wdn{b}"])
            S.add("pool", MS(sel[b][:], 0.0), writes=[f"sel{b}"])
            S.add("sp", DMA(sel[b][e_:e_ + 1, :], cst["ones32"][0:1, :]), reads=["ones32"], writes=[f"sel{b}"],
                  chan=f"sel{b}")
            for T in range(NST):
                i2 = it % 2
                it += 1
                tsl = slice(T * TS_, (T + 1) * TS_)
                xr = [("X1T", T * NSUB + q) for q in range(NSUB)]
                S.add("pe", MM(banks[4][:, 0:TS_], sel[b][:], combT[:, tsl]),
                      reads=[f"sel{b}"] + [("combT", T * NSUB + q) for q in range(NSUB)], writes=[bn[4]])
                S.add("act", ACT(cbs[i2][:], banks[4][:, 0:TS_], AF.Identity), reads=[bn[4]], writes=[f"cbs{i2}"])
                for fc in range(2):
                    hg, hu = banks[fc * 2], banks[fc * 2 + 1]
                    for kc in range(KC):
                        S.add("pe", MM(hg[:, 0:TS_], wgt[b][:, kc, fc * 128:(fc + 1) * 128], X1T[:, kc, tsl],
                                       start=(kc == 0), stop=(kc == KC - 1)),
                              reads=[f"wgt{b}"] + xr, writes=[bn[fc * 2]])
                    for kc in range(KC):
                        S.add("pe", MM(hu[:, 0:TS_], wup[b][:, kc, fc * 128:(fc + 1) * 128], X1T[:, kc, tsl],
                                       start=(kc == 0), stop=(kc == KC - 1)),
                              reads=[f"wup{b}"] + xr, writes=[bn[fc * 2 + 1]])
                    S.add("act", ACT(sl[fc][:], hg[:, 0:TS_], AF.Silu), reads=[bn[fc * 2]], writes=[f"sl{fc}"])
                    S.add("dve", TT(tl[fc][:], sl[fc][:], hu[:, 0:TS_], ALU.mult),
                          reads=[f"sl{fc}", bn[fc * 2 + 1]], writes=[f"tl{fc}"])
                    S.add("pool", TT(hid[i2][:, fc, :], tl[fc][:], cbs[i2][:], ALU.mult),
                          reads=[f"tl{fc}", f"cbs{i2}"], writes=[(f"hid{i2}", fc)])
                for sub in range(NSUB):
                    sI = T * NSUB + sub
                    for half in range(2):
                        py = banks[5 + pyi % 3]
                        pyn = bn[5 + pyi % 3]
                        pyi += 1
                        for fc in range(2):
                            S.add("pe", MM(py[:], hid[i2][:, fc, sub * 128:(sub + 1) * 128],
                                           wdn[b][:, fc, half * 512:(half + 1) * 512],
                                           start=(fc == 0), stop=(fc == 1)),
                                  reads=[(f"hid{i2}", 0), (f"hid{i2}", 1), f"wdn{b}"], writes=[pyn])
                        ysl = yacc[:, sI, half * 512:(half + 1) * 512]
                        S.add("dve", TT(ysl, ysl, py[:], ALU.add), reads=[pyn, ("yacc", sI)], writes=[("yacc", sI)])
    S.barrier()

    with ExitStack() as es:
        sb = lambda name, shape, dt: es.enter_context(nc.sbuf_tensor(name + cx.sfx, shape, dt))
        x2 = [sb(f"x2_{i}", [128, D_MODEL], F32) for i in range(2)]
        junk = sb("junk2", [128, D_MODEL], BF16)
        stt = [sb(f"lnst2_{i}", [128, 8], F32) for i in range(2)]
        xTo = [sb(f"xTo{i}", [128, KC, 128], BF16) for i in range(2)]
        want_T = d.get("xT_out") is not None
        if d.get("x_ar") is not None:
            xmk = [sb(f"xmk{i}", [128, 4, KC, 128], F32) for i in range(2)]
            pm = d["pm_tile"]
        xTo_v = d["xT_out"].rearrange("(kc p) s -> p kc s", p=128) if want_T else None
        for sI in range(NS):
            i2 = sI % 2
            tok = slice(sI * 128, (sI + 1) * 128)
            layer_norm_tile(cx, S, yacc[:, sI, :], ("yacc", sI), x2[i2][:], f"x2_{i2}", lnbc["ln2_g"], lnbc["ln2_b"],
                            stt[i2], f"lnst2_{i2}", junk, "junk2", "lnbc")
            S.add("sp", DMA(d["x_out"][tok, :], x2[i2][:]), reads=[f"x2_{i2}"], chan=f"xo{i2}")
            if not want_T:
                continue
            for kc in range(KC):
                bk = banks[(kc // 4)]
                S.add("pe", TR(bk[:, (kc % 4) * 128:(kc % 4 + 1) * 128], x2[i2][:, kc * 128:(kc + 1) * 128],
                               ident32[:]), reads=[f"x2_{i2}", "ident32"], writes=[bn[kc // 4]])
            for q in range(2):
                S.add("act" if q == 0 else "dve",
                      ACT(xTo[i2][:, q * 4:(q + 1) * 4, :], banks[q][:].rearrange("p (k t) -> p k t", k=4), AF.Identity)
                      if q == 0 else
                      CP(xTo[i2][:, q * 4:(q + 1) * 4, :], banks[q][:].rearrange("p (k t) -> p k t", k=4)),
                      reads=[bn[q]], writes=[(f"xTo{i2}", q)])
            S.add("sp", DMA(xTo_v[:, :, tok], xTo[i2][:]), reads=[(f"xTo{i2}", 0), (f"xTo{i2}", 1)], chan=f"xTo{i2}")
            if d.get("x_ar") is not None:
                xm = xmk[i2]
                for j in range(4):
                    S.add("pool" if j % 2 == 0 else "dve",
                          TS(xm[:, j, :, :], xTo[i2][:], pm[:, j:j + 1], None, ALU.mult),
                          reads=[(f"xTo{i2}", 0), (f"xTo{i2}", 1), "pm"], writes=[(f"xmk{i2}", j, 0), (f"xmk{i2}", j, 1)])
                cht = d["x_ar"][0].shape[1]
                cpq = NTOK // cht
                s8, c8 = (sI * 128) // cht, (sI * 128) % cht
                for j in range(4):
                    dst = d["x_ar"][j * cpq + s8].rearrange("(kc p) t -> p kc t", p=128)[:, :, c8:c8 + 128]
                    S.add("sp", DMA(dst, xm[:, j]), reads=[(f"xmk{i2}", j, q) for q in range(2)],
                          chan=f"xar{i2}")


def build_k2(NTOK, x_bf16):
    nc = bass.Bass("TRN2", target_bir_lowering=False)
    d = {}
    inp = lambda name, shape, dt=F32: nc.dram_tensor(name, shape, dt, kind="ExternalInput").ap()
    d["x_tok"] = inp("x_tok", [NTOK, D_MODEL])
    d["xT"] = inp("xT", [D_MODEL, NTOK], BF16 if x_bf16 else F32)
    d["yT_sb"] = inp("yT_sb", [512, NTOK], BF16)
    d["yT_ca"] = inp("yT_ca", [512, NTOK], BF16)
    d["wg"] = inp("wg", [D_MODEL, 2048])
    d["b_gate"] = inp("b_gate", [2048])
    d["w_br_sb"] = inp("w_br_sb", [512, D_MODEL])
    d["w_br_ca"] = inp("w_br_ca", [512, D_MODEL])
    d["w_out"] = inp("w_out", [D_MODEL, D_MODEL])
    for nm in ("ln1_g", "ln1_b", "ln2_g", "ln2_b"):
        d[nm] = inp(nm, [D_MODEL])
    d["w_router"] = inp("w_router", [D_MODEL, 36])
    d["b_router"] = inp("b_router", [36])
    d["w_gate"] = inp("w_gate", [32, D_MODEL, 256])
    d["w_up"] = inp("w_up", [32, D_MODEL, 256])
    d["w_down"] = inp("w_down", [32, 256, D_MODEL])
    d["x_out"] = nc.dram_tensor("x_out", [NTOK, D_MODEL], F32, kind="ExternalOutput").ap()
    d["xT_out"] = nc.dram_tensor("xT_out", [D_MODEL, NTOK], BF16, kind="ExternalOutput").ap()
    d["mT_scratch"] = nc.dram_tensor("mT_scratch", [128, 8, NTOK], BF16, kind="Internal").ap()
    with ExitStack() as es:
        cx = Ctx(nc, es)
        cst = build_consts(cx)
        k2_body(cx, cst, NTOK, x_bf16, d)
        cx.finish()
    return nc


def build_k1(S_len, x_bf16):
    nc = bass.Bass("TRN2", target_bir_lowering=False)
    xT = nc.dram_tensor("xT", [D_MODEL, S_len], BF16 if x_bf16 else F32, kind="ExternalInput").ap()
    w1 = nc.dram_tensor("w1", [D_MODEL, 768], F32, kind="ExternalInput").ap()
    cab = nc.dram_tensor("cab", [2, 128, 640], F32, kind="ExternalInput").ap()
    ot_sb = nc.dram_tensor("ot_sb", [128, S_len], BF16, kind="ExternalOutput").ap()
    ot_ca = nc.dram_tensor("ot_ca", [128, S_len], BF16, kind="ExternalOutput").ap()
    with ExitStack() as es:
        cx = Ctx(nc, es)
        cst = build_consts(cx)
        k1_body(cx, cst, S_len, x_bf16, xT, w1, cab, ot_sb, ot_ca)
        cx.finish()
    return nc


def ca_bias_table(rel_bias_l, heads):
    q = np.arange(128)[:, None]
    p = np.arange(640)[None, :]
    rel = np.clip(q + 512 - p, -128, 128) + 128
    ci = q // 64
    kc = p // 64
    valid = (kc >= ci) & (kc <= ci + 8)
    out = np.empty((len(heads), 128, 640), np.float32)
    for i, h in enumerate(heads):
        out[i] = np.where(valid, rel_bias_l[h][rel], np.float32(NEG))
    return out


def k1_weights(w_in_l, r):
    c = lambda base: w_in_l[:, base + r * 128: base + (r + 1) * 128]
    return np.ascontiguousarray(np.concatenate(
        [c(0), c(512), c(1536), c(2048), c(1024), c(2560)], axis=1))


def k2_inputs(p):
    f = np.ascontiguousarray
    m = {}
    m["wg"] = f(p["w_in"][:, 3072:5120])
    m["b_gate"] = f(p["b_gate"].reshape(2048))
    m["w_br_sb"] = f(p["w_br_sb"])
    m["w_br_ca"] = f(p["w_br_ca"])
    m["w_out"] = f(p["w_out"])
    for nm in ("ln1_g", "ln1_b", "ln2_g", "ln2_b"):
        m[nm] = f(p[nm])
    m["w_router"] = f(np.concatenate([p["w_group"]] + [p["w_erouter"][g] for g in range(4)], axis=1))
    m["b_router"] = f(np.concatenate([p["b_group"]] + [p["b_erouter"][g] for g in range(4)], axis=0))
    m["w_gate"] = f(p["w_gate"].reshape(32, D_MODEL, 256))
    m["w_up"] = f(p["w_up"].reshape(32, D_MODEL, 256))
    m["w_down"] = f(p["w_down"].reshape(32, 256, D_MODEL))
    return m


def build_fused(S_len):
    nc = bass.Bass("TRN2", target_bir_lowering=False)
    L = DEPTH
    NTOK = S_len // 4
    inp = lambda name, shape, dt=F32: nc.dram_tensor(name, shape, dt, kind="ExternalInput").ap()
    scr = lambda name, shape, dt: nc.dram_tensor(name, shape, dt, kind="Internal").ap()
    x_tok0 = inp("x_tok", [S_len, D_MODEL])
    xT0 = inp("xT", [D_MODEL, S_len])
    w1 = inp("w1", [L, 4, D_MODEL, 768])
    cab = inp("cab", [L, 4, 2, 128, 640])
    P = {}
    P["wg"] = inp("wg", [L, D_MODEL, 2048])
    P["b_gate"] = inp("b_gate", [L, 2048])
    P["w_br_sb"] = inp("w_br_sb", [L, 512, D_MODEL])
    P["w_br_ca"] = inp("w_br_ca", [L, 512, D_MODEL])
    P["w_out"] = inp("w_out", [L, D_MODEL, D_MODEL])
    for nm in ("ln1_g", "ln1_b", "ln2_g", "ln2_b"):
        P[nm] = inp(nm, [L, D_MODEL])
    P["w_router"] = inp("w_router", [L, D_MODEL, 36])
    P["b_router"] = inp("b_router", [L, 36])
    P["w_gate"] = inp("w_gate", [L, 32, D_MODEL, 256])
    P["w_up"] = inp("w_up", [L, 32, D_MODEL, 256])
    P["w_down"] = inp("w_down", [L, 32, 256, D_MODEL])
    out = nc.dram_tensor("out", [S_len, D_MODEL], F32, kind="ExternalOutput").ap()
    yT_sb = scr("yT_sb_s", [512, S_len], BF16)
    yT_ca = scr("yT_ca_s", [512, S_len], BF16)
    xT1 = scr("xT1_s", [D_MODEL, S_len], BF16)
    x_tok1 = scr("x_tok1_s", [S_len, D_MODEL], F32)
    mT_s = scr("mT_scratch", [128, 8, NTOK], BF16)
    with ExitStack() as es:
        cx = Ctx(nc, es)
        cst = build_consts(cx)
        for l in range(L):
            xT_l = xT0 if l == 0 else xT1
            xtok_l = x_tok0 if l == 0 else x_tok1
            for r in range(4):
                with ExitStack() as es2:
                    cx.es = es2
                    cx.sfx = f"_a{l}{r}"
                    k1_body(cx, cst, S_len, l > 0, xT_l, w1[l, r], cab[l, r],
                            yT_sb[r * 128:(r + 1) * 128, :], yT_ca[r * 128:(r + 1) * 128, :])
                cx.es = es
                cx.S.barrier()
            for r in range(4):
                tsl = slice(r * NTOK, (r + 1) * NTOK)
                d = {k: v[l] for k, v in P.items()}
                d["x_tok"] = xtok_l[tsl, :]
                d["xT"] = xT_l[:, tsl]
                d["yT_sb"] = yT_sb[:, tsl]
                d["yT_ca"] = yT_ca[:, tsl]
                d["mT_scratch"] = mT_s
                last = (l == L - 1)
                d["x_out"] = out[tsl, :] if last else x_tok1[tsl, :]
                d["xT_out"] = None if last else xT1[:, tsl]
                with ExitStack() as es2:
                    cx.es = es2
                    cx.sfx = f"_b{l}{r}"
                    k2_body(cx, cst, NTOK, l > 0, d)
                cx.es = es
                cx.S.barrier()
        cx.finish()
    return nc


NO_CC = False
DBG = set()


def build_fused8(S_len):
    nc = bass.Bass("TRN2", target_bir_lowering=False)
    L = DEPTH
    NTOK = S_len // 4
    TS_ = 512
    NCH = NTOK // TS_
    CHT = min(1024, NTOK)
    CPQ = NTOK // CHT
    NXC = 4 * CPQ
    groups = [[0, 1, 2, 3], [4, 5, 6, 7]]
    inp = lambda name, shape, dt=F32: nc.dram_tensor(name, shape, dt, kind="ExternalInput").ap()
    scr = lambda name, shape, dt: nc.dram_tensor(name, shape, dt, kind="Internal").ap()
    x_tok0 = inp("x_tok", [NTOK, D_MODEL])
    xT0 = inp("xT", [D_MODEL, S_len])
    xTq0 = inp("xTq", [D_MODEL, NTOK])
    pm_d = inp("pm", [128, 4])
    w1 = inp("w1", [L, D_MODEL, 768])
    cab = inp("cab", [L, 2, 128, 640])
    P = {}
    P["wg"] = inp("wg", [L, D_MODEL, 2048])
    P["b_gate"] = inp("b_gate", [L, 2048])
    P["w_br_sb"] = inp("w_br_sb", [L, 512, D_MODEL])
    P["w_br_ca"] = inp("w_br_ca", [L, 512, D_MODEL])
    P["w_out"] = inp("w_out", [L, D_MODEL, D_MODEL])
    for nm in ("ln1_g", "ln1_b", "ln2_g", "ln2_b"):
        P[nm] = inp(nm, [L, D_MODEL])
    P["w_router"] = inp("w_router", [L, D_MODEL, 36])
    P["b_router"] = inp("b_router", [L, 36])
    P["w_gate"] = inp("w_gate", [L, 32, D_MODEL, 256])
    P["w_up"] = inp("w_up", [L, 32, D_MODEL, 256])
    P["w_down"] = inp("w_down", [L, 32, 256, D_MODEL])
    out = nc.dram_tensor("out", [NTOK, D_MODEL], F32, kind="ExternalOutput").ap()
    y_in = [scr(f"y_in{c}", [4 * 1024, TS_], F32) for c in range(NCH)]
    y_rs = [scr(f"y_rs{c}", [1024, TS_], F32) for c in range(NCH)]
    x_in = [scr(f"x_in{k}", [D_MODEL, CHT], F32) for k in range(NXC)]
    x_all = [scr(f"x_all{k}", [D_MODEL, CHT], F32) for k in range(NXC)]
    xT1 = scr("xT1_s", [D_MODEL, NTOK], BF16)
    x_tok1 = scr("x_tok1_s", [NTOK, D_MODEL], F32)
    mT_s = scr("mT_scratch", [128, 8, NTOK], BF16)
    with ExitStack() as es:
        cx = Ctx(nc, es)
        S = cx.S
        cst = build_consts(cx)
        pm = cx.sb("pm_t", [128, 4], F32)
        S.add("sp", DMA(pm[:], pm_d[:, :]), writes=["pm"], chan="prm0")
        for l in range(L):
            last = (l == L - 1)
            with ExitStack() as es2:
                cx.es = es2
                cx.sfx = f"_a{l}"
                yst = [cx.sb(f"yst{i}", [64, 4, 512], F32) for i in range(2)]
                cnt = [0]

                def emit_out(branch, h, qb, ob, obn, yst=yst, cnt=cnt):
                    if "noemit" in DBG:
                        return
                    k = cnt[0] % 2
                    cnt[0] += 1
                    q, c = (qb * 512) // NTOK, ((qb * 512) % NTOK) // TS_
                    for j in range(4):
                        S.add("pool", TS(yst[k][:, j, :], ob[:], pm[0:64, j:j + 1], None, ALU.mult),
                              reads=[obn, "pm"], writes=[(f"yst{k}", j)])
                    dst = y_in[c].rearrange("(q b j w) s -> q b w j s", q=4, b=2, j=4)[q][branch][h * 64:(h + 1) * 64]
                    S.add("sp", DMA(dst, yst[k][:]), reads=[(f"yst{k}", j) for j in range(4)], chan=f"yo{k}")

                if l == 0:
                    xt_tile = None
                else:
                    def xt_tile(t):
                        kk, c0 = (t * 256) // CHT, (t * 256) % CHT
                        return x_all[kk].rearrange("(kc p) s -> p kc s", p=128)[:, :, c0:c0 + 256]
                k1_body(cx, cst, S_len, False, xT0, w1[l], cab[l], None, None, xt_tile=xt_tile, emit_out=emit_out)
            cx.es = es
            S.barrier()
            for c in range(NCH if not NO_CC else 0):
                S.add("pool", lambda e, i_=y_in[c][:, :], o_=y_rs[c][:, :]: e.collective_compute(
                    "ReduceScatter", op=ALU.add, replica_groups=groups, ins=[i_], outs=[o_]),
                    chan="cc", inc=1)
            S.barrier()
            d = {k: v[l] for k, v in P.items()}
            d["x_tok"] = x_tok0 if l == 0 else x_tok1
            d["xT"] = xTq0 if l == 0 else xT1
            d["y_rs"] = [y_rs[c] for c in range(NCH)]
            d["mT_scratch"] = mT_s
            d["x_out"] = out if last else x_tok1
            d["xT_out"] = None if last else xT1
            d["x_ar"] = None if (last or "noxar" in DBG) else x_in
            d["pm_tile"] = pm
            with ExitStack() as es2:
                cx.es = es2
                cx.sfx = f"_b{l}"
                k2_body(cx, cst, NTOK, l > 0, d)
            cx.es = es
            S.barrier()
            if not last:
                for k in range(NXC if not NO_CC else 0):
                    S.add("pool", lambda e, i_=x_in[k][:, :], o_=x_all[k][:, :]: e.collective_compute(
                        "AllReduce", op=ALU.add, replica_groups=groups, ins=[i_], outs=[o_]),
                        chan="cc", inc=1)
                S.barrier()
        cx.finish()
    return nc


def kernel_fused8(p):
    x = p["x"]
    B, S_len, D = x.shape
    NTOK = S_len // 4
    f = np.ascontiguousarray
    key = ("fused8", S_len)
    if key not in _NC_CACHE:
        _NC_CACHE[key] = build_fused8(S_len)
    nc = _NC_CACHE[key]
    per_l = [k2_inputs({k: v[l] for k, v in p.items() if k != "x"}) for l in range(DEPTH)]
    shared = {k: f(np.stack([per_l[l][k] for l in range(DEPTH)])) for k in per_l[0]}
    xT_b = [f(x[b].T) for b in range(B)]
    in_maps = []
    for c in range(8):
        b, r = c // 4, c % 4
        tsl = slice(r * NTOK, (r + 1) * NTOK)
        m = dict(shared)
        m["x_tok"] = f(x[b, tsl, :])
        m["xT"] = xT_b[b]
        m["xTq"] = f(xT_b[b][:, tsl])
        pmv = np.zeros((128, 4), np.float32)
        pmv[:, r] = 1.0
        m["pm"] = pmv
        m["w1"] = f(np.stack([k1_weights(p["w_in"][l], r) for l in range(DEPTH)]))
        m["cab"] = f(np.stack([ca_bias_table(p["rel_bias"][l], [2 * r, 2 * r + 1]) for l in range(DEPTH)]))
        in_maps.append(m)
    res = run_bass_kernel_spmd(nc, in_maps, core_ids=list(range(8))).results
    return np.stack([np.concatenate([np.asarray(res[b * 4 + r]["out"]) for r in range(4)], axis=0)
                     for b in range(B)], axis=0).astype(np.float32)


FUSED = 8


def kernel_fused(p):
    x = p["x"]
    B, S_len, D = x.shape
    f = np.ascontiguousarray
    key = ("fused", S_len)
    if key not in _NC_CACHE:
        _NC_CACHE[key] = build_fused(S_len)
    nc = _NC_CACHE[key]
    shared = {}
    shared["w1"] = f(np.stack([np.stack([k1_weights(p["w_in"][l], r) for r in range(4)]) for l in range(DEPTH)]))
    shared["cab"] = f(np.stack([np.stack([ca_bias_table(p["rel_bias"][l], [2 * r, 2 * r + 1]) for r in range(4)])
                                for l in range(DEPTH)]))
    per_l = [k2_inputs({k: v[l] for k, v in p.items() if k != "x"}) for l in range(DEPTH)]
    for k in per_l[0]:
        shared[k] = f(np.stack([per_l[l][k] for l in range(DEPTH)]))
    in_maps = []
    for b in range(B):
        m = dict(shared)
        m["x_tok"] = f(x[b])
        m["xT"] = f(x[b].T)
        in_maps.append(m)
    res = run_bass_kernel_spmd(nc, in_maps, core_ids=list(range(B))).results
    return np.stack([np.asarray(res[b]["out"]) for b in range(B)], axis=0).astype(np.float32)


_NC_CACHE = {}


def _get_nc(kind, *args):
    key = (kind,) + args
    if key not in _NC_CACHE:
        _NC_CACHE[key] = build_k1(*args) if kind == "k1" else build_k2(*args)
    return _NC_CACHE[key]


def kernel(**inputs):
    p = {k: np.asarray(v) for k, v in inputs.items()}
    if FUSED == 8:
        return kernel_fused8(p)
    if FUSED:
        return kernel_fused(p)
    x = p["x"]
    B, S_len, D = x.shape
    NTOK = S_len // 4
    cores = list(range(8))
    f = np.ascontiguousarray
    x_cur = x
    xT_full = None
    xT_prev = None
    for l in range(DEPTH):
        first = (l == 0)
        lp = {k: v[l] for k, v in p.items() if k != "x"}
        if first:
            xT_b = [f(x[b].T) for b in range(B)]
        else:
            xT_b = xT_full
        nc1 = _get_nc("k1", S_len, not first)
        in1 = []
        for c in cores:
            b, r = c // 4, c % 4
            in1.append({"xT": xT_b[b], "w1": k1_weights(lp["w_in"], r),
                        "cab": ca_bias_table(lp["rel_bias"], [2 * r, 2 * r + 1])})
        r1 = run_bass_kernel_spmd(nc1, in1, core_ids=cores).results
        yT_sb = [np.concatenate([np.asarray(r1[b * 4 + r]["ot_sb"]) for r in range(4)], axis=0) for b in range(B)]
        yT_ca = [np.concatenate([np.asarray(r1[b * 4 + r]["ot_ca"]) for r in range(4)], axis=0) for b in range(B)]
        nc2 = _get_nc("k2", NTOK, not first)
        wk2 = k2_inputs(lp)
        in2 = []
        for c in cores:
            b, r = c // 4, c % 4
            tsl = slice(r * NTOK, (r + 1) * NTOK)
            m = dict(wk2)
            m["x_tok"] = f(x_cur[b, tsl, :])
            m["xT"] = f(xT_b[b][:, tsl]) if first else xT_prev[c]
            m["yT_sb"] = f(yT_sb[b][:, tsl])
            m["yT_ca"] = f(yT_ca[b][:, tsl])
            in2.append(m)
        r2 = run_bass_kernel_spmd(nc2, in2, core_ids=cores).results
        x_cur = np.stack([np.concatenate([np.asarray(r2[b * 4 + r]["x_out"]) for r in range(4)], axis=0)
                          for b in range(B)], axis=0)
        xT_prev = [np.asarray(r2[c]["xT_out"]) for c in cores]
        xT_full = [f(np.concatenate([xT_prev[b * 4 + r] for r in range(4)], axis=1)) for b in range(B)]
    return x_cur.astype(np.float32)
```

```python
import numpy as np
from contextlib import ExitStack
import concourse.bass as bass
import concourse.mybir as mybir
from concourse.bass_utils import run_bass_kernel_spmd

F32 = mybir.dt.float32
BF16 = mybir.dt.bfloat16
AF = mybir.ActivationFunctionType
ALU = mybir.AluOpType
AX = mybir.AxisListType

D_MODEL = 1024
BATCH = 2
SEQ = 8192
DEPTH = 2
HD = 64
ALPHA = (2.0 * DEPTH) ** 0.25
LN_EPS = 1e-5
NEG = -30000.0
HEAT = 0


class Sched:
    ENG = ("pe", "act", "dve", "pool", "sp")

    def __init__(self, nc):
        self.nc = nc
        self.ops = []
        self.last_w = {}
        self.readers = {}
        self.chan_last = {}
        self.chans = []
        self.eng_last = {}
        self.bar = set()

    def barrier(self):
        self.bar = set(self.eng_last.values()) | set(self.chan_last.values())

    def add(self, eng, fn, reads=(), writes=(), chan=None, inc=16):
        idx = len(self.ops)
        deps = set(self.bar)
        for r in reads:
            if r in self.last_w:
                deps.add(self.last_w[r])
        for w in writes:
            if w in self.last_w:
                deps.add(self.last_w[w])
            for rd in self.readers.get(w, ()):
                deps.add(rd)
        if chan is not None:
            if chan in self.chan_last:
                deps.add(self.chan_last[chan])
            else:
                self.chans.append(chan)
            self.chan_last[chan] = idx
        for r in reads:
            self.readers.setdefault(r, []).append(idx)
        for w in writes:
            self.last_w[w] = idx
            self.readers[w] = []
        self.ops.append(dict(eng=eng, fn=fn, deps=deps, chan=chan, inc=inc))
        if chan is None:
            self.eng_last[eng] = idx
        return idx

    def emit(self, block, sems, final_wait_eng="sp"):
        ops = self.ops
        n = len(ops)
        has_dep = [False] * n
        for o in ops:
            for d in o["deps"]:
                has_dep[d] = True
        eng_cnt = {e: 0 for e in self.ENG}
        chan_cnt = {}
        sig = [None] * n
        for i, o in enumerate(ops):
            if o["chan"] is not None:
                c = o["chan"]
                chan_cnt[c] = chan_cnt.get(c, 0) + o["inc"]
                sig[i] = ("ch:" + c, chan_cnt[c])
            elif has_dep[i]:
                e = o["eng"]
                eng_cnt[e] += 1
                sig[i] = (e, eng_cnt[e])
        per_eng = {e: [] for e in self.ENG}
        for i, o in enumerate(ops):
            per_eng[o["eng"]].append(i)
        self.eng_cnt = eng_cnt

        def run(ename, eng):
            seen = {}
            for i in per_eng[ename]:
                o = ops[i]
                need = {}
                for d in o["deps"]:
                    od = ops[d]
                    if od["chan"] is None and od["eng"] == "pe" and ename == "pe" and o["chan"] is None:
                        continue
                    s, v = sig[d]
                    if need.get(s, 0) < v:
                        need[s] = v
                for s, v in need.items():
                    if seen.get(s, 0) >= v:
                        continue
                    eng.wait_ge(sems[s], v)
                    seen[s] = v
                ins = o["fn"](eng)
                if sig[i] is not None:
                    s, v = sig[i]
                    ins.then_inc(sems[s], o["inc"] if o["chan"] is not None else 1)
            if ename == final_wait_eng:
                for c, v in chan_cnt.items():
                    if seen.get("ch:" + c, 0) < v:
                        eng.wait_ge(sems["ch:" + c], v)

        @block.tensor
        def _(e):
            run("pe", e)

        @block.scalar
        def _(e):
            run("act", e)

        @block.vector
        def _(e):
            run("dve", e)

        @block.gpsimd
        def _(e):
            run("pool", e)

        @block.sync
        def _(e):
            run("sp", e)


class Ctx:
    def __init__(self, nc, es):
        self.nc = nc
        self.es = es
        self.S = Sched(nc)
        self.sfx = ""

    def sb(self, name, shape, dt):
        return self.es.enter_context(self.nc.sbuf_tensor(name + self.sfx, shape, dt))

    def ps(self, name, shape, dt):
        return self.es.enter_context(self.nc.psum_tensor(name + self.sfx, shape, dt))

    def finish(self):
        nc, es, S = self.nc, self.es, self.S
        sems = {}
        for e in Sched.ENG:
            sems[e] = es.enter_context(nc.semaphore("sem_" + e))
        for c in S.chans:
            sems["ch:" + c] = es.enter_context(nc.semaphore("semch_" + c))
        block = es.enter_context(nc.Block())
        S.emit(block, sems)


def build_consts(cx):
    S = cx.S
    c = {}
    ident = cx.sb("ident", [128, 128], BF16)
    ident32 = cx.sb("ident32", [128, 128], F32)
    negtri = cx.sb("negtri", [128, 128], BF16)
    negones = cx.sb("negones", [128, 128], BF16)
    m01 = cx.sb("m01", [128, 896], BF16)
    mbias = cx.sb("mbias", [128, 896], BF16)
    S.add("pool", lambda e: e.memset(ident[:], 0.0), writes=["ident"])
    S.add("pool", lambda e: e.affine_select(out=ident[:], in_=ident[:], pattern=[[-1, 128]],
                                            compare_op=ALU.not_equal, fill=1.0, base=0,
                                            channel_multiplier=1), reads=["ident"], writes=["ident"])
    S.add("pool", lambda e: e.memset(ident32[:], 0.0), writes=["ident32"])
    S.add("pool", lambda e: e.affine_select(out=ident32[:], in_=ident32[:], pattern=[[-1, 128]],
                                            compare_op=ALU.not_equal, fill=1.0, base=0,
                                            channel_multiplier=1), reads=["ident32"], writes=["ident32"])
    S.add("pool", lambda e: e.memset(negtri[:], -1.0), writes=["negtri"])
    S.add("pool", lambda e: e.affine_select(out=negtri[:], in_=negtri[:], pattern=[[-1, 128]],
                                            compare_op=ALU.is_ge, fill=0.0, base=0,
                                            channel_multiplier=1), reads=["negtri"], writes=["negtri"])
    S.add("pool", lambda e: e.memset(negones[:], -1.0), writes=["negones"])
    S.add("pool", lambda e: e.memset(m01[:], 1.0), writes=["m01"])
    S.add("pool", lambda e: e.affine_select(out=m01[:], in_=m01[:], pattern=[[1, 896]],
                                            compare_op=ALU.is_gt, fill=0.0, base=-384,
                                            channel_multiplier=-1), reads=["m01"], writes=["m01"])
    S.add("pool", lambda e: e.memset(mbias[:], 0.0), writes=["mbias"])
    S.add("pool", lambda e: e.affine_select(out=mbias[:], in_=mbias[:], pattern=[[1, 896]],
                                            compare_op=ALU.is_gt, fill=NEG, base=-384,
                                            channel_multiplier=-1), reads=["mbias"], writes=["mbias"])
    ones32 = cx.sb("ones32", [1, 128], F32)
    S.add("pool", lambda e: e.memset(ones32[:], 1.0), writes=["ones32"])
    c["ones32"] = ones32
    c.update(ident=ident, ident32=ident32, negtri=negtri, negones=negones, m01=m01, mbias=mbias)
    return c


def MM(out, lhsT, rhs, start=True, stop=True, skip=False):
    if skip:
        return lambda e: e.matmul(out, lhsT=lhsT, rhs=rhs, start=start, stop=stop, skip_group_check=True)
    return lambda e: e.matmul(out, lhsT=lhsT, rhs=rhs, start=start, stop=stop)


def ACT(out, in_, func, **kw):
    return lambda e: e.activation(out=out, in_=in_, func=func, **kw)


def TT(out, in0, in1, op):
    return lambda e: e.tensor_tensor(out=out, in0=in0, in1=in1, op=op)


def TS(out, in0, s1, s2, op0, op1=None):
    if op1 is None:
        return lambda e: e.tensor_scalar(out=out, in0=in0, scalar1=s1, scalar2=s2, op0=op0)
    return lambda e: e.tensor_scalar(out=out, in0=in0, scalar1=s1, scalar2=s2, op0=op0, op1=op1)


def STT(out, in0, scalar, in1, op0, op1):
    return lambda e: e.scalar_tensor_tensor(out=out, in0=in0, scalar=scalar, in1=in1, op0=op0, op1=op1)


def CP(out, in_):
    return lambda e: e.tensor_copy(out=out, in_=in_)


def DMA(out, in_):
    return lambda e: e.dma_start(out=out, in_=in_)


def TR(out, in_, ident):
    return lambda e: e.transpose(out, in_, ident)


def RED(out, in_, op):
    return lambda e: e.tensor_reduce(out=out, in_=in_, axis=AX.X, op=op)


def MS(ap, val):
    return lambda e: e.memset(ap, val)


def RCP(out, in_):
    return lambda e: e.reciprocal(out=out, in_=in_)


def k1_body(cx, cst, S_len, x_bf16, xT, w1, cab, ot_sb, ot_ca, xt_tile=None, emit_out=None):
    nc, S = cx.nc, cx.S
    NT = S_len // 256
    NB = S_len // 128
    NQ = S_len // 512
    KC = D_MODEL // 128

    QT_sb = cx.sb("QT_sb", [128, S_len], BF16)
    KT_sb = cx.sb("KT_sb", [128, S_len], BF16)
    QT_ca = cx.sb("QT_ca", [128, S_len], BF16)
    KT_ca = cx.sb("KT_ca", [128, S_len], BF16)
    V_all = cx.sb("V_all", [128, NB, 256], BF16)
    x32 = [cx.sb(f"x32_{i}", [128, KC, 256], F32) for i in range(2)]
    xbf = [cx.sb(f"xbf_{i}", [128, KC, 256], BF16) for i in range(2)]
    wbf = cx.sb("wbf", [128, KC, 768], BF16)
    cabs = cx.sb("cabs", [128, 2, 640], F32)

    banks = [cx.ps(f"bank{i}", [128, 512], F32) for i in range(7)]
    bankT = cx.ps("bankT", [128, 512], F32)

    if xt_tile is None:
        xT_v = xT.rearrange("(kc p) s -> p kc s", p=128)
        xt_tile = lambda t: xT_v[:, :, t * 256:(t + 1) * 256]
    w1_v = w1.rearrange("(kc p) n -> p kc n", p=128)

    S.add("sp", DMA(cabs[:], cab.rearrange("h q k -> q h k")), writes=["cabs"], chan="cab")

    for pc in range(3):
        buf = x32[pc % 2]
        nm = f"x32_{pc % 2}"
        S.add("sp", DMA(buf[:], w1_v[:, :, pc * 256:(pc + 1) * 256]), writes=[nm], chan=f"x{pc % 2}")
        S.add("dve", CP(wbf[:, :, pc * 256:(pc + 1) * 256], buf[:]), reads=[nm], writes=[f"wbf{pc}"])
    wres = ["wbf0", "wbf1", "wbf2"]

    dsts = [QT_sb, KT_sb, QT_ca, KT_ca]
    dnames = ["QT_sb", "KT_sb", "QT_ca", "KT_ca"]
    for t in range(NT):
        i2 = t % 2
        xb = xbf[i2]
        xbn = f"xbf_{i2}"
        tsl = slice(t * 256, (t + 1) * 256)
        if x_bf16:
            S.add("sp", DMA(xb[:], xt_tile(t)), writes=[xbn], chan=f"x{i2}")
            xr = [xbn]
        else:
            xs = x32[i2]
            xsn = f"x32_{i2}"
            S.add("sp", DMA(xs[:], xt_tile(t)), writes=[xsn], chan=f"x{i2}")
            S.add("dve", CP(xb[:, 0:4, :], xs[:, 0:4, :]), reads=[xsn], writes=[xbn + "a"])
            S.add("pool", CP(xb[:, 4:8, :], xs[:, 4:8, :]), reads=[xsn], writes=[xbn + "b"])
            xr = [xbn + "a", xbn + "b"]
        for g in range(4):
            bk = banks[g]
            bkn = f"bank{g}"
            for kc in range(KC):
                S.add("pe", MM(bk[:, 0:256], wbf[:, kc, g * 128:(g + 1) * 128], xb[:, kc, :],
                               start=(kc == 0), stop=(kc == KC - 1)), reads=wres + xr, writes=[bkn])
            dst = dsts[g]
            if g in (0, 2):
                S.add("act", ACT(dst[:, tsl], bk[:, 0:256], AF.Identity, scale=0.125),
                      reads=[bkn], writes=[(dnames[g], t // 2)])
            else:
                S.add("dve", CP(dst[:, tsl], bk[:, 0:256]), reads=[bkn], writes=[(dnames[g], t // 2)])
        for sub in range(2):
            bk = banks[4 + sub]
            bkn = f"bank{4 + sub}"
            for kc in range(KC):
                S.add("pe", MM(bk[:, 0:256], xb[:, kc, sub * 128:(sub + 1) * 128], wbf[:, kc, 512:768],
                               start=(kc == 0), stop=(kc == KC - 1)), reads=wres + xr, writes=[bkn])
            blk = t * 2 + sub
            if sub == 0:
                S.add("dve", CP(V_all[:, blk, :], bk[:, 0:256]), reads=[bkn], writes=[("V", blk)])
            else:
                S.add("act", ACT(V_all[:, blk, :], bk[:, 0:256], AF.Identity), reads=[bkn], writes=[("V", blk)])

    negtri, negones, ident, m01, mbias = cst["negtri"], cst["negones"], cst["ident"], cst["m01"], cst["mbias"]
    E32 = [cx.sb(f"E32_{i}", [128, 512], F32) for i in range(2)]
    SPb = [cx.sb(f"SP_{i}", [128, 512], BF16) for i in range(3)]
    Wb = [cx.sb(f"W_{i}", [128, 512], BF16) for i in range(2)]
    C32 = cx.sb("C32", [128, 512], F32)
    Cbf = [cx.sb(f"Cbf_{i}", [128, 512], BF16) for i in range(4)]
    osb = [cx.sb(f"osb_{i}", [64, 512], BF16) for i in range(2)]
    blocks = []
    oi = 0
    for h in range(2):
        for qb in range(NQ):
            n = 4 * (qb + 1)
            for i in range(n):
                blocks.append(dict(h=h, qb=qb, i=i, n=n, kb=n - 1 - i, oi=oi))
            oi += 1
    NBK = len(blocks)

    def R(g):
        bl = blocks[g]
        h, qb, kb = bl["h"], bl["qb"], bl["kb"]
        d_ = dict(bl)
        d_.update(hp=slice(h * 64, (h + 1) * 64), qs=slice(qb * 512, (qb + 1) * 512),
                  pz=banks[g % 6], pzn=f"bank{g % 6}", E=E32[g % 2], En=f"E32_{g % 2}",
                  SP=SPb[g % 3], SPn=f"SP_{g % 3}", W=Wb[g % 2], Wn=f"W_{g % 2}",
                  C=Cbf[g % 4], Cn=f"Cbf_{g % 4}", Cp=Cbf[(g + 1) % 4], Cpn=f"Cbf_{(g + 1) % 4}",
                  po=banks[6] if bl["oi"] % 2 == 0 else bankT, pon="bank6" if bl["oi"] % 2 == 0 else "bankT",
                  diag=kb >= 4 * qb, off=(kb - 4 * qb) * 128)
        return d_

    for t in range(NBK + 5):
        g = t
        if g < NBK:
            r = R(g)
            S.add("pe", MM(r["pz"][:], KT_sb[r["hp"], r["kb"] * 128:(r["kb"] + 1) * 128], QT_sb[r["hp"], r["qs"]]),
                  reads=[("KT_sb", r["kb"] // 4), ("QT_sb", r["qb"])], writes=[r["pzn"]])
        g = t - 1
        if 0 <= g < NBK:
            r = R(g)
            S.add("act", ACT(r["E"][:], r["pz"][:], AF.Exp), reads=[r["pzn"]], writes=[r["En"]])
        g = t - 2
        if 0 <= g < NBK:
            r = R(g)
            off = r["off"]
            S.add("act", ACT(r["SP"][:], r["E"][:], AF.Ln, bias=1.0), reads=[r["En"]], writes=[r["SPn"]])
            if r["diag"]:
                S.add("dve", TT(r["SP"][:], r["SP"][:], m01[:, 384 - off:896 - off], ALU.mult),
                      reads=[r["SPn"], "m01"], writes=[r["SPn"]])
            if r["i"] < r["n"] - 1:
                if r["i"] == 0:
                    S.add("dve", CP(C32[:], r["SP"][:]), reads=[r["SPn"]], writes=["C32"])
                    S.add("dve", CP(r["Cp"][:], r["SP"][:]), reads=[r["SPn"]], writes=[r["Cpn"]])
                else:
                    S.add("dve", TT(C32[:], C32[:], r["SP"][:], ALU.add), reads=[r["SPn"], "C32"], writes=["C32"])
                    S.add("dve", CP(r["Cp"][:], C32[:]), reads=["C32"], writes=[r["Cpn"]])
        g = t - 3
        if 0 <= g < NBK:
            r = R(g)
            off = r["off"]
            S.add("pe", MM(r["pz"][:], negtri[:], r["SP"][:], start=False, stop=False, skip=True),
                  reads=[r["SPn"], "negtri"], writes=[r["pzn"]])
            if r["i"] > 0:
                S.add("pe", MM(r["pz"][:], negones[:], r["C"][:], start=False, stop=False, skip=True),
                      reads=[r["Cn"], "negones"], writes=[r["pzn"]])
            if r["diag"]:
                S.add("pe", MM(r["pz"][:], ident[:], mbias[:, 384 - off:896 - off],
                               start=False, stop=False, skip=True),
                      reads=["ident", "mbias"], writes=[r["pzn"]])
        g = t - 4
        if 0 <= g < NBK:
            r = R(g)
            S.add("act", ACT(r["W"][:], r["pz"][:], AF.Exp), reads=[r["pzn"]], writes=[r["Wn"]])
        g = t - 5
        if 0 <= g < NBK:
            r = R(g)
            h = r["h"]
            po, pon = r["po"], r["pon"]
            S.add("pe", MM(po[0:64, 0:512], V_all[:, r["kb"], h * 64:(h + 1) * 64], r["W"][:],
                           start=(r["i"] == 0), stop=(r["i"] == r["n"] - 1)),
                  reads=[r["Wn"], ("V", r["kb"])], writes=[pon])
            for _ in range(HEAT):
                S.add("pe", MM(po[64:128, 0:512], negones[:, 0:64], m01[:, 0:512]), reads=["negones", "m01"])
            if r["i"] == r["n"] - 1:
                ob = osb[r["oi"] % 2]
                obn = f"osb_{r['oi'] % 2}"
                S.add("dve", CP(ob[:], po[0:64, 0:512]), reads=[pon], writes=[obn])
                if emit_out is not None:
                    emit_out(0, h, r["qb"], ob, obn)
                else:
                    S.add("sp", DMA(ot_sb[h * 64:(h + 1) * 64, r["qs"]], ob[:]), reads=[obn], chan=f"o{r['oi'] % 2}")

    T32 = [cx.sb(f"T32_{i}", [128, 640], F32) for i in range(2)]
    Pb = [cx.sb(f"P_{i}", [128, 640], BF16) for i in range(2)]
    Pn = [cx.sb(f"Pn_{i}", [128, 640], BF16) for i in range(2)]
    WT = [cx.sb(f"WT_{i}", [128, 640], BF16) for i in range(2)]
    st = [cx.sb(f"st_{i}", [128, 4], F32) for i in range(2)]
    oca = [cx.sb(f"oca_{i}", [64, 512], BF16) for i in range(2)]
    ci = 0
    po = banks[6]
    bankTb = bankT[:].bitcast(BF16)
    for h in range(2):
        hp = slice(h * 64, (h + 1) * 64)
        for m in range(NB):
            nk = min(640, 128 * (m + 1))
            ks = 128 * (m + 1) - nk
            p0 = 640 - nk
            n1 = min(nk, 512)
            i2 = ci % 2
            pa = banks[i2 * 2]
            pan = f"bank{i2 * 2}"
            pb = banks[i2 * 2 + 1]
            pbn = f"bank{i2 * 2 + 1}"
            T, Tn = T32[i2], f"T32_{i2}"
            P, Pnm = Pb[i2], f"P_{i2}"
            PN, PNn = Pn[i2], f"Pn_{i2}"
            W, Wn = WT[i2], f"WT_{i2}"
            sx, sxn = st[i2], f"st_{i2}"
            qsl = slice(m * 128, (m + 1) * 128)
            krd = [("KT_ca", j) for j in range(ks // 512, (ks + nk - 1) // 512 + 1)]
            S.add("pe", MM(pa[:, 0:n1], QT_ca[hp, qsl], KT_ca[hp, ks:ks + n1]),
                  reads=[("QT_ca", m // 4)] + krd, writes=[pan])
            S.add("dve", TT(T[:, 0:n1], pa[:, 0:n1], cabs[:, h, p0:p0 + n1], ALU.add),
                  reads=[pan, "cabs"], writes=[Tn])
            if nk > 512:
                S.add("pe", MM(pb[:, 0:128], QT_ca[hp, qsl], KT_ca[hp, ks + 512:ks + 640]),
                      reads=[("QT_ca", m // 4)] + krd, writes=[pbn])
                S.add("dve", TT(T[:, 512:640], pb[:, 0:128], cabs[:, h, 512:640], ALU.add),
                      reads=[pbn, "cabs", Tn], writes=[Tn])
            S.add("dve", RED(sx[:, 0:1], T[:, 0:nk], ALU.max), reads=[Tn], writes=[sxn])
            S.add("dve", TS(sx[:, 1:2], sx[:, 0:1], -1.0, None, ALU.mult), reads=[sxn], writes=[sxn])
            S.add("pool", MS(sx[:, 2:3], 0.0), reads=[sxn], writes=[sxn])
            S.add("act", ACT(P[:, 0:nk], T[:, 0:nk], AF.Exp, bias=sx[:, 1:2], accum_out=sx[:, 2:3]),
                  reads=[Tn, sxn], writes=[Pnm, sxn])
            S.add("dve", RCP(sx[:, 3:4], sx[:, 2:3]), reads=[sxn], writes=[sxn])
            S.add("dve", TS(PN[:, 0:nk], P[:, 0:nk], sx[:, 3:4], None, ALU.mult),
                  reads=[Pnm, sxn], writes=[PNn])
            nj = nk // 128
            for j in range(nj):
                S.add("pe", TR(bankTb[:, j * 128:(j + 1) * 128], PN[:, j * 128:(j + 1) * 128], ident[:]),
                      reads=[PNn, "ident"], writes=["bankT"])
            S.add("act", ACT(W[:, 0:nk], bankTb[:, 0:nk], AF.Identity), reads=["bankT"], writes=[Wn])
            mcol = (m % 4) * 128
            for j in range(nj):
                kb = ks // 128 + j
                S.add("pe", MM(po[0:64, mcol:mcol + 128], V_all[:, kb, 128 + h * 64:128 + (h + 1) * 64],
                               W[:, j * 128:(j + 1) * 128], start=(j == 0), stop=(j == nj - 1)),
                      reads=[Wn, ("V", kb)], writes=["bank6"])
            if m % 4 == 3:
                oq = (h * NB + m) // 4
                ob, obn = oca[oq % 2], f"oca_{oq % 2}"
                S.add("act", ACT(ob[:], po[0:64, :], AF.Identity), reads=["bank6"], writes=[obn])
                if emit_out is not None:
                    emit_out(1, h, m // 4, ob, obn)
                else:
                    S.add("sp", DMA(ot_ca[h * 64:(h + 1) * 64, (m - 3) * 128:(m + 1) * 128], ob[:]),
                          reads=[obn], chan=f"oc{oq % 2}")
            ci += 1


def layer_norm_tile(cx, S, u, un, out, outn, gbc, bbc, st, stn, junk, junkn, gn, tmp=None, tmpn=None):
    D = D_MODEL
    S.add("pool", MS(st[:, 0:2], 0.0), reads=[stn], writes=[stn])
    S.add("act", ACT(junk[:], u, AF.Identity, accum_out=st[:, 0:1]), reads=[un, stn], writes=[junkn, stn])
    S.add("act", ACT(junk[:], u, AF.Square, accum_out=st[:, 1:2]), reads=[un, stn, junkn], writes=[junkn, stn])
    S.add("dve", TS(st[:, 2:4], st[:, 0:2], 1.0 / D, None, ALU.mult), reads=[stn], writes=[stn])
    S.add("dve", STT(st[:, 4:5], st[:, 2:3], st[:, 2:3], st[:, 3:4], ALU.mult, ALU.subtract),
          reads=[stn], writes=[stn])
    S.add("dve", TS(st[:, 5:6], st[:, 4:5], -1.0, LN_EPS, ALU.mult, ALU.add), reads=[stn], writes=[stn])
    S.add("act", ACT(st[:, 5:6], st[:, 5:6], AF.Ln), reads=[stn], writes=[stn])
    S.add("act", ACT(st[:, 6:7], st[:, 5:6], AF.Exp, scale=-0.5), reads=[stn], writes=[stn])
    S.add("dve", STT(st[:, 7:8], st[:, 2:3], -1.0, st[:, 6:7], ALU.mult, ALU.mult), reads=[stn], writes=[stn])
    S.add("act", ACT(out, u, AF.Identity, scale=st[:, 6:7], bias=st[:, 7:8]), reads=[un, stn], writes=[outn])
    S.add("dve", TT(out, out, gbc[:], ALU.mult), reads=[outn, gn], writes=[outn])
    S.add("pool", TT(out, out, bbc[:], ALU.add), reads=[outn, gn], writes=[outn])


def k2_body(cx, cst, NTOK, x_bf16, d):
    nc, S = cx.nc, cx.S
    KC = D_MODEL // 128
    TS_ = min(512, NTOK)
    NST = NTOK // TS_
    NSUB = TS_ // 128
    NS = NTOK // 128
    ident32 = cst["ident32"]
    banks = [cx.ps(f"kb{i}", [128, 512], F32) for i in range(8)]
    bn = [f"kb{i}" for i in range(8)]

    lnbc = {}
    for nm in ("ln1_g", "ln1_b", "ln2_g", "ln2_b"):
        t = cx.sb("bc_" + nm, [128, D_MODEL], F32)
        S.add("sp", DMA(t[:], d[nm].partition_broadcast(128)), writes=["lnbc"], chan="prm")
        lnbc[nm] = t
    bg = cx.sb("bg", [128, 16], F32)
    S.add("sp", lambda e, o_=bg[:], i_=d["b_gate"].rearrange("(j p) -> p j", p=128): e.dma_start(
        out=o_, in_=i_, allow_slow_non_contiguous=True), writes=["bg"], chan="prm")
    wr32 = cx.sb("wr32", [128, KC, 36], F32)
    S.add("sp", DMA(wr32[:], d["w_router"].rearrange("(kc p) n -> p kc n", p=128)), writes=["wr32"], chan="prm")
    brt = cx.sb("brt", [128, 36], F32)
    S.add("sp", DMA(brt[:], d["b_router"].partition_broadcast(128)), writes=["brt"], chan="prm")

    mT_d = d["mT_scratch"]
    xT_v = d["xT"].rearrange("(kc p) s -> p kc s", p=128)
    if d.get("y_rs") is None:
        ysb_v = d["yT_sb"].rearrange("(kc p) s -> p kc s", p=128)
        yca_v = d["yT_ca"].rearrange("(kc p) s -> p kc s", p=128)

    with ExitStack() as es:
        sb = lambda name, shape, dt: es.enter_context(nc.sbuf_tensor(name + cx.sfx, shape, dt))
        wg_bf = sb("wg_bf", [128, KC, 2048], BF16)
        wbs_bf = sb("wbs_bf", [128, 4, 1024], BF16)
        wbc_bf = sb("wbc_bf", [128, 4, 1024], BF16)
        stg = [sb(f"stgA{i}", [128, 2048], F32) for i in range(2)]
        xbf = [sb(f"xbfA{i}", [128, KC, TS_], BF16) for i in range(2)]
        ysb = [sb(f"ysb{i}", [128, 4, TS_], BF16) for i in range(2)]
        yca = [sb(f"yca{i}", [128, 4, TS_], BF16) for i in range(2)]
        gs = [sb(f"gs{i}", [128, TS_], F32) for i in range(2)]
        gc = [sb(f"gc{i}", [128, TS_], F32) for i in range(2)]
        t1 = sb("t1", [128, TS_], F32)
        t2 = sb("t2", [128, TS_], F32)
        mT = [sb(f"mT{i}", [128, KC, TS_], BF16) for i in range(2)]
        si = 0

        def stage_cast(dst, src_ap, ncols, rn, k=None):
            nonlocal si
            b = si % 2
            si += 1
            sv = stg[b][:, 0:ncols]
            if k is not None:
                sv = sv.rearrange("p (k t) -> p k t", k=k)
            S.add("sp", DMA(sv, src_ap), writes=[f"stgA{b}"], chan=f"stgA{b}")
            S.add("dve" if b == 0 else "pool", CP(dst, sv), reads=[f"stgA{b}"], writes=[rn])

        wg_v = d["wg"].rearrange("(kc p) n -> p kc n", p=128)
        for kc in range(KC):
            stage_cast(wg_bf[:, kc, :], wg_v[:, kc, :], 2048, "wg_bf")
        wbs_v = d["w_br_sb"].rearrange("(kc p) n -> p kc n", p=128)
        wbc_v = d["w_br_ca"].rearrange("(kc p) n -> p kc n", p=128)
        for kc in range(0, 4, 2):
            stage_cast(wbs_bf[:, kc:kc + 2, :], wbs_v[:, kc:kc + 2, :], 2048, "wbs_bf", k=2)
            stage_cast(wbc_bf[:, kc:kc + 2, :], wbc_v[:, kc:kc + 2, :], 2048, "wbc_bf", k=2)
        for T in range(NST):
            i2 = T % 2
            tsl = slice(T * TS_, (T + 1) * TS_)
            xb, xbn = xbf[i2], f"xbfA{i2}"
            if x_bf16:
                S.add("sp", DMA(xb[:], xT_v[:, :, tsl]), writes=[xbn], chan=f"xA{i2}")
            else:
                per = 2048 // TS_
                for k0 in range(0, KC, per):
                    stage_cast(xb[:, k0:k0 + per, :], xT_v[:, k0:k0 + per, tsl], per * TS_, xbn, k=per)
            if d.get("y_rs") is not None:
                yv = d["y_rs"][T].rearrange("(b kc p) s -> b p kc s", b=2, p=128)
                per = 2048 // TS_
                for k0 in range(0, 4, per):
                    stage_cast(ysb[i2][:, k0:k0 + per, :], yv[0][:, k0:k0 + per, :], per * TS_, f"ysb{i2}", k=per)
                    stage_cast(yca[i2][:, k0:k0 + per, :], yv[1][:, k0:k0 + per, :], per * TS_, f"yca{i2}", k=per)
            else:
                S.add("sp", DMA(ysb[i2][:], ysb_v[:, :, tsl]), writes=[f"ysb{i2}"], chan=f"yA{i2}")
                S.add("sp", DMA(yca[i2][:], yca_v[:, :, tsl]), writes=[f"yca{i2}"], chan=f"yB{i2}")
            mt, mtn = mT[i2], f"mT{i2}"
            for fo in range(KC):
                j2 = fo % 2
                fsl = slice(fo * 128, (fo + 1) * 128)
                fsl2 = slice(1024 + fo * 128, 1024 + (fo + 1) * 128)
                b0, b1, b2, b3 = j2 * 4, j2 * 4 + 1, j2 * 4 + 2, j2 * 4 + 3
                for kc in range(KC):
                    S.add("pe", MM(banks[b0][:, 0:TS_], wg_bf[:, kc, fsl], xb[:, kc, :],
                                   start=(kc == 0), stop=(kc == KC - 1)), reads=["wg_bf", xbn], writes=[bn[b0]])
                S.add("act", ACT(gs[j2][:], banks[b0][:, 0:TS_], AF.Sigmoid, bias=bg[:, fo:fo + 1]),
                      reads=[bn[b0], "bg"], writes=[f"gs{j2}"])
                for kc in range(KC):
                    S.add("pe", MM(banks[b1][:, 0:TS_], wg_bf[:, kc, fsl2], xb[:, kc, :],
                                   start=(kc == 0), stop=(kc == KC - 1)), reads=["wg_bf", xbn], writes=[bn[b1]])
                S.add("act", ACT(gc[j2][:], banks[b1][:, 0:TS_], AF.Sigmoid, bias=bg[:, 8 + fo:9 + fo]),
                      reads=[bn[b1], "bg"], writes=[f"gc{j2}"])
                for kc in range(4):
                    S.add("pe", MM(banks[b2][:, 0:TS_], wbs_bf[:, kc, fsl], ysb[i2][:, kc, :],
                                   start=(kc == 0), stop=(kc == 3)), reads=["wbs_bf", f"ysb{i2}"], writes=[bn[b2]])
                for kc in range(4):
                    S.add("pe", MM(banks[b3][:, 0:TS_], wbc_bf[:, kc, fsl], yca[i2][:, kc, :],
                                   start=(kc == 0), stop=(kc == 3)), reads=["wbc_bf", f"yca{i2}"], writes=[bn[b3]])
                S.add("dve", TT(t1[:], gs[j2][:], banks[b2][:, 0:TS_], ALU.mult),
                      reads=[f"gs{j2}", bn[b2]], writes=["t1"])
                S.add("dve", TT(t2[:], gc[j2][:], banks[b3][:, 0:TS_], ALU.mult),
                      reads=[f"gc{j2}", bn[b3]], writes=["t2"])
                S.add("pool", TT(mt[:, fo, :], t1[:], t2[:], ALU.add), reads=["t1", "t2"], writes=[mtn])
            S.add("sp", DMA(mT_d[:, :, tsl], mt[:]), reads=[mtn], writes=["mT_d"], chan=f"mTo{i2}")
    S.barrier()

    yacc = cx.sb("yacc", [128, NS, D_MODEL], F32)
    X1T = cx.sb("X1T", [128, KC, NTOK], BF16)
    combT = cx.sb("combT", [32, NTOK], F32)

    with ExitStack() as es:
        sb = lambda name, shape, dt: es.enter_context(nc.sbuf_tensor(name + cx.sfx, shape, dt))
        wo_bf = sb("wo_bf", [128, KC, 1024], BF16)
        stg = [sb(f"stgB{i}", [128, 2048], F32) for i in range(2)]
        wo_v = d["w_out"].rearrange("(kc p) n -> p kc n", p=128)
        for k0 in range(0, KC, 2):
            b = (k0 // 2) % 2
            sv = stg[b][:].rearrange("p (k t) -> p k t", k=2)
            S.add("sp", DMA(sv, wo_v[:, k0:k0 + 2, :]), writes=[f"stgB{b}"], chan=f"stgB{b}")
            S.add("dve" if b == 0 else "pool", CP(wo_bf[:, k0:k0 + 2, :], sv),
                  reads=[f"stgB{b}"], writes=["wo_bf"])
        mTt = [sb(f"mTt{i}", [128, KC, 128], BF16) for i in range(2)]
        xtok = [sb(f"xtok{i}", [128, D_MODEL], F32) for i in range(2)]
        u = [sb(f"u{i}", [128, D_MODEL], F32) for i in range(2)]
        x1 = [sb(f"x1_{i}", [128, D_MODEL], F32) for i in range(2)]
        junk = sb("junk", [128, D_MODEL], BF16)
        x1T32 = sb("x1T32", [128, KC, 128], F32)
        stt = [sb(f"lnst{i}", [128, 8], F32) for i in range(2)]
        rt = [sb(f"rt{i}", [128, 128], F32) for i in range(2)]
        comb = [sb(f"comb{i}", [128, 32], F32) for i in range(2)]
        for sI in range(NS):
            i2 = sI % 2
            tok = slice(sI * 128, (sI + 1) * 128)
            S.add("sp", DMA(mTt[i2][:], mT_d[:, :, tok]), reads=["mT_d"], writes=[f"mTt{i2}"], chan=f"mTi{i2}")
            S.add("sp", DMA(xtok[i2][:], d["x_tok"][tok, :]), writes=[f"xtok{i2}"], chan=f"xt{i2}")
            for half in range(2):
                bk = banks[half]
                for kc in range(KC):
                    S.add("pe", MM(bk[:], mTt[i2][:, kc, :], wo_bf[:, kc, half * 512:(half + 1) * 512],
                                   start=(kc == 0), stop=(kc == KC - 1)),
                          reads=[f"mTt{i2}", "wo_bf"], writes=[bn[half]])
                S.add("dve", STT(u[i2][:, half * 512:(half + 1) * 512], xtok[i2][:, half * 512:(half + 1) * 512],
                                 ALPHA, bk[:], ALU.mult, ALU.add),
                      reads=[f"xtok{i2}", bn[half]], writes=[f"u{i2}"])
            layer_norm_tile(cx, S, u[i2][:], f"u{i2}", x1[i2][:], f"x1_{i2}", lnbc["ln1_g"], lnbc["ln1_b"],
                            stt[i2], f"lnst{i2}", junk, "junk", "lnbc")
            S.add("pool", TS(yacc[:, sI, :], x1[i2][:], ALPHA, None, ALU.mult),
                  reads=[f"x1_{i2}"], writes=[("yacc", sI)])
            for kc in range(KC):
                bk = banks[2 + (kc // 4)]
                S.add("pe", TR(bk[:, (kc % 4) * 128:(kc % 4 + 1) * 128], x1[i2][:, kc * 128:(kc + 1) * 128],
                               ident32[:]), reads=[f"x1_{i2}", "ident32"], writes=[bn[2 + kc // 4]])
            for q in range(2):
                S.add("act", ACT(x1T32[:, q * 4:(q + 1) * 4, :],
                                 banks[2 + q][:].rearrange("p (k t) -> p k t", k=4), AF.Identity),
                      reads=[bn[2 + q]], writes=["x1T32"])
            S.add("pool", CP(X1T[:, :, tok], x1T32[:]), reads=["x1T32"], writes=[("X1T", sI)])
            lg = banks[4]
            for kc in range(KC):
                S.add("pe", MM(lg[:, 0:36], x1T32[:, kc, :], wr32[:, kc, :], start=(kc == 0), stop=(kc == KC - 1)),
                      reads=["x1T32", "wr32"], writes=[bn[4]])
            R_, rn = rt[i2], f"rt{i2}"
            L = R_[:, 0:36]

            def V(eng, fn):
                S.add(eng, fn, reads=[rn, "brt", bn[4]] if eng != "pool" else [rn, "brt"], writes=[rn])
            V("dve", TT(L, lg[:, 0:36], brt[:], ALU.add))
            gmax, ngmax, sg, gval = R_[:, 36:37], R_[:, 37:38], R_[:, 38:39], R_[:, 39:40]
            ohg, eg, esel = R_[:, 40:44], R_[:, 44:48], R_[:, 48:56]
            m1, oh1, e2, m2, oh2 = R_[:, 56:57], R_[:, 64:72], R_[:, 72:80], R_[:, 57:58], R_[:, 80:88]
            dd, ed, den, w1, w2 = R_[:, 58:59], R_[:, 59:60], R_[:, 60:61], R_[:, 61:62], R_[:, 62:63]
            ew, gw = R_[:, 88:96], R_[:, 96:100]
            V("dve", RED(gmax, L[:, 0:4], ALU.max))
            V("dve", TS(ohg, L[:, 0:4], gmax, None, ALU.is_equal))
            V("dve", TS(ngmax, gmax, -1.0, None, ALU.mult))
            V("pool", MS(sg, 0.0))
            V("act", ACT(eg, L[:, 0:4], AF.Exp, bias=ngmax, accum_out=sg))
            V("dve", RCP(gval, sg))
            V("dve", TS(esel, L[:, 4:12], ohg[:, 0:1], None, ALU.mult))
            for g in range(1, 4):
                V("dve", STT(esel, L[:, 4 + 8 * g:12 + 8 * g], ohg[:, g:g + 1], esel, ALU.mult, ALU.add))
            V("dve", RED(m1, esel, ALU.max))
            V("dve", TS(oh1, esel, m1, None, ALU.is_equal))
            V("dve", STT(e2, oh1, -1e30, esel, ALU.mult, ALU.add))
            V("dve", RED(m2, e2, ALU.max))
            V("dve", TS(oh2, e2, m2, None, ALU.is_equal))
            V("dve", TT(dd, m2, m1, ALU.subtract))
            V("act", ACT(ed, dd, AF.Exp))
            V("dve", TS(den, ed, 1.0, None, ALU.add))
            V("dve", RCP(w1, den))
            V("dve", TT(w2, ed, w1, ALU.mult))
            V("dve", TS(ew, oh1, w1, None, ALU.mult))
            V("dve", STT(ew, oh2, w2, ew, ALU.mult, ALU.add))
            V("dve", TS(gw, ohg, gval, None, ALU.mult))
            cb_, cbn = comb[i2], f"comb{i2}"
            for g in range(4):
                S.add("dve", TS(cb_[:, 8 * g:8 * g + 8], ew, gw[:, g:g + 1], None, ALU.mult),
                      reads=[rn], writes=[cbn])
            S.add("pe", TR(banks[5][0:32, 0:128], cb_[:], ident32[:]), reads=[cbn, "ident32"], writes=[bn[5]])
            S.add("act", ACT(combT[:, tok], banks[5][0:32, 0:128], AF.Identity), reads=[bn[5]],
                  writes=[("combT", sI)])
    S.barrier()

    NE = 32
    with ExitStack() as es:
        sb = lambda name, shape, dt: es.enter_context(nc.sbuf_tensor(name + cx.sfx, shape, dt))
        wgt = [sb(f"wgt{i}", [128, KC, 256], BF16) for i in range(2)]
        wup = [sb(f"wup{i}", [128, KC, 256], BF16) for i in range(2)]
        wdn = [sb(f"wdn{i}", [128, 2, 1024], BF16) for i in range(2)]
        stg = [sb(f"stgC{i}", [128, 2048], F32) for i in range(3)]
        sel = [sb(f"sel{i}", [32, 128], F32) for i in range(2)]
        sl = [sb(f"sl{i}", [128, TS_], F32) for i in range(2)]
        tl = [sb(f"tl{i}", [128, TS_], F32) for i in range(2)]
        cbs = [sb(f"cbs{i}", [128, TS_], F32) for i in range(2)]
        hid = [sb(f"hid{i}", [128, 2, TS_], BF16) for i in range(2)]
        wg_v = d["w_gate"].rearrange("e (kc p) f -> e p kc f", p=128)
        wu_v = d["w_up"].rearrange("e (kc p) f -> e p kc f", p=128)
        wd_v = d["w_down"].rearrange("e (fc p) n -> e p fc n", p=128)
        it = 0
        pyi = 0
        for e_ in range(NE):
            b = e_ % 2
            S.add("sp", DMA(stg[0][:].rearrange("p (k f) -> p k f", k=KC), wg_v[e_]), writes=["stgC0"], chan="stgC0")
            S.add("act", ACT(wgt[b][:], stg[0][:].rearrange("p (k f) -> p k f", k=KC), AF.Identity),
                  reads=["stgC0"], writes=[f"wgt{b}"])
            S.add("sp", DMA(stg[1][:].rearrange("p (k f) -> p k f", k=KC), wu_v[e_]), writes=["stgC1"], chan="stgC1")
            S.add("act", ACT(wup[b][:], stg[1][:].rearrange("p (k f) -> p k f", k=KC), AF.Identity),
                  reads=["stgC1"], writes=[f"wup{b}"])
            S.add("sp", DMA(stg[2][:].rearrange("p (k f) -> p k f", k=2), wd_v[e_]), writes=["stgC2"], chan="stgC2")
            S.add("pool", CP(wdn[b][:], stg[2][:].rearrange("p (k f) -> p k f", k=2)),
                  reads=["stgC2"], writes=[f"wdn{b}"])
            S.add("pool", MS(sel[b][:], 0.0), writes=[f"sel{b}"])
            S.add("sp", DMA(sel[b][e_:e_ + 1, :], cst["ones32"][0:1, :]), reads=["ones32"], writes=[f"sel{b}"],
                  chan=f"sel{b}")
            for T in range(NST):
                i2 = it % 2
                it += 1
                tsl = slice(T * TS_, (T + 1) * TS_)
                xr = [("X1T", T * NSUB + q) for q in range(NSUB)]
                S.add("pe", MM(banks[4][:, 0:TS_], sel[b][:], combT[:, tsl]),
                      reads=[f"sel{b}"] + [("combT", T * NSUB + q) for q in range(NSUB)], writes=[bn[4]])
                S.add("act", ACT(cbs[i2][:], banks[4][:, 0:TS_], AF.Identity), reads=[bn[4]], writes=[f"cbs{i2}"])
                for fc in range(2):
                    hg, hu = banks[fc * 2], banks[fc * 2 + 1]
                    for kc in range(KC):
                        S.add("pe", MM(hg[:, 0:TS_], wgt[b][:, kc, fc * 128:(fc + 1) * 128], X1T[:, kc, tsl],
                                       start=(kc == 0), stop=(kc == KC - 1)),
                              reads=[f"wgt{b}"] + xr, writes=[bn[fc * 2]])
                    for kc in range(KC):
                        S.add("pe", MM(hu[:, 0:TS_], wup[b][:, kc, fc * 128:(fc + 1) * 128], X1T[:, kc, tsl],
                                       start=(kc == 0), stop=(kc == KC - 1)),
                              reads=[f"wup{b}"] + xr, writes=[bn[fc * 2 + 1]])
                    S.add("act", ACT(sl[fc][:], hg[:, 0:TS_], AF.Silu), reads=[bn[fc * 2]], writes=[f"sl{fc}"])
                    S.add("dve", TT(tl[fc][:], sl[fc][:], hu[:, 0:TS_], ALU.mult),
                          reads=[f"sl{fc}", bn[fc * 2 + 1]], writes=[f"tl{fc}"])
                    S.add("pool", TT(hid[i2][:, fc, :], tl[fc][:], cbs[i2][:], ALU.mult),
                          reads=[f"tl{fc}", f"cbs{i2}"], writes=[(f"hid{i2}", fc)])
                for sub in range(NSUB):
                    sI = T * NSUB + sub
                    for half in range(2):
                        py = banks[5 + pyi % 3]
                        pyn = bn[5 + pyi % 3]
                        pyi += 1
                        for fc in range(2):
                            S.add("pe", MM(py[:], hid[i2][:, fc, sub * 128:(sub + 1) * 128],
                                           wdn[b][:, fc, half * 512:(half + 1) * 512],
                                           start=(fc == 0), stop=(fc == 1)),
                                  reads=[(f"hid{i2}", 0), (f"hid{i2}", 1), f"wdn{b}"], writes=[pyn])
                        ysl = yacc[:, sI, half * 512:(half + 1) * 512]
                        S.add("dve", TT(ysl, ysl, py[:], ALU.add), reads=[pyn, ("yacc", sI)], writes=[("yacc", sI)])
    S.barrier()

    with ExitStack() as es:
        sb = lambda name, shape, dt: es.enter_context(nc.sbuf_tensor(name + cx.sfx, shape, dt))
        x2 = [sb(f"x2_{i}", [128, D_MODEL], F32) for i in range(2)]
        junk = sb("junk2", [128, D_MODEL], BF16)
        stt = [sb(f"lnst2_{i}", [128, 8], F32) for i in range(2)]
        xTo = [sb(f"xTo{i}", [128, KC, 128], BF16) for i in range(2)]
        want_T = d.get("xT_out") is not None
        if d.get("x_ar") is not None:
            xmk = [sb(f"xmk{i}", [128, 4, KC, 128], F32) for i in range(2)]
            pm = d["pm_tile"]
        xTo_v = d["xT_out"].rearrange("(kc p) s -> p kc s", p=128) if want_T else None
        for sI in range(NS):
            i2 = sI % 2
            tok = slice(sI * 128, (sI + 1) * 128)
            layer_norm_tile(cx, S, yacc[:, sI, :], ("yacc", sI), x2[i2][:], f"x2_{i2}", lnbc["ln2_g"], lnbc["ln2_b"],
                            stt[i2], f"lnst2_{i2}", junk, "junk2", "lnbc")
            S.add("sp", DMA(d["x_out"][tok, :], x2[i2][:]), reads=[f"x2_{i2}"], chan=f"xo{i2}")
            if not want_T:
                continue
            for kc in range(KC):
                bk = banks[(kc // 4)]
                S.add("pe", TR(bk[:, (kc % 4) * 128:(kc % 4 + 1) * 128], x2[i2][:, kc * 128:(kc + 1) * 128],
                               ident32[:]), reads=[f"x2_{i2}", "ident32"], writes=[bn[kc // 4]])
            for q in range(2):
                S.add("act" if q == 0 else "dve",
                      ACT(xTo[i2][:, q * 4:(q + 1) * 4, :], banks[q][:].rearrange("p (k t) -> p k t", k=4), AF.Identity)
                      if q == 0 else
                      CP(xTo[i2][:, q * 4:(q + 1) * 4, :], banks[q][:].rearrange("p (k t) -> p k t", k=4)),
                      reads=[bn[q]], writes=[(f"xTo{i2}", q)])
            S.add("sp", DMA(xTo_v[:, :, tok], xTo[i2][:]), reads=[(f"xTo{i2}", 0), (f"xTo{i2}", 1)], chan=f"xTo{i2}")
            if d.get("x_ar") is not None:
                xm = xmk[i2]
                for j in range(4):
                    S.add("dve",
                          TS(xm[:, j, :, :], xTo[i2][:], pm[:, j:j + 1], None, ALU.mult),
                          reads=[(f"xTo{i2}", 0), (f"xTo{i2}", 1), "pm"], writes=[(f"xmk{i2}", j, 0), (f"xmk{i2}", j, 1)])
                cht = d["x_ar"][0].shape[1]
                cpq = NTOK // cht
                s8, c8 = (sI * 128) // cht, (sI * 128) % cht
                for j in range(4):
                    dst = d["x_ar"][j * cpq + s8].rearrange("(kc p) t -> p kc t", p=128)[:, :, c8:c8 + 128]
                    S.add("sp", DMA(dst, xm[:, j]), reads=[(f"xmk{i2}", j, q) for q in range(2)],
                          chan=f"xar{i2}")


def build_k2(NTOK, x_bf16):
    nc = bass.Bass("TRN2", target_bir_lowering=False)
    d = {}
    inp = lambda name, shape, dt=F32: nc.dram_tensor(name, shape, dt, kind="ExternalInput").ap()
    d["x_tok"] = inp("x_tok", [NTOK, D_MODEL])
    d["xT"] = inp("xT", [D_MODEL, NTOK], BF16 if x_bf16 else F32)
    d["yT_sb"] = inp("yT_sb", [512, NTOK], BF16)
    d["yT_ca"] = inp("yT_ca", [512, NTOK], BF16)
    d["wg"] = inp("wg", [D_MODEL, 2048])
    d["b_gate"] = inp("b_gate", [2048])
    d["w_br_sb"] = inp("w_br_sb", [512, D_MODEL])
    d["w_br_ca"] = inp("w_br_ca", [512, D_MODEL])
    d["w_out"] = inp("w_out", [D_MODEL, D_MODEL])
    for nm in ("ln1_g", "ln1_b", "ln2_g", "ln2_b"):
        d[nm] = inp(nm, [D_MODEL])
    d["w_router"] = inp("w_router", [D_MODEL, 36])
    d["b_router"] = inp("b_router", [36])
    d["w_gate"] = inp("w_gate", [32, D_MODEL, 256])
    d["w_up"] = inp("w_up", [32, D_MODEL, 256])
    d["w_down"] = inp("w_down", [32, 256, D_MODEL])
    d["x_out"] = nc.dram_tensor("x_out", [NTOK, D_MODEL], F32, kind="ExternalOutput").ap()
    d["xT_out"] = nc.dram_tensor("xT_out", [D_MODEL, NTOK], BF16, kind="ExternalOutput").ap()
    d["mT_scratch"] = nc.dram_tensor("mT_scratch", [128, 8, NTOK], BF16, kind="Internal").ap()
    with ExitStack() as es:
        cx = Ctx(nc, es)
        cst = build_consts(cx)
        k2_body(cx, cst, NTOK, x_bf16, d)
        cx.finish()
    return nc


def build_k1(S_len, x_bf16):
    nc = bass.Bass("TRN2", target_bir_lowering=False)
    xT = nc.dram_tensor("xT", [D_MODEL, S_len], BF16 if x_bf16 else F32, kind="ExternalInput").ap()
    w1 = nc.dram_tensor("w1", [D_MODEL, 768], F32, kind="ExternalInput").ap()
    cab = nc.dram_tensor("cab", [2, 128, 640], F32, kind="ExternalInput").ap()
    ot_sb = nc.dram_tensor("ot_sb", [128, S_len], BF16, kind="ExternalOutput").ap()
    ot_ca = nc.dram_tensor("ot_ca", [128, S_len], BF16, kind="ExternalOutput").ap()
    with ExitStack() as es:
        cx = Ctx(nc, es)
        cst = build_consts(cx)
        k1_body(cx, cst, S_len, x_bf16, xT, w1, cab, ot_sb, ot_ca)
        cx.finish()
    return nc


def ca_bias_table(rel_bias_l, heads):
    q = np.arange(128)[:, None]
    p = np.arange(640)[None, :]
    rel = np.clip(q + 512 - p, -128, 128) + 128
    ci = q // 64
    kc = p // 64
    valid = (kc >= ci) & (kc <= ci + 8)
    out = np.empty((len(heads), 128, 640), np.float32)
    for i, h in enumerate(heads):
        out[i] = np.where(valid, rel_bias_l[h][rel], np.float32(NEG))
    return out


def k1_weights(w_in_l, r):
    c = lambda base: w_in_l[:, base + r * 128: base + (r + 1) * 128]
    return np.ascontiguousarray(np.concatenate(
        [c(0), c(512), c(1536), c(2048), c(1024), c(2560)], axis=1))


def k2_inputs(p):
    f = np.ascontiguousarray
    m = {}
    m["wg"] = f(p["w_in"][:, 3072:5120])
    m["b_gate"] = f(p["b_gate"].reshape(2048))
    m["w_br_sb"] = f(p["w_br_sb"])
    m["w_br_ca"] = f(p["w_br_ca"])
    m["w_out"] = f(p["w_out"])
    for nm in ("ln1_g", "ln1_b", "ln2_g", "ln2_b"):
        m[nm] = f(p[nm])
    m["w_router"] = f(np.concatenate([p["w_group"]] + [p["w_erouter"][g] for g in range(4)], axis=1))
    m["b_router"] = f(np.concatenate([p["b_group"]] + [p["b_erouter"][g] for g in range(4)], axis=0))
    m["w_gate"] = f(p["w_gate"].reshape(32, D_MODEL, 256))
    m["w_up"] = f(p["w_up"].reshape(32, D_MODEL, 256))
    m["w_down"] = f(p["w_down"].reshape(32, 256, D_MODEL))
    return m


def build_fused(S_len):
    nc = bass.Bass("TRN2", target_bir_lowering=False)
    L = DEPTH
    NTOK = S_len // 4
    inp = lambda name, shape, dt=F32: nc.dram_tensor(name, shape, dt, kind="ExternalInput").ap()
    scr = lambda name, shape, dt: nc.dram_tensor(name, shape, dt, kind="Internal").ap()
    x_tok0 = inp("x_tok", [S_len, D_MODEL])
    xT0 = inp("xT", [D_MODEL, S_len])
    w1 = inp("w1", [L, 4, D_MODEL, 768])
    cab = inp("cab", [L, 4, 2, 128, 640])
    P = {}
    P["wg"] = inp("wg", [L, D_MODEL, 2048])
    P["b_gate"] = inp("b_gate", [L, 2048])
    P["w_br_sb"] = inp("w_br_sb", [L, 512, D_MODEL])
    P["w_br_ca"] = inp("w_br_ca", [L, 512, D_MODEL])
    P["w_out"] = inp("w_out", [L, D_MODEL, D_MODEL])
    for nm in ("ln1_g", "ln1_b", "ln2_g", "ln2_b"):
        P[nm] = inp(nm, [L, D_MODEL])
    P["w_router"] = inp("w_router", [L, D_MODEL, 36])
    P["b_router"] = inp("b_router", [L, 36])
    P["w_gate"] = inp("w_gate", [L, 32, D_MODEL, 256])
    P["w_up"] = inp("w_up", [L, 32, D_MODEL, 256])
    P["w_down"] = inp("w_down", [L, 32, 256, D_MODEL])
    out = nc.dram_tensor("out", [S_len, D_MODEL], F32, kind="ExternalOutput").ap()
    yT_sb = scr("yT_sb_s", [512, S_len], BF16)
    yT_ca = scr("yT_ca_s", [512, S_len], BF16)
    xT1 = scr("xT1_s", [D_MODEL, S_len], BF16)
    x_tok1 = scr("x_tok1_s", [S_len, D_MODEL], F32)
    mT_s = scr("mT_scratch", [128, 8, NTOK], BF16)
    with ExitStack() as es:
        cx = Ctx(nc, es)
        cst = build_consts(cx)
        for l in range(L):
            xT_l = xT0 if l == 0 else xT1
            xtok_l = x_tok0 if l == 0 else x_tok1
            for r in range(4):
                with ExitStack() as es2:
                    cx.es = es2
                    cx.sfx = f"_a{l}{r}"
                    k1_body(cx, cst, S_len, l > 0, xT_l, w1[l, r], cab[l, r],
                            yT_sb[r * 128:(r + 1) * 128, :], yT_ca[r * 128:(r + 1) * 128, :])
                cx.es = es
                cx.S.barrier()
            for r in range(4):
                tsl = slice(r * NTOK, (r + 1) * NTOK)
                d = {k: v[l] for k, v in P.items()}
                d["x_tok"] = xtok_l[tsl, :]
                d["xT"] = xT_l[:, tsl]
                d["yT_sb"] = yT_sb[:, tsl]
                d["yT_ca"] = yT_ca[:, tsl]
                d["mT_scratch"] = mT_s
                last = (l == L - 1)
                d["x_out"] = out[tsl, :] if last else x_tok1[tsl, :]
                d["xT_out"] = None if last else xT1[:, tsl]
                with ExitStack() as es2:
                    cx.es = es2
                    cx.sfx = f"_b{l}{r}"
                    k2_body(cx, cst, NTOK, l > 0, d)
                cx.es = es
                cx.S.barrier()
        cx.finish()
    return nc


NO_CC = False
DBG = set()


def build_fused8(S_len):
    nc = bass.Bass("TRN2", target_bir_lowering=False)
    L = DEPTH
    NTOK = S_len // 4
    TS_ = 512
    NCH = NTOK // TS_
    CHT = min(1024, NTOK)
    CPQ = NTOK // CHT
    NXC = 4 * CPQ
    groups = [[0, 1, 2, 3], [4, 5, 6, 7]]
    inp = lambda name, shape, dt=F32: nc.dram_tensor(name, shape, dt, kind="ExternalInput").ap()
    scr = lambda name, shape, dt: nc.dram_tensor(name, shape, dt, kind="Internal").ap()
    x_tok0 = inp("x_tok", [NTOK, D_MODEL])
    xT0 = inp("xT", [D_MODEL, S_len])
    xTq0 = inp("xTq", [D_MODEL, NTOK])
    pm_d = inp("pm", [128, 4])
    w1 = inp("w1", [L, D_MODEL, 768])
    cab = inp("cab", [L, 2, 128, 640])
    P = {}
    P["wg"] = inp("wg", [L, D_MODEL, 2048])
    P["b_gate"] = inp("b_gate", [L, 2048])
    P["w_br_sb"] = inp("w_br_sb", [L, 512, D_MODEL])
    P["w_br_ca"] = inp("w_br_ca", [L, 512, D_MODEL])
    P["w_out"] = inp("w_out", [L, D_MODEL, D_MODEL])
    for nm in ("ln1_g", "ln1_b", "ln2_g", "ln2_b"):
        P[nm] = inp(nm, [L, D_MODEL])
    P["w_router"] = inp("w_router", [L, D_MODEL, 36])
    P["b_router"] = inp("b_router", [L, 36])
    P["w_gate"] = inp("w_gate", [L, 32, D_MODEL, 256])
    P["w_up"] = inp("w_up", [L, 32, D_MODEL, 256])
    P["w_down"] = inp("w_down", [L, 32, 256, D_MODEL])
    out = nc.dram_tensor("out", [NTOK, D_MODEL], F32, kind="ExternalOutput").ap()
    y_in = [scr(f"y_in{c}", [4 * 1024, TS_], F32) for c in range(NCH)]
    y_rs = [scr(f"y_rs{c}", [1024, TS_], F32) for c in range(NCH)]
    x_in = [scr(f"x_in{k}", [D_MODEL, CHT], F32) for k in range(NXC)]
    x_all = [scr(f"x_all{k}", [D_MODEL, CHT], F32) for k in range(NXC)]
    xT1 = scr("xT1_s", [D_MODEL, NTOK], BF16)
    x_tok1 = scr("x_tok1_s", [NTOK, D_MODEL], F32)
    mT_s = scr("mT_scratch", [128, 8, NTOK], BF16)
    with ExitStack() as es:
        cx = Ctx(nc, es)
        S = cx.S
        cst = build_consts(cx)
        pm = cx.sb("pm_t", [128, 4], F32)
        S.add("sp", DMA(pm[:], pm_d[:, :]), writes=["pm"], chan="prm0")
        for l in range(L):
            last = (l == L - 1)
            with ExitStack() as es2:
                cx.es = es2
                cx.sfx = f"_a{l}"
                yst = [cx.sb(f"yst{i}", [64, 4, 512], F32) for i in range(2)]
                cnt = [0]

                def emit_out(branch, h, qb, ob, obn, yst=yst, cnt=cnt):
                    if "noemit" in DBG:
                        return
                    k = cnt[0] % 2
                    cnt[0] += 1
                    q, c = (qb * 512) // NTOK, ((qb * 512) % NTOK) // TS_
                    for j in range(4):
                        S.add("dve", TS(yst[k][:, j, :], ob[:], pm[0:64, j:j + 1], None, ALU.mult),
                              reads=[obn, "pm"], writes=[(f"yst{k}", j)])
                    dst = y_in[c].rearrange("(q b j w) s -> q b w j s", q=4, b=2, j=4)[q][branch][h * 64:(h + 1) * 64]
                    S.add("sp", DMA(dst, yst[k][:]), reads=[(f"yst{k}", j) for j in range(4)], chan=f"yo{k}")

                if l == 0:
                    xt_tile = None
                else:
                    def xt_tile(t):
                        kk, c0 = (t * 256) // CHT, (t * 256) % CHT
                        return x_all[kk].rearrange("(kc p) s -> p kc s", p=128)[:, :, c0:c0 + 256]
                k1_body(cx, cst, S_len, False, xT0, w1[l], cab[l], None, None, xt_tile=xt_tile, emit_out=emit_out)
            cx.es = es
            S.barrier()
            for c in range(NCH if not NO_CC else 0):
                S.add("pool", lambda e, i_=y_in[c][:, :], o_=y_rs[c][:, :]: e.collective_compute(
                    "ReduceScatter", op=ALU.add, replica_groups=groups, ins=[i_], outs=[o_]),
                    chan="cc", inc=1)
            S.barrier()
            d = {k: v[l] for k, v in P.items()}
            d["x_tok"] = x_tok0 if l == 0 else x_tok1
            d["xT"] = xTq0 if l == 0 else xT1
            d["y_rs"] = [y_rs[c] for c in range(NCH)]
            d["mT_scratch"] = mT_s
            d["x_out"] = out if last else x_tok1
            d["xT_out"] = None if last else xT1
            d["x_ar"] = None if (last or "noxar" in DBG) else x_in
            d["pm_tile"] = pm
            with ExitStack() as es2:
                cx.es = es2
                cx.sfx = f"_b{l}"
                k2_body(cx, cst, NTOK, l > 0, d)
            cx.es = es
            S.barrier()
            if not last:
                for k in range(NXC if not NO_CC else 0):
                    S.add("pool", lambda e, i_=x_in[k][:, :], o_=x_all[k][:, :]: e.collective_compute(
                        "AllReduce", op=ALU.add, replica_groups=groups, ins=[i_], outs=[o_]),
                        chan="cc", inc=1)
                S.barrier()
        cx.finish()
    return nc


def kernel_fused8(p):
    x = p["x"]
    B, S_len, D = x.shape
    NTOK = S_len // 4
    f = np.ascontiguousarray
    key = ("fused8", S_len)
    if key not in _NC_CACHE:
        _NC_CACHE[key] = build_fused8(S_len)
    nc = _NC_CACHE[key]
    per_l = [k2_inputs({k: v[l] for k, v in p.items() if k != "x"}) for l in range(DEPTH)]
    shared = {k: f(np.stack([per_l[l][k] for l in range(DEPTH)])) for k in per_l[0]}
    xT_b = [f(x[b].T) for b in range(B)]
    in_maps = []
    for c in range(8):
        b, r = c // 4, c % 4
        tsl = slice(r * NTOK, (r + 1) * NTOK)
        m = dict(shared)
        m["x_tok"] = f(x[b, tsl, :])
        m["xT"] = xT_b[b]
        m["xTq"] = f(xT_b[b][:, tsl])
        pmv = np.zeros((128, 4), np.float32)
        pmv[:, r] = 1.0
        m["pm"] = pmv
        m["w1"] = f(np.stack([k1_weights(p["w_in"][l], r) for l in range(DEPTH)]))
        m["cab"] = f(np.stack([ca_bias_table(p["rel_bias"][l], [2 * r, 2 * r + 1]) for l in range(DEPTH)]))
        in_maps.append(m)
    res = run_bass_kernel_spmd(nc, in_maps, core_ids=list(range(8))).results
    return np.stack([np.concatenate([np.asarray(res[b * 4 + r]["out"]) for r in range(4)], axis=0)
                     for b in range(B)], axis=0).astype(np.float32)


FUSED = 8


def kernel_fused(p):
    x = p["x"]
    B, S_len, D = x.shape
    f = np.ascontiguousarray
    key = ("fused", S_len)
    if key not in _NC_CACHE:
        _NC_CACHE[key] = build_fused(S_len)
    nc = _NC_CACHE[key]
    shared = {}
    shared["w1"] = f(np.stack([np.stack([k1_weights(p["w_in"][l], r) for r in range(4)]) for l in range(DEPTH)]))
    shared["cab"] = f(np.stack([np.stack([ca_bias_table(p["rel_bias"][l], [2 * r, 2 * r + 1]) for r in range(4)])
                                for l in range(DEPTH)]))
    per_l = [k2_inputs({k: v[l] for k, v in p.items() if k != "x"}) for l in range(DEPTH)]
    for k in per_l[0]:
        shared[k] = f(np.stack([per_l[l][k] for l in range(DEPTH)]))
    in_maps = []
    for b in range(B):
        m = dict(shared)
        m["x_tok"] = f(x[b])
        m["xT"] = f(x[b].T)
        in_maps.append(m)
    res = run_bass_kernel_spmd(nc, in_maps, core_ids=list(range(B))).results
    return np.stack([np.asarray(res[b]["out"]) for b in range(B)], axis=0).astype(np.float32)


_NC_CACHE = {}


def _get_nc(kind, *args):
    key = (kind,) + args
    if key not in _NC_CACHE:
        _NC_CACHE[key] = build_k1(*args) if kind == "k1" else build_k2(*args)
    return _NC_CACHE[key]


def kernel(**inputs):
    p = {k: np.asarray(v) for k, v in inputs.items()}
    if FUSED == 8:
        return kernel_fused8(p)
    if FUSED:
        return kernel_fused(p)
    x = p["x"]
    B, S_len, D = x.shape
    NTOK = S_len // 4
    cores = list(range(8))
    f = np.ascontiguousarray
    x_cur = x
    xT_full = None
    xT_prev = None
    for l in range(DEPTH):
        first = (l == 0)
        lp = {k: v[l] for k, v in p.items() if k != "x"}
        if first:
            xT_b = [f(x[b].T) for b in range(B)]
        else:
            xT_b = xT_full
        nc1 = _get_nc("k1", S_len, not first)
        in1 = []
        for c in cores:
            b, r = c // 4, c % 4
            in1.append({"xT": xT_b[b], "w1": k1_weights(lp["w_in"], r),
                        "cab": ca_bias_table(lp["rel_bias"], [2 * r, 2 * r + 1])})
        r1 = run_bass_kernel_spmd(nc1, in1, core_ids=cores).results
        yT_sb = [np.concatenate([np.asarray(r1[b * 4 + r]["ot_sb"]) for r in range(4)], axis=0) for b in range(B)]
        yT_ca = [np.concatenate([np.asarray(r1[b * 4 + r]["ot_ca"]) for r in range(4)], axis=0) for b in range(B)]
        nc2 = _get_nc("k2", NTOK, not first)
        wk2 = k2_inputs(lp)
        in2 = []
        for c in cores:
            b, r = c // 4, c % 4
            tsl = slice(r * NTOK, (r + 1) * NTOK)
            m = dict(wk2)
            m["x_tok"] = f(x_cur[b, tsl, :])
            m["xT"] = f(xT_b[b][:, tsl]) if first else xT_prev[c]
            m["yT_sb"] = f(yT_sb[b][:, tsl])
            m["yT_ca"] = f(yT_ca[b][:, tsl])
            in2.append(m)
        r2 = run_bass_kernel_spmd(nc2, in2, core_ids=cores).results
        x_cur = np.stack([np.concatenate([np.asarray(r2[b * 4 + r]["x_out"]) for r in range(4)], axis=0)
                          for b in range(B)], axis=0)
        xT_prev = [np.asarray(r2[c]["xT_out"]) for c in cores]
        xT_full = [f(np.concatenate([xT_prev[b * 4 + r] for r in range(4)], axis=1)) for b in range(B)]
    return x_cur.astype(np.float32)
```

```python
import numpy as np
from contextlib import ExitStack
import concourse.bass as bass
import concourse.mybir as mybir
from concourse.bass_utils import run_bass_kernel_spmd

F32 = mybir.dt.float32
BF16 = mybir.dt.bfloat16
AF = mybir.ActivationFunctionType
ALU = mybir.AluOpType
AX = mybir.AxisListType

D_MODEL = 1024
BATCH = 2
SEQ = 8192
DEPTH = 2
HD = 64
ALPHA = (2.0 * DEPTH) ** 0.25
LN_EPS = 1e-5
NEG = -30000.0
HEAT = 0


class Sched:
    ENG = ("pe", "act", "dve", "pool", "sp")

    def __init__(self, nc):
        self.nc = nc
        self.ops = []
        self.last_w = {}
        self.readers = {}
        self.chan_last = {}
        self.chans = []
        self.eng_last = {}
        self.bar = set()

    def barrier(self, skip_prefix=None):
        self.bar = set(self.eng_last.values()) | set(
            v for k, v in self.chan_last.items() if not (skip_prefix and k.startswith(skip_prefix)))

    def add(self, eng, fn, reads=(), writes=(), chan=None, inc=16):
        idx = len(self.ops)
        deps = set(self.bar)
        for r in reads:
            if r in self.last_w:
                deps.add(self.last_w[r])
        for w in writes:
            if w in self.last_w:
                deps.add(self.last_w[w])
            for rd in self.readers.get(w, ()):
                deps.add(rd)
        if chan is not None:
            if chan in self.chan_last:
                deps.add(self.chan_last[chan])
            else:
                self.chans.append(chan)
            self.chan_last[chan] = idx
        for r in reads:
            self.readers.setdefault(r, []).append(idx)
        for w in writes:
            self.last_w[w] = idx
            self.readers[w] = []
        self.ops.append(dict(eng=eng, fn=fn, deps=deps, chan=chan, inc=inc))
        if chan is None:
            self.eng_last[eng] = idx
        return idx

    def emit(self, block, sems, final_wait_eng="sp"):
        ops = self.ops
        n = len(ops)
        has_dep = [False] * n
        for o in ops:
            for d in o["deps"]:
                has_dep[d] = True
        eng_cnt = {e: 0 for e in self.ENG}
        chan_cnt = {}
        sig = [None] * n
        for i, o in enumerate(ops):
            if o["chan"] is not None:
                c = o["chan"]
                chan_cnt[c] = chan_cnt.get(c, 0) + o["inc"]
                sig[i] = ("ch:" + c, chan_cnt[c])
            elif has_dep[i]:
                e = o["eng"]
                eng_cnt[e] += 1
                sig[i] = (e, eng_cnt[e])
        per_eng = {e: [] for e in self.ENG}
        for i, o in enumerate(ops):
            per_eng[o["eng"]].append(i)
        self.eng_cnt = eng_cnt

        def run(ename, eng):
            seen = {}
            for i in per_eng[ename]:
                o = ops[i]
                need = {}
                for d in o["deps"]:
                    od = ops[d]
                    if od["chan"] is None and od["eng"] == "pe" and ename == "pe" and o["chan"] is None:
                        continue
                    s, v = sig[d]
                    if need.get(s, 0) < v:
                        need[s] = v
                for s, v in need.items():
                    if seen.get(s, 0) >= v:
                        continue
                    eng.wait_ge(sems[s], v)
                    seen[s] = v
                ins = o["fn"](eng)
                if sig[i] is not None:
                    s, v = sig[i]
                    ins.then_inc(sems[s], o["inc"] if o["chan"] is not None else 1)
            if ename == final_wait_eng:
                for c, v in chan_cnt.items():
                    if seen.get("ch:" + c, 0) < v:
                        eng.wait_ge(sems["ch:" + c], v)

        @block.tensor
        def _(e):
            run("pe", e)

        @block.scalar
        def _(e):
            run("act", e)

        @block.vector
        def _(e):
            run("dve", e)

        @block.gpsimd
        def _(e):
            run("pool", e)

        @block.sync
        def _(e):
            run("sp", e)


class Ctx:
    def __init__(self, nc, es):
        self.nc = nc
        self.es = es
        self.S = Sched(nc)
        self.sfx = ""

    def sb(self, name, shape, dt):
        return self.es.enter_context(self.nc.sbuf_tensor(name + self.sfx, shape, dt))

    def ps(self, name, shape, dt):
        return self.es.enter_context(self.nc.psum_tensor(name + self.sfx, shape, dt))

    def finish(self):
        nc, es, S = self.nc, self.es, self.S
        sems = {}
        for e in Sched.ENG:
            sems[e] = es.enter_context(nc.semaphore("sem_" + e))
        for c in S.chans:
            sems["ch:" + c] = es.enter_context(nc.semaphore("semch_" + c))
        block = es.enter_context(nc.Block())
        S.emit(block, sems)


def build_consts(cx):
    S = cx.S
    c = {}
    ident = cx.sb("ident", [128, 128], BF16)
    ident32 = cx.sb("ident32", [128, 128], F32)
    negtri = cx.sb("negtri", [128, 128], BF16)
    negones = cx.sb("negones", [128, 128], BF16)
    m01 = cx.sb("m01", [128, 896], BF16)
    mbias = cx.sb("mbias", [128, 896], BF16)
    S.add("pool", lambda e: e.memset(ident[:], 0.0), writes=["ident"])
    S.add("pool", lambda e: e.affine_select(out=ident[:], in_=ident[:], pattern=[[-1, 128]],
                                            compare_op=ALU.not_equal, fill=1.0, base=0,
                                            channel_multiplier=1), reads=["ident"], writes=["ident"])
    S.add("pool", lambda e: e.memset(ident32[:], 0.0), writes=["ident32"])
    S.add("pool", lambda e: e.affine_select(out=ident32[:], in_=ident32[:], pattern=[[-1, 128]],
                                            compare_op=ALU.not_equal, fill=1.0, base=0,
                                            channel_multiplier=1), reads=["ident32"], writes=["ident32"])
    S.add("pool", lambda e: e.memset(negtri[:], -1.0), writes=["negtri"])
    S.add("pool", lambda e: e.affine_select(out=negtri[:], in_=negtri[:], pattern=[[-1, 128]],
                                            compare_op=ALU.is_ge, fill=0.0, base=0,
                                            channel_multiplier=1), reads=["negtri"], writes=["negtri"])
    S.add("pool", lambda e: e.memset(negones[:], -1.0), writes=["negones"])
    S.add("pool", lambda e: e.memset(m01[:], 1.0), writes=["m01"])
    S.add("pool", lambda e: e.affine_select(out=m01[:], in_=m01[:], pattern=[[1, 896]],
                                            compare_op=ALU.is_gt, fill=0.0, base=-384,
                                            channel_multiplier=-1), reads=["m01"], writes=["m01"])
    S.add("pool", lambda e: e.memset(mbias[:], 0.0), writes=["mbias"])
    S.add("pool", lambda e: e.affine_select(out=mbias[:], in_=mbias[:], pattern=[[1, 896]],
                                            compare_op=ALU.is_gt, fill=NEG, base=-384,
                                            channel_multiplier=-1), reads=["mbias"], writes=["mbias"])
    ones32 = cx.sb("ones32", [1, 128], F32)
    S.add("pool", lambda e: e.memset(ones32[:], 1.0), writes=["ones32"])
    c["ones32"] = ones32
    c.update(ident=ident, ident32=ident32, negtri=negtri, negones=negones, m01=m01, mbias=mbias)
    return c


def MM(out, lhsT, rhs, start=True, stop=True, skip=False):
    if skip:
        return lambda e: e.matmul(out, lhsT=lhsT, rhs=rhs, start=start, stop=stop, skip_group_check=True)
    return lambda e: e.matmul(out, lhsT=lhsT, rhs=rhs, start=start, stop=stop)


def ACT(out, in_, func, **kw):
    return lambda e: e.activation(out=out, in_=in_, func=func, **kw)


def TT(out, in0, in1, op):
    return lambda e: e.tensor_tensor(out=out, in0=in0, in1=in1, op=op)


def TS(out, in0, s1, s2, op0, op1=None):
    if op1 is None:
        return lambda e: e.tensor_scalar(out=out, in0=in0, scalar1=s1, scalar2=s2, op0=op0)
    return lambda e: e.tensor_scalar(out=out, in0=in0, scalar1=s1, scalar2=s2, op0=op0, op1=op1)


def STT(out, in0, scalar, in1, op0, op1):
    return lambda e: e.scalar_tensor_tensor(out=out, in0=in0, scalar=scalar, in1=in1, op0=op0, op1=op1)


def CP(out, in_):
    return lambda e: e.tensor_copy(out=out, in_=in_)


def DMA(out, in_):
    return lambda e: e.dma_start(out=out, in_=in_)


def TR(out, in_, ident):
    return lambda e: e.transpose(out, in_, ident)


def RED(out, in_, op):
    return lambda e: e.tensor_reduce(out=out, in_=in_, axis=AX.X, op=op)


def MS(ap, val):
    return lambda e: e.memset(ap, val)


def RCP(out, in_):
    return lambda e: e.reciprocal(out=out, in_=in_)


def k1_body(cx, cst, S_len, x_bf16, xT, w1, cab, ot_sb, ot_ca, xt_tile=None, emit_out=None):
    nc, S = cx.nc, cx.S
    NT = S_len // 256
    NB = S_len // 128
    NQ = S_len // 512
    KC = D_MODEL // 128

    QT_sb = cx.sb("QT_sb", [128, S_len], BF16)
    KT_sb = cx.sb("KT_sb", [128, S_len], BF16)
    QT_ca = cx.sb("QT_ca", [128, S_len], BF16)
    KT_ca = cx.sb("KT_ca", [128, S_len], BF16)
    V_all = cx.sb("V_all", [128, NB, 256], BF16)
    x32 = [cx.sb(f"x32_{i}", [128, KC, 256], F32) for i in range(2)]
    xbf = [cx.sb(f"xbf_{i}", [128, KC, 256], BF16) for i in range(2)]
    wbf = cx.sb("wbf", [128, KC, 768], BF16)
    cabs = cx.sb("cabs", [128, 2, 640], F32)

    banks = [cx.ps(f"bank{i}", [128, 512], F32) for i in range(7)]
    bankT = cx.ps("bankT", [128, 512], F32)

    if xt_tile is None:
        xT_v = xT.rearrange("(kc p) s -> p kc s", p=128)
        xt_tile = lambda t: xT_v[:, :, t * 256:(t + 1) * 256]
    w1_v = w1.rearrange("(kc p) n -> p kc n", p=128)

    S.add("sp", DMA(cabs[:], cab.rearrange("h q k -> q h k")), writes=["cabs"], chan="cab")

    for pc in range(3):
        buf = x32[pc % 2]
        nm = f"x32_{pc % 2}"
        S.add("sp", DMA(buf[:], w1_v[:, :, pc * 256:(pc + 1) * 256]), writes=[nm], chan=f"x{pc % 2}")
        S.add("dve", CP(wbf[:, :, pc * 256:(pc + 1) * 256], buf[:]), reads=[nm], writes=[f"wbf{pc}"])
    wres = ["wbf0", "wbf1", "wbf2"]

    dsts = [QT_sb, KT_sb, QT_ca, KT_ca]
    dnames = ["QT_sb", "KT_sb", "QT_ca", "KT_ca"]
    for t in range(NT):
        i2 = t % 2
        xb = xbf[i2]
        xbn = f"xbf_{i2}"
        tsl = slice(t * 256, (t + 1) * 256)
        if x_bf16:
            S.add("sp", DMA(xb[:], xt_tile(t)), writes=[xbn], chan=f"x{i2}")
            xr = [xbn]
        else:
            xs = x32[i2]
            xsn = f"x32_{i2}"
            S.add("sp", DMA(xs[:], xt_tile(t)), writes=[xsn], chan=f"x{i2}")
            S.add("dve", CP(xb[:, 0:4, :], xs[:, 0:4, :]), reads=[xsn], writes=[xbn + "a"])
            S.add("pool", CP(xb[:, 4:8, :], xs[:, 4:8, :]), reads=[xsn], writes=[xbn + "b"])
            xr = [xbn + "a", xbn + "b"]
        for g in range(4):
            bk = banks[g]
            bkn = f"bank{g}"
            for kc in range(KC):
                S.add("pe", MM(bk[:, 0:256], wbf[:, kc, g * 128:(g + 1) * 128], xb[:, kc, :],
                               start=(kc == 0), stop=(kc == KC - 1)), reads=wres + xr, writes=[bkn])
            dst = dsts[g]
            if g in (0, 2):
                S.add("act", ACT(dst[:, tsl], bk[:, 0:256], AF.Identity, scale=0.125),
                      reads=[bkn], writes=[(dnames[g], t // 2)])
            else:
                S.add("dve", CP(dst[:, tsl], bk[:, 0:256]), reads=[bkn], writes=[(dnames[g], t // 2)])
        for sub in range(2):
            bk = banks[4 + sub]
            bkn = f"bank{4 + sub}"
            for kc in range(KC):
                S.add("pe", MM(bk[:, 0:256], xb[:, kc, sub * 128:(sub + 1) * 128], wbf[:, kc, 512:768],
                               start=(kc == 0), stop=(kc == KC - 1)), reads=wres + xr, writes=[bkn])
            blk = t * 2 + sub
            if sub == 0:
                S.add("dve", CP(V_all[:, blk, :], bk[:, 0:256]), reads=[bkn], writes=[("V", blk)])
            else:
                S.add("act", ACT(V_all[:, blk, :], bk[:, 0:256], AF.Identity), reads=[bkn], writes=[("V", blk)])

    negtri, negones, ident, m01, mbias = cst["negtri"], cst["negones"], cst["ident"], cst["m01"], cst["mbias"]
    T32 = [cx.sb(f"T32_{i}", [128, 640], F32) for i in range(2)]
    Pb = [cx.sb(f"P_{i}", [128, 640], BF16) for i in range(2)]
    Pn = [cx.sb(f"Pn_{i}", [128, 640], BF16) for i in range(2)]
    WT = [cx.sb(f"WT_{i}", [128, 640], BF16) for i in range(2)]
    st = [cx.sb(f"st_{i}", [128, 4], F32) for i in range(4)]
    oca = [cx.sb(f"oca_{i}", [64, 512], BF16) for i in range(2)]
    po = banks[6]
    tb = [banks[4][:].bitcast(BF16), banks[5][:].bitcast(BF16)]
    tbn = ["bank4", "bank5"]
    its = [(h, m) for h in range(2) for m in range(NB)]
    NIT = len(its)

    def RC(ci):
        h, m = its[ci]
        nk = min(640, 128 * (m + 1))
        i2 = ci % 2
        return dict(h=h, m=m, nk=nk, ks=128 * (m + 1) - nk, p0=640 - nk, n1=min(nk, 512), nj=nk // 128,
                    hp=slice(h * 64, (h + 1) * 64), qsl=slice(m * 128, (m + 1) * 128),
                    pa=banks[i2 * 2], pan=f"bank{i2 * 2}", pb=banks[i2 * 2 + 1], pbn=f"bank{i2 * 2 + 1}",
                    T=T32[i2], Tn=f"T32_{i2}", P=Pb[i2], Pnm=f"P_{i2}", PN=Pn[i2], PNn=f"Pn_{i2}",
                    W=WT[i2], Wn=f"WT_{i2}", sx=st[ci % 4], sxn=f"st_{ci % 4}", tb=tb[i2], tbn=tbn[i2])

    for t in range(NIT + 6):
        ci = t
        if ci < NIT:
            r = RC(ci)
            ks, nk, n1, m, hp = r["ks"], r["nk"], r["n1"], r["m"], r["hp"]
            krd = [("KT_ca", j) for j in range(ks // 512, (ks + nk - 1) // 512 + 1)]
            S.add("pe", MM(r["pa"][:, 0:n1], QT_ca[hp, r["qsl"]], KT_ca[hp, ks:ks + n1]),
                  reads=[("QT_ca", m // 4)] + krd, writes=[r["pan"]])
            if nk > 512:
                S.add("pe", MM(r["pb"][:, 0:128], QT_ca[hp, r["qsl"]], KT_ca[hp, ks + 512:ks + 640]),
                      reads=[("QT_ca", m // 4)] + krd, writes=[r["pbn"]])
        ci = t - 1
        if 0 <= ci < NIT:
            r = RC(ci)
            T, sx, nk, n1, p0, h = r["T"], r["sx"], r["nk"], r["n1"], r["p0"], r["h"]
            S.add("dve", TT(T[:, 0:n1], r["pa"][:, 0:n1], cabs[:, h, p0:p0 + n1], ALU.add),
                  reads=[r["pan"], "cabs"], writes=[r["Tn"]])
            if nk > 512:
                S.add("dve", TT(T[:, 512:640], r["pb"][:, 0:128], cabs[:, h, 512:640], ALU.add),
                      reads=[r["pbn"], "cabs", r["Tn"]], writes=[r["Tn"]])
            S.add("dve", RED(sx[:, 0:1], T[:, 0:nk], ALU.max), reads=[r["Tn"]], writes=[r["sxn"]])
            S.add("dve", TS(sx[:, 1:2], sx[:, 0:1], -1.0, None, ALU.mult), reads=[r["sxn"]], writes=[r["sxn"]])
            S.add("pool", MS(sx[:, 2:3], 0.0), reads=[r["sxn"]], writes=[r["sxn"]])
        ci = t - 2
        if 0 <= ci < NIT:
            r = RC(ci)
            nk, sx = r["nk"], r["sx"]
            S.add("act", ACT(r["P"][:, 0:nk], r["T"][:, 0:nk], AF.Exp, bias=sx[:, 1:2], accum_out=sx[:, 2:3]),
                  reads=[r["Tn"], r["sxn"]], writes=[r["Pnm"], r["sxn"]])
        ci = t - 3
        if 0 <= ci < NIT:
            r = RC(ci)
            nk, sx = r["nk"], r["sx"]
            S.add("dve", RCP(sx[:, 3:4], sx[:, 2:3]), reads=[r["sxn"]], writes=[r["sxn"]])
            S.add("dve", TS(r["PN"][:, 0:nk], r["P"][:, 0:nk], sx[:, 3:4], None, ALU.mult),
                  reads=[r["Pnm"], r["sxn"]], writes=[r["PNn"]])
        ci = t - 4
        if 0 <= ci < NIT:
            r = RC(ci)
            for j in range(r["nj"]):
                S.add("pe", TR(r["tb"][:, j * 128:(j + 1) * 128], r["PN"][:, j * 128:(j + 1) * 128], ident[:]),
                      reads=[r["PNn"], "ident"], writes=[r["tbn"]])
        ci = t - 5
        if 0 <= ci < NIT:
            r = RC(ci)
            nk = r["nk"]
            S.add("act", ACT(r["W"][:, 0:nk], r["tb"][:, 0:nk], AF.Identity), reads=[r["tbn"]], writes=[r["Wn"]])
        ci = t - 6
        if 0 <= ci < NIT:
            r = RC(ci)
            h, m, nj, ks = r["h"], r["m"], r["nj"], r["ks"]
            mcol = (m % 4) * 128
            for j in range(nj):
                kb = ks // 128 + j
                S.add("pe", MM(po[0:64, mcol:mcol + 128], V_all[:, kb, 128 + h * 64:128 + (h + 1) * 64],
                               r["W"][:, j * 128:(j + 1) * 128], start=(j == 0), stop=(j == nj - 1)),
                      reads=[r["Wn"], ("V", kb)], writes=["bank6"])
            if m % 4 == 3:
                oq = (h * NB + m) // 4
                ob, obn = oca[oq % 2], f"oca_{oq % 2}"
                S.add("act", ACT(ob[:], po[0:64, :], AF.Identity), reads=["bank6"], writes=[obn])
                if emit_out is not None:
                    emit_out(1, h, m // 4, ob, obn)
                else:
                    S.add("sp", DMA(ot_ca[h * 64:(h + 1) * 64, (m - 3) * 128:(m + 1) * 128], ob[:]),
                          reads=[obn], chan=f"oc{oq % 2}")

    E32 = [cx.sb(f"E32_{i}", [128, 512], F32) for i in range(2)]
    SPb = [cx.sb(f"SP_{i}", [128, 512], BF16) for i in range(3)]
    Wb = [cx.sb(f"W_{i}", [128, 512], BF16) for i in range(2)]
    C32 = cx.sb("C32", [128, 512], F32)
    Cbf = [cx.sb(f"Cbf_{i}", [128, 512], BF16) for i in range(4)]
    osb = [cx.sb(f"osb_{i}", [64, 512], BF16) for i in range(2)]
    blocks = []
    oi = 0
    for qb in range(NQ):
        for h in range(2):
            n = 4 * (qb + 1)
            for i in range(n):
                blocks.append(dict(h=h, qb=qb, i=i, n=n, kb=n - 1 - i, oi=oi))
            oi += 1
    NBK = len(blocks)

    def R(g):
        bl = blocks[g]
        h, qb, kb = bl["h"], bl["qb"], bl["kb"]
        d_ = dict(bl)
        d_.update(hp=slice(h * 64, (h + 1) * 64), qs=slice(qb * 512, (qb + 1) * 512),
                  pz=banks[g % 6], pzn=f"bank{g % 6}", E=E32[g % 2], En=f"E32_{g % 2}",
                  SP=SPb[g % 3], SPn=f"SP_{g % 3}", W=Wb[g % 2], Wn=f"W_{g % 2}",
                  C=Cbf[g % 4], Cn=f"Cbf_{g % 4}", Cp=Cbf[(g + 1) % 4], Cpn=f"Cbf_{(g + 1) % 4}",
                  po=banks[6] if bl["oi"] % 2 == 0 else bankT, pon="bank6" if bl["oi"] % 2 == 0 else "bankT",
                  diag=kb >= 4 * qb, off=(kb - 4 * qb) * 128)
        return d_

    for t in range(NBK + 5):
        g = t
        if g < NBK:
            r = R(g)
            S.add("pe", MM(r["pz"][:], KT_sb[r["hp"], r["kb"] * 128:(r["kb"] + 1) * 128], QT_sb[r["hp"], r["qs"]]),
                  reads=[("KT_sb", r["kb"] // 4), ("QT_sb", r["qb"])], writes=[r["pzn"]])
        g = t - 1
        if 0 <= g < NBK:
            r = R(g)
            S.add("act", ACT(r["E"][:], r["pz"][:], AF.Exp), reads=[r["pzn"]], writes=[r["En"]])
        g = t - 2
        if 0 <= g < NBK:
            r = R(g)
            off = r["off"]
            S.add("act", ACT(r["SP"][:], r["E"][:], AF.Ln, bias=1.0), reads=[r["En"]], writes=[r["SPn"]])
            if r["diag"]:
                S.add("dve", TT(r["SP"][:], r["SP"][:], m01[:, 384 - off:896 - off], ALU.mult),
                      reads=[r["SPn"], "m01"], writes=[r["SPn"]])
            if r["i"] < r["n"] - 1:
                if r["i"] == 0:
                    S.add("dve", CP(C32[:], r["SP"][:]), reads=[r["SPn"]], writes=["C32"])
                    S.add("dve", CP(r["Cp"][:], r["SP"][:]), reads=[r["SPn"]], writes=[r["Cpn"]])
                else:
                    S.add("dve", TT(C32[:], C32[:], r["SP"][:], ALU.add), reads=[r["SPn"], "C32"], writes=["C32"])
                    S.add("dve", CP(r["Cp"][:], C32[:]), reads=["C32"], writes=[r["Cpn"]])
        g = t - 3
        if 0 <= g < NBK:
            r = R(g)
            off = r["off"]
            S.add("pe", MM(r["pz"][:], negtri[:], r["SP"][:], start=False, stop=False, skip=True),
                  reads=[r["SPn"], "negtri"], writes=[r["pzn"]])
            if r["i"] > 0:
                S.add("pe", MM(r["pz"][:], negones[:], r["C"][:], start=False, stop=False, skip=True),
                      reads=[r["Cn"], "negones"], writes=[r["pzn"]])
            if r["diag"]:
                S.add("pe", MM(r["pz"][:], ident[:], mbias[:, 384 - off:896 - off],
                               start=False, stop=False, skip=True),
                      reads=["ident", "mbias"], writes=[r["pzn"]])
        g = t - 4
        if 0 <= g < NBK:
            r = R(g)
            S.add("act", ACT(r["W"][:], r["pz"][:], AF.Exp), reads=[r["pzn"]], writes=[r["Wn"]])
        g = t - 5
        if 0 <= g < NBK:
            r = R(g)
            h = r["h"]
            po, pon = r["po"], r["pon"]
            S.add("pe", MM(po[0:64, 0:512], V_all[:, r["kb"], h * 64:(h + 1) * 64], r["W"][:],
                           start=(r["i"] == 0), stop=(r["i"] == r["n"] - 1)),
                  reads=[r["Wn"], ("V", r["kb"])], writes=[pon])
            for _ in range(HEAT):
                S.add("pe", MM(po[64:128, 0:512], negones[:, 0:64], m01[:, 0:512]), reads=["negones", "m01"])
            if r["i"] == r["n"] - 1:
                ob = osb[r["oi"] % 2]
                obn = f"osb_{r['oi'] % 2}"
                S.add("dve", CP(ob[:], po[0:64, 0:512]), reads=[pon], writes=[obn])
                if emit_out is not None:
                    emit_out(0, h, r["qb"], ob, obn)
                else:
                    S.add("sp", DMA(ot_sb[h * 64:(h + 1) * 64, r["qs"]], ob[:]), reads=[obn], chan=f"o{r['oi'] % 2}")


def layer_norm_tile(cx, S, u, un, out, outn, gbc, bbc, st, stn, junk, junkn, gn, tmp=None, tmpn=None):
    D = D_MODEL
    S.add("pool", MS(st[:, 0:2], 0.0), reads=[stn], writes=[stn])
    S.add("act", ACT(junk[:], u, AF.Identity, accum_out=st[:, 0:1]), reads=[un, stn], writes=[junkn, stn])
    S.add("act", ACT(junk[:], u, AF.Square, accum_out=st[:, 1:2]), reads=[un, stn, junkn], writes=[junkn, stn])
    S.add("dve", TS(st[:, 2:4], st[:, 0:2], 1.0 / D, None, ALU.mult), reads=[stn], writes=[stn])
    S.add("dve", STT(st[:, 4:5], st[:, 2:3], st[:, 2:3], st[:, 3:4], ALU.mult, ALU.subtract),
          reads=[stn], writes=[stn])
    S.add("dve", TS(st[:, 5:6], st[:, 4:5], -1.0, LN_EPS, ALU.mult, ALU.add), reads=[stn], writes=[stn])
    S.add("act", ACT(st[:, 5:6], st[:, 5:6], AF.Ln), reads=[stn], writes=[stn])
    S.add("act", ACT(st[:, 6:7], st[:, 5:6], AF.Exp, scale=-0.5), reads=[stn], writes=[stn])
    S.add("dve", STT(st[:, 7:8], st[:, 2:3], -1.0, st[:, 6:7], ALU.mult, ALU.mult), reads=[stn], writes=[stn])
    S.add("act", ACT(out, u, AF.Identity, scale=st[:, 6:7], bias=st[:, 7:8]), reads=[un, stn], writes=[outn])
    S.add("dve", TT(out, out, gbc[:], ALU.mult), reads=[outn, gn], writes=[outn])
    S.add("pool", TT(out, out, bbc[:], ALU.add), reads=[outn, gn], writes=[outn])


def k2_body(cx, cst, NTOK, x_bf16, d):
    nc, S = cx.nc, cx.S
    KC = D_MODEL // 128
    TS_ = min(512, NTOK)
    NST = NTOK // TS_
    NSUB = TS_ // 128
    NS = NTOK // 128
    ident32 = cst["ident32"]
    banks = [cx.ps(f"kb{i}", [128, 512], F32) for i in range(8)]
    bn = [f"kb{i}" for i in range(8)]

    lnbc = {}
    for nm in ("ln1_g", "ln1_b", "ln2_g", "ln2_b"):
        t = cx.sb("bc_" + nm, [128, D_MODEL], F32)
        S.add("sp", DMA(t[:], d[nm].partition_broadcast(128)), writes=["lnbc"], chan="prm")
        lnbc[nm] = t
    bg = cx.sb("bg", [128, 16], F32)
    S.add("sp", lambda e, o_=bg[:], i_=d["b_gate"].rearrange("(j p) -> p j", p=128): e.dma_start(
        out=o_, in_=i_, allow_slow_non_contiguous=True), writes=["bg"], chan="prm")
    wr32 = cx.sb("wr32", [128, KC, 36], F32)
    S.add("sp", DMA(wr32[:], d["w_router"].rearrange("(kc p) n -> p kc n", p=128)), writes=["wr32"], chan="prm")
    brt = cx.sb("brt", [128, 36], F32)
    S.add("sp", DMA(brt[:], d["b_router"].partition_broadcast(128)), writes=["brt"], chan="prm")

    mT_d = d["mT_scratch"]
    xT_v = d["xT"].rearrange("(kc p) s -> p kc s", p=128)
    if d.get("y_rs") is None:
        ysb_v = d["yT_sb"].rearrange("(kc p) s -> p kc s", p=128)
        yca_v = d["yT_ca"].rearrange("(kc p) s -> p kc s", p=128)

    with ExitStack() as es:
        sb = lambda name, shape, dt: es.enter_context(nc.sbuf_tensor(name + cx.sfx, shape, dt))
        wg_bf = sb("wg_bf", [128, KC, 2048], BF16)
        wbs_bf = sb("wbs_bf", [128, 4, 1024], BF16)
        wbc_bf = sb("wbc_bf", [128, 4, 1024], BF16)
        stg = [sb(f"stgA{i}", [128, 2048], F32) for i in range(2)]
        xbf = [sb(f"xbfA{i}", [128, KC, TS_], BF16) for i in range(2)]
        ysb = [sb(f"ysb{i}", [128, 4, TS_], BF16) for i in range(2)]
        yca = [sb(f"yca{i}", [128, 4, TS_], BF16) for i in range(2)]
        gs = [sb(f"gs{i}", [128, TS_], F32) for i in range(2)]
        gc = [sb(f"gc{i}", [128, TS_], F32) for i in range(2)]
        t1 = sb("t1", [128, TS_], F32)
        t2 = sb("t2", [128, TS_], F32)
        mT = [sb(f"mT{i}", [128, KC, TS_], BF16) for i in range(2)]
        si = 0

        def stage_cast(dst, src_ap, ncols, rn, k=None, rd=()):
            nonlocal si
            b = si % 2
            si += 1
            sv = stg[b][:, 0:ncols]
            if k is not None:
                sv = sv.rearrange("p (k t) -> p k t", k=k)
            S.add("sp", DMA(sv, src_ap), reads=list(rd), writes=[f"stgA{b}"], chan=f"stgA{b}")
            S.add("dve" if b == 0 else "pool", CP(dst, sv), reads=[f"stgA{b}"], writes=[rn])

        wg_v = d["wg"].rearrange("(kc p) n -> p kc n", p=128)
        for kc in range(KC):
            stage_cast(wg_bf[:, kc, :], wg_v[:, kc, :], 2048, "wg_bf")
        wbs_v = d["w_br_sb"].rearrange("(kc p) n -> p kc n", p=128)
        wbc_v = d["w_br_ca"].rearrange("(kc p) n -> p kc n", p=128)
        for kc in range(0, 4, 2):
            stage_cast(wbs_bf[:, kc:kc + 2, :], wbs_v[:, kc:kc + 2, :], 2048, "wbs_bf", k=2)
            stage_cast(wbc_bf[:, kc:kc + 2, :], wbc_v[:, kc:kc + 2, :], 2048, "wbc_bf", k=2)
        for T in range(NST):
            i2 = T % 2
            tsl = slice(T * TS_, (T + 1) * TS_)
            xb, xbn = xbf[i2], f"xbfA{i2}"
            if x_bf16:
                S.add("sp", DMA(xb[:], xT_v[:, :, tsl]), writes=[xbn], chan=f"xA{i2}")
            else:
                per = 2048 // TS_
                for k0 in range(0, KC, per):
                    stage_cast(xb[:, k0:k0 + per, :], xT_v[:, k0:k0 + per, tsl], per * TS_, xbn, k=per)
            if d.get("y_rs") is not None:
                yv = d["y_rs"][T].rearrange("(b kc p) s -> b p kc s", b=2, p=128)
                per = 2048 // TS_
                for k0 in range(0, 4, per):
                    stage_cast(ysb[i2][:, k0:k0 + per, :], yv[0][:, k0:k0 + per, :], per * TS_, f"ysb{i2}", k=per,
                               rd=[("y_rs", T)])
                    stage_cast(yca[i2][:, k0:k0 + per, :], yv[1][:, k0:k0 + per, :], per * TS_, f"yca{i2}", k=per,
                               rd=[("y_rs", T)])
            else:
                S.add("sp", DMA(ysb[i2][:], ysb_v[:, :, tsl]), writes=[f"ysb{i2}"], chan=f"yA{i2}")
                S.add("sp", DMA(yca[i2][:], yca_v[:, :, tsl]), writes=[f"yca{i2}"], chan=f"yB{i2}")
            mt, mtn = mT[i2], f"mT{i2}"
            for fo in range(KC):
                j2 = fo % 2
                fsl = slice(fo * 128, (fo + 1) * 128)
                fsl2 = slice(1024 + fo * 128, 1024 + (fo + 1) * 128)
                b0, b1, b2, b3 = j2 * 4, j2 * 4 + 1, j2 * 4 + 2, j2 * 4 + 3
                for kc in range(KC):
                    S.add("pe", MM(banks[b0][:, 0:TS_], wg_bf[:, kc, fsl], xb[:, kc, :],
                                   start=(kc == 0), stop=(kc == KC - 1)), reads=["wg_bf", xbn], writes=[bn[b0]])
                S.add("act", ACT(gs[j2][:], banks[b0][:, 0:TS_], AF.Sigmoid, bias=bg[:, fo:fo + 1]),
                      reads=[bn[b0], "bg"], writes=[f"gs{j2}"])
                for kc in range(KC):
                    S.add("pe", MM(banks[b1][:, 0:TS_], wg_bf[:, kc, fsl2], xb[:, kc, :],
                                   start=(kc == 0), stop=(kc == KC - 1)), reads=["wg_bf", xbn], writes=[bn[b1]])
                S.add("act", ACT(gc[j2][:], banks[b1][:, 0:TS_], AF.Sigmoid, bias=bg[:, 8 + fo:9 + fo]),
                      reads=[bn[b1], "bg"], writes=[f"gc{j2}"])
                for kc in range(4):
                    S.add("pe", MM(banks[b2][:, 0:TS_], wbs_bf[:, kc, fsl], ysb[i2][:, kc, :],
                                   start=(kc == 0), stop=(kc == 3)), reads=["wbs_bf", f"ysb{i2}"], writes=[bn[b2]])
                for kc in range(4):
                    S.add("pe", MM(banks[b3][:, 0:TS_], wbc_bf[:, kc, fsl], yca[i2][:, kc, :],
                                   start=(kc == 0), stop=(kc == 3)), reads=["wbc_bf", f"yca{i2}"], writes=[bn[b3]])
                S.add("dve", TT(t1[:], gs[j2][:], banks[b2][:, 0:TS_], ALU.mult),
                      reads=[f"gs{j2}", bn[b2]], writes=["t1"])
                S.add("dve", TT(t2[:], gc[j2][:], banks[b3][:, 0:TS_], ALU.mult),
                      reads=[f"gc{j2}", bn[b3]], writes=["t2"])
                S.add("pool", TT(mt[:, fo, :], t1[:], t2[:], ALU.add), reads=["t1", "t2"], writes=[mtn])
            S.add("sp", DMA(mT_d[:, :, tsl], mt[:]), reads=[mtn], writes=["mT_d"], chan=f"mTo{i2}")
    S.barrier()

    yacc = cx.sb("yacc", [128, NS, D_MODEL], F32)
    X1T = cx.sb("X1T", [128, KC, NTOK], BF16)
    combT = cx.sb("combT", [32, NTOK], F32)

    with ExitStack() as es:
        sb = lambda name, shape, dt: es.enter_context(nc.sbuf_tensor(name + cx.sfx, shape, dt))
        wo_bf = sb("wo_bf", [128, KC, 1024], BF16)
        stg = [sb(f"stgB{i}", [128, 2048], F32) for i in range(2)]
        wo_v = d["w_out"].rearrange("(kc p) n -> p kc n", p=128)
        for k0 in range(0, KC, 2):
            b = (k0 // 2) % 2
            sv = stg[b][:].rearrange("p (k t) -> p k t", k=2)
            S.add("sp", DMA(sv, wo_v[:, k0:k0 + 2, :]), writes=[f"stgB{b}"], chan=f"stgB{b}")
            S.add("dve" if b == 0 else "pool", CP(wo_bf[:, k0:k0 + 2, :], sv),
                  reads=[f"stgB{b}"], writes=["wo_bf"])
        mTt = [sb(f"mTt{i}", [128, KC, 128], BF16) for i in range(2)]
        xtok = [sb(f"xtok{i}", [128, D_MODEL], F32) for i in range(2)]
        u = [sb(f"u{i}", [128, D_MODEL], F32) for i in range(2)]
        x1 = [sb(f"x1_{i}", [128, D_MODEL], F32) for i in range(2)]
        junk = sb("junk", [128, D_MODEL], BF16)
        x1T32 = sb("x1T32", [128, KC, 128], F32)
        stt = [sb(f"lnst{i}", [128, 8], F32) for i in range(2)]
        rt = [sb(f"rt{i}", [128, 128], F32) for i in range(2)]
        comb = [sb(f"comb{i}", [128, 32], F32) for i in range(2)]
        for sI in range(NS):
            i2 = sI % 2
            tok = slice(sI * 128, (sI + 1) * 128)
            S.add("sp", DMA(mTt[i2][:], mT_d[:, :, tok]), reads=["mT_d"], writes=[f"mTt{i2}"], chan=f"mTi{i2}")
            S.add("sp", DMA(xtok[i2][:], d["x_tok"][tok, :]), writes=[f"xtok{i2}"], chan=f"xt{i2}")
            for half in range(2):
                bk = banks[half]
                for kc in range(KC):
                    S.add("pe", MM(bk[:], mTt[i2][:, kc, :], wo_bf[:, kc, half * 512:(half + 1) * 512],
                                   start=(kc == 0), stop=(kc == KC - 1)),
                          reads=[f"mTt{i2}", "wo_bf"], writes=[bn[half]])
                S.add("dve", STT(u[i2][:, half * 512:(half + 1) * 512], xtok[i2][:, half * 512:(half + 1) * 512],
                                 ALPHA, bk[:], ALU.mult, ALU.add),
                      reads=[f"xtok{i2}", bn[half]], writes=[f"u{i2}"])
            layer_norm_tile(cx, S, u[i2][:], f"u{i2}", x1[i2][:], f"x1_{i2}", lnbc["ln1_g"], lnbc["ln1_b"],
                            stt[i2], f"lnst{i2}", junk, "junk", "lnbc")
            S.add("pool", TS(yacc[:, sI, :], x1[i2][:], ALPHA, None, ALU.mult),
                  reads=[f"x1_{i2}"], writes=[("yacc", sI)])
            for kc in range(KC):
                bk = banks[2 + (kc // 4)]
                S.add("pe", TR(bk[:, (kc % 4) * 128:(kc % 4 + 1) * 128], x1[i2][:, kc * 128:(kc + 1) * 128],
                               ident32[:]), reads=[f"x1_{i2}", "ident32"], writes=[bn[2 + kc // 4]])
            for q in range(2):
                S.add("act", ACT(x1T32[:, q * 4:(q + 1) * 4, :],
                                 banks[2 + q][:].rearrange("p (k t) -> p k t", k=4), AF.Identity),
                      reads=[bn[2 + q]], writes=["x1T32"])
            S.add("pool", CP(X1T[:, :, tok], x1T32[:]), reads=["x1T32"], writes=[("X1T", sI)])
            lg = banks[4]
            for kc in range(KC):
                S.add("pe", MM(lg[:, 0:36], x1T32[:, kc, :], wr32[:, kc, :], start=(kc == 0), stop=(kc == KC - 1)),
                      reads=["x1T32", "wr32"], writes=[bn[4]])
            R_, rn = rt[i2], f"rt{i2}"
            L = R_[:, 0:36]

            def V(eng, fn):
                S.add(eng, fn, reads=[rn, "brt", bn[4]] if eng != "pool" else [rn, "brt"], writes=[rn])
            V("dve", TT(L, lg[:, 0:36], brt[:], ALU.add))
            gmax, ngmax, sg, gval = R_[:, 36:37], R_[:, 37:38], R_[:, 38:39], R_[:, 39:40]
            ohg, eg, esel = R_[:, 40:44], R_[:, 44:48], R_[:, 48:56]
            m1, oh1, e2, m2, oh2 = R_[:, 56:57], R_[:, 64:72], R_[:, 72:80], R_[:, 57:58], R_[:, 80:88]
            dd, ed, den, w1, w2 = R_[:, 58:59], R_[:, 59:60], R_[:, 60:61], R_[:, 61:62], R_[:, 62:63]
            ew, gw = R_[:, 88:96], R_[:, 96:100]
            V("dve", RED(gmax, L[:, 0:4], ALU.max))
            V("dve", TS(ohg, L[:, 0:4], gmax, None, ALU.is_equal))
            V("dve", TS(ngmax, gmax, -1.0, None, ALU.mult))
            V("pool", MS(sg, 0.0))
            V("act", ACT(eg, L[:, 0:4], AF.Exp, bias=ngmax, accum_out=sg))
            V("dve", RCP(gval, sg))
            V("dve", TS(esel, L[:, 4:12], ohg[:, 0:1], None, ALU.mult))
            for g in range(1, 4):
                V("dve", STT(esel, L[:, 4 + 8 * g:12 + 8 * g], ohg[:, g:g + 1], esel, ALU.mult, ALU.add))
            V("dve", RED(m1, esel, ALU.max))
            V("dve", TS(oh1, esel, m1, None, ALU.is_equal))
            V("dve", STT(e2, oh1, -1e30, esel, ALU.mult, ALU.add))
            V("dve", RED(m2, e2, ALU.max))
            V("dve", TS(oh2, e2, m2, None, ALU.is_equal))
            V("dve", TT(dd, m2, m1, ALU.subtract))
            V("act", ACT(ed, dd, AF.Exp))
            V("dve", TS(den, ed, 1.0, None, ALU.add))
            V("dve", RCP(w1, den))
            V("dve", TT(w2, ed, w1, ALU.mult))
            V("dve", TS(ew, oh1, w1, None, ALU.mult))
            V("dve", STT(ew, oh2, w2, ew, ALU.mult, ALU.add))
            V("dve", TS(gw, ohg, gval, None, ALU.mult))
            cb_, cbn = comb[i2], f"comb{i2}"
            for g in range(4):
                S.add("dve", TS(cb_[:, 8 * g:8 * g + 8], ew, gw[:, g:g + 1], None, ALU.mult),
                      reads=[rn], writes=[cbn])
            S.add("pe", TR(banks[5][0:32, 0:128], cb_[:], ident32[:]), reads=[cbn, "ident32"], writes=[bn[5]])
            S.add("act", ACT(combT[:, tok], banks[5][0:32, 0:128], AF.Identity), reads=[bn[5]],
                  writes=[("combT", sI)])
    S.barrier()

    NE = 32
    with ExitStack() as es:
        sb = lambda name, shape, dt: es.enter_context(nc.sbuf_tensor(name + cx.sfx, shape, dt))
        wgt = [sb(f"wgt{i}", [128, KC, 256], BF16) for i in range(2)]
        wup = [sb(f"wup{i}", [128, KC, 256], BF16) for i in range(2)]
        wdn = [sb(f"wdn{i}", [128, 2, 1024], BF16) for i in range(2)]
        stg = [sb(f"stgC{i}", [128, 2048], F32) for i in range(3)]
        sel = [sb(f"sel{i}", [32, 128], F32) for i in range(2)]
        sl = [sb(f"sl{i}", [128, TS_], F32) for i in range(2)]
        tl = [sb(f"tl{i}", [128, TS_], F32) for i in range(2)]
        cbs = [sb(f"cbs{i}", [128, TS_], F32) for i in range(2)]
        hid = [sb(f"hid{i}", [128, 2, TS_], BF16) for i in range(2)]
        wg_v = d["w_gate"].rearrange("e (kc p) f -> e p kc f", p=128)
        wu_v = d["w_up"].rearrange("e (kc p) f -> e p kc f", p=128)
        wd_v = d["w_down"].rearrange("e (fc p) n -> e p fc n", p=128)
        it = 0
        pyi = 0
        for e_ in range(NE):
            b = e_ % 2
            S.add("sp", DMA(stg[0][:].rearrange("p (k f) -> p k f", k=KC), wg_v[e_]), writes=["stgC0"], chan="stgC0")
            S.add("act", ACT(wgt[b][:], stg[0][:].rearrange("p (k f) -> p k f", k=KC), AF.Identity),
                  reads=["stgC0"], writes=[f"wgt{b}"])
            S.add("sp", DMA(stg[1][:].rearrange("p (k f) -> p k f", k=KC), wu_v[e_]), writes=["stgC1"], chan="stgC1")
            S.add("act", ACT(wup[b][:], stg[1][:].rearrange("p (k f) -> p k f", k=KC), AF.Identity),
                  reads=["stgC1"], writes=[f"wup{b}"])
            S.add("sp", DMA(stg[2][:].rearrange("p (k f) -> p k f", k=2), wd_v[e_]), writes=["stgC2"], chan="stgC2")
            S.add("pool", CP(wdn[b][:], stg[2][:].rearrange("p (k f) -> p k f", k=2)),
                  reads=["stgC2"], writes=[f"wdn{b}"])
            S.add("pool", MS(sel[b][:], 0.0), writes=[f"sel{b}"])
            S.add("sp", DMA(sel[b][e_:e_ + 1, :], cst["ones32"][0:1, :]), reads=["ones32"], writes=[f"sel{b}"],
                  chan=f"sel{b}")
            for T in range(NST):
                i2 = it % 2
                it += 1
                tsl = slice(T * TS_, (T + 1) * TS_)
                xr = [("X1T", T * NSUB + q) for q in range(NSUB)]
                S.add("pe", MM(banks[4][:, 0:TS_], sel[b][:], combT[:, tsl]),
                      reads=[f"sel{b}"] + [("combT", T * NSUB + q) for q in range(NSUB)], writes=[bn[4]])
                S.add("act", ACT(cbs[i2][:], banks[4][:, 0:TS_], AF.Identity), reads=[bn[4]], writes=[f"cbs{i2}"])
                for fc in range(2):
                    hg, hu = banks[fc * 2], banks[fc * 2 + 1]
                    for kc in range(KC):
                        S.add("pe", MM(hg[:, 0:TS_], wgt[b][:, kc, fc * 128:(fc + 1) * 128], X1T[:, kc, tsl],
                                       start=(kc == 0), stop=(kc == KC - 1)),
                              reads=[f"wgt{b}"] + xr, writes=[bn[fc * 2]])
                    for kc in range(KC):
                        S.add("pe", MM(hu[:, 0:TS_], wup[b][:, kc, fc * 128:(fc + 1) * 128], X1T[:, kc, tsl],
                                       start=(kc == 0), stop=(kc == KC - 1)),
                              reads=[f"wup{b}"] + xr, writes=[bn[fc * 2 + 1]])
                    S.add("act", ACT(sl[fc][:], hg[:, 0:TS_], AF.Silu), reads=[bn[fc * 2]], writes=[f"sl{fc}"])
                    S.add("dve", TT(tl[fc][:], sl[fc][:], hu[:, 0:TS_], ALU.mult),
                          reads=[f"sl{fc}", bn[fc * 2 + 1]], writes=[f"tl{fc}"])
                    S.add("pool", TT(hid[i2][:, fc, :], tl[fc][:], cbs[i2][:], ALU.mult),
                          reads=[f"tl{fc}", f"cbs{i2}"], writes=[(f"hid{i2}", fc)])
                for sub in range(NSUB):
                    sI = T * NSUB + sub
                    for half in range(2):
                        py = banks[5 + pyi % 3]
                        pyn = bn[5 + pyi % 3]
                        pyi += 1
                        for fc in range(2):
                            S.add("pe", MM(py[:], hid[i2][:, fc, sub * 128:(sub + 1) * 128],
                                           wdn[b][:, fc, half * 512:(half + 1) * 512],
                                           start=(fc == 0), stop=(fc == 1)),
                                  reads=[(f"hid{i2}", 0), (f"hid{i2}", 1), f"wdn{b}"], writes=[pyn])
                        ysl = yacc[:, sI, half * 512:(half + 1) * 512]
                        S.add("dve", TT(ysl, ysl, py[:], ALU.add), reads=[pyn, ("yacc", sI)], writes=[("yacc", sI)])
    S.barrier()

    with ExitStack() as es:
        sb = lambda name, shape, dt: es.enter_context(nc.sbuf_tensor(name + cx.sfx, shape, dt))
        x2 = [sb(f"x2_{i}", [128, D_MODEL], F32) for i in range(2)]
        junk = sb("junk2", [128, D_MODEL], BF16)
        stt = [sb(f"lnst2_{i}", [128, 8], F32) for i in range(2)]
        xTo = [sb(f"xTo{i}", [128, KC, 128], BF16) for i in range(2)]
        want_T = d.get("xT_out") is not None
        if d.get("x_ar") is not None:
            xmk = [sb(f"xmk{i}", [128, 4, KC, 128], F32) for i in range(2)]
            pm = d["pm_tile"]
        xTo_v = d["xT_out"].rearrange("(kc p) s -> p kc s", p=128) if want_T else None
        for sI in range(NS):
            i2 = sI % 2
            tok = slice(sI * 128, (sI + 1) * 128)
            layer_norm_tile(cx, S, yacc[:, sI, :], ("yacc", sI), x2[i2][:], f"x2_{i2}", lnbc["ln2_g"], lnbc["ln2_b"],
                            stt[i2], f"lnst2_{i2}", junk, "junk2", "lnbc")
            S.add("sp", DMA(d["x_out"][tok, :], x2[i2][:]), reads=[f"x2_{i2}"], chan=f"xo{i2}")
            if not want_T:
                continue
            for kc in range(KC):
                bk = banks[(kc // 4)]
                S.add("pe", TR(bk[:, (kc % 4) * 128:(kc % 4 + 1) * 128], x2[i2][:, kc * 128:(kc + 1) * 128],
                               ident32[:]), reads=[f"x2_{i2}", "ident32"], writes=[bn[kc // 4]])
            for q in range(2):
                S.add("act" if q == 0 else "dve",
                      ACT(xTo[i2][:, q * 4:(q + 1) * 4, :], banks[q][:].rearrange("p (k t) -> p k t", k=4), AF.Identity)
                      if q == 0 else
                      CP(xTo[i2][:, q * 4:(q + 1) * 4, :], banks[q][:].rearrange("p (k t) -> p k t", k=4)),
                      reads=[bn[q]], writes=[(f"xTo{i2}", q)])
            S.add("sp", DMA(xTo_v[:, :, tok], xTo[i2][:]), reads=[(f"xTo{i2}", 0), (f"xTo{i2}", 1)], chan=f"xTo{i2}")
            if d.get("x_ar") is not None:
                xm = xmk[i2]
                for j in range(4):
                    S.add("dve",
                          TS(xm[:, j, :, :], xTo[i2][:], pm[:, j:j + 1], None, ALU.mult),
                          reads=[(f"xTo{i2}", 0), (f"xTo{i2}", 1), "pm"], writes=[(f"xmk{i2}", j, 0), (f"xmk{i2}", j, 1)])
                cht = d["x_ar"][0].shape[1]
                cpq = NTOK // cht
                s8, c8 = (sI * 128) // cht, (sI * 128) % cht
                for j in range(4):
                    dst = d["x_ar"][j * cpq + s8].rearrange("(kc p) t -> p kc t", p=128)[:, :, c8:c8 + 128]
                    S.add("sp", DMA(dst, xm[:, j]), reads=[(f"xmk{i2}", j, q) for q in range(2)],
                          chan=f"xar{i2}")


def build_k2(NTOK, x_bf16):
    nc = bass.Bass("TRN2", target_bir_lowering=False)
    d = {}
    inp = lambda name, shape, dt=F32: nc.dram_tensor(name, shape, dt, kind="ExternalInput").ap()
    d["x_tok"] = inp("x_tok", [NTOK, D_MODEL])
    d["xT"] = inp("xT", [D_MODEL, NTOK], BF16 if x_bf16 else F32)
    d["yT_sb"] = inp("yT_sb", [512, NTOK], BF16)
    d["yT_ca"] = inp("yT_ca", [512, NTOK], BF16)
    d["wg"] = inp("wg", [D_MODEL, 2048])
    d["b_gate"] = inp("b_gate", [2048])
    d["w_br_sb"] = inp("w_br_sb", [512, D_MODEL])
    d["w_br_ca"] = inp("w_br_ca", [512, D_MODEL])
    d["w_out"] = inp("w_out", [D_MODEL, D_MODEL])
    for nm in ("ln1_g", "ln1_b", "ln2_g", "ln2_b"):
        d[nm] = inp(nm, [D_MODEL])
    d["w_router"] = inp("w_router", [D_MODEL, 36])
    d["b_router"] = inp("b_router", [36])
    d["w_gate"] = inp("w_gate", [32, D_MODEL, 256])
    d["w_up"] = inp("w_up", [32, D_MODEL, 256])
    d["w_down"] = inp("w_down", [32, 256, D_MODEL])
    d["x_out"] = nc.dram_tensor("x_out", [NTOK, D_MODEL], F32, kind="ExternalOutput").ap()
    d["xT_out"] = nc.dram_tensor("xT_out", [D_MODEL, NTOK], BF16, kind="ExternalOutput").ap()
    d["mT_scratch"] = nc.dram_tensor("mT_scratch", [128, 8, NTOK], BF16, kind="Internal").ap()
    with ExitStack() as es:
        cx = Ctx(nc, es)
        cst = build_consts(cx)
        k2_body(cx, cst, NTOK, x_bf16, d)
        cx.finish()
    return nc


def build_k1(S_len, x_bf16):
    nc = bass.Bass("TRN2", target_bir_lowering=False)
    xT = nc.dram_tensor("xT", [D_MODEL, S_len], BF16 if x_bf16 else F32, kind="ExternalInput").ap()
    w1 = nc.dram_tensor("w1", [D_MODEL, 768], F32, kind="ExternalInput").ap()
    cab = nc.dram_tensor("cab", [2, 128, 640], F32, kind="ExternalInput").ap()
    ot_sb = nc.dram_tensor("ot_sb", [128, S_len], BF16, kind="ExternalOutput").ap()
    ot_ca = nc.dram_tensor("ot_ca", [128, S_len], BF16, kind="ExternalOutput").ap()
    with ExitStack() as es:
        cx = Ctx(nc, es)
        cst = build_consts(cx)
        k1_body(cx, cst, S_len, x_bf16, xT, w1, cab, ot_sb, ot_ca)
        cx.finish()
    return nc


def ca_bias_table(rel_bias_l, heads):
    q = np.arange(128)[:, None]
    p = np.arange(640)[None, :]
    rel = np.clip(q + 512 - p, -128, 128) + 128
    ci = q // 64
    kc = p // 64
    valid = (kc >= ci) & (kc <= ci + 8)
    out = np.empty((len(heads), 128, 640), np.float32)
    for i, h in enumerate(heads):
        out[i] = np.where(valid, rel_bias_l[h][rel], np.float32(NEG))
    return out


def k1_weights(w_in_l, r):
    c = lambda base: w_in_l[:, base + r * 128: base + (r + 1) * 128]
    return np.ascontiguousarray(np.concatenate(
        [c(0), c(512), c(1536), c(2048), c(1024), c(2560)], axis=1))


def k2_inputs(p):
    f = np.ascontiguousarray
    m = {}
    m["wg"] = f(p["w_in"][:, 3072:5120])
    m["b_gate"] = f(p["b_gate"].reshape(2048))
    m["w_br_sb"] = f(p["w_br_sb"])
    m["w_br_ca"] = f(p["w_br_ca"])
    m["w_out"] = f(p["w_out"])
    for nm in ("ln1_g", "ln1_b", "ln2_g", "ln2_b"):
        m[nm] = f(p[nm])
    m["w_router"] = f(np.concatenate([p["w_group"]] + [p["w_erouter"][g] for g in range(4)], axis=1))
    m["b_router"] = f(np.concatenate([p["b_group"]] + [p["b_erouter"][g] for g in range(4)], axis=0))
    m["w_gate"] = f(p["w_gate"].reshape(32, D_MODEL, 256))
    m["w_up"] = f(p["w_up"].reshape(32, D_MODEL, 256))
    m["w_down"] = f(p["w_down"].reshape(32, 256, D_MODEL))
    return m


def build_fused(S_len):
    nc = bass.Bass("TRN2", target_bir_lowering=False)
    L = DEPTH
    NTOK = S_len // 4
    inp = lambda name, shape, dt=F32: nc.dram_tensor(name, shape, dt, kind="ExternalInput").ap()
    scr = lambda name, shape, dt: nc.dram_tensor(name, shape, dt, kind="Internal").ap()
    x_tok0 = inp("x_tok", [S_len, D_MODEL])
    xT0 = inp("xT", [D_MODEL, S_len])
    w1 = inp("w1", [L, 4, D_MODEL, 768])
    cab = inp("cab", [L, 4, 2, 128, 640])
    P = {}
    P["wg"] = inp("wg", [L, D_MODEL, 2048])
    P["b_gate"] = inp("b_gate", [L, 2048])
    P["w_br_sb"] = inp("w_br_sb", [L, 512, D_MODEL])
    P["w_br_ca"] = inp("w_br_ca", [L, 512, D_MODEL])
    P["w_out"] = inp("w_out", [L, D_MODEL, D_MODEL])
    for nm in ("ln1_g", "ln1_b", "ln2_g", "ln2_b"):
        P[nm] = inp(nm, [L, D_MODEL])
    P["w_router"] = inp("w_router", [L, D_MODEL, 36])
    P["b_router"] = inp("b_router", [L, 36])
    P["w_gate"] = inp("w_gate", [L, 32, D_MODEL, 256])
    P["w_up"] = inp("w_up", [L, 32, D_MODEL, 256])
    P["w_down"] = inp("w_down", [L, 32, 256, D_MODEL])
    out = nc.dram_tensor("out", [S_len, D_MODEL], F32, kind="ExternalOutput").ap()
    yT_sb = scr("yT_sb_s", [512, S_len], BF16)
    yT_ca = scr("yT_ca_s", [512, S_len], BF16)
    xT1 = scr("xT1_s", [D_MODEL, S_len], BF16)
    x_tok1 = scr("x_tok1_s", [S_len, D_MODEL], F32)
    mT_s = scr("mT_scratch", [128, 8, NTOK], BF16)
    with ExitStack() as es:
        cx = Ctx(nc, es)
        cst = build_consts(cx)
        for l in range(L):
            xT_l = xT0 if l == 0 else xT1
            xtok_l = x_tok0 if l == 0 else x_tok1
            for r in range(4):
                with ExitStack() as es2:
                    cx.es = es2
                    cx.sfx = f"_a{l}{r}"
                    k1_body(cx, cst, S_len, l > 0, xT_l, w1[l, r], cab[l, r],
                            yT_sb[r * 128:(r + 1) * 128, :], yT_ca[r * 128:(r + 1) * 128, :])
                cx.es = es
                cx.S.barrier()
            for r in range(4):
                tsl = slice(r * NTOK, (r + 1) * NTOK)
                d = {k: v[l] for k, v in P.items()}
                d["x_tok"] = xtok_l[tsl, :]
                d["xT"] = xT_l[:, tsl]
                d["yT_sb"] = yT_sb[:, tsl]
                d["yT_ca"] = yT_ca[:, tsl]
                d["mT_scratch"] = mT_s
                last = (l == L - 1)
                d["x_out"] = out[tsl, :] if last else x_tok1[tsl, :]
                d["xT_out"] = None if last else xT1[:, tsl]
                with ExitStack() as es2:
                    cx.es = es2
                    cx.sfx = f"_b{l}{r}"
                    k2_body(cx, cst, NTOK, l > 0, d)
                cx.es = es
                cx.S.barrier()
        cx.finish()
    return nc


NO_CC = False
DBG = set()


def build_fused8(S_len):
    nc = bass.Bass("TRN2", target_bir_lowering=False)
    L = DEPTH
    NTOK = S_len // 4
    TS_ = 512
    NCH = NTOK // TS_
    CHT = min(1024, NTOK)
    CPQ = NTOK // CHT
    NXC = 4 * CPQ
    groups = [[0, 1, 2, 3], [4, 5, 6, 7]]
    inp = lambda name, shape, dt=F32: nc.dram_tensor(name, shape, dt, kind="ExternalInput").ap()
    scr = lambda name, shape, dt: nc.dram_tensor(name, shape, dt, kind="Internal").ap()
    x_tok0 = inp("x_tok", [NTOK, D_MODEL])
    xT0 = inp("xT", [D_MODEL, S_len])
    xTq0 = inp("xTq", [D_MODEL, NTOK])
    pm_d = inp("pm", [128, 4])
    w1 = inp("w1", [L, D_MODEL, 768])
    cab = inp("cab", [L, 2, 128, 640])
    P = {}
    P["wg"] = inp("wg", [L, D_MODEL, 2048])
    P["b_gate"] = inp("b_gate", [L, 2048])
    P["w_br_sb"] = inp("w_br_sb", [L, 512, D_MODEL])
    P["w_br_ca"] = inp("w_br_ca", [L, 512, D_MODEL])
    P["w_out"] = inp("w_out", [L, D_MODEL, D_MODEL])
    for nm in ("ln1_g", "ln1_b", "ln2_g", "ln2_b"):
        P[nm] = inp(nm, [L, D_MODEL])
    P["w_router"] = inp("w_router", [L, D_MODEL, 36])
    P["b_router"] = inp("b_router", [L, 36])
    P["w_gate"] = inp("w_gate", [L, 32, D_MODEL, 256])
    P["w_up"] = inp("w_up", [L, 32, D_MODEL, 256])
    P["w_down"] = inp("w_down", [L, 32, 256, D_MODEL])
    out = nc.dram_tensor("out", [NTOK, D_MODEL], F32, kind="ExternalOutput").ap()
    y_in = [scr(f"y_in{c}", [4 * 1024, TS_], F32) for c in range(NCH)]
    y_rs = [scr(f"y_rs{c}", [1024, TS_], F32) for c in range(NCH)]
    x_in = [scr(f"x_in{k}", [D_MODEL, CHT], F32) for k in range(NXC)]
    x_all = [scr(f"x_all{k}", [D_MODEL, CHT], F32) for k in range(NXC)]
    xT1 = scr("xT1_s", [D_MODEL, NTOK], BF16)
    x_tok1 = scr("x_tok1_s", [NTOK, D_MODEL], F32)
    mT_s = scr("mT_scratch", [128, 8, NTOK], BF16)
    with ExitStack() as es:
        cx = Ctx(nc, es)
        S = cx.S
        cst = build_consts(cx)
        pm = cx.sb("pm_t", [128, 4], F32)
        S.add("sp", DMA(pm[:], pm_d[:, :]), writes=["pm"], chan="prm0")
        for l in range(L):
            last = (l == L - 1)
            with ExitStack() as es2:
                cx.es = es2
                cx.sfx = f"_a{l}"
                yst = [cx.sb(f"yst{i}", [64, 4, 512], F32) for i in range(2)]
                cnt = [0]

                chunk_res = {c: [] for c in range(NCH)}
                tiles_per_chunk = 2 * 2 * (S_len // 512) // NCH

                def emit_out(branch, h, qb, ob, obn, yst=yst, cnt=cnt, chunk_res=chunk_res, l=l):
                    k = cnt[0] % 2
                    cnt[0] += 1
                    q, c = (qb * 512) // NTOK, ((qb * 512) % NTOK) // TS_
                    for j in range(4):
                        S.add("dve", TS(yst[k][:, j, :], ob[:], pm[0:64, j:j + 1], None, ALU.mult),
                              reads=[obn, "pm"], writes=[(f"yst{k}", j)])
                    dst = y_in[c].rearrange("(q b j w) s -> q b w j s", q=4, b=2, j=4)[q][branch][h * 64:(h + 1) * 64]
                    rn = ("y_in", c, branch, h, q)
                    S.add("sp", DMA(dst, yst[k][:]), reads=[(f"yst{k}", j) for j in range(4)], writes=[rn],
                          chan=f"yo{k}")
                    chunk_res[c].append(rn)
                    if len(chunk_res[c]) == tiles_per_chunk:
                        S.add("pool", lambda e, i_=y_in[c][:, :], o_=y_rs[c][:, :]: e.collective_compute(
                            "ReduceScatter", op=ALU.add, replica_groups=groups, ins=[i_], outs=[o_]),
                            reads=list(chunk_res[c]), writes=[("y_rs", c)], chan=f"ccy{l}_{c}", inc=1)

                if l == 0:
                    xt_tile = None
                else:
                    def xt_tile(t):
                        kk, c0 = (t * 256) // CHT, (t * 256) % CHT
                        return x_all[kk].rearrange("(kc p) s -> p kc s", p=128)[:, :, c0:c0 + 256]
                k1_body(cx, cst, S_len, False, xT0, w1[l], cab[l], None, None, xt_tile=xt_tile, emit_out=emit_out)
            cx.es = es
            S.barrier(skip_prefix="ccy")
            d = {k: v[l] for k, v in P.items()}
            d["x_tok"] = x_tok0 if l == 0 else x_tok1
            d["xT"] = xTq0 if l == 0 else xT1
            d["y_rs"] = [y_rs[c] for c in range(NCH)]
            d["mT_scratch"] = mT_s
            d["x_out"] = out if last else x_tok1
            d["xT_out"] = None if last else xT1
            d["x_ar"] = None if (last or "noxar" in DBG) else x_in
            d["pm_tile"] = pm
            with ExitStack() as es2:
                cx.es = es2
                cx.sfx = f"_b{l}"
                k2_body(cx, cst, NTOK, l > 0, d)
            cx.es = es
            S.barrier()
            if not last:
                for k in range(NXC if not NO_CC else 0):
                    S.add("pool", lambda e, i_=x_in[k][:, :], o_=x_all[k][:, :]: e.collective_compute(
                        "AllReduce", op=ALU.add, replica_groups=groups, ins=[i_], outs=[o_]),
                        chan="cc", inc=1)
                S.barrier()
        cx.finish()
    return nc


def kernel_fused8(p):
    x = p["x"]
    B, S_len, D = x.shape
    NTOK = S_len // 4
    f = np.ascontiguousarray
    key = ("fused8", S_len)
    if key not in _NC_CACHE:
        _NC_CACHE[key] = build_fused8(S_len)
    nc = _NC_CACHE[key]
    per_l = [k2_inputs({k: v[l] for k, v in p.items() if k != "x"}) for l in range(DEPTH)]
    shared = {k: f(np.stack([per_l[l][k] for l in range(DEPTH)])) for k in per_l[0]}
    xT_b = [f(x[b].T) for b in range(B)]
    in_maps = []
    for c in range(8):
        b, r = c // 4, c % 4
        tsl = slice(r * NTOK, (r + 1) * NTOK)
        m = dict(shared)
        m["x_tok"] = f(x[b, tsl, :])
        m["xT"] = xT_b[b]
        m["xTq"] = f(xT_b[b][:, tsl])
        pmv = np.zeros((128, 4), np.float32)
        pmv[:, r] = 1.0
        m["pm"] = pmv
        m["w1"] = f(np.stack([k1_weights(p["w_in"][l], r) for l in range(DEPTH)]))
        m["cab"] = f(np.stack([ca_bias_table(p["rel_bias"][l], [2 * r, 2 * r + 1]) for l in range(DEPTH)]))
        in_maps.append(m)
    res = run_bass_kernel_spmd(nc, in_maps, core_ids=list(range(8))).results
    return np.stack([np.concatenate([np.asarray(res[b * 4 + r]["out"]) for r in range(4)], axis=0)
                     for b in range(B)], axis=0).astype(np.float32)


FUSED = 8


def kernel_fused(p):
    x = p["x"]
    B, S_len, D = x.shape
    f = np.ascontiguousarray
    key = ("fused", S_len)
    if key not in _NC_CACHE:
        _NC_CACHE[key] = build_fused(S_len)
    nc = _NC_CACHE[key]
    shared = {}
    shared["w1"] = f(np.stack([np.stack([k1_weights(p["w_in"][l], r) for r in range(4)]) for l in range(DEPTH)]))
    shared["cab"] = f(np.stack([np.stack([ca_bias_table(p["rel_bias"][l], [2 * r, 2 * r + 1]) for r in range(4)])
                                for l in range(DEPTH)]))
    per_l = [k2_inputs({k: v[l] for k, v in p.items() if k != "x"}) for l in range(DEPTH)]
    for k in per_l[0]:
        shared[k] = f(np.stack([per_l[l][k] for l in range(DEPTH)]))
    in_maps = []
    for b in range(B):
        m = dict(shared)
        m["x_tok"] = f(x[b])
        m["xT"] = f(x[b].T)
        in_maps.append(m)
    res = run_bass_kernel_spmd(nc, in_maps, core_ids=list(range(B))).results
    return np.stack([np.asarray(res[b]["out"]) for b in range(B)], axis=0).astype(np.float32)


_NC_CACHE = {}


def _get_nc(kind, *args):
    key = (kind,) + args
    if key not in _NC_CACHE:
        _NC_CACHE[key] = build_k1(*args) if kind == "k1" else build_k2(*args)
    return _NC_CACHE[key]


def kernel(**inputs):
    p = {k: np.asarray(v) for k, v in inputs.items()}
    if FUSED == 8:
        return kernel_fused8(p)
    if FUSED:
        return kernel_fused(p)
    x = p["x"]
    B, S_len, D = x.shape
    NTOK = S_len // 4
    cores = list(range(8))
    f = np.ascontiguousarray
    x_cur = x
    xT_full = None
    xT_prev = None
    for l in range(DEPTH):
        first = (l == 0)
        lp = {k: v[l] for k, v in p.items() if k != "x"}
        if first:
            xT_b = [f(x[b].T) for b in range(B)]
        else:
            xT_b = xT_full
        nc1 = _get_nc("k1", S_len, not first)
        in1 = []
        for c in cores:
            b, r = c // 4, c % 4
            in1.append({"xT": xT_b[b], "w1": k1_weights(lp["w_in"], r),
                        "cab": ca_bias_table(lp["rel_bias"], [2 * r, 2 * r + 1])})
        r1 = run_bass_kernel_spmd(nc1, in1, core_ids=cores).results
        yT_sb = [np.concatenate([np.asarray(r1[b * 4 + r]["ot_sb"]) for r in range(4)], axis=0) for b in range(B)]
        yT_ca = [np.concatenate([np.asarray(r1[b * 4 + r]["ot_ca"]) for r in range(4)], axis=0) for b in range(B)]
        nc2 = _get_nc("k2", NTOK, not first)
        wk2 = k2_inputs(lp)
        in2 = []
        for c in cores:
            b, r = c // 4, c % 4
            tsl = slice(r * NTOK, (r + 1) * NTOK)
            m = dict(wk2)
            m["x_tok"] = f(x_cur[b, tsl, :])
            m["xT"] = f(xT_b[b][:, tsl]) if first else xT_prev[c]
            m["yT_sb"] = f(yT_sb[b][:, tsl])
            m["yT_ca"] = f(yT_ca[b][:, tsl])
            in2.append(m)
        r2 = run_bass_kernel_spmd(nc2, in2, core_ids=cores).results
        x_cur = np.stack([np.concatenate([np.asarray(r2[b * 4 + r]["x_out"]) for r in range(4)], axis=0)
                          for b in range(B)], axis=0)
        xT_prev = [np.asarray(r2[c]["xT_out"]) for c in cores]
        xT_full = [f(np.concatenate([xT_prev[b * 4 + r] for r in range(4)], axis=1)) for b in range(B)]
    return x_cur.astype(np.float32)
```

```python
import numpy as np
from contextlib import ExitStack
import concourse.bass as bass
import concourse.mybir as mybir
from concourse.bass_utils import run_bass_kernel_spmd

F32 = mybir.dt.float32
BF16 = mybir.dt.bfloat16
AF = mybir.ActivationFunctionType
ALU = mybir.AluOpType
AX = mybir.AxisListType

D_MODEL = 1024
BATCH = 2
SEQ = 8192
DEPTH = 2
HD = 64
ALPHA = (2.0 * DEPTH) ** 0.25
LN_EPS = 1e-5
NEG = -30000.0
HEAT = 0


class Sched:
    ENG = ("pe", "act", "dve", "pool", "sp")

    def __init__(self, nc):
        self.nc = nc
        self.ops = []
        self.last_w = {}
        self.readers = {}
        self.chan_last = {}
        self.chans = []
        self.eng_last = {}
        self.bar = set()

    def barrier(self, skip_prefix=None):
        self.bar = set(self.eng_last.values()) | set(
            v for k, v in self.chan_last.items() if not (skip_prefix and k.startswith(skip_prefix)))

    def add(self, eng, fn, reads=(), writes=(), chan=None, inc=16):
        idx = len(self.ops)
        deps = set(self.bar)
        for r in reads:
            if r in self.last_w:
                deps.add(self.last_w[r])
        for w in writes:
            if w in self.last_w:
                deps.add(self.last_w[w])
            for rd in self.readers.get(w, ()):
                deps.add(rd)
        if chan is not None:
            if chan in self.chan_last:
                deps.add(self.chan_last[chan])
            else:
                self.chans.append(chan)
            self.chan_last[chan] = idx
        for r in reads:
            self.readers.setdefault(r, []).append(idx)
        for w in writes:
            self.last_w[w] = idx
            self.readers[w] = []
        self.ops.append(dict(eng=eng, fn=fn, deps=deps, chan=chan, inc=inc))
        if chan is None:
            self.eng_last[eng] = idx
        return idx

    def emit(self, block, sems, final_wait_eng="sp"):
        ops = self.ops
        n = len(ops)
        has_dep = [False] * n
        for o in ops:
            for d in o["deps"]:
                has_dep[d] = True
        eng_cnt = {e: 0 for e in self.ENG}
        chan_cnt = {}
        sig = [None] * n
        for i, o in enumerate(ops):
            if o["chan"] is not None:
                c = o["chan"]
                chan_cnt[c] = chan_cnt.get(c, 0) + o["inc"]
                sig[i] = ("ch:" + c, chan_cnt[c])
            elif has_dep[i]:
                e = o["eng"]
                eng_cnt[e] += 1
                sig[i] = (e, eng_cnt[e])
        per_eng = {e: [] for e in self.ENG}
        for i, o in enumerate(ops):
            per_eng[o["eng"]].append(i)
        self.eng_cnt = eng_cnt

        def run(ename, eng):
            seen = {}
            for i in per_eng[ename]:
                o = ops[i]
                need = {}
                for d in o["deps"]:
                    od = ops[d]
                    if od["chan"] is None and od["eng"] == "pe" and ename == "pe" and o["chan"] is None:
                        continue
                    s, v = sig[d]
                    if need.get(s, 0) < v:
                        need[s] = v
                for s, v in need.items():
                    if seen.get(s, 0) >= v:
                        continue
                    eng.wait_ge(sems[s], v)
                    seen[s] = v
                ins = o["fn"](eng)
                if sig[i] is not None:
                    s, v = sig[i]
                    ins.then_inc(sems[s], o["inc"] if o["chan"] is not None else 1)
            if ename == final_wait_eng:
                for c, v in chan_cnt.items():
                    if seen.get("ch:" + c, 0) < v:
                        eng.wait_ge(sems["ch:" + c], v)

        @block.tensor
        def _(e):
            run("pe", e)

        @block.scalar
        def _(e):
            run("act", e)

        @block.vector
        def _(e):
            run("dve", e)

        @block.gpsimd
        def _(e):
            run("pool", e)

        @block.sync
        def _(e):
            run("sp", e)


class Ctx:
    def __init__(self, nc, es):
        self.nc = nc
        self.es = es
        self.S = Sched(nc)
        self.sfx = ""

    def sb(self, name, shape, dt):
        return self.es.enter_context(self.nc.sbuf_tensor(name + self.sfx, shape, dt))

    def ps(self, name, shape, dt):
        return self.es.enter_context(self.nc.psum_tensor(name + self.sfx, shape, dt))

    def finish(self):
        nc, es, S = self.nc, self.es, self.S
        sems = {}
        for e in Sched.ENG:
            sems[e] = es.enter_context(nc.semaphore("sem_" + e))
        for c in S.chans:
            sems["ch:" + c] = es.enter_context(nc.semaphore("semch_" + c))
        block = es.enter_context(nc.Block())
        S.emit(block, sems)


def build_consts(cx):
    S = cx.S
    c = {}
    ident = cx.sb("ident", [128, 128], BF16)
    ident32 = cx.sb("ident32", [128, 128], F32)
    negtri = cx.sb("negtri", [128, 128], BF16)
    negones = cx.sb("negones", [128, 128], BF16)
    m01 = cx.sb("m01", [128, 896], BF16)
    mbias = cx.sb("mbias", [128, 896], BF16)
    S.add("pool", lambda e: e.memset(ident[:], 0.0), writes=["ident"])
    S.add("pool", lambda e: e.affine_select(out=ident[:], in_=ident[:], pattern=[[-1, 128]],
                                            compare_op=ALU.not_equal, fill=1.0, base=0,
                                            channel_multiplier=1), reads=["ident"], writes=["ident"])
    S.add("pool", lambda e: e.memset(ident32[:], 0.0), writes=["ident32"])
    S.add("pool", lambda e: e.affine_select(out=ident32[:], in_=ident32[:], pattern=[[-1, 128]],
                                            compare_op=ALU.not_equal, fill=1.0, base=0,
                                            channel_multiplier=1), reads=["ident32"], writes=["ident32"])
    S.add("pool", lambda e: e.memset(negtri[:], -1.0), writes=["negtri"])
    S.add("pool", lambda e: e.affine_select(out=negtri[:], in_=negtri[:], pattern=[[-1, 128]],
                                            compare_op=ALU.is_ge, fill=0.0, base=0,
                                            channel_multiplier=1), reads=["negtri"], writes=["negtri"])
    S.add("pool", lambda e: e.memset(negones[:], -1.0), writes=["negones"])
    S.add("pool", lambda e: e.memset(m01[:], 1.0), writes=["m01"])
    S.add("pool", lambda e: e.affine_select(out=m01[:], in_=m01[:], pattern=[[1, 896]],
                                            compare_op=ALU.is_gt, fill=0.0, base=-384,
                                            channel_multiplier=-1), reads=["m01"], writes=["m01"])
    S.add("pool", lambda e: e.memset(mbias[:], 0.0), writes=["mbias"])
    S.add("pool", lambda e: e.affine_select(out=mbias[:], in_=mbias[:], pattern=[[1, 896]],
                                            compare_op=ALU.is_gt, fill=NEG, base=-384,
                                            channel_multiplier=-1), reads=["mbias"], writes=["mbias"])
    ones32 = cx.sb("ones32", [1, 128], F32)
    S.add("pool", lambda e: e.memset(ones32[:], 1.0), writes=["ones32"])
    c["ones32"] = ones32
    c.update(ident=ident, ident32=ident32, negtri=negtri, negones=negones, m01=m01, mbias=mbias)
    return c


def MM(out, lhsT, rhs, start=True, stop=True, skip=False):
    if skip:
        return lambda e: e.matmul(out, lhsT=lhsT, rhs=rhs, start=start, stop=stop, skip_group_check=True)
    return lambda e: e.matmul(out, lhsT=lhsT, rhs=rhs, start=start, stop=stop)


def ACT(out, in_, func, **kw):
    return lambda e: e.activation(out=out, in_=in_, func=func, **kw)


def TT(out, in0, in1, op):
    return lambda e: e.tensor_tensor(out=out, in0=in0, in1=in1, op=op)


def TS(out, in0, s1, s2, op0, op1=None):
    if op1 is None:
        return lambda e: e.tensor_scalar(out=out, in0=in0, scalar1=s1, scalar2=s2, op0=op0)
    return lambda e: e.tensor_scalar(out=out, in0=in0, scalar1=s1, scalar2=s2, op0=op0, op1=op1)


def STT(out, in0, scalar, in1, op0, op1):
    return lambda e: e.scalar_tensor_tensor(out=out, in0=in0, scalar=scalar, in1=in1, op0=op0, op1=op1)


def CP(out, in_):
    return lambda e: e.tensor_copy(out=out, in_=in_)


def DMA(out, in_):
    return lambda e: e.dma_start(out=out, in_=in_)


def TR(out, in_, ident):
    return lambda e: e.transpose(out, in_, ident)


def RED(out, in_, op):
    return lambda e: e.tensor_reduce(out=out, in_=in_, axis=AX.X, op=op)


def MS(ap, val):
    return lambda e: e.memset(ap, val)


def RCP(out, in_):
    return lambda e: e.reciprocal(out=out, in_=in_)


def k1_body(cx, cst, S_len, x_bf16, xT, w1, cab, ot_sb, ot_ca, xt_tile=None, emit_out=None):
    nc, S = cx.nc, cx.S
    NT = S_len // 256
    NB = S_len // 128
    NQ = S_len // 512
    KC = D_MODEL // 128

    QT_sb = cx.sb("QT_sb", [128, S_len], BF16)
    KT_sb = cx.sb("KT_sb", [128, S_len], BF16)
    QT_ca = cx.sb("QT_ca", [128, S_len], BF16)
    KT_ca = cx.sb("KT_ca", [128, S_len], BF16)
    V_all = cx.sb("V_all", [128, NB, 256], BF16)
    x32 = [cx.sb(f"x32_{i}", [128, KC, 256], F32) for i in range(2)]
    xbf = [cx.sb(f"xbf_{i}", [128, KC, 256], BF16) for i in range(2)]
    wbf = cx.sb("wbf", [128, KC, 768], BF16)
    cabs = cx.sb("cabs", [128, 2, 640], F32)

    banks = [cx.ps(f"bank{i}", [128, 512], F32) for i in range(7)]
    bankT = cx.ps("bankT", [128, 512], F32)

    if xt_tile is None:
        xT_v = xT.rearrange("(kc p) s -> p kc s", p=128)
        xt_tile = lambda t: xT_v[:, :, t * 256:(t + 1) * 256]
    w1_v = w1.rearrange("(kc p) n -> p kc n", p=128)

    S.add("sp", DMA(cabs[:], cab.rearrange("h q k -> q h k")), writes=["cabs"], chan="cab")

    for pc in range(3):
        buf = x32[pc % 2]
        nm = f"x32_{pc % 2}"
        S.add("sp", DMA(buf[:], w1_v[:, :, pc * 256:(pc + 1) * 256]), writes=[nm], chan=f"x{pc % 2}")
        S.add("dve", CP(wbf[:, :, pc * 256:(pc + 1) * 256], buf[:]), reads=[nm], writes=[f"wbf{pc}"])
    wres = ["wbf0", "wbf1", "wbf2"]

    dsts = [QT_sb, KT_sb, QT_ca, KT_ca]
    dnames = ["QT_sb", "KT_sb", "QT_ca", "KT_ca"]
    for t in range(NT):
        i2 = t % 2
        xb = xbf[i2]
        xbn = f"xbf_{i2}"
        tsl = slice(t * 256, (t + 1) * 256)
        if x_bf16:
            S.add("sp", DMA(xb[:], xt_tile(t)), writes=[xbn], chan=f"x{i2}")
            xr = [xbn]
        else:
            xs = x32[i2]
            xsn = f"x32_{i2}"
            S.add("sp", DMA(xs[:], xt_tile(t)), writes=[xsn], chan=f"x{i2}")
            S.add("dve", CP(xb[:, 0:4, :], xs[:, 0:4, :]), reads=[xsn], writes=[xbn + "a"])
            S.add("pool", CP(xb[:, 4:8, :], xs[:, 4:8, :]), reads=[xsn], writes=[xbn + "b"])
            xr = [xbn + "a", xbn + "b"]
        for g in range(4):
            bk = banks[g]
            bkn = f"bank{g}"
            for kc in range(KC):
                S.add("pe", MM(bk[:, 0:256], wbf[:, kc, g * 128:(g + 1) * 128], xb[:, kc, :],
                               start=(kc == 0), stop=(kc == KC - 1)), reads=wres + xr, writes=[bkn])
            dst = dsts[g]
            if g in (0, 2):
                S.add("act", ACT(dst[:, tsl], bk[:, 0:256], AF.Identity, scale=0.125),
                      reads=[bkn], writes=[(dnames[g], t // 2)])
            else:
                S.add("dve", CP(dst[:, tsl], bk[:, 0:256]), reads=[bkn], writes=[(dnames[g], t // 2)])
        for sub in range(2):
            bk = banks[4 + sub]
            bkn = f"bank{4 + sub}"
            for kc in range(KC):
                S.add("pe", MM(bk[:, 0:256], xb[:, kc, sub * 128:(sub + 1) * 128], wbf[:, kc, 512:768],
                               start=(kc == 0), stop=(kc == KC - 1)), reads=wres + xr, writes=[bkn])
            blk = t * 2 + sub
            if sub == 0:
                S.add("dve", CP(V_all[:, blk, :], bk[:, 0:256]), reads=[bkn], writes=[("V", blk)])
            else:
                S.add("act", ACT(V_all[:, blk, :], bk[:, 0:256], AF.Identity), reads=[bkn], writes=[("V", blk)])

    negtri, negones, ident, m01, mbias = cst["negtri"], cst["negones"], cst["ident"], cst["m01"], cst["mbias"]
    T32 = [cx.sb(f"T32_{i}", [128, 640], F32) for i in range(2)]
    Pb = [cx.sb(f"P_{i}", [128, 640], BF16) for i in range(2)]
    Pn = [cx.sb(f"Pn_{i}", [128, 640], BF16) for i in range(2)]
    WT = [cx.sb(f"WT_{i}", [128, 640], BF16) for i in range(2)]
    st = [cx.sb(f"st_{i}", [128, 4], F32) for i in range(4)]
    oca = [cx.sb(f"oca_{i}", [64, 512], BF16) for i in range(2)]
    po = banks[6]
    tb = [banks[4][:].bitcast(BF16), banks[5][:].bitcast(BF16)]
    tbn = ["bank4", "bank5"]
    its = [(h, m) for h in range(2) for m in range(NB)]
    NIT = len(its)

    def RC(ci):
        h, m = its[ci]
        nk = min(640, 128 * (m + 1))
        i2 = ci % 2
        return dict(h=h, m=m, nk=nk, ks=128 * (m + 1) - nk, p0=640 - nk, n1=min(nk, 512), nj=nk // 128,
                    hp=slice(h * 64, (h + 1) * 64), qsl=slice(m * 128, (m + 1) * 128),
                    pa=banks[i2 * 2], pan=f"bank{i2 * 2}", pb=banks[i2 * 2 + 1], pbn=f"bank{i2 * 2 + 1}",
                    T=T32[i2], Tn=f"T32_{i2}", P=Pb[i2], Pnm=f"P_{i2}", PN=Pn[i2], PNn=f"Pn_{i2}",
                    W=WT[i2], Wn=f"WT_{i2}", sx=st[ci % 4], sxn=f"st_{ci % 4}", tb=tb[i2], tbn=tbn[i2])

    for t in range(NIT + 6):
        ci = t
        if ci < NIT:
            r = RC(ci)
            ks, nk, n1, m, hp = r["ks"], r["nk"], r["n1"], r["m"], r["hp"]
            krd = [("KT_ca", j) for j in range(ks // 512, (ks + nk - 1) // 512 + 1)]
            S.add("pe", MM(r["pa"][:, 0:n1], QT_ca[hp, r["qsl"]], KT_ca[hp, ks:ks + n1]),
                  reads=[("QT_ca", m // 4)] + krd, writes=[r["pan"]])
            if nk > 512:
                S.add("pe", MM(r["pb"][:, 0:128], QT_ca[hp, r["qsl"]], KT_ca[hp, ks + 512:ks + 640]),
                      reads=[("QT_ca", m // 4)] + krd, writes=[r["pbn"]])
        ci = t - 1
        if 0 <= ci < NIT:
            r = RC(ci)
            T, sx, nk, n1, p0, h = r["T"], r["sx"], r["nk"], r["n1"], r["p0"], r["h"]
            S.add("dve", TT(T[:, 0:n1], r["pa"][:, 0:n1], cabs[:, h, p0:p0 + n1], ALU.add),
                  reads=[r["pan"], "cabs"], writes=[r["Tn"]])
            if nk > 512:
                S.add("dve", TT(T[:, 512:640], r["pb"][:, 0:128], cabs[:, h, 512:640], ALU.add),
                      reads=[r["pbn"], "cabs", r["Tn"]], writes=[r["Tn"]])
            S.add("dve", RED(sx[:, 0:1], T[:, 0:nk], ALU.max), reads=[r["Tn"]], writes=[r["sxn"]])
            S.add("dve", TS(sx[:, 1:2], sx[:, 0:1], -1.0, None, ALU.mult), reads=[r["sxn"]], writes=[r["sxn"]])
            S.add("pool", MS(sx[:, 2:3], 0.0), reads=[r["sxn"]], writes=[r["sxn"]])
        ci = t - 2
        if 0 <= ci < NIT:
            r = RC(ci)
            nk, sx = r["nk"], r["sx"]
            S.add("act", ACT(r["P"][:, 0:nk], r["T"][:, 0:nk], AF.Exp, bias=sx[:, 1:2], accum_out=sx[:, 2:3]),
                  reads=[r["Tn"], r["sxn"]], writes=[r["Pnm"], r["sxn"]])
        ci = t - 3
        if 0 <= ci < NIT:
            r = RC(ci)
            nk, sx = r["nk"], r["sx"]
            S.add("dve", RCP(sx[:, 3:4], sx[:, 2:3]), reads=[r["sxn"]], writes=[r["sxn"]])
            S.add("dve", TS(r["PN"][:, 0:nk], r["P"][:, 0:nk], sx[:, 3:4], None, ALU.mult),
                  reads=[r["Pnm"], r["sxn"]], writes=[r["PNn"]])
        ci = t - 4
        if 0 <= ci < NIT:
            r = RC(ci)
            for j in range(r["nj"]):
                S.add("pe", TR(r["tb"][:, j * 128:(j + 1) * 128], r["PN"][:, j * 128:(j + 1) * 128], ident[:]),
                      reads=[r["PNn"], "ident"], writes=[r["tbn"]])
        ci = t - 5
        if 0 <= ci < NIT:
            r = RC(ci)
            nk = r["nk"]
            S.add("act", ACT(r["W"][:, 0:nk], r["tb"][:, 0:nk], AF.Identity), reads=[r["tbn"]], writes=[r["Wn"]])
        ci = t - 6
        if 0 <= ci < NIT:
            r = RC(ci)
            h, m, nj, ks = r["h"], r["m"], r["nj"], r["ks"]
            mcol = (m % 4) * 128
            for j in range(nj):
                kb = ks // 128 + j
                S.add("pe", MM(po[0:64, mcol:mcol + 128], V_all[:, kb, 128 + h * 64:128 + (h + 1) * 64],
                               r["W"][:, j * 128:(j + 1) * 128], start=(j == 0), stop=(j == nj - 1)),
                      reads=[r["Wn"], ("V", kb)], writes=["bank6"])
            if m % 4 == 3:
                oq = (h * NB + m) // 4
                ob, obn = oca[oq % 2], f"oca_{oq % 2}"
                S.add("act", ACT(ob[:], po[0:64, :], AF.Identity), reads=["bank6"], writes=[obn])
                if emit_out is not None:
                    emit_out(1, h, m // 4, ob, obn)
                else:
                    S.add("sp", DMA(ot_ca[h * 64:(h + 1) * 64, (m - 3) * 128:(m + 1) * 128], ob[:]),
                          reads=[obn], chan=f"oc{oq % 2}")

    E32 = [cx.sb(f"E32_{i}", [128, 512], F32) for i in range(2)]
    SPb = [cx.sb(f"SP_{i}", [128, 512], BF16) for i in range(3)]
    Wb = [cx.sb(f"W_{i}", [128, 512], BF16) for i in range(2)]
    C32 = cx.sb("C32", [128, 512], F32)
    Cbf = [cx.sb(f"Cbf_{i}", [128, 512], BF16) for i in range(4)]
    osb = [cx.sb(f"osb_{i}", [64, 512], BF16) for i in range(2)]
    blocks = []
    oi = 0
    for qb in range(NQ):
        for h in range(2):
            n = 4 * (qb + 1)
            for i in range(n):
                blocks.append(dict(h=h, qb=qb, i=i, n=n, kb=n - 1 - i, oi=oi))
            oi += 1
    NBK = len(blocks)

    def R(g):
        bl = blocks[g]
        h, qb, kb = bl["h"], bl["qb"], bl["kb"]
        d_ = dict(bl)
        d_.update(hp=slice(h * 64, (h + 1) * 64), qs=slice(qb * 512, (qb + 1) * 512),
                  pz=banks[g % 6], pzn=f"bank{g % 6}", E=E32[g % 2], En=f"E32_{g % 2}",
                  SP=SPb[g % 3], SPn=f"SP_{g % 3}", W=Wb[g % 2], Wn=f"W_{g % 2}",
                  C=Cbf[g % 4], Cn=f"Cbf_{g % 4}", Cp=Cbf[(g + 1) % 4], Cpn=f"Cbf_{(g + 1) % 4}",
                  po=banks[6] if bl["oi"] % 2 == 0 else bankT, pon="bank6" if bl["oi"] % 2 == 0 else "bankT",
                  diag=kb >= 4 * qb, off=(kb - 4 * qb) * 128)
        return d_

    for t in range(NBK + 5):
        g = t
        if g < NBK:
            r = R(g)
            S.add("pe", MM(r["pz"][:], KT_sb[r["hp"], r["kb"] * 128:(r["kb"] + 1) * 128], QT_sb[r["hp"], r["qs"]]),
                  reads=[("KT_sb", r["kb"] // 4), ("QT_sb", r["qb"])], writes=[r["pzn"]])
        g = t - 1
        if 0 <= g < NBK:
            r = R(g)
            S.add("act", ACT(r["E"][:], r["pz"][:], AF.Exp), reads=[r["pzn"]], writes=[r["En"]])
        g = t - 2
        if 0 <= g < NBK:
            r = R(g)
            off = r["off"]
            S.add("act", ACT(r["SP"][:], r["E"][:], AF.Ln, bias=1.0), reads=[r["En"]], writes=[r["SPn"]])
            if r["diag"]:
                S.add("dve", TT(r["SP"][:], r["SP"][:], m01[:, 384 - off:896 - off], ALU.mult),
                      reads=[r["SPn"], "m01"], writes=[r["SPn"]])
            if r["i"] < r["n"] - 1:
                if r["i"] == 0:
                    S.add("dve", CP(C32[:], r["SP"][:]), reads=[r["SPn"]], writes=["C32"])
                    S.add("dve", CP(r["Cp"][:], r["SP"][:]), reads=[r["SPn"]], writes=[r["Cpn"]])
                else:
                    S.add("dve", TT(C32[:], C32[:], r["SP"][:], ALU.add), reads=[r["SPn"], "C32"], writes=["C32"])
                    S.add("dve", CP(r["Cp"][:], C32[:]), reads=["C32"], writes=[r["Cpn"]])
        g = t - 3
        if 0 <= g < NBK:
            r = R(g)
            off = r["off"]
            S.add("pe", MM(r["pz"][:], negtri[:], r["SP"][:], start=False, stop=False, skip=True),
                  reads=[r["SPn"], "negtri"], writes=[r["pzn"]])
            if r["i"] > 0:
                S.add("pe", MM(r["pz"][:], negones[:], r["C"][:], start=False, stop=False, skip=True),
                      reads=[r["Cn"], "negones"], writes=[r["pzn"]])
            if r["diag"]:
                S.add("pe", MM(r["pz"][:], ident[:], mbias[:, 384 - off:896 - off],
                               start=False, stop=False, skip=True),
                      reads=["ident", "mbias"], writes=[r["pzn"]])
        g = t - 4
        if 0 <= g < NBK:
            r = R(g)
            S.add("act", ACT(r["W"][:], r["pz"][:], AF.Exp), reads=[r["pzn"]], writes=[r["Wn"]])
        g = t - 5
        if 0 <= g < NBK:
            r = R(g)
            h = r["h"]
            po, pon = r["po"], r["pon"]
            S.add("pe", MM(po[0:64, 0:512], V_all[:, r["kb"], h * 64:(h + 1) * 64], r["W"][:],
                           start=(r["i"] == 0), stop=(r["i"] == r["n"] - 1)),
                  reads=[r["Wn"], ("V", r["kb"])], writes=[pon])
            for _ in range(HEAT):
                S.add("pe", MM(po[64:128, 0:512], negones[:, 0:64], m01[:, 0:512]), reads=["negones", "m01"])
            if r["i"] == r["n"] - 1:
                ob = osb[r["oi"] % 2]
                obn = f"osb_{r['oi'] % 2}"
                S.add("dve", CP(ob[:], po[0:64, 0:512]), reads=[pon], writes=[obn])
                if emit_out is not None:
                    emit_out(0, h, r["qb"], ob, obn)
                else:
                    S.add("sp", DMA(ot_sb[h * 64:(h + 1) * 64, r["qs"]], ob[:]), reads=[obn], chan=f"o{r['oi'] % 2}")


def layer_norm_tile(cx, S, u, un, out, outn, gbc, bbc, st, stn, junk, junkn, gn, tmp=None, tmpn=None):
    D = D_MODEL
    S.add("pool", MS(st[:, 0:2], 0.0), reads=[stn], writes=[stn])
    S.add("act", ACT(junk[:], u, AF.Identity, accum_out=st[:, 0:1]), reads=[un, stn], writes=[junkn, stn])
    S.add("act", ACT(junk[:], u, AF.Square, accum_out=st[:, 1:2]), reads=[un, stn, junkn], writes=[junkn, stn])
    S.add("dve", TS(st[:, 2:4], st[:, 0:2], 1.0 / D, None, ALU.mult), reads=[stn], writes=[stn])
    S.add("dve", STT(st[:, 4:5], st[:, 2:3], st[:, 2:3], st[:, 3:4], ALU.mult, ALU.subtract),
          reads=[stn], writes=[stn])
    S.add("dve", TS(st[:, 5:6], st[:, 4:5], -1.0, LN_EPS, ALU.mult, ALU.add), reads=[stn], writes=[stn])
    S.add("act", ACT(st[:, 5:6], st[:, 5:6], AF.Ln), reads=[stn], writes=[stn])
    S.add("act", ACT(st[:, 6:7], st[:, 5:6], AF.Exp, scale=-0.5), reads=[stn], writes=[stn])
    S.add("dve", STT(st[:, 7:8], st[:, 2:3], -1.0, st[:, 6:7], ALU.mult, ALU.mult), reads=[stn], writes=[stn])
    S.add("act", ACT(out, u, AF.Identity, scale=st[:, 6:7], bias=st[:, 7:8]), reads=[un, stn], writes=[outn])
    S.add("dve", TT(out, out, gbc[:], ALU.mult), reads=[outn, gn], writes=[outn])
    S.add("dve", TT(out, out, bbc[:], ALU.add), reads=[outn, gn], writes=[outn])


def k2_body(cx, cst, NTOK, x_bf16, d):
    nc, S = cx.nc, cx.S
    KC = D_MODEL // 128
    TS_ = min(512, NTOK)
    NST = NTOK // TS_
    NSUB = TS_ // 128
    NS = NTOK // 128
    ident32 = cst["ident32"]
    banks = [cx.ps(f"kb{i}", [128, 512], F32) for i in range(8)]
    bn = [f"kb{i}" for i in range(8)]

    lnbc = {}
    for nm in ("ln1_g", "ln1_b", "ln2_g", "ln2_b"):
        t = cx.sb("bc_" + nm, [128, D_MODEL], F32)
        S.add("sp", DMA(t[:], d[nm].partition_broadcast(128)), writes=["lnbc"], chan="prm")
        lnbc[nm] = t
    bg = cx.sb("bg", [128, 16], F32)
    S.add("sp", lambda e, o_=bg[:], i_=d["b_gate"].rearrange("(j p) -> p j", p=128): e.dma_start(
        out=o_, in_=i_, allow_slow_non_contiguous=True), writes=["bg"], chan="prm")
    wr32 = cx.sb("wr32", [128, KC, 36], F32)
    S.add("sp", DMA(wr32[:], d["w_router"].rearrange("(kc p) n -> p kc n", p=128)), writes=["wr32"], chan="prm")
    brt = cx.sb("brt", [128, 36], F32)
    S.add("sp", DMA(brt[:], d["b_router"].partition_broadcast(128)), writes=["brt"], chan="prm")

    mT_d = d["mT_scratch"]
    xT_v = d["xT"].rearrange("(kc p) s -> p kc s", p=128)
    if d.get("y_rs") is None:
        ysb_v = d["yT_sb"].rearrange("(kc p) s -> p kc s", p=128)
        yca_v = d["yT_ca"].rearrange("(kc p) s -> p kc s", p=128)

    with ExitStack() as es:
        sb = lambda name, shape, dt: es.enter_context(nc.sbuf_tensor(name + cx.sfx, shape, dt))
        wg_bf = sb("wg_bf", [128, KC, 2048], BF16)
        wbs_bf = sb("wbs_bf", [128, 4, 1024], BF16)
        wbc_bf = sb("wbc_bf", [128, 4, 1024], BF16)
        stg = [sb(f"stgA{i}", [128, 2048], F32) for i in range(2)]
        xbf = [sb(f"xbfA{i}", [128, KC, TS_], BF16) for i in range(2)]
        ysb = [sb(f"ysb{i}", [128, 4, TS_], BF16) for i in range(2)]
        yca = [sb(f"yca{i}", [128, 4, TS_], BF16) for i in range(2)]
        gs = [sb(f"gs{i}", [128, TS_], F32) for i in range(2)]
        gc = [sb(f"gc{i}", [128, TS_], F32) for i in range(2)]
        t1 = sb("t1", [128, TS_], F32)
        t2 = sb("t2", [128, TS_], F32)
        mT = [sb(f"mT{i}", [128, KC, TS_], BF16) for i in range(2)]
        si = 0

        def stage_cast(dst, src_ap, ncols, rn, k=None, rd=()):
            nonlocal si
            b = si % 2
            si += 1
            sv = stg[b][:, 0:ncols]
            if k is not None:
                sv = sv.rearrange("p (k t) -> p k t", k=k)
            S.add("sp", DMA(sv, src_ap), reads=list(rd), writes=[f"stgA{b}"], chan=f"stgA{b}")
            S.add("dve" if b == 0 else "pool", CP(dst, sv), reads=[f"stgA{b}"], writes=[rn])

        wg_v = d["wg"].rearrange("(kc p) n -> p kc n", p=128)
        for kc in range(KC):
            stage_cast(wg_bf[:, kc, :], wg_v[:, kc, :], 2048, "wg_bf")
        wbs_v = d["w_br_sb"].rearrange("(kc p) n -> p kc n", p=128)
        wbc_v = d["w_br_ca"].rearrange("(kc p) n -> p kc n", p=128)
        for kc in range(0, 4, 2):
            stage_cast(wbs_bf[:, kc:kc + 2, :], wbs_v[:, kc:kc + 2, :], 2048, "wbs_bf", k=2)
            stage_cast(wbc_bf[:, kc:kc + 2, :], wbc_v[:, kc:kc + 2, :], 2048, "wbc_bf", k=2)
        for T in range(NST):
            i2 = T % 2
            tsl = slice(T * TS_, (T + 1) * TS_)
            xb, xbn = xbf[i2], f"xbfA{i2}"
            if x_bf16:
                S.add("sp", DMA(xb[:], xT_v[:, :, tsl]), writes=[xbn], chan=f"xA{i2}")
            else:
                per = 2048 // TS_
                for k0 in range(0, KC, per):
                    stage_cast(xb[:, k0:k0 + per, :], xT_v[:, k0:k0 + per, tsl], per * TS_, xbn, k=per)
            if d.get("y_rs") is not None:
                yv = d["y_rs"][T].rearrange("(b kc p) s -> b p kc s", b=2, p=128)
                per = 2048 // TS_
                for k0 in range(0, 4, per):
                    stage_cast(ysb[i2][:, k0:k0 + per, :], yv[0][:, k0:k0 + per, :], per * TS_, f"ysb{i2}", k=per,
                               rd=[("y_rs", T)])
                    stage_cast(yca[i2][:, k0:k0 + per, :], yv[1][:, k0:k0 + per, :], per * TS_, f"yca{i2}", k=per,
                               rd=[("y_rs", T)])
            else:
                S.add("sp", DMA(ysb[i2][:], ysb_v[:, :, tsl]), writes=[f"ysb{i2}"], chan=f"yA{i2}")
                S.add("sp", DMA(yca[i2][:], yca_v[:, :, tsl]), writes=[f"yca{i2}"], chan=f"yB{i2}")
            mt, mtn = mT[i2], f"mT{i2}"
            for fo in range(KC):
                j2 = fo % 2
                fsl = slice(fo * 128, (fo + 1) * 128)
                fsl2 = slice(1024 + fo * 128, 1024 + (fo + 1) * 128)
                b0, b1, b2, b3 = j2 * 4, j2 * 4 + 1, j2 * 4 + 2, j2 * 4 + 3
                for kc in range(KC):
                    S.add("pe", MM(banks[b0][:, 0:TS_], wg_bf[:, kc, fsl], xb[:, kc, :],
                                   start=(kc == 0), stop=(kc == KC - 1)), reads=["wg_bf", xbn], writes=[bn[b0]])
                S.add("act", ACT(gs[j2][:], banks[b0][:, 0:TS_], AF.Sigmoid, bias=bg[:, fo:fo + 1]),
                      reads=[bn[b0], "bg"], writes=[f"gs{j2}"])
                for kc in range(KC):
                    S.add("pe", MM(banks[b1][:, 0:TS_], wg_bf[:, kc, fsl2], xb[:, kc, :],
                                   start=(kc == 0), stop=(kc == KC - 1)), reads=["wg_bf", xbn], writes=[bn[b1]])
                S.add("act", ACT(gc[j2][:], banks[b1][:, 0:TS_], AF.Sigmoid, bias=bg[:, 8 + fo:9 + fo]),
                      reads=[bn[b1], "bg"], writes=[f"gc{j2}"])
                for kc in range(4):
                    S.add("pe", MM(banks[b2][:, 0:TS_], wbs_bf[:, kc, fsl], ysb[i2][:, kc, :],
                                   start=(kc == 0), stop=(kc == 3)), reads=["wbs_bf", f"ysb{i2}"], writes=[bn[b2]])
                for kc in range(4):
                    S.add("pe", MM(banks[b3][:, 0:TS_], wbc_bf[:, kc, fsl], yca[i2][:, kc, :],
                                   start=(kc == 0), stop=(kc == 3)), reads=["wbc_bf", f"yca{i2}"], writes=[bn[b3]])
                S.add("dve", TT(t1[:], gs[j2][:], banks[b2][:, 0:TS_], ALU.mult),
                      reads=[f"gs{j2}", bn[b2]], writes=["t1"])
                S.add("dve", TT(t2[:], gc[j2][:], banks[b3][:, 0:TS_], ALU.mult),
                      reads=[f"gc{j2}", bn[b3]], writes=["t2"])
                S.add("pool", TT(mt[:, fo, :], t1[:], t2[:], ALU.add), reads=["t1", "t2"], writes=[mtn])
            S.add("sp", DMA(mT_d[:, :, tsl], mt[:]), reads=[mtn], writes=["mT_d"], chan=f"mTo{i2}")
    S.barrier()

    yacc = cx.sb("yacc", [128, NS, D_MODEL], F32)
    X1T = cx.sb("X1T", [128, KC, NTOK], BF16)
    combT = cx.sb("combT", [32, NTOK], F32)

    with ExitStack() as es:
        sb = lambda name, shape, dt: es.enter_context(nc.sbuf_tensor(name + cx.sfx, shape, dt))
        wo_bf = sb("wo_bf", [128, KC, 1024], BF16)
        stg = [sb(f"stgB{i}", [128, 2048], F32) for i in range(2)]
        wo_v = d["w_out"].rearrange("(kc p) n -> p kc n", p=128)
        for k0 in range(0, KC, 2):
            b = (k0 // 2) % 2
            sv = stg[b][:].rearrange("p (k t) -> p k t", k=2)
            S.add("sp", DMA(sv, wo_v[:, k0:k0 + 2, :]), writes=[f"stgB{b}"], chan=f"stgB{b}")
            S.add("dve" if b == 0 else "pool", CP(wo_bf[:, k0:k0 + 2, :], sv),
                  reads=[f"stgB{b}"], writes=["wo_bf"])
        mTt = [sb(f"mTt{i}", [128, KC, 128], BF16) for i in range(2)]
        xtok = [sb(f"xtok{i}", [128, D_MODEL], F32) for i in range(2)]
        u = [sb(f"u{i}", [128, D_MODEL], F32) for i in range(2)]
        x1 = [sb(f"x1_{i}", [128, D_MODEL], F32) for i in range(2)]
        junk = sb("junk", [128, D_MODEL], BF16)
        x1T32 = sb("x1T32", [128, KC, 128], F32)
        stt = [sb(f"lnst{i}", [128, 8], F32) for i in range(2)]
        rt = [sb(f"rt{i}", [128, 128], F32) for i in range(2)]
        comb = [sb(f"comb{i}", [128, 32], F32) for i in range(2)]
        for sI in range(NS):
            i2 = sI % 2
            tok = slice(sI * 128, (sI + 1) * 128)
            S.add("sp", DMA(mTt[i2][:], mT_d[:, :, tok]), reads=["mT_d"], writes=[f"mTt{i2}"], chan=f"mTi{i2}")
            S.add("sp", DMA(xtok[i2][:], d["x_tok"][tok, :]), writes=[f"xtok{i2}"], chan=f"xt{i2}")
            for half in range(2):
                bk = banks[half]
                for kc in range(KC):
                    S.add("pe", MM(bk[:], mTt[i2][:, kc, :], wo_bf[:, kc, half * 512:(half + 1) * 512],
                                   start=(kc == 0), stop=(kc == KC - 1)),
                          reads=[f"mTt{i2}", "wo_bf"], writes=[bn[half]])
                S.add("dve", STT(u[i2][:, half * 512:(half + 1) * 512], xtok[i2][:, half * 512:(half + 1) * 512],
                                 ALPHA, bk[:], ALU.mult, ALU.add),
                      reads=[f"xtok{i2}", bn[half]], writes=[f"u{i2}"])
            layer_norm_tile(cx, S, u[i2][:], f"u{i2}", x1[i2][:], f"x1_{i2}", lnbc["ln1_g"], lnbc["ln1_b"],
                            stt[i2], f"lnst{i2}", junk, "junk", "lnbc")
            S.add("act", ACT(yacc[:, sI, :], x1[i2][:], AF.Identity, scale=ALPHA),
                  reads=[f"x1_{i2}"], writes=[("yacc", sI)])
            for kc in range(KC):
                bk = banks[2 + (kc // 4)]
                S.add("pe", TR(bk[:, (kc % 4) * 128:(kc % 4 + 1) * 128], x1[i2][:, kc * 128:(kc + 1) * 128],
                               ident32[:]), reads=[f"x1_{i2}", "ident32"], writes=[bn[2 + kc // 4]])
            for q in range(2):
                S.add("act", ACT(x1T32[:, q * 4:(q + 1) * 4, :],
                                 banks[2 + q][:].rearrange("p (k t) -> p k t", k=4), AF.Identity),
                      reads=[bn[2 + q]], writes=["x1T32"])
            S.add("act", ACT(X1T[:, :, tok], x1T32[:], AF.Identity), reads=["x1T32"], writes=[("X1T", sI)])
            lg = banks[4]
            for kc in range(KC):
                S.add("pe", MM(lg[:, 0:36], x1T32[:, kc, :], wr32[:, kc, :], start=(kc == 0), stop=(kc == KC - 1)),
                      reads=["x1T32", "wr32"], writes=[bn[4]])
            R_, rn = rt[i2], f"rt{i2}"
            L = R_[:, 0:36]

            def V(eng, fn):
                S.add(eng, fn, reads=[rn, "brt", bn[4]] if eng != "pool" else [rn, "brt"], writes=[rn])
            V("dve", TT(L, lg[:, 0:36], brt[:], ALU.add))
            gmax, ngmax, sg, gval = R_[:, 36:37], R_[:, 37:38], R_[:, 38:39], R_[:, 39:40]
            ohg, eg, esel = R_[:, 40:44], R_[:, 44:48], R_[:, 48:56]
            m1, oh1, e2, m2, oh2 = R_[:, 56:57], R_[:, 64:72], R_[:, 72:80], R_[:, 57:58], R_[:, 80:88]
            dd, ed, den, w1, w2 = R_[:, 58:59], R_[:, 59:60], R_[:, 60:61], R_[:, 61:62], R_[:, 62:63]
            ew, gw = R_[:, 88:96], R_[:, 96:100]
            V("dve", RED(gmax, L[:, 0:4], ALU.max))
            V("dve", TS(ohg, L[:, 0:4], gmax, None, ALU.is_equal))
            V("dve", TS(ngmax, gmax, -1.0, None, ALU.mult))
            V("pool", MS(sg, 0.0))
            V("act", ACT(eg, L[:, 0:4], AF.Exp, bias=ngmax, accum_out=sg))
            V("dve", RCP(gval, sg))
            V("dve", TS(esel, L[:, 4:12], ohg[:, 0:1], None, ALU.mult))
            for g in range(1, 4):
                V("dve", STT(esel, L[:, 4 + 8 * g:12 + 8 * g], ohg[:, g:g + 1], esel, ALU.mult, ALU.add))
            V("dve", RED(m1, esel, ALU.max))
            V("dve", TS(oh1, esel, m1, None, ALU.is_equal))
            V("dve", STT(e2, oh1, -1e30, esel, ALU.mult, ALU.add))
            V("dve", RED(m2, e2, ALU.max))
            V("dve", TS(oh2, e2, m2, None, ALU.is_equal))
            V("dve", TT(dd, m2, m1, ALU.subtract))
            V("act", ACT(ed, dd, AF.Exp))
            V("dve", TS(den, ed, 1.0, None, ALU.add))
            V("dve", RCP(w1, den))
            V("dve", TT(w2, ed, w1, ALU.mult))
            V("dve", TS(ew, oh1, w1, None, ALU.mult))
            V("dve", STT(ew, oh2, w2, ew, ALU.mult, ALU.add))
            V("dve", TS(gw, ohg, gval, None, ALU.mult))
            cb_, cbn = comb[i2], f"comb{i2}"
            for g in range(4):
                S.add("dve", TS(cb_[:, 8 * g:8 * g + 8], ew, gw[:, g:g + 1], None, ALU.mult),
                      reads=[rn], writes=[cbn])
            S.add("pe", TR(banks[5][0:32, 0:128], cb_[:], ident32[:]), reads=[cbn, "ident32"], writes=[bn[5]])
            S.add("act", ACT(combT[:, tok], banks[5][0:32, 0:128], AF.Identity), reads=[bn[5]],
                  writes=[("combT", sI)])
    S.barrier()

    NE = 32
    with ExitStack() as es:
        sb = lambda name, shape, dt: es.enter_context(nc.sbuf_tensor(name + cx.sfx, shape, dt))
        wgt = [sb(f"wgt{i}", [128, KC, 256], BF16) for i in range(2)]
        wup = [sb(f"wup{i}", [128, KC, 256], BF16) for i in range(2)]
        wdn = [sb(f"wdn{i}", [128, 2, 1024], BF16) for i in range(2)]
        stg = [sb(f"stgC{i}", [128, 2048], F32) for i in range(3)]
        sel = [sb(f"sel{i}", [32, 128], F32) for i in range(2)]
        sl = [sb(f"sl{i}", [128, TS_], F32) for i in range(2)]
        tl = [sb(f"tl{i}", [128, TS_], F32) for i in range(2)]
        cbs = [sb(f"cbs{i}", [128, TS_], F32) for i in range(2)]
        hid = [sb(f"hid{i}", [128, 2, TS_], BF16) for i in range(2)]
        wg_v = d["w_gate"].rearrange("e (kc p) f -> e p kc f", p=128)
        wu_v = d["w_up"].rearrange("e (kc p) f -> e p kc f", p=128)
        wd_v = d["w_down"].rearrange("e (fc p) n -> e p fc n", p=128)
        it = 0
        pyi = 0
        for e_ in range(NE):
            b = e_ % 2
            S.add("sp", DMA(stg[0][:].rearrange("p (k f) -> p k f", k=KC), wg_v[e_]), writes=["stgC0"], chan="stgC0")
            S.add("act", ACT(wgt[b][:], stg[0][:].rearrange("p (k f) -> p k f", k=KC), AF.Identity),
                  reads=["stgC0"], writes=[f"wgt{b}"])
            S.add("sp", DMA(stg[1][:].rearrange("p (k f) -> p k f", k=KC), wu_v[e_]), writes=["stgC1"], chan="stgC1")
            S.add("act", ACT(wup[b][:], stg[1][:].rearrange("p (k f) -> p k f", k=KC), AF.Identity),
                  reads=["stgC1"], writes=[f"wup{b}"])
            S.add("sp", DMA(stg[2][:].rearrange("p (k f) -> p k f", k=2), wd_v[e_]), writes=["stgC2"], chan="stgC2")
            S.add("pool", CP(wdn[b][:], stg[2][:].rearrange("p (k f) -> p k f", k=2)),
                  reads=["stgC2"], writes=[f"wdn{b}"])
            S.add("pool", MS(sel[b][:], 0.0), writes=[f"sel{b}"])
            S.add("sp", DMA(sel[b][e_:e_ + 1, :], cst["ones32"][0:1, :]), reads=["ones32"], writes=[f"sel{b}"],
                  chan=f"sel{b}")
            for T in range(NST):
                i2 = it % 2
                it += 1
                tsl = slice(T * TS_, (T + 1) * TS_)
                xr = [("X1T", T * NSUB + q) for q in range(NSUB)]
                S.add("pe", MM(banks[4][:, 0:TS_], sel[b][:], combT[:, tsl]),
                      reads=[f"sel{b}"] + [("combT", T * NSUB + q) for q in range(NSUB)], writes=[bn[4]])
                S.add("act", ACT(cbs[i2][:], banks[4][:, 0:TS_], AF.Identity), reads=[bn[4]], writes=[f"cbs{i2}"])
                for fc in range(2):
                    hg, hu = banks[fc * 2], banks[fc * 2 + 1]
                    for kc in range(KC):
                        S.add("pe", MM(hg[:, 0:TS_], wgt[b][:, kc, fc * 128:(fc + 1) * 128], X1T[:, kc, tsl],
                                       start=(kc == 0), stop=(kc == KC - 1)),
                              reads=[f"wgt{b}"] + xr, writes=[bn[fc * 2]])
                    for kc in range(KC):
                        S.add("pe", MM(hu[:, 0:TS_], wup[b][:, kc, fc * 128:(fc + 1) * 128], X1T[:, kc, tsl],
                                       start=(kc == 0), stop=(kc == KC - 1)),
                              reads=[f"wup{b}"] + xr, writes=[bn[fc * 2 + 1]])
                    S.add("act", ACT(sl[fc][:], hg[:, 0:TS_], AF.Silu), reads=[bn[fc * 2]], writes=[f"sl{fc}"])
                    S.add("dve", TT(tl[fc][:], sl[fc][:], hu[:, 0:TS_], ALU.mult),
                          reads=[f"sl{fc}", bn[fc * 2 + 1]], writes=[f"tl{fc}"])
                    S.add("pool", TT(hid[i2][:, fc, :], tl[fc][:], cbs[i2][:], ALU.mult),
                          reads=[f"tl{fc}", f"cbs{i2}"], writes=[(f"hid{i2}", fc)])
                for sub in range(NSUB):
                    sI = T * NSUB + sub
                    for half in range(2):
                        py = banks[5 + pyi % 3]
                        pyn = bn[5 + pyi % 3]
                        pyi += 1
                        for fc in range(2):
                            S.add("pe", MM(py[:], hid[i2][:, fc, sub * 128:(sub + 1) * 128],
                                           wdn[b][:, fc, half * 512:(half + 1) * 512],
                                           start=(fc == 0), stop=(fc == 1)),
                                  reads=[(f"hid{i2}", 0), (f"hid{i2}", 1), f"wdn{b}"], writes=[pyn])
                        ysl = yacc[:, sI, half * 512:(half + 1) * 512]
                        S.add("dve", TT(ysl, ysl, py[:], ALU.add), reads=[pyn, ("yacc", sI)], writes=[("yacc", sI)])
    S.barrier()

    with ExitStack() as es:
        sb = lambda name, shape, dt: es.enter_context(nc.sbuf_tensor(name + cx.sfx, shape, dt))
        x2 = [sb(f"x2_{i}", [128, D_MODEL], F32) for i in range(2)]
        junk = sb("junk2", [128, D_MODEL], BF16)
        stt = [sb(f"lnst2_{i}", [128, 8], F32) for i in range(2)]
        xTo = [sb(f"xTo{i}", [128, KC, 128], BF16) for i in range(2)]
        want_T = d.get("xT_out") is not None
        if d.get("x_ar") is not None:
            xmk = [sb(f"xmk{i}", [128, 4, KC, 128], F32) for i in range(2)]
            pm = d["pm_tile"]
        xTo_v = d["xT_out"].rearrange("(kc p) s -> p kc s", p=128) if want_T else None
        for sI in range(NS):
            i2 = sI % 2
            tok = slice(sI * 128, (sI + 1) * 128)
            layer_norm_tile(cx, S, yacc[:, sI, :], ("yacc", sI), x2[i2][:], f"x2_{i2}", lnbc["ln2_g"], lnbc["ln2_b"],
                            stt[i2], f"lnst2_{i2}", junk, "junk2", "lnbc")
            S.add("sp", DMA(d["x_out"][tok, :], x2[i2][:]), reads=[f"x2_{i2}"], chan=f"xo{i2}")
            if not want_T:
                continue
            for kc in range(KC):
                bk = banks[(kc // 4)]
                S.add("pe", TR(bk[:, (kc % 4) * 128:(kc % 4 + 1) * 128], x2[i2][:, kc * 128:(kc + 1) * 128],
                               ident32[:]), reads=[f"x2_{i2}", "ident32"], writes=[bn[kc // 4]])
            for q in range(2):
                S.add("act" if q == 0 else "dve",
                      ACT(xTo[i2][:, q * 4:(q + 1) * 4, :], banks[q][:].rearrange("p (k t) -> p k t", k=4), AF.Identity)
                      if q == 0 else
                      CP(xTo[i2][:, q * 4:(q + 1) * 4, :], banks[q][:].rearrange("p (k t) -> p k t", k=4)),
                      reads=[bn[q]], writes=[(f"xTo{i2}", q)])
            S.add("sp", DMA(xTo_v[:, :, tok], xTo[i2][:]), reads=[(f"xTo{i2}", 0), (f"xTo{i2}", 1)], chan=f"xTo{i2}")
            if d.get("x_ar") is not None:
                xm = xmk[i2]
                for j in range(4):
                    S.add("dve",
                          TS(xm[:, j, :, :], xTo[i2][:], pm[:, j:j + 1], None, ALU.mult),
                          reads=[(f"xTo{i2}", 0), (f"xTo{i2}", 1), "pm"], writes=[(f"xmk{i2}", j, 0), (f"xmk{i2}", j, 1)])
                cht = d["x_ar"][0].shape[1]
                cpq = NTOK // cht
                s8, c8 = (sI * 128) // cht, (sI * 128) % cht
                for j in range(4):
                    dst = d["x_ar"][j * cpq + s8].rearrange("(kc p) t -> p kc t", p=128)[:, :, c8:c8 + 128]
                    S.add("sp", DMA(dst, xm[:, j]), reads=[(f"xmk{i2}", j, q) for q in range(2)],
                          chan=f"xar{i2}")


def build_k2(NTOK, x_bf16):
    nc = bass.Bass("TRN2", target_bir_lowering=False)
    d = {}
    inp = lambda name, shape, dt=F32: nc.dram_tensor(name, shape, dt, kind="ExternalInput").ap()
    d["x_tok"] = inp("x_tok", [NTOK, D_MODEL])
    d["xT"] = inp("xT", [D_MODEL, NTOK], BF16 if x_bf16 else F32)
    d["yT_sb"] = inp("yT_sb", [512, NTOK], BF16)
    d["yT_ca"] = inp("yT_ca", [512, NTOK], BF16)
    d["wg"] = inp("wg", [D_MODEL, 2048])
    d["b_gate"] = inp("b_gate", [2048])
    d["w_br_sb"] = inp("w_br_sb", [512, D_MODEL])
    d["w_br_ca"] = inp("w_br_ca", [512, D_MODEL])
    d["w_out"] = inp("w_out", [D_MODEL, D_MODEL])
    for nm in ("ln1_g", "ln1_b", "ln2_g", "ln2_b"):
        d[nm] = inp(nm, [D_MODEL])
    d["w_router"] = inp("w_router", [D_MODEL, 36])
    d["b_router"] = inp("b_router", [36])
    d["w_gate"] = inp("w_gate", [32, D_MODEL, 256])
    d["w_up"] = inp("w_up", [32, D_MODEL, 256])
    d["w_down"] = inp("w_down", [32, 256, D_MODEL])
    d["x_out"] = nc.dram_tensor("x_out", [NTOK, D_MODEL], F32, kind="ExternalOutput").ap()
    d["xT_out"] = nc.dram_tensor("xT_out", [D_MODEL, NTOK], BF16, kind="ExternalOutput").ap()
    d["mT_scratch"] = nc.dram_tensor("mT_scratch", [128, 8, NTOK], BF16, kind="Internal").ap()
    with ExitStack() as es:
        cx = Ctx(nc, es)
        cst = build_consts(cx)
        k2_body(cx, cst, NTOK, x_bf16, d)
        cx.finish()
    return nc


def build_k1(S_len, x_bf16):
    nc = bass.Bass("TRN2", target_bir_lowering=False)
    xT = nc.dram_tensor("xT", [D_MODEL, S_len], BF16 if x_bf16 else F32, kind="ExternalInput").ap()
    w1 = nc.dram_tensor("w1", [D_MODEL, 768], F32, kind="ExternalInput").ap()
    cab = nc.dram_tensor("cab", [2, 128, 640], F32, kind="ExternalInput").ap()
    ot_sb = nc.dram_tensor("ot_sb", [128, S_len], BF16, kind="ExternalOutput").ap()
    ot_ca = nc.dram_tensor("ot_ca", [128, S_len], BF16, kind="ExternalOutput").ap()
    with ExitStack() as es:
        cx = Ctx(nc, es)
        cst = build_consts(cx)
        k1_body(cx, cst, S_len, x_bf16, xT, w1, cab, ot_sb, ot_ca)
        cx.finish()
    return nc


def ca_bias_table(rel_bias_l, heads):
    q = np.arange(128)[:, None]
    p = np.arange(640)[None, :]
    rel = np.clip(q + 512 - p, -128, 128) + 128
    ci = q // 64
    kc = p // 64
    valid = (kc >= ci) & (kc <= ci + 8)
    out = np.empty((len(heads), 128, 640), np.float32)
    for i, h in enumerate(heads):
        out[i] = np.where(valid, rel_bias_l[h][rel], np.float32(NEG))
    return out


def k1_weights(w_in_l, r):
    c = lambda base: w_in_l[:, base + r * 128: base + (r + 1) * 128]
    return np.ascontiguousarray(np.concatenate(
        [c(0), c(512), c(1536), c(2048), c(1024), c(2560)], axis=1))


def k2_inputs(p):
    f = np.ascontiguousarray
    m = {}
    m["wg"] = f(p["w_in"][:, 3072:5120])
    m["b_gate"] = f(p["b_gate"].reshape(2048))
    m["w_br_sb"] = f(p["w_br_sb"])
    m["w_br_ca"] = f(p["w_br_ca"])
    m["w_out"] = f(p["w_out"])
    for nm in ("ln1_g", "ln1_b", "ln2_g", "ln2_b"):
        m[nm] = f(p[nm])
    m["w_router"] = f(np.concatenate([p["w_group"]] + [p["w_erouter"][g] for g in range(4)], axis=1))
    m["b_router"] = f(np.concatenate([p["b_group"]] + [p["b_erouter"][g] for g in range(4)], axis=0))
    m["w_gate"] = f(p["w_gate"].reshape(32, D_MODEL, 256))
    m["w_up"] = f(p["w_up"].reshape(32, D_MODEL, 256))
    m["w_down"] = f(p["w_down"].reshape(32, 256, D_MODEL))
    return m


def build_fused(S_len):
    nc = bass.Bass("TRN2", target_bir_lowering=False)
    L = DEPTH
    NTOK = S_len // 4
    inp = lambda name, shape, dt=F32: nc.dram_tensor(name, shape, dt, kind="ExternalInput").ap()
    scr = lambda name, shape, dt: nc.dram_tensor(name, shape, dt, kind="Internal").ap()
    x_tok0 = inp("x_tok", [S_len, D_MODEL])
    xT0 = inp("xT", [D_MODEL, S_len])
    w1 = inp("w1", [L, 4, D_MODEL, 768])
    cab = inp("cab", [L, 4, 2, 128, 640])
    P = {}
    P["wg"] = inp("wg", [L, D_MODEL, 2048])
    P["b_gate"] = inp("b_gate", [L, 2048])
    P["w_br_sb"] = inp("w_br_sb", [L, 512, D_MODEL])
    P["w_br_ca"] = inp("w_br_ca", [L, 512, D_MODEL])
    P["w_out"] = inp("w_out", [L, D_MODEL, D_MODEL])
    for nm in ("ln1_g", "ln1_b", "ln2_g", "ln2_b"):
        P[nm] = inp(nm, [L, D_MODEL])
    P["w_router"] = inp("w_router", [L, D_MODEL, 36])
    P["b_router"] = inp("b_router", [L, 36])
    P["w_gate"] = inp("w_gate", [L, 32, D_MODEL, 256])
    P["w_up"] = inp("w_up", [L, 32, D_MODEL, 256])
    P["w_down"] = inp("w_down", [L, 32, 256, D_MODEL])
    out = nc.dram_tensor("out", [S_len, D_MODEL], F32, kind="ExternalOutput").ap()
    yT_sb = scr("yT_sb_s", [512, S_len], BF16)
    yT_ca = scr("yT_ca_s", [512, S_len], BF16)
    xT1 = scr("xT1_s", [D_MODEL, S_len], BF16)
    x_tok1 = scr("x_tok1_s", [S_len, D_MODEL], F32)
    mT_s = scr("mT_scratch", [128, 8, NTOK], BF16)
    with ExitStack() as es:
        cx = Ctx(nc, es)
        cst = build_consts(cx)
        for l in range(L):
            xT_l = xT0 if l == 0 else xT1
            xtok_l = x_tok0 if l == 0 else x_tok1
            for r in range(4):
                with ExitStack() as es2:
                    cx.es = es2
                    cx.sfx = f"_a{l}{r}"
                    k1_body(cx, cst, S_len, l > 0, xT_l, w1[l, r], cab[l, r],
                            yT_sb[r * 128:(r + 1) * 128, :], yT_ca[r * 128:(r + 1) * 128, :])
                cx.es = es
                cx.S.barrier()
            for r in range(4):
                tsl = slice(r * NTOK, (r + 1) * NTOK)
                d = {k: v[l] for k, v in P.items()}
                d["x_tok"] = xtok_l[tsl, :]
                d["xT"] = xT_l[:, tsl]
                d["yT_sb"] = yT_sb[:, tsl]
                d["yT_ca"] = yT_ca[:, tsl]
                d["mT_scratch"] = mT_s
                last = (l == L - 1)
                d["x_out"] = out[tsl, :] if last else x_tok1[tsl, :]
                d["xT_out"] = None if last else xT1[:, tsl]
                with ExitStack() as es2:
                    cx.es = es2
                    cx.sfx = f"_b{l}{r}"
                    k2_body(cx, cst, NTOK, l > 0, d)
                cx.es = es
                cx.S.barrier()
        cx.finish()
    return nc


NO_CC = False
DBG = set()


def build_fused8(S_len):
    nc = bass.Bass("TRN2", target_bir_lowering=False)
    L = DEPTH
    NTOK = S_len // 4
    TS_ = 512
    NCH = NTOK // TS_
    CHT = min(1024, NTOK)
    CPQ = NTOK // CHT
    NXC = 4 * CPQ
    groups = [[0, 1, 2, 3], [4, 5, 6, 7]]
    inp = lambda name, shape, dt=F32: nc.dram_tensor(name, shape, dt, kind="ExternalInput").ap()
    scr = lambda name, shape, dt: nc.dram_tensor(name, shape, dt, kind="Internal").ap()
    x_tok0 = inp("x_tok", [NTOK, D_MODEL])
    xT0 = inp("xT", [D_MODEL, S_len])
    xTq0 = inp("xTq", [D_MODEL, NTOK])
    pm_d = inp("pm", [128, 4])
    w1 = inp("w1", [L, D_MODEL, 768])
    cab = inp("cab", [L, 2, 128, 640])
    P = {}
    P["wg"] = inp("wg", [L, D_MODEL, 2048])
    P["b_gate"] = inp("b_gate", [L, 2048])
    P["w_br_sb"] = inp("w_br_sb", [L, 512, D_MODEL])
    P["w_br_ca"] = inp("w_br_ca", [L, 512, D_MODEL])
    P["w_out"] = inp("w_out", [L, D_MODEL, D_MODEL])
    for nm in ("ln1_g", "ln1_b", "ln2_g", "ln2_b"):
        P[nm] = inp(nm, [L, D_MODEL])
    P["w_router"] = inp("w_router", [L, D_MODEL, 36])
    P["b_router"] = inp("b_router", [L, 36])
    P["w_gate"] = inp("w_gate", [L, 32, D_MODEL, 256])
    P["w_up"] = inp("w_up", [L, 32, D_MODEL, 256])
    P["w_down"] = inp("w_down", [L, 32, 256, D_MODEL])
    out = nc.dram_tensor("out", [NTOK, D_MODEL], F32, kind="ExternalOutput").ap()
    y_in = [scr(f"y_in{c}", [4 * 1024, TS_], F32) for c in range(NCH)]
    y_rs = [scr(f"y_rs{c}", [1024, TS_], F32) for c in range(NCH)]
    x_in = [scr(f"x_in{k}", [D_MODEL, CHT], F32) for k in range(NXC)]
    x_all = [scr(f"x_all{k}", [D_MODEL, CHT], F32) for k in range(NXC)]
    xT1 = scr("xT1_s", [D_MODEL, NTOK], BF16)
    x_tok1 = scr("x_tok1_s", [NTOK, D_MODEL], F32)
    mT_s = scr("mT_scratch", [128, 8, NTOK], BF16)
    with ExitStack() as es:
        cx = Ctx(nc, es)
        S = cx.S
        cst = build_consts(cx)
        pm = cx.sb("pm_t", [128, 4], F32)
        S.add("sp", DMA(pm[:], pm_d[:, :]), writes=["pm"], chan="prm0")
        for l in range(L):
            last = (l == L - 1)
            with ExitStack() as es2:
                cx.es = es2
                cx.sfx = f"_a{l}"
                yst = [cx.sb(f"yst{i}", [64, 4, 512], F32) for i in range(2)]
                cnt = [0]

                chunk_res = {c: [] for c in range(NCH)}
                tiles_per_chunk = 2 * 2 * (S_len // 512) // NCH

                def emit_out(branch, h, qb, ob, obn, yst=yst, cnt=cnt, chunk_res=chunk_res, l=l):
                    k = cnt[0] % 2
                    cnt[0] += 1
                    q, c = (qb * 512) // NTOK, ((qb * 512) % NTOK) // TS_
                    for j in range(4):
                        S.add("dve", TS(yst[k][:, j, :], ob[:], pm[0:64, j:j + 1], None, ALU.mult),
                              reads=[obn, "pm"], writes=[(f"yst{k}", j)])
                    dst = y_in[c].rearrange("(q b j w) s -> q b w j s", q=4, b=2, j=4)[q][branch][h * 64:(h + 1) * 64]
                    rn = ("y_in", c, branch, h, q)
                    S.add("sp", DMA(dst, yst[k][:]), reads=[(f"yst{k}", j) for j in range(4)], writes=[rn],
                          chan=f"yo{k}")
                    chunk_res[c].append(rn)
                    if len(chunk_res[c]) == tiles_per_chunk:
                        S.add("pool", lambda e, i_=y_in[c][:, :], o_=y_rs[c][:, :]: e.collective_compute(
                            "ReduceScatter", op=ALU.add, replica_groups=groups, ins=[i_], outs=[o_]),
                            reads=list(chunk_res[c]), writes=[("y_rs", c)], chan=f"ccy{l}_{c}", inc=1)

                if l == 0:
                    xt_tile = None
                else:
                    def xt_tile(t):
                        kk, c0 = (t * 256) // CHT, (t * 256) % CHT
                        return x_all[kk].rearrange("(kc p) s -> p kc s", p=128)[:, :, c0:c0 + 256]
                k1_body(cx, cst, S_len, False, xT0, w1[l], cab[l], None, None, xt_tile=xt_tile, emit_out=emit_out)
            cx.es = es
            S.barrier(skip_prefix="ccy")
            d = {k: v[l] for k, v in P.items()}
            d["x_tok"] = x_tok0 if l == 0 else x_tok1
            d["xT"] = xTq0 if l == 0 else xT1
            d["y_rs"] = [y_rs[c] for c in range(NCH)]
            d["mT_scratch"] = mT_s
            d["x_out"] = out if last else x_tok1
            d["xT_out"] = None if last else xT1
            d["x_ar"] = None if (last or "noxar" in DBG) else x_in
            d["pm_tile"] = pm
            with ExitStack() as es2:
                cx.es = es2
                cx.sfx = f"_b{l}"
                k2_body(cx, cst, NTOK, l > 0, d)
            cx.es = es
            S.barrier()
            if not last:
                for k in range(NXC if not NO_CC else 0):
                    S.add("pool", lambda e, i_=x_in[k][:, :], o_=x_all[k][:, :]: e.collective_compute(
                        "AllReduce", op=ALU.add, replica_groups=groups, ins=[i_], outs=[o_]),
                        chan="cc", inc=1)
                S.barrier()
        cx.finish()
    return nc


def kernel_fused8(p):
    x = p["x"]
    B, S_len, D = x.shape
    NTOK = S_len // 4
    f = np.ascontiguousarray
    key = ("fused8", S_len)
    if key not in _NC_CACHE:
        _NC_CACHE[key] = build_fused8(S_len)
    nc = _NC_CACHE[key]
    per_l = [k2_inputs({k: v[l] for k, v in p.items() if k != "x"}) for l in range(DEPTH)]
    shared = {k: f(np.stack([per_l[l][k] for l in range(DEPTH)])) for k in per_l[0]}
    xT_b = [f(x[b].T) for b in range(B)]
    in_maps = []
    for c in range(8):
        b, r = c // 4, c % 4
        tsl = slice(r * NTOK, (r + 1) * NTOK)
        m = dict(shared)
        m["x_tok"] = f(x[b, tsl, :])
        m["xT"] = xT_b[b]
        m["xTq"] = f(xT_b[b][:, tsl])
        pmv = np.zeros((128, 4), np.float32)
        pmv[:, r] = 1.0
        m["pm"] = pmv
        m["w1"] = f(np.stack([k1_weights(p["w_in"][l], r) for l in range(DEPTH)]))
        m["cab"] = f(np.stack([ca_bias_table(p["rel_bias"][l], [2 * r, 2 * r + 1]) for l in range(DEPTH)]))
        in_maps.append(m)
    res = run_bass_kernel_spmd(nc, in_maps, core_ids=list(range(8))).results
    return np.stack([np.concatenate([np.asarray(res[b * 4 + r]["out"]) for r in range(4)], axis=0)
                     for b in range(B)], axis=0).astype(np.float32)


FUSED = 8


def kernel_fused(p):
    x = p["x"]
    B, S_len, D = x.shape
    f = np.ascontiguousarray
    key = ("fused", S_len)
    if key not in _NC_CACHE:
        _NC_CACHE[key] = build_fused(S_len)
    nc = _NC_CACHE[key]
    shared = {}
    shared["w1"] = f(np.stack([np.stack([k1_weights(p["w_in"][l], r) for r in range(4)]) for l in range(DEPTH)]))
    shared["cab"] = f(np.stack([np.stack([ca_bias_table(p["rel_bias"][l], [2 * r, 2 * r + 1]) for r in range(4)])
                                for l in range(DEPTH)]))
    per_l = [k2_inputs({k: v[l] for k, v in p.items() if k != "x"}) for l in range(DEPTH)]
    for k in per_l[0]:
        shared[k] = f(np.stack([per_l[l][k] for l in range(DEPTH)]))
    in_maps = []
    for b in range(B):
        m = dict(shared)
        m["x_tok"] = f(x[b])
        m["xT"] = f(x[b].T)
        in_maps.append(m)
    res = run_bass_kernel_spmd(nc, in_maps, core_ids=list(range(B))).results
    return np.stack([np.asarray(res[b]["out"]) for b in range(B)], axis=0).astype(np.float32)


_NC_CACHE = {}


def _get_nc(kind, *args):
    key = (kind,) + args
    if key not in _NC_CACHE:
        _NC_CACHE[key] = build_k1(*args) if kind == "k1" else build_k2(*args)
    return _NC_CACHE[key]


def kernel(**inputs):
    p = {k: np.asarray(v) for k, v in inputs.items()}
    if FUSED == 8:
        return kernel_fused8(p)
    if FUSED:
        return kernel_fused(p)
    x = p["x"]
    B, S_len, D = x.shape
    NTOK = S_len // 4
    cores = list(range(8))
    f = np.ascontiguousarray
    x_cur = x
    xT_full = None
    xT_prev = None
    for l in range(DEPTH):
        first = (l == 0)
        lp = {k: v[l] for k, v in p.items() if k != "x"}
        if first:
            xT_b = [f(x[b].T) for b in range(B)]
        else:
            xT_b = xT_full
        nc1 = _get_nc("k1", S_len, not first)
        in1 = []
        for c in cores:
            b, r = c // 4, c % 4
            in1.append({"xT": xT_b[b], "w1": k1_weights(lp["w_in"], r),
                        "cab": ca_bias_table(lp["rel_bias"], [2 * r, 2 * r + 1])})
        r1 = run_bass_kernel_spmd(nc1, in1, core_ids=cores).results
        yT_sb = [np.concatenate([np.asarray(r1[b * 4 + r]["ot_sb"]) for r in range(4)], axis=0) for b in range(B)]
        yT_ca = [np.concatenate([np.asarray(r1[b * 4 + r]["ot_ca"]) for r in range(4)], axis=0) for b in range(B)]
        nc2 = _get_nc("k2", NTOK, not first)
        wk2 = k2_inputs(lp)
        in2 = []
        for c in cores:
            b, r = c // 4, c % 4
            tsl = slice(r * NTOK, (r + 1) * NTOK)
            m = dict(wk2)
            m["x_tok"] = f(x_cur[b, tsl, :])
            m["xT"] = f(xT_b[b][:, tsl]) if first else xT_prev[c]
            m["yT_sb"] = f(yT_sb[b][:, tsl])
            m["yT_ca"] = f(yT_ca[b][:, tsl])
            in2.append(m)
        r2 = run_bass_kernel_spmd(nc2, in2, core_ids=cores).results
        x_cur = np.stack([np.concatenate([np.asarray(r2[b * 4 + r]["x_out"]) for r in range(4)], axis=0)
                          for b in range(B)], axis=0)
        xT_prev = [np.asarray(r2[c]["xT_out"]) for c in cores]
        xT_full = [f(np.concatenate([xT_prev[b * 4 + r] for r in range(4)], axis=1)) for b in range(B)]
    return x_cur.astype(np.float32)
```

```python
import numpy as np
from contextlib import ExitStack
import concourse.bass as bass
import concourse.mybir as mybir
from concourse.bass_utils import run_bass_kernel_spmd

F32 = mybir.dt.float32
BF16 = mybir.dt.bfloat16
AF = mybir.ActivationFunctionType
ALU = mybir.AluOpType
AX = mybir.AxisListType

D_MODEL = 1024
BATCH = 2
SEQ = 8192
DEPTH = 2
HD = 64
ALPHA = (2.0 * DEPTH) ** 0.25
LN_EPS = 1e-5
NEG = -30000.0
HEAT = 0


class Sched:
    ENG = ("pe", "act", "dve", "pool", "sp")

    def __init__(self, nc):
        self.nc = nc
        self.ops = []
        self.last_w = {}
        self.readers = {}
        self.chan_last = {}
        self.chans = []
        self.eng_last = {}
        self.bar = set()

    def barrier(self, skip_prefix=None):
        self.bar = set(self.eng_last.values()) | set(
            v for k, v in self.chan_last.items() if not (skip_prefix and k.startswith(skip_prefix)))

    def add(self, eng, fn, reads=(), writes=(), chan=None, inc=16):
        idx = len(self.ops)
        deps = set(self.bar)
        for r in reads:
            if r in self.last_w:
                deps.add(self.last_w[r])
        for w in writes:
            if w in self.last_w:
                deps.add(self.last_w[w])
            for rd in self.readers.get(w, ()):
                deps.add(rd)
        if chan is not None:
            if chan in self.chan_last:
                deps.add(self.chan_last[chan])
            else:
                self.chans.append(chan)
            self.chan_last[chan] = idx
        for r in reads:
            self.readers.setdefault(r, []).append(idx)
        for w in writes:
            self.last_w[w] = idx
            self.readers[w] = []
        self.ops.append(dict(eng=eng, fn=fn, deps=deps, chan=chan, inc=inc))
        if chan is None:
            self.eng_last[eng] = idx
        return idx

    def emit(self, block, sems, final_wait_eng="sp"):
        ops = self.ops
        n = len(ops)
        has_dep = [False] * n
        for o in ops:
            for d in o["deps"]:
                has_dep[d] = True
        eng_cnt = {e: 0 for e in self.ENG}
        chan_cnt = {}
        sig = [None] * n
        for i, o in enumerate(ops):
            if o["chan"] is not None:
                c = o["chan"]
                chan_cnt[c] = chan_cnt.get(c, 0) + o["inc"]
                sig[i] = ("ch:" + c, chan_cnt[c])
            elif has_dep[i]:
                e = o["eng"]
                eng_cnt[e] += 1
                sig[i] = (e, eng_cnt[e])
        per_eng = {e: [] for e in self.ENG}
        for i, o in enumerate(ops):
            per_eng[o["eng"]].append(i)
        self.eng_cnt = eng_cnt

        def run(ename, eng):
            seen = {}
            for i in per_eng[ename]:
                o = ops[i]
                need = {}
                for d in o["deps"]:
                    od = ops[d]
                    if od["chan"] is None and od["eng"] == "pe" and ename == "pe" and o["chan"] is None:
                        continue
                    s, v = sig[d]
                    if need.get(s, 0) < v:
                        need[s] = v
                for s, v in need.items():
                    if seen.get(s, 0) >= v:
                        continue
                    eng.wait_ge(sems[s], v)
                    seen[s] = v
                ins = o["fn"](eng)
                if sig[i] is not None:
                    s, v = sig[i]
                    ins.then_inc(sems[s], o["inc"] if o["chan"] is not None else 1)
            if ename == final_wait_eng:
                for c, v in chan_cnt.items():
                    if seen.get("ch:" + c, 0) < v:
                        eng.wait_ge(sems["ch:" + c], v)

        @block.tensor
        def _(e):
            run("pe", e)

        @block.scalar
        def _(e):
            run("act", e)

        @block.vector
        def _(e):
            run("dve", e)

        @block.gpsimd
        def _(e):
            run("pool", e)

        @block.sync
        def _(e):
            run("sp", e)


class Ctx:
    def __init__(self, nc, es):
        self.nc = nc
        self.es = es
        self.S = Sched(nc)
        self.sfx = ""

    def sb(self, name, shape, dt):
        return self.es.enter_context(self.nc.sbuf_tensor(name + self.sfx, shape, dt))

    def ps(self, name, shape, dt):
        return self.es.enter_context(self.nc.psum_tensor(name + self.sfx, shape, dt))

    def finish(self):
        nc, es, S = self.nc, self.es, self.S
        sems = {}
        for e in Sched.ENG:
            sems[e] = es.enter_context(nc.semaphore("sem_" + e))
        for c in S.chans:
            sems["ch:" + c] = es.enter_context(nc.semaphore("semch_" + c))
        block = es.enter_context(nc.Block())
        S.emit(block, sems)


def build_consts(cx):
    S = cx.S
    c = {}
    ident = cx.sb("ident", [128, 128], BF16)
    ident32 = cx.sb("ident32", [128, 128], F32)
    negtri = cx.sb("negtri", [128, 128], BF16)
    negones = cx.sb("negones", [128, 128], BF16)
    m01 = cx.sb("m01", [128, 896], BF16)
    mbias = cx.sb("mbias", [128, 896], BF16)
    S.add("pool", lambda e: e.memset(ident[:], 0.0), writes=["ident"])
    S.add("pool", lambda e: e.affine_select(out=ident[:], in_=ident[:], pattern=[[-1, 128]],
                                            compare_op=ALU.not_equal, fill=1.0, base=0,
                                            channel_multiplier=1), reads=["ident"], writes=["ident"])
    S.add("pool", lambda e: e.memset(ident32[:], 0.0), writes=["ident32"])
    S.add("pool", lambda e: e.affine_select(out=ident32[:], in_=ident32[:], pattern=[[-1, 128]],
                                            compare_op=ALU.not_equal, fill=1.0, base=0,
                                            channel_multiplier=1), reads=["ident32"], writes=["ident32"])
    S.add("pool", lambda e: e.memset(negtri[:], -1.0), writes=["negtri"])
    S.add("pool", lambda e: e.affine_select(out=negtri[:], in_=negtri[:], pattern=[[-1, 128]],
                                            compare_op=ALU.is_ge, fill=0.0, base=0,
                                            channel_multiplier=1), reads=["negtri"], writes=["negtri"])
    S.add("pool", lambda e: e.memset(negones[:], -1.0), writes=["negones"])
    S.add("pool", lambda e: e.memset(m01[:], 1.0), writes=["m01"])
    S.add("pool", lambda e: e.affine_select(out=m01[:], in_=m01[:], pattern=[[1, 896]],
                                            compare_op=ALU.is_gt, fill=0.0, base=-384,
                                            channel_multiplier=-1), reads=["m01"], writes=["m01"])
    S.add("pool", lambda e: e.memset(mbias[:], 0.0), writes=["mbias"])
    S.add("pool", lambda e: e.affine_select(out=mbias[:], in_=mbias[:], pattern=[[1, 896]],
                                            compare_op=ALU.is_gt, fill=NEG, base=-384,
                                            channel_multiplier=-1), reads=["mbias"], writes=["mbias"])
    ones32 = cx.sb("ones32", [1, 128], F32)
    S.add("pool", lambda e: e.memset(ones32[:], 1.0), writes=["ones32"])
    c["ones32"] = ones32
    c.update(ident=ident, ident32=ident32, negtri=negtri, negones=negones, m01=m01, mbias=mbias)
    return c


def MM(out, lhsT, rhs, start=True, stop=True, skip=False):
    if skip:
        return lambda e: e.matmul(out, lhsT=lhsT, rhs=rhs, start=start, stop=stop, skip_group_check=True)
    return lambda e: e.matmul(out, lhsT=lhsT, rhs=rhs, start=start, stop=stop)


def ACT(out, in_, func, **kw):
    return lambda e: e.activation(out=out, in_=in_, func=func, **kw)


def TT(out, in0, in1, op):
    return lambda e: e.tensor_tensor(out=out, in0=in0, in1=in1, op=op)


def TS(out, in0, s1, s2, op0, op1=None):
    if op1 is None:
        return lambda e: e.tensor_scalar(out=out, in0=in0, scalar1=s1, scalar2=s2, op0=op0)
    return lambda e: e.tensor_scalar(out=out, in0=in0, scalar1=s1, scalar2=s2, op0=op0, op1=op1)


def STT(out, in0, scalar, in1, op0, op1):
    return lambda e: e.scalar_tensor_tensor(out=out, in0=in0, scalar=scalar, in1=in1, op0=op0, op1=op1)


def CP(out, in_):
    return lambda e: e.tensor_copy(out=out, in_=in_)


def DMA(out, in_):
    return lambda e: e.dma_start(out=out, in_=in_)


def TR(out, in_, ident):
    return lambda e: e.transpose(out, in_, ident)


def RED(out, in_, op):
    return lambda e: e.tensor_reduce(out=out, in_=in_, axis=AX.X, op=op)


def MS(ap, val):
    return lambda e: e.memset(ap, val)


def RCP(out, in_):
    return lambda e: e.reciprocal(out=out, in_=in_)


def k1_body(cx, cst, S_len, x_bf16, xT, w1, cab, ot_sb, ot_ca, xt_tile=None, emit_out=None):
    nc, S = cx.nc, cx.S
    NT = S_len // 256
    NB = S_len // 128
    NQ = S_len // 512
    KC = D_MODEL // 128

    QT_sb = cx.sb("QT_sb", [128, S_len], BF16)
    KT_sb = cx.sb("KT_sb", [128, S_len], BF16)
    QT_ca = cx.sb("QT_ca", [128, S_len], BF16)
    KT_ca = cx.sb("KT_ca", [128, S_len], BF16)
    V_all = cx.sb("V_all", [128, NB, 256], BF16)
    x32 = [cx.sb(f"x32_{i}", [128, KC, 256], F32) for i in range(2)]
    xbf = [cx.sb(f"xbf_{i}", [128, KC, 256], BF16) for i in range(2)]
    wbf = cx.sb("wbf", [128, KC, 768], BF16)
    cabs = cx.sb("cabs", [128, 2, 640], F32)

    banks = [cx.ps(f"bank{i}", [128, 512], F32) for i in range(7)]
    bankT = cx.ps("bankT", [128, 512], F32)

    if xt_tile is None:
        xT_v = xT.rearrange("(kc p) s -> p kc s", p=128)
        xt_tile = lambda t: xT_v[:, :, t * 256:(t + 1) * 256]
    w1_v = w1.rearrange("(kc p) n -> p kc n", p=128)

    S.add("sp", DMA(cabs[:], cab.rearrange("h q k -> q h k")), writes=["cabs"], chan="cab")

    for pc in range(3):
        buf = x32[pc % 2]
        nm = f"x32_{pc % 2}"
        S.add("sp", DMA(buf[:], w1_v[:, :, pc * 256:(pc + 1) * 256]), writes=[nm], chan=f"x{pc % 2}")
        S.add("dve", CP(wbf[:, :, pc * 256:(pc + 1) * 256], buf[:]), reads=[nm], writes=[f"wbf{pc}"])
    wres = ["wbf0", "wbf1", "wbf2"]

    dsts = [QT_sb, KT_sb, QT_ca, KT_ca]
    dnames = ["QT_sb", "KT_sb", "QT_ca", "KT_ca"]
    for t in range(NT):
        i2 = t % 2
        xb = xbf[i2]
        xbn = f"xbf_{i2}"
        tsl = slice(t * 256, (t + 1) * 256)
        if x_bf16:
            S.add("sp", DMA(xb[:], xt_tile(t)), writes=[xbn], chan=f"x{i2}")
            xr = [xbn]
        else:
            xs = x32[i2]
            xsn = f"x32_{i2}"
            S.add("sp", DMA(xs[:], xt_tile(t)), writes=[xsn], chan=f"x{i2}")
            S.add("dve", CP(xb[:, 0:4, :], xs[:, 0:4, :]), reads=[xsn], writes=[xbn + "a"])
            S.add("pool", CP(xb[:, 4:8, :], xs[:, 4:8, :]), reads=[xsn], writes=[xbn + "b"])
            xr = [xbn + "a", xbn + "b"]
        for g in range(4):
            bk = banks[g]
            bkn = f"bank{g}"
            for kc in range(KC):
                S.add("pe", MM(bk[:, 0:256], wbf[:, kc, g * 128:(g + 1) * 128], xb[:, kc, :],
                               start=(kc == 0), stop=(kc == KC - 1)), reads=wres + xr, writes=[bkn])
            dst = dsts[g]
            if g in (0, 2):
                S.add("act", ACT(dst[:, tsl], bk[:, 0:256], AF.Identity, scale=0.125),
                      reads=[bkn], writes=[(dnames[g], t // 2)])
            else:
                S.add("dve", CP(dst[:, tsl], bk[:, 0:256]), reads=[bkn], writes=[(dnames[g], t // 2)])
        for sub in range(2):
            bk = banks[4 + sub]
            bkn = f"bank{4 + sub}"
            for kc in range(KC):
                S.add("pe", MM(bk[:, 0:256], xb[:, kc, sub * 128:(sub + 1) * 128], wbf[:, kc, 512:768],
                               start=(kc == 0), stop=(kc == KC - 1)), reads=wres + xr, writes=[bkn])
            blk = t * 2 + sub
            if sub == 0:
                S.add("dve", CP(V_all[:, blk, :], bk[:, 0:256]), reads=[bkn], writes=[("V", blk)])
            else:
                S.add("act", ACT(V_all[:, blk, :], bk[:, 0:256], AF.Identity), reads=[bkn], writes=[("V", blk)])

    negtri, negones, ident, m01, mbias = cst["negtri"], cst["negones"], cst["ident"], cst["m01"], cst["mbias"]
    T32 = [cx.sb(f"T32_{i}", [128, 640], F32) for i in range(2)]
    Pb = [cx.sb(f"P_{i}", [128, 640], BF16) for i in range(2)]
    Pn = [cx.sb(f"Pn_{i}", [128, 640], BF16) for i in range(2)]
    WT = [cx.sb(f"WT_{i}", [128, 640], BF16) for i in range(2)]
    st = [cx.sb(f"st_{i}", [128, 4], F32) for i in range(4)]
    oca = [cx.sb(f"oca_{i}", [64, 512], BF16) for i in range(2)]
    po = banks[6]
    tb = [banks[4][:].bitcast(BF16), banks[5][:].bitcast(BF16)]
    tbn = ["bank4", "bank5"]
    its = [(h, m) for h in range(2) for m in range(NB)]
    NIT = len(its)

    def RC(ci):
        h, m = its[ci]
        nk = min(640, 128 * (m + 1))
        i2 = ci % 2
        return dict(h=h, m=m, nk=nk, ks=128 * (m + 1) - nk, p0=640 - nk, n1=min(nk, 512), nj=nk // 128,
                    hp=slice(h * 64, (h + 1) * 64), qsl=slice(m * 128, (m + 1) * 128),
                    pa=banks[i2 * 2], pan=f"bank{i2 * 2}", pb=banks[i2 * 2 + 1], pbn=f"bank{i2 * 2 + 1}",
                    T=T32[i2], Tn=f"T32_{i2}", P=Pb[i2], Pnm=f"P_{i2}", PN=Pn[i2], PNn=f"Pn_{i2}",
                    W=WT[i2], Wn=f"WT_{i2}", sx=st[ci % 4], sxn=f"st_{ci % 4}", tb=tb[i2], tbn=tbn[i2])

    for t in range(NIT + 6):
        ci = t
        if ci < NIT:
            r = RC(ci)
            ks, nk, n1, m, hp = r["ks"], r["nk"], r["n1"], r["m"], r["hp"]
            krd = [("KT_ca", j) for j in range(ks // 512, (ks + nk - 1) // 512 + 1)]
            S.add("pe", MM(r["pa"][:, 0:n1], QT_ca[hp, r["qsl"]], KT_ca[hp, ks:ks + n1]),
                  reads=[("QT_ca", m // 4)] + krd, writes=[r["pan"]])
            if nk > 512:
                S.add("pe", MM(r["pb"][:, 0:128], QT_ca[hp, r["qsl"]], KT_ca[hp, ks + 512:ks + 640]),
                      reads=[("QT_ca", m // 4)] + krd, writes=[r["pbn"]])
        ci = t - 1
        if 0 <= ci < NIT:
            r = RC(ci)
            T, sx, nk, n1, p0, h = r["T"], r["sx"], r["nk"], r["n1"], r["p0"], r["h"]
            S.add("dve", TT(T[:, 0:n1], r["pa"][:, 0:n1], cabs[:, h, p0:p0 + n1], ALU.add),
                  reads=[r["pan"], "cabs"], writes=[r["Tn"]])
            if nk > 512:
                S.add("dve", TT(T[:, 512:640], r["pb"][:, 0:128], cabs[:, h, 512:640], ALU.add),
                      reads=[r["pbn"], "cabs", r["Tn"]], writes=[r["Tn"]])
            S.add("dve", RED(sx[:, 0:1], T[:, 0:nk], ALU.max), reads=[r["Tn"]], writes=[r["sxn"]])
            S.add("dve", TS(sx[:, 1:2], sx[:, 0:1], -1.0, None, ALU.mult), reads=[r["sxn"]], writes=[r["sxn"]])
            S.add("pool", MS(sx[:, 2:3], 0.0), reads=[r["sxn"]], writes=[r["sxn"]])
        ci = t - 2
        if 0 <= ci < NIT:
            r = RC(ci)
            nk, sx = r["nk"], r["sx"]
            S.add("act", ACT(r["P"][:, 0:nk], r["T"][:, 0:nk], AF.Exp, bias=sx[:, 1:2], accum_out=sx[:, 2:3]),
                  reads=[r["Tn"], r["sxn"]], writes=[r["Pnm"], r["sxn"]])
        ci = t - 3
        if 0 <= ci < NIT:
            r = RC(ci)
            nk, sx = r["nk"], r["sx"]
            S.add("dve", RCP(sx[:, 3:4], sx[:, 2:3]), reads=[r["sxn"]], writes=[r["sxn"]])
            S.add("dve", TS(r["PN"][:, 0:nk], r["P"][:, 0:nk], sx[:, 3:4], None, ALU.mult),
                  reads=[r["Pnm"], r["sxn"]], writes=[r["PNn"]])
        ci = t - 4
        if 0 <= ci < NIT:
            r = RC(ci)
            for j in range(r["nj"]):
                S.add("pe", TR(r["tb"][:, j * 128:(j + 1) * 128], r["PN"][:, j * 128:(j + 1) * 128], ident[:]),
                      reads=[r["PNn"], "ident"], writes=[r["tbn"]])
        ci = t - 5
        if 0 <= ci < NIT:
            r = RC(ci)
            nk = r["nk"]
            S.add("act", ACT(r["W"][:, 0:nk], r["tb"][:, 0:nk], AF.Identity), reads=[r["tbn"]], writes=[r["Wn"]])
        ci = t - 6
        if 0 <= ci < NIT:
            r = RC(ci)
            h, m, nj, ks = r["h"], r["m"], r["nj"], r["ks"]
            mcol = (m % 4) * 128
            for j in range(nj):
                kb = ks // 128 + j
                S.add("pe", MM(po[0:64, mcol:mcol + 128], V_all[:, kb, 128 + h * 64:128 + (h + 1) * 64],
                               r["W"][:, j * 128:(j + 1) * 128], start=(j == 0), stop=(j == nj - 1)),
                      reads=[r["Wn"], ("V", kb)], writes=["bank6"])
            if m % 4 == 3:
                oq = (h * NB + m) // 4
                ob, obn = oca[oq % 2], f"oca_{oq % 2}"
                S.add("act", ACT(ob[:], po[0:64, :], AF.Identity), reads=["bank6"], writes=[obn])
                if emit_out is not None:
                    emit_out(1, h, m // 4, ob, obn)
                else:
                    S.add("sp", DMA(ot_ca[h * 64:(h + 1) * 64, (m - 3) * 128:(m + 1) * 128], ob[:]),
                          reads=[obn], chan=f"oc{oq % 2}")

    E32 = [cx.sb(f"E32_{i}", [128, 512], F32) for i in range(2)]
    SPb = [cx.sb(f"SP_{i}", [128, 512], BF16) for i in range(3)]
    Wb = [cx.sb(f"W_{i}", [128, 512], BF16) for i in range(2)]
    C32 = cx.sb("C32", [128, 512], F32)
    Cbf = [cx.sb(f"Cbf_{i}", [128, 512], BF16) for i in range(4)]
    osb = [cx.sb(f"osb_{i}", [64, 512], BF16) for i in range(2)]
    blocks = []
    oi = 0
    for qb in range(NQ):
        for h in range(2):
            n = 4 * (qb + 1)
            for i in range(n):
                blocks.append(dict(h=h, qb=qb, i=i, n=n, kb=n - 1 - i, oi=oi))
            oi += 1
    NBK = len(blocks)

    def R(g):
        bl = blocks[g]
        h, qb, kb = bl["h"], bl["qb"], bl["kb"]
        d_ = dict(bl)
        d_.update(hp=slice(h * 64, (h + 1) * 64), qs=slice(qb * 512, (qb + 1) * 512),
                  pz=banks[g % 6], pzn=f"bank{g % 6}", E=E32[g % 2], En=f"E32_{g % 2}",
                  SP=SPb[g % 3], SPn=f"SP_{g % 3}", W=Wb[g % 2], Wn=f"W_{g % 2}",
                  C=Cbf[g % 4], Cn=f"Cbf_{g % 4}", Cp=Cbf[(g + 1) % 4], Cpn=f"Cbf_{(g + 1) % 4}",
                  po=banks[6] if bl["oi"] % 2 == 0 else bankT, pon="bank6" if bl["oi"] % 2 == 0 else "bankT",
                  diag=kb >= 4 * qb, off=(kb - 4 * qb) * 128)
        return d_

    for t in range(NBK + 5):
        g = t
        if g < NBK:
            r = R(g)
            S.add("pe", MM(r["pz"][:], KT_sb[r["hp"], r["kb"] * 128:(r["kb"] + 1) * 128], QT_sb[r["hp"], r["qs"]]),
                  reads=[("KT_sb", r["kb"] // 4), ("QT_sb", r["qb"])], writes=[r["pzn"]])
        g = t - 1
        if 0 <= g < NBK:
            r = R(g)
            S.add("act", ACT(r["E"][:], r["pz"][:], AF.Exp), reads=[r["pzn"]], writes=[r["En"]])
        g = t - 2
        if 0 <= g < NBK:
            r = R(g)
            off = r["off"]
            S.add("act", ACT(r["SP"][:], r["E"][:], AF.Ln, bias=1.0), reads=[r["En"]], writes=[r["SPn"]])
            if r["diag"]:
                S.add("dve", TT(r["SP"][:], r["SP"][:], m01[:, 384 - off:896 - off], ALU.mult),
                      reads=[r["SPn"], "m01"], writes=[r["SPn"]])
            if r["i"] < r["n"] - 1:
                if r["i"] == 0:
                    S.add("dve", CP(C32[:], r["SP"][:]), reads=[r["SPn"]], writes=["C32"])
                    S.add("dve", CP(r["Cp"][:], r["SP"][:]), reads=[r["SPn"]], writes=[r["Cpn"]])
                else:
                    S.add("dve", TT(C32[:], C32[:], r["SP"][:], ALU.add), reads=[r["SPn"], "C32"], writes=["C32"])
                    S.add("dve", CP(r["Cp"][:], C32[:]), reads=["C32"], writes=[r["Cpn"]])
        g = t - 3
        if 0 <= g < NBK:
            r = R(g)
            off = r["off"]
            S.add("pe", MM(r["pz"][:], negtri[:], r["SP"][:], start=False, stop=False, skip=True),
                  reads=[r["SPn"], "negtri"], writes=[r["pzn"]])
            if r["i"] > 0:
                S.add("pe", MM(r["pz"][:], negones[:], r["C"][:], start=False, stop=False, skip=True),
                      reads=[r["Cn"], "negones"], writes=[r["pzn"]])
            if r["diag"]:
                S.add("pe", MM(r["pz"][:], ident[:], mbias[:, 384 - off:896 - off],
                               start=False, stop=False, skip=True),
                      reads=["ident", "mbias"], writes=[r["pzn"]])
        g = t - 4
        if 0 <= g < NBK:
            r = R(g)
            S.add("act", ACT(r["W"][:], r["pz"][:], AF.Exp), reads=[r["pzn"]], writes=[r["Wn"]])
        g = t - 5
        if 0 <= g < NBK:
            r = R(g)
            h = r["h"]
            po, pon = r["po"], r["pon"]
            S.add("pe", MM(po[0:64, 0:512], V_all[:, r["kb"], h * 64:(h + 1) * 64], r["W"][:],
                           start=(r["i"] == 0), stop=(r["i"] == r["n"] - 1)),
                  reads=[r["Wn"], ("V", r["kb"])], writes=[pon])
            for _ in range(HEAT):
                S.add("pe", MM(po[64:128, 0:512], negones[:, 0:64], m01[:, 0:512]), reads=["negones", "m01"])
            if r["i"] == r["n"] - 1:
                ob = osb[r["oi"] % 2]
                obn = f"osb_{r['oi'] % 2}"
                S.add("dve", CP(ob[:], po[0:64, 0:512]), reads=[pon], writes=[obn])
                if emit_out is not None:
                    emit_out(0, h, r["qb"], ob, obn)
                else:
                    S.add("sp", DMA(ot_sb[h * 64:(h + 1) * 64, r["qs"]], ob[:]), reads=[obn], chan=f"o{r['oi'] % 2}")


def layer_norm_tile(cx, S, u, un, out, outn, gbc, bbc, st, stn, junk, junkn, gn, tmp=None, tmpn=None):
    D = D_MODEL
    S.add("pool", MS(st[:, 0:2], 0.0), reads=[stn], writes=[stn])
    S.add("act", ACT(junk[:], u, AF.Identity, accum_out=st[:, 0:1]), reads=[un, stn], writes=[junkn, stn])
    S.add("act", ACT(junk[:], u, AF.Square, accum_out=st[:, 1:2]), reads=[un, stn, junkn], writes=[junkn, stn])
    S.add("dve", TS(st[:, 2:4], st[:, 0:2], 1.0 / D, None, ALU.mult), reads=[stn], writes=[stn])
    S.add("dve", STT(st[:, 4:5], st[:, 2:3], st[:, 2:3], st[:, 3:4], ALU.mult, ALU.subtract),
          reads=[stn], writes=[stn])
    S.add("dve", TS(st[:, 5:6], st[:, 4:5], -1.0, LN_EPS, ALU.mult, ALU.add), reads=[stn], writes=[stn])
    S.add("act", ACT(st[:, 5:6], st[:, 5:6], AF.Ln), reads=[stn], writes=[stn])
    S.add("act", ACT(st[:, 6:7], st[:, 5:6], AF.Exp, scale=-0.5), reads=[stn], writes=[stn])
    S.add("dve", STT(st[:, 7:8], st[:, 2:3], -1.0, st[:, 6:7], ALU.mult, ALU.mult), reads=[stn], writes=[stn])
    S.add("act", ACT(out, u, AF.Identity, scale=st[:, 6:7], bias=st[:, 7:8]), reads=[un, stn], writes=[outn])
    S.add("dve", TT(out, out, gbc[:], ALU.mult), reads=[outn, gn], writes=[outn])
    S.add("dve", TT(out, out, bbc[:], ALU.add), reads=[outn, gn], writes=[outn])


def k2_body(cx, cst, NTOK, x_bf16, d):
    nc, S = cx.nc, cx.S
    KC = D_MODEL // 128
    TS_ = min(512, NTOK)
    NST = NTOK // TS_
    NSUB = TS_ // 128
    NS = NTOK // 128
    ident32 = cst["ident32"]
    banks = [cx.ps(f"kb{i}", [128, 512], F32) for i in range(8)]
    bn = [f"kb{i}" for i in range(8)]

    lnbc = {}
    for nm in ("ln1_g", "ln1_b", "ln2_g", "ln2_b"):
        t = cx.sb("bc_" + nm, [128, D_MODEL], F32)
        S.add("sp", DMA(t[:], d[nm].partition_broadcast(128)), writes=["lnbc"], chan="prm_" + nm)
        lnbc[nm] = t
    bg = cx.sb("bg", [128, 16], F32)
    S.add("sp", DMA(bg[:], d["b_gate"]), writes=["bg"], chan="prm_bg")
    wr32 = cx.sb("wr32", [128, KC, 36], F32)
    S.add("sp", DMA(wr32[:], d["w_router"].rearrange("(kc p) n -> p kc n", p=128)), writes=["wr32"], chan="prm")
    brt = cx.sb("brt", [128, 36], F32)
    S.add("sp", DMA(brt[:], d["b_router"].partition_broadcast(128)), writes=["brt"], chan="prm")

    mT_d = d["mT_scratch"]
    xT_v = d["xT"].rearrange("(kc p) s -> p kc s", p=128)
    if d.get("y_rs") is None:
        ysb_v = d["yT_sb"].rearrange("(kc p) s -> p kc s", p=128)
        yca_v = d["yT_ca"].rearrange("(kc p) s -> p kc s", p=128)

    with ExitStack() as es:
        sb = lambda name, shape, dt: es.enter_context(nc.sbuf_tensor(name + cx.sfx, shape, dt))
        wg_bf = sb("wg_bf", [128, KC, 2048], BF16)
        wbs_bf = sb("wbs_bf", [128, 4, 1024], BF16)
        wbc_bf = sb("wbc_bf", [128, 4, 1024], BF16)
        stg = [sb(f"stgA{i}", [128, 2048], F32) for i in range(2)]
        xbf = [sb(f"xbfA{i}", [128, KC, TS_], BF16) for i in range(2)]
        ysb = [sb(f"ysb{i}", [128, 4, TS_], BF16) for i in range(2)]
        yca = [sb(f"yca{i}", [128, 4, TS_], BF16) for i in range(2)]
        gs = [sb(f"gs{i}", [128, TS_], F32) for i in range(2)]
        gc = [sb(f"gc{i}", [128, TS_], F32) for i in range(2)]
        t1 = sb("t1", [128, TS_], F32)
        t2 = sb("t2", [128, TS_], F32)
        mT = [sb(f"mT{i}", [128, KC, TS_], BF16) for i in range(2)]
        si = 0

        def stage_cast(dst, src_ap, ncols, rn, k=None, rd=()):
            nonlocal si
            b = si % 2
            si += 1
            sv = stg[b][:, 0:ncols]
            if k is not None:
                sv = sv.rearrange("p (k t) -> p k t", k=k)
            S.add("sp", DMA(sv, src_ap), reads=list(rd), writes=[f"stgA{b}"], chan=f"stgA{b}")
            S.add("dve", CP(dst, sv), reads=[f"stgA{b}"], writes=[rn])

        wg_v = d["wg"].rearrange("(kc p) n -> p kc n", p=128)
        for kc in range(KC):
            stage_cast(wg_bf[:, kc, :], wg_v[:, kc, :], 2048, "wg_bf")
        wbs_v = d["w_br_sb"].rearrange("(kc p) n -> p kc n", p=128)
        wbc_v = d["w_br_ca"].rearrange("(kc p) n -> p kc n", p=128)
        for kc in range(0, 4, 2):
            stage_cast(wbs_bf[:, kc:kc + 2, :], wbs_v[:, kc:kc + 2, :], 2048, "wbs_bf", k=2)
            stage_cast(wbc_bf[:, kc:kc + 2, :], wbc_v[:, kc:kc + 2, :], 2048, "wbc_bf", k=2)
        for T in range(NST):
            i2 = T % 2
            tsl = slice(T * TS_, (T + 1) * TS_)
            xb, xbn = xbf[i2], f"xbfA{i2}"
            if x_bf16:
                S.add("sp", DMA(xb[:], xT_v[:, :, tsl]), writes=[xbn], chan=f"xA{i2}")
            else:
                per = 2048 // TS_
                for k0 in range(0, KC, per):
                    stage_cast(xb[:, k0:k0 + per, :], xT_v[:, k0:k0 + per, tsl], per * TS_, xbn, k=per)
            if d.get("y_rs") is not None:
                yv = d["y_rs"][T].rearrange("(b kc p) s -> b p kc s", b=2, p=128)
                per = 2048 // TS_
                for k0 in range(0, 4, per):
                    stage_cast(ysb[i2][:, k0:k0 + per, :], yv[0][:, k0:k0 + per, :], per * TS_, f"ysb{i2}", k=per,
                               rd=[("y_rs", T)])
                    stage_cast(yca[i2][:, k0:k0 + per, :], yv[1][:, k0:k0 + per, :], per * TS_, f"yca{i2}", k=per,
                               rd=[("y_rs", T)])
            else:
                S.add("sp", DMA(ysb[i2][:], ysb_v[:, :, tsl]), writes=[f"ysb{i2}"], chan=f"yA{i2}")
                S.add("sp", DMA(yca[i2][:], yca_v[:, :, tsl]), writes=[f"yca{i2}"], chan=f"yB{i2}")
            mt, mtn = mT[i2], f"mT{i2}"
            for fo in range(KC):
                j2 = fo % 2
                fsl = slice(fo * 128, (fo + 1) * 128)
                fsl2 = slice(1024 + fo * 128, 1024 + (fo + 1) * 128)
                b0, b1, b2, b3 = j2 * 4, j2 * 4 + 1, j2 * 4 + 2, j2 * 4 + 3
                for kc in range(KC):
                    S.add("pe", MM(banks[b0][:, 0:TS_], wg_bf[:, kc, fsl], xb[:, kc, :],
                                   start=(kc == 0), stop=(kc == KC - 1)), reads=["wg_bf", xbn], writes=[bn[b0]])
                S.add("act", ACT(gs[j2][:], banks[b0][:, 0:TS_], AF.Sigmoid, bias=bg[:, fo:fo + 1]),
                      reads=[bn[b0], "bg"], writes=[f"gs{j2}"])
                for kc in range(KC):
                    S.add("pe", MM(banks[b1][:, 0:TS_], wg_bf[:, kc, fsl2], xb[:, kc, :],
                                   start=(kc == 0), stop=(kc == KC - 1)), reads=["wg_bf", xbn], writes=[bn[b1]])
                S.add("act", ACT(gc[j2][:], banks[b1][:, 0:TS_], AF.Sigmoid, bias=bg[:, 8 + fo:9 + fo]),
                      reads=[bn[b1], "bg"], writes=[f"gc{j2}"])
                for kc in range(4):
                    S.add("pe", MM(banks[b2][:, 0:TS_], wbs_bf[:, kc, fsl], ysb[i2][:, kc, :],
                                   start=(kc == 0), stop=(kc == 3)), reads=["wbs_bf", f"ysb{i2}"], writes=[bn[b2]])
                for kc in range(4):
                    S.add("pe", MM(banks[b3][:, 0:TS_], wbc_bf[:, kc, fsl], yca[i2][:, kc, :],
                                   start=(kc == 0), stop=(kc == 3)), reads=["wbc_bf", f"yca{i2}"], writes=[bn[b3]])
                S.add("dve", TT(t1[:], gs[j2][:], banks[b2][:, 0:TS_], ALU.mult),
                      reads=[f"gs{j2}", bn[b2]], writes=["t1"])
                S.add("dve", TT(t2[:], gc[j2][:], banks[b3][:, 0:TS_], ALU.mult),
                      reads=[f"gc{j2}", bn[b3]], writes=["t2"])
                S.add("pool", TT(mt[:, fo, :], t1[:], t2[:], ALU.add), reads=["t1", "t2"], writes=[mtn])
            S.add("sp", DMA(mT_d[:, :, tsl], mt[:]), reads=[mtn], writes=["mT_d"], chan=f"mTo{i2}")
    S.barrier()

    yacc = cx.sb("yacc", [128, NS, D_MODEL], F32)
    X1T = cx.sb("X1T", [128, KC, NTOK], BF16)
    combT = cx.sb("combT", [32, NTOK], F32)

    with ExitStack() as es:
        sb = lambda name, shape, dt: es.enter_context(nc.sbuf_tensor(name + cx.sfx, shape, dt))
        wo_bf = sb("wo_bf", [128, KC, 1024], BF16)
        stg = [sb(f"stgB{i}", [128, 2048], F32) for i in range(2)]
        wo_v = d["w_out"].rearrange("(kc p) n -> p kc n", p=128)
        for k0 in range(0, KC, 2):
            b = (k0 // 2) % 2
            sv = stg[b][:].rearrange("p (k t) -> p k t", k=2)
            S.add("sp", DMA(sv, wo_v[:, k0:k0 + 2, :]), writes=[f"stgB{b}"], chan=f"stgB{b}")
            S.add("dve" if b == 0 else "pool", CP(wo_bf[:, k0:k0 + 2, :], sv),
                  reads=[f"stgB{b}"], writes=["wo_bf"])
        mTt = [sb(f"mTt{i}", [128, KC, 128], BF16) for i in range(2)]
        xtok = [sb(f"xtok{i}", [128, D_MODEL], F32) for i in range(2)]
        u = [sb(f"u{i}", [128, D_MODEL], F32) for i in range(2)]
        x1 = [sb(f"x1_{i}", [128, D_MODEL], F32) for i in range(2)]
        junk = sb("junk", [128, D_MODEL], BF16)
        x1T32 = sb("x1T32", [128, KC, 128], F32)
        stt = [sb(f"lnst{i}", [128, 8], F32) for i in range(2)]
        rt = [sb(f"rt{i}", [128, 128], F32) for i in range(2)]
        comb = [sb(f"comb{i}", [128, 32], F32) for i in range(2)]
        for sI in range(NS):
            i2 = sI % 2
            tok = slice(sI * 128, (sI + 1) * 128)
            S.add("sp", DMA(mTt[i2][:], mT_d[:, :, tok]), reads=["mT_d"], writes=[f"mTt{i2}"], chan=f"mTi{i2}")
            S.add("sp", DMA(xtok[i2][:], d["x_tok"][tok, :]), writes=[f"xtok{i2}"], chan=f"xt{i2}")
            for half in range(2):
                bk = banks[half]
                for kc in range(KC):
                    S.add("pe", MM(bk[:], mTt[i2][:, kc, :], wo_bf[:, kc, half * 512:(half + 1) * 512],
                                   start=(kc == 0), stop=(kc == KC - 1)),
                          reads=[f"mTt{i2}", "wo_bf"], writes=[bn[half]])
                S.add("dve", STT(u[i2][:, half * 512:(half + 1) * 512], xtok[i2][:, half * 512:(half + 1) * 512],
                                 ALPHA, bk[:], ALU.mult, ALU.add),
                      reads=[f"xtok{i2}", bn[half]], writes=[f"u{i2}"])
            layer_norm_tile(cx, S, u[i2][:], f"u{i2}", x1[i2][:], f"x1_{i2}", lnbc["ln1_g"], lnbc["ln1_b"],
                            stt[i2], f"lnst{i2}", junk, "junk", "lnbc")
            S.add("act", ACT(yacc[:, sI, :], x1[i2][:], AF.Identity, scale=ALPHA),
                  reads=[f"x1_{i2}"], writes=[("yacc", sI)])
            for kc in range(KC):
                bk = banks[2 + (kc // 4)]
                S.add("pe", TR(bk[:, (kc % 4) * 128:(kc % 4 + 1) * 128], x1[i2][:, kc * 128:(kc + 1) * 128],
                               ident32[:]), reads=[f"x1_{i2}", "ident32"], writes=[bn[2 + kc // 4]])
            for q in range(2):
                S.add("act", ACT(x1T32[:, q * 4:(q + 1) * 4, :],
                                 banks[2 + q][:].rearrange("p (k t) -> p k t", k=4), AF.Identity),
                      reads=[bn[2 + q]], writes=["x1T32"])
            S.add("act", ACT(X1T[:, :, tok], x1T32[:], AF.Identity), reads=["x1T32"], writes=[("X1T", sI)])
            lg = banks[4]
            for kc in range(KC):
                S.add("pe", MM(lg[:, 0:36], x1T32[:, kc, :], wr32[:, kc, :], start=(kc == 0), stop=(kc == KC - 1)),
                      reads=["x1T32", "wr32"], writes=[bn[4]])
            R_, rn = rt[i2], f"rt{i2}"
            L = R_[:, 0:36]

            def V(eng, fn):
                S.add(eng, fn, reads=[rn, "brt", bn[4]] if eng != "pool" else [rn, "brt"], writes=[rn])
            V("dve", TT(L, lg[:, 0:36], brt[:], ALU.add))
            gmax, ngmax, sg, gval = R_[:, 36:37], R_[:, 37:38], R_[:, 38:39], R_[:, 39:40]
            ohg, eg, esel = R_[:, 40:44], R_[:, 44:48], R_[:, 48:56]
            m1, oh1, e2, m2, oh2 = R_[:, 56:57], R_[:, 64:72], R_[:, 72:80], R_[:, 57:58], R_[:, 80:88]
            dd, ed, den, w1, w2 = R_[:, 58:59], R_[:, 59:60], R_[:, 60:61], R_[:, 61:62], R_[:, 62:63]
            ew, gw = R_[:, 88:96], R_[:, 96:100]
            RE = "dve"
            tmpm = R_[:, 100:104]

            def tree_max(dst, src, n):
                cur = src
                while n > 1:
                    n //= 2
                    o_ = dst if n == 1 else tmpm[:, 0:n]
                    V(RE, TT(o_, cur[:, 0:n], cur[:, n:2 * n], ALU.max))
                    cur = tmpm
            V(RE, RED(gmax, L[:, 0:4], ALU.max))
            V(RE, TS(ohg, L[:, 0:4], gmax, None, ALU.is_equal))
            V(RE, TS(ngmax, gmax, -1.0, None, ALU.mult))
            V("pool", MS(sg, 0.0))
            V("act", ACT(eg, L[:, 0:4], AF.Exp, bias=ngmax, accum_out=sg))
            V("dve", RCP(gval, sg))
            V(RE, TS(esel, L[:, 4:12], ohg[:, 0:1], None, ALU.mult))
            for g in range(1, 4):
                V(RE, STT(esel, L[:, 4 + 8 * g:12 + 8 * g], ohg[:, g:g + 1], esel, ALU.mult, ALU.add))
            V(RE, RED(m1, esel, ALU.max))
            V(RE, TS(oh1, esel, m1, None, ALU.is_equal))
            V(RE, STT(e2, oh1, -1e30, esel, ALU.mult, ALU.add))
            V(RE, RED(m2, e2, ALU.max))
            V(RE, TS(oh2, e2, m2, None, ALU.is_equal))
            V(RE, TT(dd, m2, m1, ALU.subtract))
            V("act", ACT(ed, dd, AF.Exp))
            V(RE, TS(den, ed, 1.0, None, ALU.add))
            V("dve", RCP(w1, den))
            V(RE, TT(w2, ed, w1, ALU.mult))
            V(RE, TS(ew, oh1, w1, None, ALU.mult))
            V(RE, STT(ew, oh2, w2, ew, ALU.mult, ALU.add))
            V(RE, TS(gw, ohg, gval, None, ALU.mult))
            cb_, cbn = comb[i2], f"comb{i2}"
            for g in range(4):
                S.add(RE, TS(cb_[:, 8 * g:8 * g + 8], ew, gw[:, g:g + 1], None, ALU.mult),
                      reads=[rn], writes=[cbn])
            S.add("pe", TR(banks[5][0:32, 0:128], cb_[:], ident32[:]), reads=[cbn, "ident32"], writes=[bn[5]])
            S.add("act", ACT(combT[:, tok], banks[5][0:32, 0:128], AF.Identity), reads=[bn[5]],
                  writes=[("combT", sI)])
    S.barrier()

    NE = 32
    with ExitStack() as es:
        sb = lambda name, shape, dt: es.enter_context(nc.sbuf_tensor(name + cx.sfx, shape, dt))
        wgt = [sb(f"wgt{i}", [128, KC, 256], BF16) for i in range(2)]
        wup = [sb(f"wup{i}", [128, KC, 256], BF16) for i in range(2)]
        wdn = [sb(f"wdn{i}", [128, 2, 1024], BF16) for i in range(2)]
        stg = [sb(f"stgC{i}", [128, 2048], F32) for i in range(3)]
        sel = [sb(f"sel{i}", [32, 128], F32) for i in range(2)]
        sl = [sb(f"sl{i}", [128, TS_], F32) for i in range(2)]
        tl = [sb(f"tl{i}", [128, TS_], F32) for i in range(2)]
        cbs = [sb(f"cbs{i}", [128, TS_], F32) for i in range(2)]
        hid = [sb(f"hid{i}", [128, 2, TS_], BF16) for i in range(2)]
        wg_v = d["w_gate"].rearrange("e (kc p) f -> e p kc f", p=128)
        wu_v = d["w_up"].rearrange("e (kc p) f -> e p kc f", p=128)
        wd_v = d["w_down"].rearrange("e (fc p) n -> e p fc n", p=128)
        it = 0
        pyi = 0
        for e_ in range(NE):
            b = e_ % 2
            S.add("sp", DMA(stg[0][:].rearrange("p (k f) -> p k f", k=KC), wg_v[e_]), writes=["stgC0"], chan="stgC0")
            S.add("act", ACT(wgt[b][:], stg[0][:].rearrange("p (k f) -> p k f", k=KC), AF.Identity),
                  reads=["stgC0"], writes=[f"wgt{b}"])
            S.add("sp", DMA(stg[1][:].rearrange("p (k f) -> p k f", k=KC), wu_v[e_]), writes=["stgC1"], chan="stgC1")
            S.add("act", ACT(wup[b][:], stg[1][:].rearrange("p (k f) -> p k f", k=KC), AF.Identity),
                  reads=["stgC1"], writes=[f"wup{b}"])
            S.add("sp", DMA(stg[2][:].rearrange("p (k f) -> p k f", k=2), wd_v[e_]), writes=["stgC2"], chan="stgC2")
            S.add("pool", CP(wdn[b][:], stg[2][:].rearrange("p (k f) -> p k f", k=2)),
                  reads=["stgC2"], writes=[f"wdn{b}"])
            S.add("pool", MS(sel[b][:], 0.0), writes=[f"sel{b}"])
            S.add("sp", DMA(sel[b][e_:e_ + 1, :], cst["ones32"][0:1, :]), reads=["ones32"], writes=[f"sel{b}"],
                  chan=f"sel{b}")
            for T in range(NST):
                i2 = it % 2
                it += 1
                tsl = slice(T * TS_, (T + 1) * TS_)
                xr = [("X1T", T * NSUB + q) for q in range(NSUB)]
                S.add("pe", MM(banks[4][:, 0:TS_], sel[b][:], combT[:, tsl]),
                      reads=[f"sel{b}"] + [("combT", T * NSUB + q) for q in range(NSUB)], writes=[bn[4]])
                S.add("act", ACT(cbs[i2][:], banks[4][:, 0:TS_], AF.Identity), reads=[bn[4]], writes=[f"cbs{i2}"])
                for fc in range(2):
                    hg, hu = banks[fc * 2], banks[fc * 2 + 1]
                    for kc in range(KC):
                        S.add("pe", MM(hg[:, 0:TS_], wgt[b][:, kc, fc * 128:(fc + 1) * 128], X1T[:, kc, tsl],
                                       start=(kc == 0), stop=(kc == KC - 1)),
                              reads=[f"wgt{b}"] + xr, writes=[bn[fc * 2]])
                    for kc in range(KC):
                        S.add("pe", MM(hu[:, 0:TS_], wup[b][:, kc, fc * 128:(fc + 1) * 128], X1T[:, kc, tsl],
                                       start=(kc == 0), stop=(kc == KC - 1)),
                              reads=[f"wup{b}"] + xr, writes=[bn[fc * 2 + 1]])
                    S.add("act", ACT(sl[fc][:], hg[:, 0:TS_], AF.Silu), reads=[bn[fc * 2]], writes=[f"sl{fc}"])
                    S.add("dve", TT(tl[fc][:], sl[fc][:], hu[:, 0:TS_], ALU.mult),
                          reads=[f"sl{fc}", bn[fc * 2 + 1]], writes=[f"tl{fc}"])
                    S.add("pool", TT(hid[i2][:, fc, :], tl[fc][:], cbs[i2][:], ALU.mult),
                          reads=[f"tl{fc}", f"cbs{i2}"], writes=[(f"hid{i2}", fc)])
                for sub in range(NSUB):
                    sI = T * NSUB + sub
                    for half in range(2):
                        py = banks[5 + pyi % 3]
                        pyn = bn[5 + pyi % 3]
                        pyi += 1
                        for fc in range(2):
                            S.add("pe", MM(py[:], hid[i2][:, fc, sub * 128:(sub + 1) * 128],
                                           wdn[b][:, fc, half * 512:(half + 1) * 512],
                                           start=(fc == 0), stop=(fc == 1)),
                                  reads=[(f"hid{i2}", 0), (f"hid{i2}", 1), f"wdn{b}"], writes=[pyn])
                        ysl = yacc[:, sI, half * 512:(half + 1) * 512]
                        S.add("dve", TT(ysl, ysl, py[:], ALU.add), reads=[pyn, ("yacc", sI)], writes=[("yacc", sI)])
    S.barrier()

    with ExitStack() as es:
        sb = lambda name, shape, dt: es.enter_context(nc.sbuf_tensor(name + cx.sfx, shape, dt))
        x2 = [sb(f"x2_{i}", [128, D_MODEL], F32) for i in range(2)]
        junk = sb("junk2", [128, D_MODEL], BF16)
        stt = [sb(f"lnst2_{i}", [128, 8], F32) for i in range(2)]
        xTo = [sb(f"xTo{i}", [128, KC, 128], BF16) for i in range(2)]
        want_T = d.get("xT_out") is not None
        if d.get("x_ar") is not None:
            xmk = [sb(f"xmk{i}", [128, 4, KC, 128], F32) for i in range(2)]
            pm = d["pm_tile"]
        xTo_v = d["xT_out"].rearrange("(kc p) s -> p kc s", p=128) if want_T else None
        for sI in range(NS):
            i2 = sI % 2
            tok = slice(sI * 128, (sI + 1) * 128)
            layer_norm_tile(cx, S, yacc[:, sI, :], ("yacc", sI), x2[i2][:], f"x2_{i2}", lnbc["ln2_g"], lnbc["ln2_b"],
                            stt[i2], f"lnst2_{i2}", junk, "junk2", "lnbc")
            S.add("sp", DMA(d["x_out"][tok, :], x2[i2][:]), reads=[f"x2_{i2}"], chan=f"xo{i2}")
            if not want_T:
                continue
            for kc in range(KC):
                bk = banks[(kc // 4)]
                S.add("pe", TR(bk[:, (kc % 4) * 128:(kc % 4 + 1) * 128], x2[i2][:, kc * 128:(kc + 1) * 128],
                               ident32[:]), reads=[f"x2_{i2}", "ident32"], writes=[bn[kc // 4]])
            for q in range(2):
                S.add("act" if q == 0 else "dve",
                      ACT(xTo[i2][:, q * 4:(q + 1) * 4, :], banks[q][:].rearrange("p (k t) -> p k t", k=4), AF.Identity)
                      if q == 0 else
                      CP(xTo[i2][:, q * 4:(q + 1) * 4, :], banks[q][:].rearrange("p (k t) -> p k t", k=4)),
                      reads=[bn[q]], writes=[(f"xTo{i2}", q)])
            S.add("sp", DMA(xTo_v[:, :, tok], xTo[i2][:]), reads=[(f"xTo{i2}", 0), (f"xTo{i2}", 1)], chan=f"xTo{i2}")
            if d.get("x_ar") is not None:
                xm = xmk[i2]
                for j in range(4):
                    S.add("dve",
                          TS(xm[:, j, :, :], xTo[i2][:], pm[:, j:j + 1], None, ALU.mult),
                          reads=[(f"xTo{i2}", 0), (f"xTo{i2}", 1), "pm"], writes=[(f"xmk{i2}", j, 0), (f"xmk{i2}", j, 1)])
                cht = d["x_ar"][0].shape[1]
                cpq = NTOK // cht
                s8, c8 = (sI * 128) // cht, (sI * 128) % cht
                for j in range(4):
                    dst = d["x_ar"][j * cpq + s8].rearrange("(kc p) t -> p kc t", p=128)[:, :, c8:c8 + 128]
                    S.add("sp", DMA(dst, xm[:, j]), reads=[(f"xmk{i2}", j, q) for q in range(2)],
                          chan=f"xar{i2}")


def build_k2(NTOK, x_bf16):
    nc = bass.Bass("TRN2", target_bir_lowering=False)
    d = {}
    inp = lambda name, shape, dt=F32: nc.dram_tensor(name, shape, dt, kind="ExternalInput").ap()
    d["x_tok"] = inp("x_tok", [NTOK, D_MODEL])
    d["xT"] = inp("xT", [D_MODEL, NTOK], BF16 if x_bf16 else F32)
    d["yT_sb"] = inp("yT_sb", [512, NTOK], BF16)
    d["yT_ca"] = inp("yT_ca", [512, NTOK], BF16)
    d["wg"] = inp("wg", [D_MODEL, 2048])
    d["b_gate"] = inp("b_gate", [128, 16])
    d["w_br_sb"] = inp("w_br_sb", [512, D_MODEL])
    d["w_br_ca"] = inp("w_br_ca", [512, D_MODEL])
    d["w_out"] = inp("w_out", [D_MODEL, D_MODEL])
    for nm in ("ln1_g", "ln1_b", "ln2_g", "ln2_b"):
        d[nm] = inp(nm, [D_MODEL])
    d["w_router"] = inp("w_router", [D_MODEL, 36])
    d["b_router"] = inp("b_router", [36])
    d["w_gate"] = inp("w_gate", [32, D_MODEL, 256])
    d["w_up"] = inp("w_up", [32, D_MODEL, 256])
    d["w_down"] = inp("w_down", [32, 256, D_MODEL])
    d["x_out"] = nc.dram_tensor("x_out", [NTOK, D_MODEL], F32, kind="ExternalOutput").ap()
    d["xT_out"] = nc.dram_tensor("xT_out", [D_MODEL, NTOK], BF16, kind="ExternalOutput").ap()
    d["mT_scratch"] = nc.dram_tensor("mT_scratch", [128, 8, NTOK], BF16, kind="Internal").ap()
    with ExitStack() as es:
        cx = Ctx(nc, es)
        cst = build_consts(cx)
        k2_body(cx, cst, NTOK, x_bf16, d)
        cx.finish()
    return nc


def build_k1(S_len, x_bf16):
    nc = bass.Bass("TRN2", target_bir_lowering=False)
    xT = nc.dram_tensor("xT", [D_MODEL, S_len], BF16 if x_bf16 else F32, kind="ExternalInput").ap()
    w1 = nc.dram_tensor("w1", [D_MODEL, 768], F32, kind="ExternalInput").ap()
    cab = nc.dram_tensor("cab", [2, 128, 640], F32, kind="ExternalInput").ap()
    ot_sb = nc.dram_tensor("ot_sb", [128, S_len], BF16, kind="ExternalOutput").ap()
    ot_ca = nc.dram_tensor("ot_ca", [128, S_len], BF16, kind="ExternalOutput").ap()
    with ExitStack() as es:
        cx = Ctx(nc, es)
        cst = build_consts(cx)
        k1_body(cx, cst, S_len, x_bf16, xT, w1, cab, ot_sb, ot_ca)
        cx.finish()
    return nc


def ca_bias_table(rel_bias_l, heads):
    q = np.arange(128)[:, None]
    p = np.arange(640)[None, :]
    rel = np.clip(q + 512 - p, -128, 128) + 128
    ci = q // 64
    kc = p // 64
    valid = (kc >= ci) & (kc <= ci + 8)
    out = np.empty((len(heads), 128, 640), np.float32)
    for i, h in enumerate(heads):
        out[i] = np.where(valid, rel_bias_l[h][rel], np.float32(NEG))
    return out


def k1_weights(w_in_l, r):
    c = lambda base: w_in_l[:, base + r * 128: base + (r + 1) * 128]
    return np.ascontiguousarray(np.concatenate(
        [c(0), c(512), c(1536), c(2048), c(1024), c(2560)], axis=1))


def k2_inputs(p):
    f = np.ascontiguousarray
    m = {}
    m["wg"] = f(p["w_in"][:, 3072:5120])
    m["b_gate"] = f(p["b_gate"].reshape(16, 128).T)
    m["w_br_sb"] = f(p["w_br_sb"])
    m["w_br_ca"] = f(p["w_br_ca"])
    m["w_out"] = f(p["w_out"])
    for nm in ("ln1_g", "ln1_b", "ln2_g", "ln2_b"):
        m[nm] = f(p[nm])
    m["w_router"] = f(np.concatenate([p["w_group"]] + [p["w_erouter"][g] for g in range(4)], axis=1))
    m["b_router"] = f(np.concatenate([p["b_group"]] + [p["b_erouter"][g] for g in range(4)], axis=0))
    m["w_gate"] = f(p["w_gate"].reshape(32, D_MODEL, 256))
    m["w_up"] = f(p["w_up"].reshape(32, D_MODEL, 256))
    m["w_down"] = f(p["w_down"].reshape(32, 256, D_MODEL))
    return m


def build_fused(S_len):
    nc = bass.Bass("TRN2", target_bir_lowering=False)
    L = DEPTH
    NTOK = S_len // 4
    inp = lambda name, shape, dt=F32: nc.dram_tensor(name, shape, dt, kind="ExternalInput").ap()
    scr = lambda name, shape, dt: nc.dram_tensor(name, shape, dt, kind="Internal").ap()
    x_tok0 = inp("x_tok", [S_len, D_MODEL])
    xT0 = inp("xT", [D_MODEL, S_len])
    w1 = inp("w1", [L, 4, D_MODEL, 768])
    cab = inp("cab", [L, 4, 2, 128, 640])
    P = {}
    P["wg"] = inp("wg", [L, D_MODEL, 2048])
    P["b_gate"] = inp("b_gate", [L, 128, 16])
    P["w_br_sb"] = inp("w_br_sb", [L, 512, D_MODEL])
    P["w_br_ca"] = inp("w_br_ca", [L, 512, D_MODEL])
    P["w_out"] = inp("w_out", [L, D_MODEL, D_MODEL])
    for nm in ("ln1_g", "ln1_b", "ln2_g", "ln2_b"):
        P[nm] = inp(nm, [L, D_MODEL])
    P["w_router"] = inp("w_router", [L, D_MODEL, 36])
    P["b_router"] = inp("b_router", [L, 36])
    P["w_gate"] = inp("w_gate", [L, 32, D_MODEL, 256])
    P["w_up"] = inp("w_up", [L, 32, D_MODEL, 256])
    P["w_down"] = inp("w_down", [L, 32, 256, D_MODEL])
    out = nc.dram_tensor("out", [S_len, D_MODEL], F32, kind="ExternalOutput").ap()
    yT_sb = scr("yT_sb_s", [512, S_len], BF16)
    yT_ca = scr("yT_ca_s", [512, S_len], BF16)
    xT1 = scr("xT1_s", [D_MODEL, S_len], BF16)
    x_tok1 = scr("x_tok1_s", [S_len, D_MODEL], F32)
    mT_s = scr("mT_scratch", [128, 8, NTOK], BF16)
    with ExitStack() as es:
        cx = Ctx(nc, es)
        cst = build_consts(cx)
        for l in range(L):
            xT_l = xT0 if l == 0 else xT1
            xtok_l = x_tok0 if l == 0 else x_tok1
            for r in range(4):
                with ExitStack() as es2:
                    cx.es = es2
                    cx.sfx = f"_a{l}{r}"
                    k1_body(cx, cst, S_len, l > 0, xT_l, w1[l, r], cab[l, r],
                            yT_sb[r * 128:(r + 1) * 128, :], yT_ca[r * 128:(r + 1) * 128, :])
                cx.es = es
                cx.S.barrier()
            for r in range(4):
                tsl = slice(r * NTOK, (r + 1) * NTOK)
                d = {k: v[l] for k, v in P.items()}
                d["x_tok"] = xtok_l[tsl, :]
                d["xT"] = xT_l[:, tsl]
                d["yT_sb"] = yT_sb[:, tsl]
                d["yT_ca"] = yT_ca[:, tsl]
                d["mT_scratch"] = mT_s
                last = (l == L - 1)
                d["x_out"] = out[tsl, :] if last else x_tok1[tsl, :]
                d["xT_out"] = None if last else xT1[:, tsl]
                with ExitStack() as es2:
                    cx.es = es2
                    cx.sfx = f"_b{l}{r}"
                    k2_body(cx, cst, NTOK, l > 0, d)
                cx.es = es
                cx.S.barrier()
        cx.finish()
    return nc


NO_CC = False
DBG = set()


def build_fused8(S_len):
    nc = bass.Bass("TRN2", target_bir_lowering=False)
    L = DEPTH
    NTOK = S_len // 4
    TS_ = 512
    NCH = NTOK // TS_
    CHT = min(1024, NTOK)
    CPQ = NTOK // CHT
    NXC = 4 * CPQ
    groups = [[0, 1, 2, 3], [4, 5, 6, 7]]
    inp = lambda name, shape, dt=F32: nc.dram_tensor(name, shape, dt, kind="ExternalInput").ap()
    scr = lambda name, shape, dt: nc.dram_tensor(name, shape, dt, kind="Internal").ap()
    x_tok0 = inp("x_tok", [NTOK, D_MODEL])
    xT0 = inp("xT", [D_MODEL, S_len])
    xTq0 = inp("xTq", [D_MODEL, NTOK])
    pm_d = inp("pm", [128, 4])
    w1 = inp("w1", [L, D_MODEL, 768])
    cab = inp("cab", [L, 2, 128, 640])
    P = {}
    P["wg"] = inp("wg", [L, D_MODEL, 2048])
    P["b_gate"] = inp("b_gate", [L, 128, 16])
    P["w_br_sb"] = inp("w_br_sb", [L, 512, D_MODEL])
    P["w_br_ca"] = inp("w_br_ca", [L, 512, D_MODEL])
    P["w_out"] = inp("w_out", [L, D_MODEL, D_MODEL])
    for nm in ("ln1_g", "ln1_b", "ln2_g", "ln2_b"):
        P[nm] = inp(nm, [L, D_MODEL])
    P["w_router"] = inp("w_router", [L, D_MODEL, 36])
    P["b_router"] = inp("b_router", [L, 36])
    P["w_gate"] = inp("w_gate", [L, 32, D_MODEL, 256])
    P["w_up"] = inp("w_up", [L, 32, D_MODEL, 256])
    P["w_down"] = inp("w_down", [L, 32, 256, D_MODEL])
    out = nc.dram_tensor("out", [NTOK, D_MODEL], F32, kind="ExternalOutput").ap()
    y_in = [scr(f"y_in{c}", [4 * 1024, TS_], F32) for c in range(NCH)]
    y_rs = [scr(f"y_rs{c}", [1024, TS_], F32) for c in range(NCH)]
    x_in = [scr(f"x_in{k}", [D_MODEL, CHT], F32) for k in range(NXC)]
    x_all = [scr(f"x_all{k}", [D_MODEL, CHT], F32) for k in range(NXC)]
    xT1 = scr("xT1_s", [D_MODEL, NTOK], BF16)
    x_tok1 = scr("x_tok1_s", [NTOK, D_MODEL], F32)
    mT_s = scr("mT_scratch", [128, 8, NTOK], BF16)
    with ExitStack() as es:
        cx = Ctx(nc, es)
        S = cx.S
        cst = build_consts(cx)
        pm = cx.sb("pm_t", [128, 4], F32)
        S.add("sp", DMA(pm[:], pm_d[:, :]), writes=["pm"], chan="prm0")
        for l in range(L):
            last = (l == L - 1)
            with ExitStack() as es2:
                cx.es = es2
                cx.sfx = f"_a{l}"
                yst = [cx.sb(f"yst{i}", [64, 4, 512], F32) for i in range(2)]
                cnt = [0]

                chunk_res = {c: [] for c in range(NCH)}
                tiles_per_chunk = 2 * 2 * (S_len // 512) // NCH

                def emit_out(branch, h, qb, ob, obn, yst=yst, cnt=cnt, chunk_res=chunk_res, l=l):
                    k = cnt[0] % 2
                    cnt[0] += 1
                    q, c = (qb * 512) // NTOK, ((qb * 512) % NTOK) // TS_
                    for j in range(4):
                        S.add("dve", TS(yst[k][:, j, :], ob[:], pm[0:64, j:j + 1], None, ALU.mult),
                              reads=[obn, "pm"], writes=[(f"yst{k}", j)])
                    dst = y_in[c].rearrange("(q b j w) s -> q b w j s", q=4, b=2, j=4)[q][branch][h * 64:(h + 1) * 64]
                    rn = ("y_in", c, branch, h, q)
                    S.add("sp", DMA(dst, yst[k][:]), reads=[(f"yst{k}", j) for j in range(4)], writes=[rn],
                          chan=f"yo{k}")
                    chunk_res[c].append(rn)
                    if len(chunk_res[c]) == tiles_per_chunk:
                        S.add("pool", lambda e, i_=y_in[c][:, :], o_=y_rs[c][:, :]: e.collective_compute(
                            "ReduceScatter", op=ALU.add, replica_groups=groups, ins=[i_], outs=[o_]),
                            reads=list(chunk_res[c]), writes=[("y_rs", c)], chan=f"ccy{l}_{c}", inc=1)

                if l == 0:
                    xt_tile = None
                else:
                    def xt_tile(t):
                        kk, c0 = (t * 256) // CHT, (t * 256) % CHT
                        return x_all[kk].rearrange("(kc p) s -> p kc s", p=128)[:, :, c0:c0 + 256]
                k1_body(cx, cst, S_len, False, xT0, w1[l], cab[l], None, None, xt_tile=xt_tile, emit_out=emit_out)
            cx.es = es
            S.barrier(skip_prefix="ccy")
            d = {k: v[l] for k, v in P.items()}
            d["x_tok"] = x_tok0 if l == 0 else x_tok1
            d["xT"] = xTq0 if l == 0 else xT1
            d["y_rs"] = [y_rs[c] for c in range(NCH)]
            d["mT_scratch"] = mT_s
            d["x_out"] = out if last else x_tok1
            d["xT_out"] = None if last else xT1
            d["x_ar"] = None if (last or "noxar" in DBG) else x_in
            d["pm_tile"] = pm
            with ExitStack() as es2:
                cx.es = es2
                cx.sfx = f"_b{l}"
                k2_body(cx, cst, NTOK, l > 0, d)
            cx.es = es
            S.barrier()
            if not last:
                for k in range(NXC if not NO_CC else 0):
                    S.add("pool", lambda e, i_=x_in[k][:, :], o_=x_all[k][:, :]: e.collective_compute(
                        "AllReduce", op=ALU.add, replica_groups=groups, ins=[i_], outs=[o_]),
                        chan="cc", inc=1)
                S.barrier()
        cx.finish()
    return nc


def kernel_fused8(p):
    x = p["x"]
    B, S_len, D = x.shape
    NTOK = S_len // 4
    f = np.ascontiguousarray
    key = ("fused8", S_len)
    if key not in _NC_CACHE:
        _NC_CACHE[key] = build_fused8(S_len)
    nc = _NC_CACHE[key]
    per_l = [k2_inputs({k: v[l] for k, v in p.items() if k != "x"}) for l in range(DEPTH)]
    shared = {k: f(np.stack([per_l[l][k] for l in range(DEPTH)])) for k in per_l[0]}
    xT_b = [f(x[b].T) for b in range(B)]
    in_maps = []
    for c in range(8):
        b, r = c // 4, c % 4
        tsl = slice(r * NTOK, (r + 1) * NTOK)
        m = dict(shared)
        m["x_tok"] = f(x[b, tsl, :])
        m["xT"] = xT_b[b]
        m["xTq"] = f(xT_b[b][:, tsl])
        pmv = np.zeros((128, 4), np.float32)
        pmv[:, r] = 1.0
        m["pm"] = pmv
        m["w1"] = f(np.stack([k1_weights(p["w_in"][l], r) for l in range(DEPTH)]))
        m["cab"] = f(np.stack([ca_bias_table(p["rel_bias"][l], [2 * r, 2 * r + 1]) for l in range(DEPTH)]))
        in_maps.append(m)
    res = run_bass_kernel_spmd(nc, in_maps, core_ids=list(range(8))).results
    return np.stack([np.concatenate([np.asarray(res[b * 4 + r]["out"]) for r in range(4)], axis=0)
                     for b in range(B)], axis=0).astype(np.float32)


FUSED = 8


def kernel_fused(p):
    x = p["x"]
    B, S_len, D = x.shape
    f = np.ascontiguousarray
    key = ("fused", S_len)
    if key not in _NC_CACHE:
        _NC_CACHE[key] = build_fused(S_len)
    nc = _NC_CACHE[key]
    shared = {}
    shared["w1"] = f(np.stack([np.stack([k1_weights(p["w_in"][l], r) for r in range(4)]) for l in range(DEPTH)]))
    shared["cab"] = f(np.stack([np.stack([ca_bias_table(p["rel_bias"][l], [2 * r, 2 * r + 1]) for r in range(4)])
                                for l in range(DEPTH)]))
    per_l = [k2_inputs({k: v[l] for k, v in p.items() if k != "x"}) for l in range(DEPTH)]
    for k in per_l[0]:
        shared[k] = f(np.stack([per_l[l][k] for l in range(DEPTH)]))
    in_maps = []
    for b in range(B):
        m = dict(shared)
        m["x_tok"] = f(x[b])
        m["xT"] = f(x[b].T)
        in_maps.append(m)
    res = run_bass_kernel_spmd(nc, in_maps, core_ids=list(range(B))).results
    return np.stack([np.asarray(res[b]["out"]) for b in range(B)], axis=0).astype(np.float32)


_NC_CACHE = {}


def _get_nc(kind, *args):
    key = (kind,) + args
    if key not in _NC_CACHE:
        _NC_CACHE[key] = build_k1(*args) if kind == "k1" else build_k2(*args)
    return _NC_CACHE[key]


def kernel(**inputs):
    p = {k: np.asarray(v) for k, v in inputs.items()}
    if FUSED == 8:
        return kernel_fused8(p)
    if FUSED:
        return kernel_fused(p)
    x = p["x"]
    B, S_len, D = x.shape
    NTOK = S_len // 4
    cores = list(range(8))
    f = np.ascontiguousarray
    x_cur = x
    xT_full = None
    xT_prev = None
    for l in range(DEPTH):
        first = (l == 0)
        lp = {k: v[l] for k, v in p.items() if k != "x"}
        if first:
            xT_b = [f(x[b].T) for b in range(B)]
        else:
            xT_b = xT_full
        nc1 = _get_nc("k1", S_len, not first)
        in1 = []
        for c in cores:
            b, r = c // 4, c % 4
            in1.append({"xT": xT_b[b], "w1": k1_weights(lp["w_in"], r),
                        "cab": ca_bias_table(lp["rel_bias"], [2 * r, 2 * r + 1])})
        r1 = run_bass_kernel_spmd(nc1, in1, core_ids=cores).results
        yT_sb = [np.concatenate([np.asarray(r1[b * 4 + r]["ot_sb"]) for r in range(4)], axis=0) for b in range(B)]
        yT_ca = [np.concatenate([np.asarray(r1[b * 4 + r]["ot_ca"]) for r in range(4)], axis=0) for b in range(B)]
        nc2 = _get_nc("k2", NTOK, not first)
        wk2 = k2_inputs(lp)
        in2 = []
        for c in cores:
            b, r = c // 4, c % 4
            tsl = slice(r * NTOK, (r + 1) * NTOK)
            m = dict(wk2)
            m["x_tok"] = f(x_cur[b, tsl, :])
            m["xT"] = f(xT_b[b][:, tsl]) if first else xT_prev[c]
            m["yT_sb"] = f(yT_sb[b][:, tsl])
            m["yT_ca"] = f(yT_ca[b][:, tsl])
            in2.append(m)
        r2 = run_bass_kernel_spmd(nc2, in2, core_ids=cores).results
        x_cur = np.stack([np.concatenate([np.asarray(r2[b * 4 + r]["x_out"]) for r in range(4)], axis=0)
                          for b in range(B)], axis=0)
        xT_prev = [np.asarray(r2[c]["xT_out"]) for c in cores]
        xT_full = [f(np.concatenate([xT_prev[b * 4 + r] for r in range(4)], axis=1)) for b in range(B)]
    return x_cur.astype(np.float32)
```

```python
import numpy as np
from contextlib import ExitStack
import concourse.bass as bass
import concourse.mybir as mybir
from concourse.bass_utils import run_bass_kernel_spmd

F32 = mybir.dt.float32
BF16 = mybir.dt.bfloat16
AF = mybir.ActivationFunctionType
ALU = mybir.AluOpType
AX = mybir.AxisListType

D_MODEL = 1024
BATCH = 2
SEQ = 8192
DEPTH = 2
HD = 64
ALPHA = (2.0 * DEPTH) ** 0.25
LN_EPS = 1e-5
NEG = -30000.0
HEAT = 0


class Sched:
    ENG = ("pe", "act", "dve", "pool", "sp")

    def __init__(self, nc):
        self.nc = nc
        self.ops = []
        self.last_w = {}
        self.readers = {}
        self.chan_last = {}
        self.chans = []
        self.eng_last = {}
        self.bar = set()

    def barrier(self, skip_prefix=None):
        self.bar = set(self.eng_last.values()) | set(
            v for k, v in self.chan_last.items() if not (skip_prefix and k.startswith(skip_prefix)))

    def add(self, eng, fn, reads=(), writes=(), chan=None, inc=16):
        idx = len(self.ops)
        deps = set(self.bar)
        for r in reads:
            if r in self.last_w:
                deps.add(self.last_w[r])
        for w in writes:
            if w in self.last_w:
                deps.add(self.last_w[w])
            for rd in self.readers.get(w, ()):
                deps.add(rd)
        if chan is not None:
            if chan in self.chan_last:
                deps.add(self.chan_last[chan])
            else:
                self.chans.append(chan)
            self.chan_last[chan] = idx
        for r in reads:
            self.readers.setdefault(r, []).append(idx)
        for w in writes:
            self.last_w[w] = idx
            self.readers[w] = []
        self.ops.append(dict(eng=eng, fn=fn, deps=deps, chan=chan, inc=inc))
        if chan is None:
            self.eng_last[eng] = idx
        return idx

    def emit(self, block, sems, final_wait_eng="sp"):
        ops = self.ops
        n = len(ops)
        has_dep = [False] * n
        for o in ops:
            for d in o["deps"]:
                has_dep[d] = True
        eng_cnt = {e: 0 for e in self.ENG}
        chan_cnt = {}
        sig = [None] * n
        for i, o in enumerate(ops):
            if o["chan"] is not None:
                c = o["chan"]
                chan_cnt[c] = chan_cnt.get(c, 0) + o["inc"]
                sig[i] = ("ch:" + c, chan_cnt[c])
            elif has_dep[i]:
                e = o["eng"]
                eng_cnt[e] += 1
                sig[i] = (e, eng_cnt[e])
        per_eng = {e: [] for e in self.ENG}
        for i, o in enumerate(ops):
            per_eng[o["eng"]].append(i)
        self.eng_cnt = eng_cnt

        def run(ename, eng):
            seen = {}
            for i in per_eng[ename]:
                o = ops[i]
                need = {}
                for d in o["deps"]:
                    od = ops[d]
                    if od["chan"] is None and od["eng"] == "pe" and ename == "pe" and o["chan"] is None:
                        continue
                    s, v = sig[d]
                    if need.get(s, 0) < v:
                        need[s] = v
                for s, v in need.items():
                    if seen.get(s, 0) >= v:
                        continue
                    eng.wait_ge(sems[s], v)
                    seen[s] = v
                ins = o["fn"](eng)
                if sig[i] is not None:
                    s, v = sig[i]
                    ins.then_inc(sems[s], o["inc"] if o["chan"] is not None else 1)
            if ename == final_wait_eng:
                for c, v in chan_cnt.items():
                    if seen.get("ch:" + c, 0) < v:
                        eng.wait_ge(sems["ch:" + c], v)

        @block.tensor
        def _(e):
            run("pe", e)

        @block.scalar
        def _(e):
            run("act", e)

        @block.vector
        def _(e):
            run("dve", e)

        @block.gpsimd
        def _(e):
            run("pool", e)

        @block.sync
        def _(e):
            run("sp", e)


class Ctx:
    def __init__(self, nc, es):
        self.nc = nc
        self.es = es
        self.S = Sched(nc)
        self.sfx = ""

    def sb(self, name, shape, dt):
        return self.es.enter_context(self.nc.sbuf_tensor(name + self.sfx, shape, dt))

    def ps(self, name, shape, dt):
        return self.es.enter_context(self.nc.psum_tensor(name + self.sfx, shape, dt))

    def finish(self):
        nc, es, S = self.nc, self.es, self.S
        sems = {}
        for e in Sched.ENG:
            sems[e] = es.enter_context(nc.semaphore("sem_" + e))
        for c in S.chans:
            sems["ch:" + c] = es.enter_context(nc.semaphore("semch_" + c))
        block = es.enter_context(nc.Block())
        S.emit(block, sems)


def build_consts(cx):
    S = cx.S
    c = {}
    ident = cx.sb("ident", [128, 128], BF16)
    ident32 = cx.sb("ident32", [128, 128], F32)
    negtri = cx.sb("negtri", [128, 128], BF16)
    negones = cx.sb("negones", [128, 128], BF16)
    m01 = cx.sb("m01", [128, 896], BF16)
    mbias = cx.sb("mbias", [128, 896], BF16)
    S.add("pool", lambda e: e.memset(ident[:], 0.0), writes=["ident"])
    S.add("pool", lambda e: e.affine_select(out=ident[:], in_=ident[:], pattern=[[-1, 128]],
                                            compare_op=ALU.not_equal, fill=1.0, base=0,
                                            channel_multiplier=1), reads=["ident"], writes=["ident"])
    S.add("pool", lambda e: e.memset(ident32[:], 0.0), writes=["ident32"])
    S.add("pool", lambda e: e.affine_select(out=ident32[:], in_=ident32[:], pattern=[[-1, 128]],
                                            compare_op=ALU.not_equal, fill=1.0, base=0,
                                            channel_multiplier=1), reads=["ident32"], writes=["ident32"])
    S.add("pool", lambda e: e.memset(negtri[:], -1.0), writes=["negtri"])
    S.add("pool", lambda e: e.affine_select(out=negtri[:], in_=negtri[:], pattern=[[-1, 128]],
                                            compare_op=ALU.is_ge, fill=0.0, base=0,
                                            channel_multiplier=1), reads=["negtri"], writes=["negtri"])
    S.add("pool", lambda e: e.memset(negones[:], -1.0), writes=["negones"])
    S.add("pool", lambda e: e.memset(m01[:], 1.0), writes=["m01"])
    S.add("pool", lambda e: e.affine_select(out=m01[:], in_=m01[:], pattern=[[1, 896]],
                                            compare_op=ALU.is_gt, fill=0.0, base=-384,
                                            channel_multiplier=-1), reads=["m01"], writes=["m01"])
    S.add("pool", lambda e: e.memset(mbias[:], 0.0), writes=["mbias"])
    S.add("pool", lambda e: e.affine_select(out=mbias[:], in_=mbias[:], pattern=[[1, 896]],
                                            compare_op=ALU.is_gt, fill=NEG, base=-384,
                                            channel_multiplier=-1), reads=["mbias"], writes=["mbias"])
    ones32 = cx.sb("ones32", [1, 128], F32)
    S.add("pool", lambda e: e.memset(ones32[:], 1.0), writes=["ones32"])
    c["ones32"] = ones32
    c.update(ident=ident, ident32=ident32, negtri=negtri, negones=negones, m01=m01, mbias=mbias)
    return c


def MM(out, lhsT, rhs, start=True, stop=True, skip=False):
    if skip:
        return lambda e: e.matmul(out, lhsT=lhsT, rhs=rhs, start=start, stop=stop, skip_group_check=True)
    return lambda e: e.matmul(out, lhsT=lhsT, rhs=rhs, start=start, stop=stop)


def ACT(out, in_, func, **kw):
    return lambda e: e.activation(out=out, in_=in_, func=func, **kw)


def TT(out, in0, in1, op):
    return lambda e: e.tensor_tensor(out=out, in0=in0, in1=in1, op=op)


def TS(out, in0, s1, s2, op0, op1=None):
    if op1 is None:
        return lambda e: e.tensor_scalar(out=out, in0=in0, scalar1=s1, scalar2=s2, op0=op0)
    return lambda e: e.tensor_scalar(out=out, in0=in0, scalar1=s1, scalar2=s2, op0=op0, op1=op1)


def STT(out, in0, scalar, in1, op0, op1):
    return lambda e: e.scalar_tensor_tensor(out=out, in0=in0, scalar=scalar, in1=in1, op0=op0, op1=op1)


def CP(out, in_):
    return lambda e: e.tensor_copy(out=out, in_=in_)


def DMA(out, in_):
    return lambda e: e.dma_start(out=out, in_=in_)


def TR(out, in_, ident):
    return lambda e: e.transpose(out, in_, ident)


def RED(out, in_, op):
    return lambda e: e.tensor_reduce(out=out, in_=in_, axis=AX.X, op=op)


def MS(ap, val):
    return lambda e: e.memset(ap, val)


def RCP(out, in_):
    return lambda e: e.reciprocal(out=out, in_=in_)


def k1_body(cx, cst, S_len, x_bf16, xT, w1, cab, ot_sb, ot_ca, xt_tile=None, emit_out=None, xt_rd=None):
    nc, S = cx.nc, cx.S
    NT = S_len // 256
    NB = S_len // 128
    NQ = S_len // 512
    KC = D_MODEL // 128

    QT_sb = cx.sb("QT_sb", [128, S_len], BF16)
    KT_sb = cx.sb("KT_sb", [128, S_len], BF16)
    QT_ca = cx.sb("QT_ca", [128, S_len], BF16)
    KT_ca = cx.sb("KT_ca", [128, S_len], BF16)
    V_all = cx.sb("V_all", [128, NB, 256], BF16)
    x32 = [cx.sb(f"x32_{i}", [128, KC, 256], F32) for i in range(2)]
    xbf = [cx.sb(f"xbf_{i}", [128, KC, 256], BF16) for i in range(2)]
    wbf = cx.sb("wbf", [128, KC, 768], BF16)
    cabs = cx.sb("cabs", [128, 2, 640], F32)

    banks = [cx.ps(f"bank{i}", [128, 512], F32) for i in range(7)]
    bankT = cx.ps("bankT", [128, 512], F32)

    if xt_tile is None:
        xT_v = xT.rearrange("(kc p) s -> p kc s", p=128)
        xt_tile = lambda t: xT_v[:, :, t * 256:(t + 1) * 256]
    w1_v = w1.rearrange("(kc p) n -> p kc n", p=128)

    S.add("sp", DMA(cabs[:], cab.rearrange("h q k -> q h k")), writes=["cabs"], chan="cab")

    for pc in range(3):
        buf = x32[pc % 2]
        nm = f"x32_{pc % 2}"
        S.add("sp", DMA(buf[:], w1_v[:, :, pc * 256:(pc + 1) * 256]), writes=[nm], chan=f"x{pc % 2}")
        S.add("dve", CP(wbf[:, :, pc * 256:(pc + 1) * 256], buf[:]), reads=[nm], writes=[f"wbf{pc}"])
    wres = ["wbf0", "wbf1", "wbf2"]

    dsts = [QT_sb, KT_sb, QT_ca, KT_ca]
    dnames = ["QT_sb", "KT_sb", "QT_ca", "KT_ca"]
    for t in range(NT):
        i2 = t % 2
        xb = xbf[i2]
        xbn = f"xbf_{i2}"
        tsl = slice(t * 256, (t + 1) * 256)
        if x_bf16:
            S.add("sp", DMA(xb[:], xt_tile(t)), reads=(xt_rd(t) if xt_rd else []), writes=[xbn], chan=f"x{i2}")
            xr = [xbn]
        else:
            xs = x32[i2]
            xsn = f"x32_{i2}"
            S.add("sp", DMA(xs[:], xt_tile(t)), reads=(xt_rd(t) if xt_rd else []), writes=[xsn], chan=f"x{i2}")
            S.add("dve", CP(xb[:, 0:4, :], xs[:, 0:4, :]), reads=[xsn], writes=[xbn + "a"])
            S.add("pool", CP(xb[:, 4:8, :], xs[:, 4:8, :]), reads=[xsn], writes=[xbn + "b"])
            xr = [xbn + "a", xbn + "b"]
        for g in range(4):
            bk = banks[g]
            bkn = f"bank{g}"
            for kc in range(KC):
                S.add("pe", MM(bk[:, 0:256], wbf[:, kc, g * 128:(g + 1) * 128], xb[:, kc, :],
                               start=(kc == 0), stop=(kc == KC - 1)), reads=wres + xr, writes=[bkn])
            dst = dsts[g]
            if g in (0, 2):
                S.add("act", ACT(dst[:, tsl], bk[:, 0:256], AF.Identity, scale=0.125),
                      reads=[bkn], writes=[(dnames[g], t // 2)])
            else:
                S.add("dve", CP(dst[:, tsl], bk[:, 0:256]), reads=[bkn], writes=[(dnames[g], t // 2)])
        for sub in range(2):
            bk = banks[4 + sub]
            bkn = f"bank{4 + sub}"
            for kc in range(KC):
                S.add("pe", MM(bk[:, 0:256], xb[:, kc, sub * 128:(sub + 1) * 128], wbf[:, kc, 512:768],
                               start=(kc == 0), stop=(kc == KC - 1)), reads=wres + xr, writes=[bkn])
            blk = t * 2 + sub
            if sub == 0:
                S.add("dve", CP(V_all[:, blk, :], bk[:, 0:256]), reads=[bkn], writes=[("V", blk)])
            else:
                S.add("act", ACT(V_all[:, blk, :], bk[:, 0:256], AF.Identity), reads=[bkn], writes=[("V", blk)])

    negtri, negones, ident, m01, mbias = cst["negtri"], cst["negones"], cst["ident"], cst["m01"], cst["mbias"]
    T32 = [cx.sb(f"T32_{i}", [128, 640], F32) for i in range(2)]
    Pb = [cx.sb(f"P_{i}", [128, 640], BF16) for i in range(2)]
    Pn = [cx.sb(f"Pn_{i}", [128, 640], BF16) for i in range(2)]
    WT = [cx.sb(f"WT_{i}", [128, 640], BF16) for i in range(2)]
    st = [cx.sb(f"st_{i}", [128, 4], F32) for i in range(4)]
    oca = [cx.sb(f"oca_{i}", [64, 512], BF16) for i in range(2)]
    po = banks[6]
    tb = [banks[4][:].bitcast(BF16), banks[5][:].bitcast(BF16)]
    tbn = ["bank4", "bank5"]
    its = [(h, m) for h in range(2) for m in range(NB)]
    NIT = len(its)

    def RC(ci):
        h, m = its[ci]
        nk = min(640, 128 * (m + 1))
        i2 = ci % 2
        return dict(h=h, m=m, nk=nk, ks=128 * (m + 1) - nk, p0=640 - nk, n1=min(nk, 512), nj=nk // 128,
                    hp=slice(h * 64, (h + 1) * 64), qsl=slice(m * 128, (m + 1) * 128),
                    pa=banks[i2 * 2], pan=f"bank{i2 * 2}", pb=banks[i2 * 2 + 1], pbn=f"bank{i2 * 2 + 1}",
                    T=T32[i2], Tn=f"T32_{i2}", P=Pb[i2], Pnm=f"P_{i2}", PN=Pn[i2], PNn=f"Pn_{i2}",
                    W=WT[i2], Wn=f"WT_{i2}", sx=st[ci % 4], sxn=f"st_{ci % 4}", tb=tb[i2], tbn=tbn[i2])

    for t in range(NIT + 6):
        ci = t
        if ci < NIT:
            r = RC(ci)
            ks, nk, n1, m, hp = r["ks"], r["nk"], r["n1"], r["m"], r["hp"]
            krd = [("KT_ca", j) for j in range(ks // 512, (ks + nk - 1) // 512 + 1)]
            S.add("pe", MM(r["pa"][:, 0:n1], QT_ca[hp, r["qsl"]], KT_ca[hp, ks:ks + n1]),
                  reads=[("QT_ca", m // 4)] + krd, writes=[r["pan"]])
            if nk > 512:
                S.add("pe", MM(r["pb"][:, 0:128], QT_ca[hp, r["qsl"]], KT_ca[hp, ks + 512:ks + 640]),
                      reads=[("QT_ca", m // 4)] + krd, writes=[r["pbn"]])
        ci = t - 1
        if 0 <= ci < NIT:
            r = RC(ci)
            T, sx, nk, n1, p0, h = r["T"], r["sx"], r["nk"], r["n1"], r["p0"], r["h"]
            S.add("dve", TT(T[:, 0:n1], r["pa"][:, 0:n1], cabs[:, h, p0:p0 + n1], ALU.add),
                  reads=[r["pan"], "cabs"], writes=[r["Tn"]])
            if nk > 512:
                S.add("dve", TT(T[:, 512:640], r["pb"][:, 0:128], cabs[:, h, 512:640], ALU.add),
                      reads=[r["pbn"], "cabs", r["Tn"]], writes=[r["Tn"]])
            S.add("dve", RED(sx[:, 0:1], T[:, 0:nk], ALU.max), reads=[r["Tn"]], writes=[r["sxn"]])
            S.add("dve", TS(sx[:, 1:2], sx[:, 0:1], -1.0, None, ALU.mult), reads=[r["sxn"]], writes=[r["sxn"]])
            S.add("pool", MS(sx[:, 2:3], 0.0), reads=[r["sxn"]], writes=[r["sxn"]])
        ci = t - 2
        if 0 <= ci < NIT:
            r = RC(ci)
            nk, sx = r["nk"], r["sx"]
            S.add("act", ACT(r["P"][:, 0:nk], r["T"][:, 0:nk], AF.Exp, bias=sx[:, 1:2], accum_out=sx[:, 2:3]),
                  reads=[r["Tn"], r["sxn"]], writes=[r["Pnm"], r["sxn"]])
        ci = t - 3
        if 0 <= ci < NIT:
            r = RC(ci)
            nk, sx = r["nk"], r["sx"]
            S.add("dve", RCP(sx[:, 3:4], sx[:, 2:3]), reads=[r["sxn"]], writes=[r["sxn"]])
            S.add("dve", TS(r["PN"][:, 0:nk], r["P"][:, 0:nk], sx[:, 3:4], None, ALU.mult),
                  reads=[r["Pnm"], r["sxn"]], writes=[r["PNn"]])
        ci = t - 4
        if 0 <= ci < NIT:
            r = RC(ci)
            for j in range(r["nj"]):
                S.add("pe", TR(r["tb"][:, j * 128:(j + 1) * 128], r["PN"][:, j * 128:(j + 1) * 128], ident[:]),
                      reads=[r["PNn"], "ident"], writes=[r["tbn"]])
        ci = t - 5
        if 0 <= ci < NIT:
            r = RC(ci)
            nk = r["nk"]
            S.add("act", ACT(r["W"][:, 0:nk], r["tb"][:, 0:nk], AF.Identity), reads=[r["tbn"]], writes=[r["Wn"]])
        ci = t - 6
        if 0 <= ci < NIT:
            r = RC(ci)
            h, m, nj, ks = r["h"], r["m"], r["nj"], r["ks"]
            mcol = (m % 4) * 128
            for j in range(nj):
                kb = ks // 128 + j
                S.add("pe", MM(po[0:64, mcol:mcol + 128], V_all[:, kb, 128 + h * 64:128 + (h + 1) * 64],
                               r["W"][:, j * 128:(j + 1) * 128], start=(j == 0), stop=(j == nj - 1)),
                      reads=[r["Wn"], ("V", kb)], writes=["bank6"])
            if m % 4 == 3:
                oq = (h * NB + m) // 4
                ob, obn = oca[oq % 2], f"oca_{oq % 2}"
                S.add("act", ACT(ob[:], po[0:64, :], AF.Identity), reads=["bank6"], writes=[obn])
                if emit_out is not None:
                    emit_out(1, h, m // 4, ob, obn)
                else:
                    S.add("sp", DMA(ot_ca[h * 64:(h + 1) * 64, (m - 3) * 128:(m + 1) * 128], ob[:]),
                          reads=[obn], chan=f"oc{oq % 2}")

    E32 = [cx.sb(f"E32_{i}", [128, 512], F32) for i in range(2)]
    SPb = [cx.sb(f"SP_{i}", [128, 512], BF16) for i in range(3)]
    Wb = [cx.sb(f"W_{i}", [128, 512], BF16) for i in range(2)]
    C32 = cx.sb("C32", [128, 512], F32)
    Cbf = [cx.sb(f"Cbf_{i}", [128, 512], BF16) for i in range(4)]
    osb = [cx.sb(f"osb_{i}", [64, 512], BF16) for i in range(2)]
    blocks = []
    oi = 0
    for qb in range(NQ):
        for h in range(2):
            n = 4 * (qb + 1)
            for i in range(n):
                blocks.append(dict(h=h, qb=qb, i=i, n=n, kb=n - 1 - i, oi=oi))
            oi += 1
    NBK = len(blocks)

    def R(g):
        bl = blocks[g]
        h, qb, kb = bl["h"], bl["qb"], bl["kb"]
        d_ = dict(bl)
        d_.update(hp=slice(h * 64, (h + 1) * 64), qs=slice(qb * 512, (qb + 1) * 512),
                  pz=banks[g % 6], pzn=f"bank{g % 6}", E=E32[g % 2], En=f"E32_{g % 2}",
                  SP=SPb[g % 3], SPn=f"SP_{g % 3}", W=Wb[g % 2], Wn=f"W_{g % 2}",
                  C=Cbf[g % 4], Cn=f"Cbf_{g % 4}", Cp=Cbf[(g + 1) % 4], Cpn=f"Cbf_{(g + 1) % 4}",
                  po=banks[6] if bl["oi"] % 2 == 0 else bankT, pon="bank6" if bl["oi"] % 2 == 0 else "bankT",
                  diag=kb >= 4 * qb, off=(kb - 4 * qb) * 128)
        return d_

    for t in range(NBK + 5):
        g = t
        if g < NBK:
            r = R(g)
            S.add("pe", MM(r["pz"][:], KT_sb[r["hp"], r["kb"] * 128:(r["kb"] + 1) * 128], QT_sb[r["hp"], r["qs"]]),
                  reads=[("KT_sb", r["kb"] // 4), ("QT_sb", r["qb"])], writes=[r["pzn"]])
        g = t - 1
        if 0 <= g < NBK:
            r = R(g)
            S.add("act", ACT(r["E"][:], r["pz"][:], AF.Exp), reads=[r["pzn"]], writes=[r["En"]])
        g = t - 2
        if 0 <= g < NBK:
            r = R(g)
            off = r["off"]
            S.add("act", ACT(r["SP"][:], r["E"][:], AF.Ln, bias=1.0), reads=[r["En"]], writes=[r["SPn"]])
            if r["diag"]:
                S.add("dve", TT(r["SP"][:], r["SP"][:], m01[:, 384 - off:896 - off], ALU.mult),
                      reads=[r["SPn"], "m01"], writes=[r["SPn"]])
            if r["i"] < r["n"] - 1:
                if r["i"] == 0:
                    S.add("dve", CP(C32[:], r["SP"][:]), reads=[r["SPn"]], writes=["C32"])
                    S.add("dve", CP(r["Cp"][:], r["SP"][:]), reads=[r["SPn"]], writes=[r["Cpn"]])
                else:
                    S.add("dve", TT(C32[:], C32[:], r["SP"][:], ALU.add), reads=[r["SPn"], "C32"], writes=["C32"])
                    S.add("dve", CP(r["Cp"][:], C32[:]), reads=["C32"], writes=[r["Cpn"]])
        g = t - 3
        if 0 <= g < NBK:
            r = R(g)
            off = r["off"]
            S.add("pe", MM(r["pz"][:], negtri[:], r["SP"][:], start=False, stop=False, skip=True),
                  reads=[r["SPn"], "negtri"], writes=[r["pzn"]])
            if r["i"] > 0:
                S.add("pe", MM(r["pz"][:], negones[:], r["C"][:], start=False, stop=False, skip=True),
                      reads=[r["Cn"], "negones"], writes=[r["pzn"]])
            if r["diag"]:
                S.add("pe", MM(r["pz"][:], ident[:], mbias[:, 384 - off:896 - off],
                               start=False, stop=False, skip=True),
                      reads=["ident", "mbias"], writes=[r["pzn"]])
        g = t - 4
        if 0 <= g < NBK:
            r = R(g)
            S.add("act", ACT(r["W"][:], r["pz"][:], AF.Exp), reads=[r["pzn"]], writes=[r["Wn"]])
        g = t - 5
        if 0 <= g < NBK:
            r = R(g)
            h = r["h"]
            po, pon = r["po"], r["pon"]
            S.add("pe", MM(po[0:64, 0:512], V_all[:, r["kb"], h * 64:(h + 1) * 64], r["W"][:],
                           start=(r["i"] == 0), stop=(r["i"] == r["n"] - 1)),
                  reads=[r["Wn"], ("V", r["kb"])], writes=[pon])
            for _ in range(HEAT):
                S.add("pe", MM(po[64:128, 0:512], negones[:, 0:64], m01[:, 0:512]), reads=["negones", "m01"])
            if r["i"] == r["n"] - 1:
                ob = osb[r["oi"] % 2]
                obn = f"osb_{r['oi'] % 2}"
                S.add("dve", CP(ob[:], po[0:64, 0:512]), reads=[pon], writes=[obn])
                if emit_out is not None:
                    emit_out(0, h, r["qb"], ob, obn)
                else:
                    S.add("sp", DMA(ot_sb[h * 64:(h + 1) * 64, r["qs"]], ob[:]), reads=[obn], chan=f"o{r['oi'] % 2}")


def layer_norm_tile(cx, S, u, un, out, outn, gbc, bbc, st, stn, junk, junkn, gn, tmp=None, tmpn=None):
    D = D_MODEL
    S.add("pool", MS(st[:, 0:2], 0.0), reads=[stn], writes=[stn])
    S.add("act", ACT(junk[:], u, AF.Identity, accum_out=st[:, 0:1]), reads=[un, stn], writes=[junkn, stn])
    S.add("act", ACT(junk[:], u, AF.Square, accum_out=st[:, 1:2]), reads=[un, stn, junkn], writes=[junkn, stn])
    S.add("dve", TS(st[:, 2:4], st[:, 0:2], 1.0 / D, None, ALU.mult), reads=[stn], writes=[stn])
    S.add("dve", STT(st[:, 4:5], st[:, 2:3], st[:, 2:3], st[:, 3:4], ALU.mult, ALU.subtract),
          reads=[stn], writes=[stn])
    S.add("dve", TS(st[:, 5:6], st[:, 4:5], -1.0, LN_EPS, ALU.mult, ALU.add), reads=[stn], writes=[stn])
    S.add("act", ACT(st[:, 5:6], st[:, 5:6], AF.Ln), reads=[stn], writes=[stn])
    S.add("act", ACT(st[:, 6:7], st[:, 5:6], AF.Exp, scale=-0.5), reads=[stn], writes=[stn])
    S.add("dve", STT(st[:, 7:8], st[:, 2:3], -1.0, st[:, 6:7], ALU.mult, ALU.mult), reads=[stn], writes=[stn])
    S.add("act", ACT(out, u, AF.Identity, scale=st[:, 6:7], bias=st[:, 7:8]), reads=[un, stn], writes=[outn])
    S.add("dve", TT(out, out, gbc[:], ALU.mult), reads=[outn, gn], writes=[outn])
    S.add("dve", TT(out, out, bbc[:], ALU.add), reads=[outn, gn], writes=[outn])


def k2_body(cx, cst, NTOK, x_bf16, d):
    nc, S = cx.nc, cx.S
    KC = D_MODEL // 128
    TS_ = min(512, NTOK)
    NST = NTOK // TS_
    NSUB = TS_ // 128
    NS = NTOK // 128
    ident32 = cst["ident32"]
    banks = [cx.ps(f"kb{i}", [128, 512], F32) for i in range(8)]
    bn = [f"kb{i}" for i in range(8)]

    lnbc = {}
    for nm in ("ln1_g", "ln1_b", "ln2_g", "ln2_b"):
        t = cx.sb("bc_" + nm, [128, D_MODEL], F32)
        S.add("sp", DMA(t[:], d[nm].partition_broadcast(128)), writes=["lnbc"], chan="prm_" + nm)
        lnbc[nm] = t
    bg = cx.sb("bg", [128, 16], F32)
    S.add("sp", DMA(bg[:], d["b_gate"]), writes=["bg"], chan="prm_bg")
    wr32 = cx.sb("wr32", [128, KC, 36], F32)
    S.add("sp", DMA(wr32[:], d["w_router"].rearrange("(kc p) n -> p kc n", p=128)), writes=["wr32"], chan="prm")
    brt = cx.sb("brt", [128, 36], F32)
    S.add("sp", DMA(brt[:], d["b_router"].partition_broadcast(128)), writes=["brt"], chan="prm")

    mT_d = d["mT_scratch"]
    xT_v = d["xT"].rearrange("(kc p) s -> p kc s", p=128)
    if d.get("y_rs") is None:
        ysb_v = d["yT_sb"].rearrange("(kc p) s -> p kc s", p=128)
        yca_v = d["yT_ca"].rearrange("(kc p) s -> p kc s", p=128)

    with ExitStack() as es:
        sb = lambda name, shape, dt: es.enter_context(nc.sbuf_tensor(name + cx.sfx, shape, dt))
        wg_bf = sb("wg_bf", [128, KC, 2048], BF16)
        wbs_bf = sb("wbs_bf", [128, 4, 1024], BF16)
        wbc_bf = sb("wbc_bf", [128, 4, 1024], BF16)
        stg = [sb(f"stgA{i}", [128, 2048], F32) for i in range(2)]
        xbf = [sb(f"xbfA{i}", [128, KC, TS_], BF16) for i in range(2)]
        ysb = [sb(f"ysb{i}", [128, 4, TS_], BF16) for i in range(2)]
        yca = [sb(f"yca{i}", [128, 4, TS_], BF16) for i in range(2)]
        gs = [sb(f"gs{i}", [128, TS_], F32) for i in range(2)]
        gc = [sb(f"gc{i}", [128, TS_], F32) for i in range(2)]
        t1 = sb("t1", [128, TS_], F32)
        t2 = sb("t2", [128, TS_], F32)
        mT = [sb(f"mT{i}", [128, KC, TS_], BF16) for i in range(2)]
        si = 0

        def stage_cast(dst, src_ap, ncols, rn, k=None, rd=()):
            nonlocal si
            b = si % 2
            si += 1
            sv = stg[b][:, 0:ncols]
            if k is not None:
                sv = sv.rearrange("p (k t) -> p k t", k=k)
            S.add("sp", DMA(sv, src_ap), reads=list(rd), writes=[f"stgA{b}"], chan=f"stgA{b}")
            S.add("dve", CP(dst, sv), reads=[f"stgA{b}"], writes=[rn])

        wg_v = d["wg"].rearrange("(kc p) n -> p kc n", p=128)
        for kc in range(KC):
            stage_cast(wg_bf[:, kc, :], wg_v[:, kc, :], 2048, "wg_bf")
        wbs_v = d["w_br_sb"].rearrange("(kc p) n -> p kc n", p=128)
        wbc_v = d["w_br_ca"].rearrange("(kc p) n -> p kc n", p=128)
        for kc in range(0, 4, 2):
            stage_cast(wbs_bf[:, kc:kc + 2, :], wbs_v[:, kc:kc + 2, :], 2048, "wbs_bf", k=2)
            stage_cast(wbc_bf[:, kc:kc + 2, :], wbc_v[:, kc:kc + 2, :], 2048, "wbc_bf", k=2)
        for T in range(NST):
            i2 = T % 2
            tsl = slice(T * TS_, (T + 1) * TS_)
            xb, xbn = xbf[i2], f"xbfA{i2}"
            if x_bf16:
                S.add("sp", DMA(xb[:], xT_v[:, :, tsl]), writes=[xbn], chan=f"xA{i2}")
            else:
                per = 2048 // TS_
                for k0 in range(0, KC, per):
                    stage_cast(xb[:, k0:k0 + per, :], xT_v[:, k0:k0 + per, tsl], per * TS_, xbn, k=per)
            if d.get("y_rs") is not None:
                yv = d["y_rs"][T].rearrange("(b kc p) s -> b p kc s", b=2, p=128)
                per = 2048 // TS_
                for k0 in range(0, 4, per):
                    stage_cast(ysb[i2][:, k0:k0 + per, :], yv[0][:, k0:k0 + per, :], per * TS_, f"ysb{i2}", k=per,
                               rd=[("y_rs", T)])
                    stage_cast(yca[i2][:, k0:k0 + per, :], yv[1][:, k0:k0 + per, :], per * TS_, f"yca{i2}", k=per,
                               rd=[("y_rs", T)])
            else:
                S.add("sp", DMA(ysb[i2][:], ysb_v[:, :, tsl]), writes=[f"ysb{i2}"], chan=f"yA{i2}")
                S.add("sp", DMA(yca[i2][:], yca_v[:, :, tsl]), writes=[f"yca{i2}"], chan=f"yB{i2}")
            mt, mtn = mT[i2], f"mT{i2}"
            for fo in range(KC):
                j2 = fo % 2
                fsl = slice(fo * 128, (fo + 1) * 128)
                fsl2 = slice(1024 + fo * 128, 1024 + (fo + 1) * 128)
                b0, b1, b2, b3 = j2 * 4, j2 * 4 + 1, j2 * 4 + 2, j2 * 4 + 3
                for kc in range(KC):
                    S.add("pe", MM(banks[b0][:, 0:TS_], wg_bf[:, kc, fsl], xb[:, kc, :],
                                   start=(kc == 0), stop=(kc == KC - 1)), reads=["wg_bf", xbn], writes=[bn[b0]])
                S.add("act", ACT(gs[j2][:], banks[b0][:, 0:TS_], AF.Sigmoid, bias=bg[:, fo:fo + 1]),
                      reads=[bn[b0], "bg"], writes=[f"gs{j2}"])
                for kc in range(KC):
                    S.add("pe", MM(banks[b1][:, 0:TS_], wg_bf[:, kc, fsl2], xb[:, kc, :],
                                   start=(kc == 0), stop=(kc == KC - 1)), reads=["wg_bf", xbn], writes=[bn[b1]])
                S.add("act", ACT(gc[j2][:], banks[b1][:, 0:TS_], AF.Sigmoid, bias=bg[:, 8 + fo:9 + fo]),
                      reads=[bn[b1], "bg"], writes=[f"gc{j2}"])
                for kc in range(4):
                    S.add("pe", MM(banks[b2][:, 0:TS_], wbs_bf[:, kc, fsl], ysb[i2][:, kc, :],
                                   start=(kc == 0), stop=(kc == 3)), reads=["wbs_bf", f"ysb{i2}"], writes=[bn[b2]])
                for kc in range(4):
                    S.add("pe", MM(banks[b3][:, 0:TS_], wbc_bf[:, kc, fsl], yca[i2][:, kc, :],
                                   start=(kc == 0), stop=(kc == 3)), reads=["wbc_bf", f"yca{i2}"], writes=[bn[b3]])
                S.add("dve", TT(t1[:], gs[j2][:], banks[b2][:, 0:TS_], ALU.mult),
                      reads=[f"gs{j2}", bn[b2]], writes=["t1"])
                S.add("dve", TT(t2[:], gc[j2][:], banks[b3][:, 0:TS_], ALU.mult),
                      reads=[f"gc{j2}", bn[b3]], writes=["t2"])
                S.add("pool", TT(mt[:, fo, :], t1[:], t2[:], ALU.add), reads=["t1", "t2"], writes=[mtn])
            S.add("sp", DMA(mT_d[:, :, tsl], mt[:]), reads=[mtn], writes=["mT_d"], chan=f"mTo{i2}")
    S.barrier()

    yacc = cx.sb("yacc", [128, NS, D_MODEL], F32)
    X1T = cx.sb("X1T", [128, KC, NTOK], BF16)
    combT = cx.sb("combT", [32, NTOK], F32)

    with ExitStack() as es:
        sb = lambda name, shape, dt: es.enter_context(nc.sbuf_tensor(name + cx.sfx, shape, dt))
        wo_bf = sb("wo_bf", [128, KC, 1024], BF16)
        stg = [sb(f"stgB{i}", [128, 2048], F32) for i in range(2)]
        wo_v = d["w_out"].rearrange("(kc p) n -> p kc n", p=128)
        for k0 in range(0, KC, 2):
            b = (k0 // 2) % 2
            sv = stg[b][:].rearrange("p (k t) -> p k t", k=2)
            S.add("sp", DMA(sv, wo_v[:, k0:k0 + 2, :]), writes=[f"stgB{b}"], chan=f"stgB{b}")
            S.add("dve" if b == 0 else "pool", CP(wo_bf[:, k0:k0 + 2, :], sv),
                  reads=[f"stgB{b}"], writes=["wo_bf"])
        mTt = [sb(f"mTt{i}", [128, KC, 128], BF16) for i in range(2)]
        xtok = [sb(f"xtok{i}", [128, D_MODEL], F32) for i in range(2)]
        u = [sb(f"u{i}", [128, D_MODEL], F32) for i in range(2)]
        x1 = [sb(f"x1_{i}", [128, D_MODEL], F32) for i in range(2)]
        junk = sb("junk", [128, D_MODEL], BF16)
        x1T32 = sb("x1T32", [128, KC, 128], F32)
        stt = [sb(f"lnst{i}", [128, 8], F32) for i in range(2)]
        rt = [sb(f"rt{i}", [128, 128], F32) for i in range(2)]
        comb = [sb(f"comb{i}", [128, 32], F32) for i in range(2)]
        for sI in range(NS):
            i2 = sI % 2
            tok = slice(sI * 128, (sI + 1) * 128)
            S.add("sp", DMA(mTt[i2][:], mT_d[:, :, tok]), reads=["mT_d"], writes=[f"mTt{i2}"], chan=f"mTi{i2}")
            S.add("sp", DMA(xtok[i2][:], d["x_tok"][tok, :]), writes=[f"xtok{i2}"], chan=f"xt{i2}")
            for half in range(2):
                bk = banks[half]
                for kc in range(KC):
                    S.add("pe", MM(bk[:], mTt[i2][:, kc, :], wo_bf[:, kc, half * 512:(half + 1) * 512],
                                   start=(kc == 0), stop=(kc == KC - 1)),
                          reads=[f"mTt{i2}", "wo_bf"], writes=[bn[half]])
                S.add("dve", STT(u[i2][:, half * 512:(half + 1) * 512], xtok[i2][:, half * 512:(half + 1) * 512],
                                 ALPHA, bk[:], ALU.mult, ALU.add),
                      reads=[f"xtok{i2}", bn[half]], writes=[f"u{i2}"])
            layer_norm_tile(cx, S, u[i2][:], f"u{i2}", x1[i2][:], f"x1_{i2}", lnbc["ln1_g"], lnbc["ln1_b"],
                            stt[i2], f"lnst{i2}", junk, "junk", "lnbc")
            S.add("act", ACT(yacc[:, sI, :], x1[i2][:], AF.Identity, scale=ALPHA),
                  reads=[f"x1_{i2}"], writes=[("yacc", sI)])
            for kc in range(KC):
                bk = banks[2 + (kc // 4)]
                S.add("pe", TR(bk[:, (kc % 4) * 128:(kc % 4 + 1) * 128], x1[i2][:, kc * 128:(kc + 1) * 128],
                               ident32[:]), reads=[f"x1_{i2}", "ident32"], writes=[bn[2 + kc // 4]])
            for q in range(2):
                S.add("act", ACT(x1T32[:, q * 4:(q + 1) * 4, :],
                                 banks[2 + q][:].rearrange("p (k t) -> p k t", k=4), AF.Identity),
                      reads=[bn[2 + q]], writes=["x1T32"])
            S.add("act", ACT(X1T[:, :, tok], x1T32[:], AF.Identity), reads=["x1T32"], writes=[("X1T", sI)])
            lg = banks[4]
            for kc in range(KC):
                S.add("pe", MM(lg[:, 0:36], x1T32[:, kc, :], wr32[:, kc, :], start=(kc == 0), stop=(kc == KC - 1)),
                      reads=["x1T32", "wr32"], writes=[bn[4]])
            R_, rn = rt[i2], f"rt{i2}"
            L = R_[:, 0:36]

            def V(eng, fn):
                S.add(eng, fn, reads=[rn, "brt", bn[4]] if eng != "pool" else [rn, "brt"], writes=[rn])
            V("dve", TT(L, lg[:, 0:36], brt[:], ALU.add))
            gmax, ngmax, sg, gval = R_[:, 36:37], R_[:, 37:38], R_[:, 38:39], R_[:, 39:40]
            ohg, eg, esel = R_[:, 40:44], R_[:, 44:48], R_[:, 48:56]
            m1, oh1, e2, m2, oh2 = R_[:, 56:57], R_[:, 64:72], R_[:, 72:80], R_[:, 57:58], R_[:, 80:88]
            dd, ed, den, w1, w2 = R_[:, 58:59], R_[:, 59:60], R_[:, 60:61], R_[:, 61:62], R_[:, 62:63]
            ew, gw = R_[:, 88:96], R_[:, 96:100]
            RE = "dve"
            tmpm = R_[:, 100:104]

            def tree_max(dst, src, n):
                cur = src
                while n > 1:
                    n //= 2
                    o_ = dst if n == 1 else tmpm[:, 0:n]
                    V(RE, TT(o_, cur[:, 0:n], cur[:, n:2 * n], ALU.max))
                    cur = tmpm
            V(RE, RED(gmax, L[:, 0:4], ALU.max))
            V(RE, TS(ohg, L[:, 0:4], gmax, None, ALU.is_equal))
            V(RE, TS(ngmax, gmax, -1.0, None, ALU.mult))
            V("pool", MS(sg, 0.0))
            V("act", ACT(eg, L[:, 0:4], AF.Exp, bias=ngmax, accum_out=sg))
            V("dve", RCP(gval, sg))
            V(RE, TS(esel, L[:, 4:12], ohg[:, 0:1], None, ALU.mult))
            for g in range(1, 4):
                V(RE, STT(esel, L[:, 4 + 8 * g:12 + 8 * g], ohg[:, g:g + 1], esel, ALU.mult, ALU.add))
            V(RE, RED(m1, esel, ALU.max))
            V(RE, TS(oh1, esel, m1, None, ALU.is_equal))
            V(RE, STT(e2, oh1, -1e30, esel, ALU.mult, ALU.add))
            V(RE, RED(m2, e2, ALU.max))
            V(RE, TS(oh2, e2, m2, None, ALU.is_equal))
            V(RE, TT(dd, m2, m1, ALU.subtract))
            V("act", ACT(ed, dd, AF.Exp))
            V(RE, TS(den, ed, 1.0, None, ALU.add))
            V("dve", RCP(w1, den))
            V(RE, TT(w2, ed, w1, ALU.mult))
            V(RE, TS(ew, oh1, w1, None, ALU.mult))
            V(RE, STT(ew, oh2, w2, ew, ALU.mult, ALU.add))
            V(RE, TS(gw, ohg, gval, None, ALU.mult))
            cb_, cbn = comb[i2], f"comb{i2}"
            for g in range(4):
                S.add(RE, TS(cb_[:, 8 * g:8 * g + 8], ew, gw[:, g:g + 1], None, ALU.mult),
                      reads=[rn], writes=[cbn])
            S.add("pe", TR(banks[5][0:32, 0:128], cb_[:], ident32[:]), reads=[cbn, "ident32"], writes=[bn[5]])
            S.add("act", ACT(combT[:, tok], banks[5][0:32, 0:128], AF.Identity), reads=[bn[5]],
                  writes=[("combT", sI)])
    S.barrier()

    NE = 32
    with ExitStack() as es:
        sb = lambda name, shape, dt: es.enter_context(nc.sbuf_tensor(name + cx.sfx, shape, dt))
        wgt = [sb(f"wgt{i}", [128, KC, 256], BF16) for i in range(2)]
        wup = [sb(f"wup{i}", [128, KC, 256], BF16) for i in range(2)]
        wdn = [sb(f"wdn{i}", [128, 2, 1024], BF16) for i in range(2)]
        stg = [sb(f"stgC{i}", [128, 2048], F32) for i in range(3)]
        sel = [sb(f"sel{i}", [32, 128], F32) for i in range(2)]
        sl = [sb(f"sl{i}", [128, TS_], F32) for i in range(2)]
        tl = [sb(f"tl{i}", [128, TS_], F32) for i in range(2)]
        cbs = [sb(f"cbs{i}", [128, TS_], F32) for i in range(2)]
        hid = [sb(f"hid{i}", [128, 2, TS_], BF16) for i in range(2)]
        wg_v = d["w_gate"].rearrange("e (kc p) f -> e p kc f", p=128)
        wu_v = d["w_up"].rearrange("e (kc p) f -> e p kc f", p=128)
        wd_v = d["w_down"].rearrange("e (fc p) n -> e p fc n", p=128)
        it = 0
        pyi = 0
        for e_ in range(NE):
            b = e_ % 2
            S.add("sp", DMA(stg[0][:].rearrange("p (k f) -> p k f", k=KC), wg_v[e_]), writes=["stgC0"], chan="stgC0")
            S.add("act", ACT(wgt[b][:], stg[0][:].rearrange("p (k f) -> p k f", k=KC), AF.Identity),
                  reads=["stgC0"], writes=[f"wgt{b}"])
            S.add("sp", DMA(stg[1][:].rearrange("p (k f) -> p k f", k=KC), wu_v[e_]), writes=["stgC1"], chan="stgC1")
            S.add("act", ACT(wup[b][:], stg[1][:].rearrange("p (k f) -> p k f", k=KC), AF.Identity),
                  reads=["stgC1"], writes=[f"wup{b}"])
            S.add("sp", DMA(stg[2][:].rearrange("p (k f) -> p k f", k=2), wd_v[e_]), writes=["stgC2"], chan="stgC2")
            S.add("pool", CP(wdn[b][:], stg[2][:].rearrange("p (k f) -> p k f", k=2)),
                  reads=["stgC2"], writes=[f"wdn{b}"])
            S.add("pool", MS(sel[b][:], 0.0), writes=[f"sel{b}"])
            S.add("sp", DMA(sel[b][e_:e_ + 1, :], cst["ones32"][0:1, :]), reads=["ones32"], writes=[f"sel{b}"],
                  chan=f"sel{b}")
            for T in range(NST):
                i2 = it % 2
                it += 1
                tsl = slice(T * TS_, (T + 1) * TS_)
                xr = [("X1T", T * NSUB + q) for q in range(NSUB)]
                S.add("pe", MM(banks[4][:, 0:TS_], sel[b][:], combT[:, tsl]),
                      reads=[f"sel{b}"] + [("combT", T * NSUB + q) for q in range(NSUB)], writes=[bn[4]])
                S.add("act", ACT(cbs[i2][:], banks[4][:, 0:TS_], AF.Identity), reads=[bn[4]], writes=[f"cbs{i2}"])
                for fc in range(2):
                    hg, hu = banks[fc * 2], banks[fc * 2 + 1]
                    for kc in range(KC):
                        S.add("pe", MM(hg[:, 0:TS_], wgt[b][:, kc, fc * 128:(fc + 1) * 128], X1T[:, kc, tsl],
                                       start=(kc == 0), stop=(kc == KC - 1)),
                              reads=[f"wgt{b}"] + xr, writes=[bn[fc * 2]])
                    for kc in range(KC):
                        S.add("pe", MM(hu[:, 0:TS_], wup[b][:, kc, fc * 128:(fc + 1) * 128], X1T[:, kc, tsl],
                                       start=(kc == 0), stop=(kc == KC - 1)),
                              reads=[f"wup{b}"] + xr, writes=[bn[fc * 2 + 1]])
                    S.add("act", ACT(sl[fc][:], hg[:, 0:TS_], AF.Silu), reads=[bn[fc * 2]], writes=[f"sl{fc}"])
                    S.add("dve", TT(tl[fc][:], sl[fc][:], hu[:, 0:TS_], ALU.mult),
                          reads=[f"sl{fc}", bn[fc * 2 + 1]], writes=[f"tl{fc}"])
                    S.add("pool", TT(hid[i2][:, fc, :], tl[fc][:], cbs[i2][:], ALU.mult),
                          reads=[f"tl{fc}", f"cbs{i2}"], writes=[(f"hid{i2}", fc)])
                for sub in range(NSUB):
                    sI = T * NSUB + sub
                    for half in range(2):
                        py = banks[5 + pyi % 3]
                        pyn = bn[5 + pyi % 3]
                        pyi += 1
                        for fc in range(2):
                            S.add("pe", MM(py[:], hid[i2][:, fc, sub * 128:(sub + 1) * 128],
                                           wdn[b][:, fc, half * 512:(half + 1) * 512],
                                           start=(fc == 0), stop=(fc == 1)),
                                  reads=[(f"hid{i2}", 0), (f"hid{i2}", 1), f"wdn{b}"], writes=[pyn])
                        ysl = yacc[:, sI, half * 512:(half + 1) * 512]
                        S.add("dve", TT(ysl, ysl, py[:], ALU.add), reads=[pyn, ("yacc", sI)], writes=[("yacc", sI)])
    S.barrier()

    with ExitStack() as es:
        sb = lambda name, shape, dt: es.enter_context(nc.sbuf_tensor(name + cx.sfx, shape, dt))
        x2 = [sb(f"x2_{i}", [128, D_MODEL], F32) for i in range(2)]
        junk = sb("junk2", [128, D_MODEL], BF16)
        stt = [sb(f"lnst2_{i}", [128, 8], F32) for i in range(2)]
        xTo = [sb(f"xTo{i}", [128, KC, 128], BF16) for i in range(2)]
        want_T = d.get("xT_out") is not None
        if d.get("x_ar") is not None:
            xmk = [sb(f"xmk{i}", [128, 4, KC, 128], F32) for i in range(2)]
            pm = d["pm_tile"]
            xar_res = {}
        xTo_v = d["xT_out"].rearrange("(kc p) s -> p kc s", p=128) if want_T else None
        for sI in range(NS):
            i2 = sI % 2
            tok = slice(sI * 128, (sI + 1) * 128)
            layer_norm_tile(cx, S, yacc[:, sI, :], ("yacc", sI), x2[i2][:], f"x2_{i2}", lnbc["ln2_g"], lnbc["ln2_b"],
                            stt[i2], f"lnst2_{i2}", junk, "junk2", "lnbc")
            S.add("sp", DMA(d["x_out"][tok, :], x2[i2][:]), reads=[f"x2_{i2}"], chan=f"xo{i2}")
            if not want_T:
                continue
            for kc in range(KC):
                bk = banks[(kc // 4)]
                S.add("pe", TR(bk[:, (kc % 4) * 128:(kc % 4 + 1) * 128], x2[i2][:, kc * 128:(kc + 1) * 128],
                               ident32[:]), reads=[f"x2_{i2}", "ident32"], writes=[bn[kc // 4]])
            for q in range(2):
                S.add("act" if q == 0 else "dve",
                      ACT(xTo[i2][:, q * 4:(q + 1) * 4, :], banks[q][:].rearrange("p (k t) -> p k t", k=4), AF.Identity)
                      if q == 0 else
                      CP(xTo[i2][:, q * 4:(q + 1) * 4, :], banks[q][:].rearrange("p (k t) -> p k t", k=4)),
                      reads=[bn[q]], writes=[(f"xTo{i2}", q)])
            S.add("sp", DMA(xTo_v[:, :, tok], xTo[i2][:]), reads=[(f"xTo{i2}", 0), (f"xTo{i2}", 1)], chan=f"xTo{i2}")
            if d.get("x_ar") is not None:
                xm = xmk[i2]
                for j in range(4):
                    S.add("dve",
                          TS(xm[:, j, :, :], xTo[i2][:], pm[:, j:j + 1], None, ALU.mult),
                          reads=[(f"xTo{i2}", 0), (f"xTo{i2}", 1), "pm"], writes=[(f"xmk{i2}", j, 0), (f"xmk{i2}", j, 1)])
                cht = d["x_ar"][0].shape[1]
                cpq = NTOK // cht
                s8, c8 = (sI * 128) // cht, (sI * 128) % cht
                for j in range(4):
                    dst = d["x_ar"][j * cpq + s8].rearrange("(kc p) t -> p kc t", p=128)[:, :, c8:c8 + 128]
                    rn_ = ("x_in", j * cpq + s8, sI)
                    S.add("sp", DMA(dst, xm[:, j]), reads=[(f"xmk{i2}", j, q) for q in range(2)], writes=[rn_],
                          chan=f"xar{i2}")
                    xar_res.setdefault(j * cpq + s8, []).append(rn_)
                if c8 + 128 == cht and d.get("x_ar_done") is not None:
                    for j in range(4):
                        d["x_ar_done"](j * cpq + s8, xar_res[j * cpq + s8])


def build_k2(NTOK, x_bf16):
    nc = bass.Bass("TRN2", target_bir_lowering=False)
    d = {}
    inp = lambda name, shape, dt=F32: nc.dram_tensor(name, shape, dt, kind="ExternalInput").ap()
    d["x_tok"] = inp("x_tok", [NTOK, D_MODEL])
    d["xT"] = inp("xT", [D_MODEL, NTOK], BF16 if x_bf16 else F32)
    d["yT_sb"] = inp("yT_sb", [512, NTOK], BF16)
    d["yT_ca"] = inp("yT_ca", [512, NTOK], BF16)
    d["wg"] = inp("wg", [D_MODEL, 2048])
    d["b_gate"] = inp("b_gate", [128, 16])
    d["w_br_sb"] = inp("w_br_sb", [512, D_MODEL])
    d["w_br_ca"] = inp("w_br_ca", [512, D_MODEL])
    d["w_out"] = inp("w_out", [D_MODEL, D_MODEL])
    for nm in ("ln1_g", "ln1_b", "ln2_g", "ln2_b"):
        d[nm] = inp(nm, [D_MODEL])
    d["w_router"] = inp("w_router", [D_MODEL, 36])
    d["b_router"] = inp("b_router", [36])
    d["w_gate"] = inp("w_gate", [32, D_MODEL, 256])
    d["w_up"] = inp("w_up", [32, D_MODEL, 256])
    d["w_down"] = inp("w_down", [32, 256, D_MODEL])
    d["x_out"] = nc.dram_tensor("x_out", [NTOK, D_MODEL], F32, kind="ExternalOutput").ap()
    d["xT_out"] = nc.dram_tensor("xT_out", [D_MODEL, NTOK], BF16, kind="ExternalOutput").ap()
    d["mT_scratch"] = nc.dram_tensor("mT_scratch", [128, 8, NTOK], BF16, kind="Internal").ap()
    with ExitStack() as es:
        cx = Ctx(nc, es)
        cst = build_consts(cx)
        k2_body(cx, cst, NTOK, x_bf16, d)
        cx.finish()
    return nc


def build_k1(S_len, x_bf16):
    nc = bass.Bass("TRN2", target_bir_lowering=False)
    xT = nc.dram_tensor("xT", [D_MODEL, S_len], BF16 if x_bf16 else F32, kind="ExternalInput").ap()
    w1 = nc.dram_tensor("w1", [D_MODEL, 768], F32, kind="ExternalInput").ap()
    cab = nc.dram_tensor("cab", [2, 128, 640], F32, kind="ExternalInput").ap()
    ot_sb = nc.dram_tensor("ot_sb", [128, S_len], BF16, kind="ExternalOutput").ap()
    ot_ca = nc.dram_tensor("ot_ca", [128, S_len], BF16, kind="ExternalOutput").ap()
    with ExitStack() as es:
        cx = Ctx(nc, es)
        cst = build_consts(cx)
        k1_body(cx, cst, S_len, x_bf16, xT, w1, cab, ot_sb, ot_ca)
        cx.finish()
    return nc


def ca_bias_table(rel_bias_l, heads):
    q = np.arange(128)[:, None]
    p = np.arange(640)[None, :]
    rel = np.clip(q + 512 - p, -128, 128) + 128
    ci = q // 64
    kc = p // 64
    valid = (kc >= ci) & (kc <= ci + 8)
    out = np.empty((len(heads), 128, 640), np.float32)
    for i, h in enumerate(heads):
        out[i] = np.where(valid, rel_bias_l[h][rel], np.float32(NEG))
    return out


def k1_weights(w_in_l, r):
    c = lambda base: w_in_l[:, base + r * 128: base + (r + 1) * 128]
    return np.ascontiguousarray(np.concatenate(
        [c(0), c(512), c(1536), c(2048), c(1024), c(2560)], axis=1))


def k2_inputs(p):
    f = np.ascontiguousarray
    m = {}
    m["wg"] = f(p["w_in"][:, 3072:5120])
    m["b_gate"] = f(p["b_gate"].reshape(16, 128).T)
    m["w_br_sb"] = f(p["w_br_sb"])
    m["w_br_ca"] = f(p["w_br_ca"])
    m["w_out"] = f(p["w_out"])
    for nm in ("ln1_g", "ln1_b", "ln2_g", "ln2_b"):
        m[nm] = f(p[nm])
    m["w_router"] = f(np.concatenate([p["w_group"]] + [p["w_erouter"][g] for g in range(4)], axis=1))
    m["b_router"] = f(np.concatenate([p["b_group"]] + [p["b_erouter"][g] for g in range(4)], axis=0))
    m["w_gate"] = f(p["w_gate"].reshape(32, D_MODEL, 256))
    m["w_up"] = f(p["w_up"].reshape(32, D_MODEL, 256))
    m["w_down"] = f(p["w_down"].reshape(32, 256, D_MODEL))
    return m


def build_fused(S_len):
    nc = bass.Bass("TRN2", target_bir_lowering=False)
    L = DEPTH
    NTOK = S_len // 4
    inp = lambda name, shape, dt=F32: nc.dram_tensor(name, shape, dt, kind="ExternalInput").ap()
    scr = lambda name, shape, dt: nc.dram_tensor(name, shape, dt, kind="Internal").ap()
    x_tok0 = inp("x_tok", [S_len, D_MODEL])
    xT0 = inp("xT", [D_MODEL, S_len])
    w1 = inp("w1", [L, 4, D_MODEL, 768])
    cab = inp("cab", [L, 4, 2, 128, 640])
    P = {}
    P["wg"] = inp("wg", [L, D_MODEL, 2048])
    P["b_gate"] = inp("b_gate", [L, 128, 16])
    P["w_br_sb"] = inp("w_br_sb", [L, 512, D_MODEL])
    P["w_br_ca"] = inp("w_br_ca", [L, 512, D_MODEL])
    P["w_out"] = inp("w_out", [L, D_MODEL, D_MODEL])
    for nm in ("ln1_g", "ln1_b", "ln2_g", "ln2_b"):
        P[nm] = inp(nm, [L, D_MODEL])
    P["w_router"] = inp("w_router", [L, D_MODEL, 36])
    P["b_router"] = inp("b_router", [L, 36])
    P["w_gate"] = inp("w_gate", [L, 32, D_MODEL, 256])
    P["w_up"] = inp("w_up", [L, 32, D_MODEL, 256])
    P["w_down"] = inp("w_down", [L, 32, 256, D_MODEL])
    out = nc.dram_tensor("out", [S_len, D_MODEL], F32, kind="ExternalOutput").ap()
    yT_sb = scr("yT_sb_s", [512, S_len], BF16)
    yT_ca = scr("yT_ca_s", [512, S_len], BF16)
    xT1 = scr("xT1_s", [D_MODEL, S_len], BF16)
    x_tok1 = scr("x_tok1_s", [S_len, D_MODEL], F32)
    mT_s = scr("mT_scratch", [128, 8, NTOK], BF16)
    with ExitStack() as es:
        cx = Ctx(nc, es)
        cst = build_consts(cx)
        for l in range(L):
            xT_l = xT0 if l == 0 else xT1
            xtok_l = x_tok0 if l == 0 else x_tok1
            for r in range(4):
                with ExitStack() as es2:
                    cx.es = es2
                    cx.sfx = f"_a{l}{r}"
                    k1_body(cx, cst, S_len, l > 0, xT_l, w1[l, r], cab[l, r],
                            yT_sb[r * 128:(r + 1) * 128, :], yT_ca[r * 128:(r + 1) * 128, :])
                cx.es = es
                cx.S.barrier()
            for r in range(4):
                tsl = slice(r * NTOK, (r + 1) * NTOK)
                d = {k: v[l] for k, v in P.items()}
                d["x_tok"] = xtok_l[tsl, :]
                d["xT"] = xT_l[:, tsl]
                d["yT_sb"] = yT_sb[:, tsl]
                d["yT_ca"] = yT_ca[:, tsl]
                d["mT_scratch"] = mT_s
                last = (l == L - 1)
                d["x_out"] = out[tsl, :] if last else x_tok1[tsl, :]
                d["xT_out"] = None if last else xT1[:, tsl]
                with ExitStack() as es2:
                    cx.es = es2
                    cx.sfx = f"_b{l}{r}"
                    k2_body(cx, cst, NTOK, l > 0, d)
                cx.es = es
                cx.S.barrier()
        cx.finish()
    return nc


NO_CC = False
DBG = set()


def build_fused8(S_len):
    nc = bass.Bass("TRN2", target_bir_lowering=False)
    L = DEPTH
    NTOK = S_len // 4
    TS_ = 512
    NCH = NTOK // TS_
    CHT = min(1024, NTOK)
    CPQ = NTOK // CHT
    NXC = 4 * CPQ
    groups = [[0, 1, 2, 3], [4, 5, 6, 7]]
    inp = lambda name, shape, dt=F32: nc.dram_tensor(name, shape, dt, kind="ExternalInput").ap()
    scr = lambda name, shape, dt: nc.dram_tensor(name, shape, dt, kind="Internal").ap()
    x_tok0 = inp("x_tok", [NTOK, D_MODEL])
    xT0 = inp("xT", [D_MODEL, S_len])
    xTq0 = inp("xTq", [D_MODEL, NTOK])
    pm_d = inp("pm", [128, 4])
    w1 = inp("w1", [L, D_MODEL, 768])
    cab = inp("cab", [L, 2, 128, 640])
    P = {}
    P["wg"] = inp("wg", [L, D_MODEL, 2048])
    P["b_gate"] = inp("b_gate", [L, 128, 16])
    P["w_br_sb"] = inp("w_br_sb", [L, 512, D_MODEL])
    P["w_br_ca"] = inp("w_br_ca", [L, 512, D_MODEL])
    P["w_out"] = inp("w_out", [L, D_MODEL, D_MODEL])
    for nm in ("ln1_g", "ln1_b", "ln2_g", "ln2_b"):
        P[nm] = inp(nm, [L, D_MODEL])
    P["w_router"] = inp("w_router", [L, D_MODEL, 36])
    P["b_router"] = inp("b_router", [L, 36])
    P["w_gate"] = inp("w_gate", [L, 32, D_MODEL, 256])
    P["w_up"] = inp("w_up", [L, 32, D_MODEL, 256])
    P["w_down"] = inp("w_down", [L, 32, 256, D_MODEL])
    out = nc.dram_tensor("out", [NTOK, D_MODEL], F32, kind="ExternalOutput").ap()
    y_in = [scr(f"y_in{c}", [4 * 1024, TS_], F32) for c in range(NCH)]
    y_rs = [scr(f"y_rs{c}", [1024, TS_], F32) for c in range(NCH)]
    x_in = [scr(f"x_in{k}", [D_MODEL, CHT], F32) for k in range(NXC)]
    x_all = [scr(f"x_all{k}", [D_MODEL, CHT], F32) for k in range(NXC)]
    xT1 = scr("xT1_s", [D_MODEL, NTOK], BF16)
    x_tok1 = scr("x_tok1_s", [NTOK, D_MODEL], F32)
    mT_s = scr("mT_scratch", [128, 8, NTOK], BF16)
    with ExitStack() as es:
        cx = Ctx(nc, es)
        S = cx.S
        cst = build_consts(cx)
        pm = cx.sb("pm_t", [128, 4], F32)
        S.add("sp", DMA(pm[:], pm_d[:, :]), writes=["pm"], chan="prm0")
        for l in range(L):
            last = (l == L - 1)
            with ExitStack() as es2:
                cx.es = es2
                cx.sfx = f"_a{l}"
                yst = [cx.sb(f"yst{i}", [64, 4, 512], F32) for i in range(2)]
                cnt = [0]

                chunk_res = {c: [] for c in range(NCH)}
                tiles_per_chunk = 2 * 2 * (S_len // 512) // NCH

                def emit_out(branch, h, qb, ob, obn, yst=yst, cnt=cnt, chunk_res=chunk_res, l=l):
                    k = cnt[0] % 2
                    cnt[0] += 1
                    q, c = (qb * 512) // NTOK, ((qb * 512) % NTOK) // TS_
                    for j in range(4):
                        S.add("dve", TS(yst[k][:, j, :], ob[:], pm[0:64, j:j + 1], None, ALU.mult),
                              reads=[obn, "pm"], writes=[(f"yst{k}", j)])
                    dst = y_in[c].rearrange("(q b j w) s -> q b w j s", q=4, b=2, j=4)[q][branch][h * 64:(h + 1) * 64]
                    rn = ("y_in", c, branch, h, q)
                    S.add("sp", DMA(dst, yst[k][:]), reads=[(f"yst{k}", j) for j in range(4)], writes=[rn],
                          chan=f"yo{k}")
                    chunk_res[c].append(rn)
                    if len(chunk_res[c]) == tiles_per_chunk:
                        S.add("pool", lambda e, i_=y_in[c][:, :], o_=y_rs[c][:, :]: e.collective_compute(
                            "ReduceScatter", op=ALU.add, replica_groups=groups, ins=[i_], outs=[o_]),
                            reads=list(chunk_res[c]), writes=[("y_rs", c)], chan=f"ccy{l}_{c}", inc=1)

                if l == 0:
                    xt_tile = None
                else:
                    def xt_tile(t):
                        kk, c0 = (t * 256) // CHT, (t * 256) % CHT
                        return x_all[kk].rearrange("(kc p) s -> p kc s", p=128)[:, :, c0:c0 + 256]
                xt_rd = None if l == 0 else (lambda t: [("x_all", (t * 256) // CHT)])
                k1_body(cx, cst, S_len, False, xT0, w1[l], cab[l], None, None, xt_tile=xt_tile, emit_out=emit_out,
                        xt_rd=xt_rd)
            cx.es = es
            S.barrier(skip_prefix="ccy")
            d = {k: v[l] for k, v in P.items()}
            d["x_tok"] = x_tok0 if l == 0 else x_tok1
            d["xT"] = xTq0 if l == 0 else xT1
            d["y_rs"] = [y_rs[c] for c in range(NCH)]
            d["mT_scratch"] = mT_s
            d["x_out"] = out if last else x_tok1
            d["xT_out"] = None if last else xT1
            d["x_ar"] = None if (last or "noxar" in DBG) else x_in
            d["pm_tile"] = pm

            def x_ar_done(k, res_list, l=l):
                S.add("pool", lambda e, i_=x_in[k][:, :], o_=x_all[k][:, :]: e.collective_compute(
                    "AllReduce", op=ALU.add, replica_groups=groups, ins=[i_], outs=[o_]),
                    reads=list(res_list), writes=[("x_all", k)], chan=f"ccx{l}_{k}", inc=1)
            d["x_ar_done"] = None if last else x_ar_done
            with ExitStack() as es2:
                cx.es = es2
                cx.sfx = f"_b{l}"
                k2_body(cx, cst, NTOK, l > 0, d)
            cx.es = es
            S.barrier(skip_prefix="ccx")
        cx.finish()
    return nc


def kernel_fused8(p):
    x = p["x"]
    B, S_len, D = x.shape
    NTOK = S_len // 4
    f = np.ascontiguousarray
    key = ("fused8", S_len)
    if key not in _NC_CACHE:
        _NC_CACHE[key] = build_fused8(S_len)
    nc = _NC_CACHE[key]
    per_l = [k2_inputs({k: v[l] for k, v in p.items() if k != "x"}) for l in range(DEPTH)]
    shared = {k: f(np.stack([per_l[l][k] for l in range(DEPTH)])) for k in per_l[0]}
    xT_b = [f(x[b].T) for b in range(B)]
    in_maps = []
    for c in range(8):
        b, r = c // 4, c % 4
        tsl = slice(r * NTOK, (r + 1) * NTOK)
        m = dict(shared)
        m["x_tok"] = f(x[b, tsl, :])
        m["xT"] = xT_b[b]
        m["xTq"] = f(xT_b[b][:, tsl])
        pmv = np.zeros((128, 4), np.float32)
        pmv[:, r] = 1.0
        m["pm"] = pmv
        m["w1"] = f(np.stack([k1_weights(p["w_in"][l], r) for l in range(DEPTH)]))
        m["cab"] = f(np.stack([ca_bias_table(p["rel_bias"][l], [2 * r, 2 * r + 1]) for l in range(DEPTH)]))
        in_maps.append(m)
    res = run_bass_kernel_spmd(nc, in_maps, core_ids=list(range(8))).results
    return np.stack([np.concatenate([np.asarray(res[b * 4 + r]["out"]) for r in range(4)], axis=0)
                     for b in range(B)], axis=0).astype(np.float32)


FUSED = 8


def kernel_fused(p):
    x = p["x"]
    B, S_len, D = x.shape
    f = np.ascontiguousarray
    key = ("fused", S_len)
    if key not in _NC_CACHE:
        _NC_CACHE[key] = build_fused(S_len)
    nc = _NC_CACHE[key]
    shared = {}
    shared["w1"] = f(np.stack([np.stack([k1_weights(p["w_in"][l], r) for r in range(4)]) for l in range(DEPTH)]))
    shared["cab"] = f(np.stack([np.stack([ca_bias_table(p["rel_bias"][l], [2 * r, 2 * r + 1]) for r in range(4)])
                                for l in range(DEPTH)]))
    per_l = [k2_inputs({k: v[l] for k, v in p.items() if k != "x"}) for l in range(DEPTH)]
    for k in per_l[0]:
        shared[k] = f(np.stack([per_l[l][k] for l in range(DEPTH)]))
    in_maps = []
    for b in range(B):
        m = dict(shared)
        m["x_tok"] = f(x[b])
        m["xT"] = f(x[b].T)
        in_maps.append(m)
    res = run_bass_kernel_spmd(nc, in_maps, core_ids=list(range(B))).results
    return np.stack([np.asarray(res[b]["out"]) for b in range(B)], axis=0).astype(np.float32)


_NC_CACHE = {}


def _get_nc(kind, *args):
    key = (kind,) + args
    if key not in _NC_CACHE:
        _NC_CACHE[key] = build_k1(*args) if kind == "k1" else build_k2(*args)
    return _NC_CACHE[key]


def kernel(**inputs):
    p = {k: np.asarray(v) for k, v in inputs.items()}
    if FUSED == 8:
        return kernel_fused8(p)
    if FUSED:
        return kernel_fused(p)
    x = p["x"]
    B, S_len, D = x.shape
    NTOK = S_len // 4
    cores = list(range(8))
    f = np.ascontiguousarray
    x_cur = x
    xT_full = None
    xT_prev = None
    for l in range(DEPTH):
        first = (l == 0)
        lp = {k: v[l] for k, v in p.items() if k != "x"}
        if first:
            xT_b = [f(x[b].T) for b in range(B)]
        else:
            xT_b = xT_full
        nc1 = _get_nc("k1", S_len, not first)
        in1 = []
        for c in cores:
            b, r = c // 4, c % 4
            in1.append({"xT": xT_b[b], "w1": k1_weights(lp["w_in"], r),
                        "cab": ca_bias_table(lp["rel_bias"], [2 * r, 2 * r + 1])})
        r1 = run_bass_kernel_spmd(nc1, in1, core_ids=cores).results
        yT_sb = [np.concatenate([np.asarray(r1[b * 4 + r]["ot_sb"]) for r in range(4)], axis=0) for b in range(B)]
        yT_ca = [np.concatenate([np.asarray(r1[b * 4 + r]["ot_ca"]) for r in range(4)], axis=0) for b in range(B)]
        nc2 = _get_nc("k2", NTOK, not first)
        wk2 = k2_inputs(lp)
        in2 = []
        for c in cores:
            b, r = c // 4, c % 4
            tsl = slice(r * NTOK, (r + 1) * NTOK)
            m = dict(wk2)
            m["x_tok"] = f(x_cur[b, tsl, :])
            m["xT"] = f(xT_b[b][:, tsl]) if first else xT_prev[c]
            m["yT_sb"] = f(yT_sb[b][:, tsl])
            m["yT_ca"] = f(yT_ca[b][:, tsl])
            in2.append(m)
        r2 = run_bass_kernel_spmd(nc2, in2, core_ids=cores).results
        x_cur = np.stack([np.concatenate([np.asarray(r2[b * 4 + r]["x_out"]) for r in range(4)], axis=0)
                          for b in range(B)], axis=0)
        xT_prev = [np.asarray(r2[c]["xT_out"]) for c in cores]
        xT_full = [f(np.concatenate([xT_prev[b * 4 + r] for r in range(4)], axis=1)) for b in range(B)]
    return x_cur.astype(np.float32)
```
